# Optimizing a Trainium2 kernel written in Bass

```python
import math
import jax
import jax.numpy as jnp
from jax import lax
import numpy as np

D_MODEL = 1024
BATCH = 8
SEQ = 4096
DEPTH = 1

MIX_WIDTH = D_MODEL
MLSTM_WIDTH = MIX_WIDTH // 2
MLSTM_HEADS = 4
MLSTM_HEAD_DIM = MLSTM_WIDTH // MLSTM_HEADS
MLSTM_CHUNK = 128
MLSTM_M_INIT = -1e30
HYENA_WIDTH = MIX_WIDTH - MLSTM_WIDTH
HYENA_GROUPS = 4
HYENA_GROUP_DIM = HYENA_WIDTH // HYENA_GROUPS
HYENA_ORDER = 2
HYENA_DIRS = 2
HYENA_BANDS = 16
HYENA_EMB_DIM = 1 + 2 * HYENA_BANDS
HYENA_FILTER_HIDDEN = 64
HYENA_WINDOW_SHIFT = 0.05
HYENA_SLOW_DECAY = -math.log(1e-2) / 1.5
HYENA_FAST_DECAY = -math.log(1e-2) / 0.3
SHORT_CONV = 3
N_GATE_COLS = 4 * MLSTM_HEADS
PROJ_COLS = 4 * MLSTM_WIDTH + N_GATE_COLS + (HYENA_ORDER + 1) * HYENA_WIDTH
N_EXPERTS = 16
EC_CAPACITY_FACTOR = 2
EXPERT_FF = 2 * D_MODEL
RMS_EPS = 1e-6

kernel_name = 'hybrid_mlstm_hyena_ec_moe_encoder'


def rms_norm(x, g):
    xf = x.astype(jnp.float32)
    y = xf * lax.rsqrt(jnp.mean(xf * xf, axis=-1, keepdims=True) + RMS_EPS)
    return (y * g.astype(jnp.float32)).astype(x.dtype)


def centred_short_conv(u, w, b):
    S = u.shape[1]
    r = SHORT_CONV // 2
    up = jnp.pad(u, ((0, 0), (r, r), (0, 0)))
    y = b
    for j in range(SHORT_CONV):
        y = y + up[:, j:j + S] * w[j]
    return y


def mlstm_scan(q, k, v, i_pre, logf):
    B, H, S, Dh = q.shape
    nc = S // MLSTM_CHUNK
    qc = q.reshape(B, H, nc, MLSTM_CHUNK, Dh)
    kc = k.reshape(B, H, nc, MLSTM_CHUNK, Dh)
    vc = v.reshape(B, H, nc, MLSTM_CHUNK, Dh)
    ic = i_pre.reshape(B, H, nc, MLSTM_CHUNK)
    b = jnp.cumsum(logf.reshape(B, H, nc, MLSTM_CHUNK), axis=-1)
    g = b[..., -1]
    w_end = g[..., None] - b + ic
    a = jnp.max(w_end, axis=-1)
    e_end = jnp.exp(w_end - a[..., None])
    c_loc = jnp.einsum('bhcsv,bhcsk->bhcvk', vc * e_end[..., None], kc)
    n_loc = jnp.einsum('bhcs,bhcsk->bhck', e_end, kc)

    def step(carry, inp):
        c_st, n_st, m_st = carry
        g_j, a_j, c_j, n_j = inp
        m_new = jnp.maximum(g_j + m_st, a_j)
        s_prev = jnp.exp(g_j + m_st - m_new)
        s_loc = jnp.exp(a_j - m_new)
        c_new = s_prev[..., None, None] * c_st + s_loc[..., None, None] * c_j
        n_new = s_prev[..., None] * n_st + s_loc[..., None] * n_j
        return (c_new, n_new, m_new), (c_st, n_st, m_st)

    init = (jnp.zeros((B, H, Dh, Dh), q.dtype), jnp.zeros((B, H, Dh), q.dtype),
            jnp.full((B, H), MLSTM_M_INIT, q.dtype))
    xs = tuple(jnp.moveaxis(t, 2, 0) for t in (g, a, c_loc, n_loc))
    _, (c_in, n_in, m_in) = lax.scan(step, init, xs)
    c_in = jnp.moveaxis(c_in, 0, 2)
    n_in = jnp.moveaxis(n_in, 0, 2)
    m_in = jnp.moveaxis(m_in, 0, 2)

    tri = jnp.tril(jnp.ones((MLSTM_CHUNK, MLSTM_CHUNK), dtype=bool))
    log_d = jnp.where(tri, b[..., :, None] - b[..., None, :] + ic[..., None, :], -jnp.inf)
    log_inter = b + m_in[..., None]
    m_t = jnp.maximum(log_inter, jnp.max(log_d, axis=-1))
    d = jnp.exp(log_d - m_t[..., None])
    e_inter = jnp.exp(log_inter - m_t)
    s = jnp.einsum('bhctd,bhcsd->bhcts', qc, kc) * d
    num = (jnp.einsum('bhcts,bhcsd->bhctd', s, vc)
           + e_inter[..., None] * jnp.einsum('bhcvk,bhctk->bhctv', c_in, qc))
    den = jnp.sum(s, axis=-1) + e_inter * jnp.einsum('bhck,bhctk->bhct', n_in, qc)
    nrm = jnp.maximum(jnp.abs(den), jnp.exp(-m_t))
    return (num / nrm[..., None]).reshape(B, H, S, Dh)


def mlstm_mixer(q, k, v, o, gates, conv_w, conv_b, norm_g):
    B, S, _ = q.shape
    H, Dh = MLSTM_HEADS, MLSTM_HEAD_DIM
    qk = jax.nn.silu(centred_short_conv(jnp.concatenate([q, k], axis=-1), conv_w, conv_b))
    q, k = jnp.split(qk, 2, axis=-1)

    def heads(t):
        return t.reshape(B, S, H, Dh).transpose(0, 2, 1, 3).astype(jnp.float32)

    qh, kh, vh = heads(q), heads(k) * (MLSTM_HEAD_DIM ** -0.5), heads(v)
    g4 = gates.reshape(B, S, 4, H).transpose(2, 0, 3, 1).astype(jnp.float32)
    i_f, f_f, i_b, f_b = g4[0], g4[1], g4[2], g4[3]
    h_f = mlstm_scan(qh, kh, vh, i_f, jax.nn.log_sigmoid(f_f))

    def fl(t):
        return jnp.flip(t, axis=2)

    h_b = fl(mlstm_scan(fl(qh), fl(kh), fl(vh), fl(i_b), fl(jax.nn.log_sigmoid(f_b))))
    hsum = (h_f + h_b).transpose(0, 2, 1, 3)
    hn = rms_norm(hsum, norm_g.reshape(H, Dh)).reshape(B, S, MLSTM_WIDTH)
    return (jax.nn.sigmoid(o.astype(jnp.float32)) * hn).astype(o.dtype)


def hyena_filters(L, w1, b1, w2, b2, w3, freq, deltas):
    f32 = jnp.float32
    t = jnp.linspace(0.0, 1.0, L, dtype=f32)[:, None]
    w = 2.0 * math.pi * jnp.arange(L, dtype=f32)[:, None] / L
    bands = jnp.linspace(1e-4, HYENA_BANDS - 1, HYENA_BANDS, dtype=f32)[None, :]
    z = jnp.concatenate([t, jnp.cos(bands * w), -jnp.sin(bands * w)], axis=-1)
    fr = freq.astype(f32)
    hid = jnp.sin(fr * (z @ w1.astype(f32) + b1.astype(f32)))
    hid = jnp.sin(fr * (hid @ w2.astype(f32) + b2.astype(f32)))
    h = hid @ w3.astype(f32)
    window = jnp.exp(-t * jnp.abs(deltas.astype(f32))) + HYENA_WINDOW_SHIFT
    return (h * window).reshape(L, HYENA_ORDER, HYENA_DIRS, HYENA_WIDTH)


def bidir_long_conv(u, h_fwd, h_bwd, bias):
    L, C = h_fwd.shape
    two_sided = jnp.concatenate([(h_fwd[0] + h_bwd[0])[None], h_fwd[1:],
                                 jnp.zeros((1, C), jnp.float32), jnp.flip(h_bwd[1:], axis=0)], axis=0)
    k_f = jnp.fft.rfft(two_sided, n=2 * L, axis=0)
    uf = u.astype(jnp.float32)
    u_f = jnp.fft.rfft(uf, n=2 * L, axis=1)
    y = jnp.fft.irfft(u_f * k_f[None], n=2 * L, axis=1)[:, :L]
    return (y + uf * bias.astype(jnp.float32)).astype(u.dtype)


def hyena_mixer(hx, conv_w, conv_b, filt, bias, norm_g):
    B, S, _ = hx.shape
    u = centred_short_conv(hx, conv_w, conv_b)
    x1, x2, z = jnp.split(u, HYENA_ORDER + 1, axis=-1)
    z = x1 * bidir_long_conv(z, filt[:, 0, 0], filt[:, 0, 1], bias[0])
    z = x2 * bidir_long_conv(z, filt[:, 1, 0], filt[:, 1, 1], bias[1])
    zg = z.reshape(B, S, HYENA_GROUPS, HYENA_GROUP_DIM)
    return rms_norm(zg, norm_g.reshape(HYENA_GROUPS, HYENA_GROUP_DIM)).reshape(B, S, HYENA_WIDTH)


def expert_choice_moe(xn, w_router, w_gate, w_up, w_down):
    B, T, D = xn.shape
    cap = EC_CAPACITY_FACTOR * T // N_EXPERTS
    aff = jax.nn.softmax((xn @ w_router).astype(jnp.float32), axis=-1)
    g, idx = lax.top_k(jnp.swapaxes(aff, 1, 2), cap)
    bidx = jnp.arange(B)[:, None, None]
    xs = xn[bidx, idx]
    hid = jax.nn.silu(jnp.einsum('becd,edf->becf', xs, w_gate)) * jnp.einsum('becd,edf->becf', xs, w_up)
    y = jnp.einsum('becf,efd->becd', hid, w_down) * g[..., None].astype(xn.dtype)
    return jnp.zeros_like(xn).at[bidx, idx].add(y)


def setup_inputs(seed: int = 0) -> dict:
    key = jax.random.key(seed)
    ks = jax.random.split(key, 32)
    f32 = jnp.float32
    D, L = D_MODEL, DEPTH

    def nrm(k, shape, scale):
        return jax.random.normal(k, shape, f32) * scale

    gate_off = 4 * MLSTM_WIDTH
    H = MLSTM_HEADS
    f_bias = jnp.linspace(3.0, 6.0, H, dtype=f32)
    b_in = nrm(ks[5], (L, PROJ_COLS), 0.02)
    b_in = b_in.at[:, gate_off + H:gate_off + 2 * H].add(f_bias)
    b_in = b_in.at[:, gate_off + 3 * H:gate_off + 4 * H].add(f_bias)
    n_filt = HYENA_ORDER * HYENA_DIRS * HYENA_WIDTH
    base_delta = jnp.tile(jnp.linspace(HYENA_SLOW_DECAY, HYENA_FAST_DECAY, HYENA_WIDTH, dtype=f32),
                          HYENA_ORDER * HYENA_DIRS)
    return {
        'x': nrm(ks[0], (BATCH, SEQ, D), 1.0),
        'c': nrm(ks[1], (BATCH, D), 1.0),
        'w_ada': nrm(ks[2], (L, D, 6 * D), 0.5 * D ** -0.5),
        'b_ada': nrm(ks[3], (L, 6 * D), 0.02),
        'g_mix': 1.0 + nrm(ks[4], (L, D), 0.02),
        'w_in': nrm(ks[6], (L, D, PROJ_COLS), D ** -0.5),
        'b_in': b_in,
        'conv_qk_w': nrm(ks[7], (L, SHORT_CONV, 2 * MLSTM_WIDTH), SHORT_CONV ** -0.5),
        'conv_qk_b': nrm(ks[8], (L, 2 * MLSTM_WIDTH), 0.02),
        'mlstm_norm_g': 1.0 + nrm(ks[9], (L, MLSTM_WIDTH), 0.02),
        'conv_hy_w': nrm(ks[10], (L, SHORT_CONV, (HYENA_ORDER + 1) * HYENA_WIDTH), SHORT_CONV ** -0.5),
        'conv_hy_b': nrm(ks[11], (L, (HYENA_ORDER + 1) * HYENA_WIDTH), 0.02),
        'hy_w1': nrm(ks[12], (L, HYENA_EMB_DIM, HYENA_FILTER_HIDDEN), HYENA_EMB_DIM ** -0.5),
        'hy_b1': nrm(ks[13], (L, HYENA_FILTER_HIDDEN), 0.1),
        'hy_w2': nrm(ks[14], (L, HYENA_FILTER_HIDDEN, HYENA_FILTER_HIDDEN), HYENA_FILTER_HIDDEN ** -0.5),
        'hy_b2': nrm(ks[15], (L, HYENA_FILTER_HIDDEN), 0.1),
        'hy_w3': nrm(ks[16], (L, HYENA_FILTER_HIDDEN, n_filt), 0.1 * HYENA_FILTER_HIDDEN ** -0.5),
        'hy_freq': 1.0 + nrm(ks[17], (L, HYENA_FILTER_HIDDEN), 0.02),
        'hy_deltas': base_delta[None] * (1.0 + nrm(ks[18], (L, n_filt), 0.02)),
        'hy_bias': nrm(ks[19], (L, HYENA_ORDER, HYENA_WIDTH), 0.1),
        'hyena_norm_g': 1.0 + nrm(ks[20], (L, HYENA_WIDTH), 0.02),
        'w_out': nrm(ks[21], (L, MIX_WIDTH, D), MIX_WIDTH ** -0.5),
        'g_ffn': 1.0 + nrm(ks[22], (L, D), 0.02),
        'w_router': nrm(ks[23], (L, D, N_EXPERTS), D ** -0.5),
        'w_gate': nrm(ks[24], (L, N_EXPERTS, D, EXPERT_FF), D ** -0.5),
        'w_up': nrm(ks[25], (L, N_EXPERTS, D, EXPERT_FF), D ** -0.5),
        'w_down': nrm(ks[26], (L, N_EXPERTS, EXPERT_FF, D), EXPERT_FF ** -0.5),
        'g_final': 1.0 + nrm(ks[27], (D,), 0.02),
    }


def reference(x, c, w_ada, b_ada, g_mix, w_in, b_in, conv_qk_w, conv_qk_b, mlstm_norm_g,
              conv_hy_w, conv_hy_b, hy_w1, hy_b1, hy_w2, hy_b2, hy_w3, hy_freq, hy_deltas, hy_bias,
              hyena_norm_g, w_out, g_ffn, w_router, w_gate, w_up, w_down, g_final):
    W = MLSTM_WIDTH
    split_pts = [W, 2 * W, 3 * W, 4 * W, 4 * W + N_GATE_COLS]
    seq_len = x.shape[1]
    for l in range(DEPTH):
        mod = (c @ w_ada[l] + b_ada[l])[:, None, :]
        sh1, sc1, gt1, sh2, sc2, gt2 = jnp.split(mod, 6, axis=-1)
        h = rms_norm(x, g_mix[l]) * (1 + sc1) + sh1
        p = h @ w_in[l] + b_in[l]
        q, k, v, o, gates, hx = jnp.split(p, split_pts, axis=-1)
        y_m = mlstm_mixer(q, k, v, o, gates, conv_qk_w[l], conv_qk_b[l], mlstm_norm_g[l])
        filt = hyena_filters(seq_len, hy_w1[l], hy_b1[l], hy_w2[l], hy_b2[l], hy_w3[l], hy_freq[l], hy_deltas[l])
        y_h = hyena_mixer(hx, conv_hy_w[l], conv_hy_b[l], filt, hy_bias[l], hyena_norm_g[l])
        mixed = jnp.concatenate([y_m, y_h], axis=-1) @ w_out[l]
        x = x + gt1 * mixed
        hf = rms_norm(x, g_ffn[l]) * (1 + sc2) + sh2
        x = x + gt2 * expert_choice_moe(hf, w_router[l], w_gate[l], w_up[l], w_down[l])
    return rms_norm(x, g_final)
```

```python
import math
import numpy as np
import ml_dtypes
import concourse.bass as bass
import concourse.mybir as mybir
from concourse.bass_utils import run_bass_kernel_spmd
from contextlib import ExitStack

F32 = mybir.dt.float32
BF16 = mybir.dt.bfloat16
I32 = mybir.dt.int32
AF = mybir.ActivationFunctionType
ALU = mybir.AluOpType
AX = mybir.AxisListType

SEQ = 4096
DM = 1024
NT = 32
EPS = 1e-6
EMIT_UNTIL = [None]
COMPUTE = ('pe', 'act', 'dve', 'pool')
SELF_SYNC = {'act': True, 'dve': True, 'pool': True, 'pe': False}
NDMA_SEMS = 6


def ap_box(ap):
    t = ap.tensor
    name = t.name
    dims = list(ap.ap)
    off = int(ap.offset)
    sp = str(ap.space() if callable(ap.space) else ap.space)
    is_dram = 'DRAM' in sp.upper() or 'HBM' in sp.upper() or type(t).__name__.startswith('DRAM') or type(t).__name__.startswith('Dram')
    if is_dram:
        lo = off
        hi = off
        for (st, cnt) in dims:
            st = int(st); cnt = int(cnt)
            if st >= 0:
                hi += st * (cnt - 1)
            else:
                lo += st * (cnt - 1)
        return (name, 0, 1, lo, hi + 1)
    if 'PSUM' in sp.upper() or type(t).__name__.startswith('PSum'):
        return (name, 0, 128, 0, 1 << 30)
    p0 = int(ap.start_partition())
    pc = int(dims[0][1])
    lo = off
    hi = off
    for (st, cnt) in dims[1:]:
        st = int(st); cnt = int(cnt)
        if st >= 0:
            hi += st * (cnt - 1)
        else:
            lo += st * (cnt - 1)
    return (name, p0, p0 + pc, lo, hi + 1)


class Op:
    __slots__ = ('stream', 'fn', 'deps', 'is_dma', 'signal', 'semval', 'dma_slot', 'idx', 'extra_waits')


class Sched:
    def __init__(self, nc, es):
        self.nc = nc
        self.es = es
        self.ops = []
        self.track = {}
        self.eng = {'pe': nc.tensor, 'act': nc.scalar, 'dve': nc.vector, 'pool': nc.gpsimd, 'sp': nc.sync}
        self.sem = {s: es.enter_context(nc.semaphore('sem_' + s)) for s in COMPUTE}
        self.dma_sems = {}
        for s in ('sp', 'pool', 'act'):
            self.dma_sems[s] = [es.enter_context(nc.semaphore(f'dsem_{s}{i}')) for i in range(NDMA_SEMS)]
        self.dma_count = {'sp': 0, 'pool': 0, 'act': 0}
        self.last_dma_ops = {'sp': [], 'pool': [], 'act': []}

    def _deps(self, boxes_r, boxes_w, idx, stream, is_dma):
        deps = set()
        for (boxes, is_w) in ((boxes_r, False), (boxes_w, True)):
            for b in boxes:
                lst = self.track.setdefault(b[0], [])
                keep = []
                for ent in lst:
                    eb, eidx, ew = ent
                    ov = not (eb[2] <= b[1] or b[2] <= eb[1] or eb[4] <= b[3] or b[4] <= eb[3])
                    if ov and (is_w or ew):
                        deps.add(eidx)
                    covered = (b[1] <= eb[1] and eb[2] <= b[2] and b[3] <= eb[3] and eb[4] <= b[4])
                    if is_w and covered:
                        continue
                    if (not is_w) and (not ew) and covered and (not is_dma):
                        eo = self.ops[eidx]
                        if eo.stream == stream and not eo.is_dma:
                            continue
                    keep.append(ent)
                keep.append([b, idx, is_w])
                self.track[b[0]] = keep
        deps.discard(idx)
        return deps

    def op(self, stream, fn, reads=(), writes=(), dma=False):
        o = Op()
        o.idx = len(self.ops)
        o.stream = stream
        o.fn = fn
        o.is_dma = dma
        o.signal = False
        o.semval = None
        o.dma_slot = None
        o.extra_waits = []
        br = [ap_box(a) for a in reads if a is not None and not isinstance(a, (int, float))]
        bw = [ap_box(a) for a in writes]
        self.ops.append(o)
        o.deps = self._deps(br, bw, o.idx, stream, dma)
        return o

    def emit(self):
        ops = self.ops
        for o in ops:
            for d in o.deps:
                po = ops[d]
                if po.is_dma:
                    continue
                if po.stream != o.stream or o.is_dma or SELF_SYNC.get(po.stream, True):
                    po.signal = True
        cnt = {s: 0 for s in COMPUTE}
        dcnt = {'sp': 0, 'pool': 0, 'act': 0}
        for o in ops:
            if o.is_dma:
                d = dcnt[o.stream]
                o.dma_slot = (d % NDMA_SEMS, 16 * (d // NDMA_SEMS + 1))
                dcnt[o.stream] = d + 1
            elif o.signal:
                cnt[o.stream] += 1
                o.semval = cnt[o.stream]
        waited = {s: {} for s in self.eng}
        nw = 0
        for o in ops:
            e = self.eng[o.stream]
            w = waited[o.stream]
            toks = []
            if o.is_dma:
                j, v = o.dma_slot
                if v > 16:
                    toks.append((('d', o.stream, j), self.dma_sems[o.stream][j], v - 16))
            for d in o.deps:
                po = ops[d]
                if po.is_dma:
                    j, v = po.dma_slot
                    toks.append((('d', po.stream, j), self.dma_sems[po.stream][j], v))
                else:
                    if po.stream == o.stream and not o.is_dma and not SELF_SYNC.get(po.stream, True):
                        continue
                    toks.append((('c', po.stream), self.sem[po.stream], po.semval))
            best = {}
            for key, sem, v in toks:
                if v is None:
                    raise RuntimeError('dep on non-signaling op')
                if w.get(key, 0) >= v:
                    continue
                if key not in best or best[key][1] < v:
                    best[key] = (sem, v)
            for key, (sem, v) in best.items():
                e.wait_ge(sem, v)
                w[key] = v
                nw += 1
            ins = o.fn(e)
            if ins is None:
                continue
            if o.is_dma:
                j, v = o.dma_slot
                ins.then_inc(self.dma_sems[o.stream][j], 16)
            elif o.signal:
                ins.then_inc(self.sem[o.stream], 1)
        self.n_waits = nw
        return cnt, dcnt

    def dma(self, out, in_, q='sp', **kw):
        return self.op(q, lambda e: e.dma_start(out=out, in_=in_, **kw), [in_], [out], dma=True)

    def mm(self, out, lhsT, rhs, start=True, stop=True, **kw):
        return self.op('pe', lambda e: e.matmul(out, lhsT, rhs, start=start, stop=stop, **kw),
                       [lhsT, rhs] + ([] if start else [out]), [out])

    def transpose(self, out, in_, ident):
        return self.op('pe', lambda e: e.transpose(out, in_, ident), [in_, ident], [out])

    def act(self, out, in_, func, scale=1.0, bias=0.0, accum_out=None, eng='act'):
        rd = [in_]
        if not isinstance(scale, (int, float)):
            rd.append(scale)
            if func == AF.Copy:
                func = AF.Identity
        if not isinstance(bias, (int, float)):
            rd.append(bias)
            if func == AF.Copy:
                func = AF.Identity
        wr = [out] + ([accum_out] if accum_out is not None else [])
        kw = {}
        if accum_out is not None:
            kw['accum_out'] = accum_out
        return self.op('act', lambda e: e.activation(out=out, in_=in_, func=func, scale=scale, bias=bias, **kw), rd, wr)

    def tt(self, out, in0, in1, op, eng='dve'):
        return self.op(eng, lambda e: e.tensor_tensor(out=out, in0=in0, in1=in1, op=op), [in0, in1], [out])

    def ts(self, out, in0, s1, op0, s2=None, op1=None, eng='dve', accum_out=None):
        rd = [in0]
        if not isinstance(s1, (int, float)):
            rd.append(s1)
        if s2 is not None and not isinstance(s2, (int, float)):
            rd.append(s2)
        kw = {}
        if op1 is not None:
            kw['op1'] = op1
        if accum_out is not None:
            kw['accum_out'] = accum_out
        wr = [out] + ([accum_out] if accum_out is not None else [])
        return self.op(eng, lambda e: e.tensor_scalar(out=out, in0=in0, scalar1=s1, scalar2=s2, op0=op0, **kw), rd, wr)

    def stt(self, out, in0, scalar, in1, op0, op1, eng='dve'):
        rd = [in0, in1]
        if not isinstance(scalar, (int, float)):
            rd.append(scalar)
        return self.op(eng, lambda e: e.scalar_tensor_tensor(out=out, in0=in0, scalar=scalar, in1=in1, op0=op0, op1=op1), rd, [out])

    def copy(self, out, in_, eng='dve'):
        if eng == 'act':
            return self.act(out, in_, AF.Copy)
        return self.op(eng, lambda e: e.tensor_copy(out=out, in_=in_), [in_], [out])

    def memset(self, out, val, eng='dve'):
        return self.op(eng, lambda e: e.memset(out, val), [], [out])

    def reduce(self, out, in_, op, axis=None, eng='dve'):
        axis = axis or AX.X
        return self.op(eng, lambda e: e.tensor_reduce(out=out, in_=in_, op=op, axis=axis), [in_], [out])

    def recip(self, out, in_, eng='dve'):
        return self.op(eng, lambda e: e.reciprocal(out=out, in_=in_), [in_], [out])

    def barrier(self):
        alld = set()
        for lst in self.track.values():
            for ent in lst:
                alld.add(ent[1])
        self.track = {}
        for s_ in ('pe', 'act', 'dve', 'pool', 'sp'):
            o = self.op(s_, lambda e: None, [], [])
            o.deps = set(alld)

    def scope(self):
        return _Scope(self)

    def finish(self, out_aps):
        boxes = [ap_box(a) for a in out_aps]
        o = self.op('sp', lambda e: None, list(out_aps), [])
        return o


class _Scope:
    def __init__(self, S):
        self.S = S
        self.st = ExitStack()

    def __enter__(self):
        self.st.__enter__()
        return self.st

    def __exit__(self, *a):
        self.S.barrier()
        return self.st.__exit__(*a)

_CONSTS = {}


def _bf(a):
    return np.ascontiguousarray(a.astype(np.float32)).astype(ml_dtypes.bfloat16)


def host_consts():
    if _CONSTS:
        return _CONSTS
    c = {}
    c['ident_bf'] = _bf(np.eye(128))
    c['ident_f'] = np.eye(128, dtype=np.float32)
    s = np.arange(128)[:, None]
    t = np.arange(128)[None, :]
    c['triU'] = (s <= t).astype(np.float32)
    c['triL'] = (s >= t).astype(np.float32)
    c['striU'] = (s < t).astype(np.float32)
    N = 8192
    n1 = np.arange(64); k1 = np.arange(64); n2 = np.arange(128); k2 = np.arange(128)
    ang = 2 * np.pi * np.outer(n1, k1) / 64
    c['F1'] = _bf(np.concatenate([np.cos(ang), -np.sin(ang)], 1))
    th = 2 * np.pi * ((n2[:, None, None] * (k1[None, :, None] + 64 * k2[None, None, :])) % N) / N
    c['GrT'] = _bf(np.cos(th).reshape(128, 64 * 128))
    c['GiT'] = _bf((-np.sin(th)).reshape(128, 64 * 128))
    c['GiNT'] = _bf((np.sin(th)).reshape(128, 64 * 128))
    ph = 2 * np.pi * np.outer(k2, n2) / 128
    Rr = np.cos(ph); Ri = np.sin(ph)
    c['Rc1'] = _bf(np.concatenate([Rr, Ri], 1))
    c['Rc2'] = _bf(np.concatenate([-Ri, Rr], 1))
    n1h = np.arange(32)
    thL = 2 * np.pi * (k1[:, None, None] * n1h[None, None, :] / 64 + k1[:, None, None] * n2[None, :, None] / N)
    c['LrT'] = _bf((np.cos(thL) / N).reshape(64, 128 * 32))
    c['LiNT'] = _bf((-np.sin(thL) / N).reshape(64, 128 * 32))
    L = SEQ
    f32 = np.float32
    tt = np.linspace(0.0, 1.0, L, dtype=f32)[:, None]
    w = (f32(2.0 * math.pi) * np.arange(L, dtype=f32)[:, None] / f32(L)).astype(f32)
    bands = np.linspace(1e-4, 15, 16, dtype=f32)[None, :]
    z = np.concatenate([tt, np.cos(bands * w), -np.sin(bands * w)], axis=-1).astype(f32)
    zr = np.concatenate([z[0:1], z[:0:-1]], 0)
    c['zT'] = np.ascontiguousarray(z.T)
    c['zrT'] = np.ascontiguousarray(zr.T)
    trow = tt[:, 0]
    trr = np.concatenate([trow[0:1], trow[:0:-1]])
    c['t_row'] = np.ascontiguousarray(trow[None, :]).astype(f32)
    c['tr_row'] = np.ascontiguousarray(trr[None, :]).astype(f32)
    _CONSTS.update(c)
    return _CONSTS


def colmaj(v, nk):
    return np.ascontiguousarray(np.asarray(v, dtype=np.float32).reshape(nk, 128).T)


def layout_inputs(inp, b):
    m = {}
    f = lambda a: np.ascontiguousarray(np.asarray(a, dtype=np.float32))
    m['x'] = f(inp['x'][b])
    m['ccol'] = colmaj(inp['c'][b], 8)
    m['w_ada'] = f(inp['w_ada'][0])
    m['b_ada'] = f(inp['b_ada'][0][None, :])
    m['gmix_col'] = colmaj(inp['g_mix'][0], 8)
    m['w_in'] = f(inp['w_in'][0])
    bin_ = np.asarray(inp['b_in'][0], dtype=np.float32)
    m['bin_row'] = f(bin_[None, 1024:2064])
    m['bqk_col'] = colmaj(bin_[0:1024], 8)
    m['bhy_col'] = colmaj(bin_[2064:3600], 12)
    cw = np.asarray(inp['conv_qk_w'][0], dtype=np.float32)
    m['cqk_w'] = np.ascontiguousarray(cw.reshape(3, 8, 128).transpose(2, 1, 0))
    m['cqk_b'] = colmaj(inp['conv_qk_b'][0], 8)
    cw = np.asarray(inp['conv_hy_w'][0], dtype=np.float32)
    m['chy_w'] = np.ascontiguousarray(cw.reshape(3, 12, 128).transpose(2, 1, 0))
    m['chy_b'] = colmaj(inp['conv_hy_b'][0], 12)
    m['mnorm_g'] = f(inp['mlstm_norm_g'][0][None, :])
    m['hy_w1'] = f(inp['hy_w1'][0])
    m['hy_b1c'] = f(inp['hy_b1'][0][:, None])
    m['hy_w2'] = f(inp['hy_w2'][0])
    m['hy_b2c'] = f(inp['hy_b2'][0][:, None])
    m['hy_w3'] = f(inp['hy_w3'][0])
    m['hy_frc'] = f(inp['hy_freq'][0][:, None])
    m['hy_del_col'] = colmaj(inp['hy_deltas'][0], 16)
    m['hy_bias_col'] = colmaj(np.asarray(inp['hy_bias'][0]).reshape(-1), 8)
    m['hnorm_col'] = colmaj(inp['hyena_norm_g'][0], 4)
    m['w_out'] = f(inp['w_out'][0])
    m['gffn_row'] = f(inp['g_ffn'][0][None, :])
    m['w_router'] = f(inp['w_router'][0])
    m['w_gate'] = f(inp['w_gate'][0]).reshape(16 * 1024, 2048)
    m['w_up'] = f(inp['w_up'][0]).reshape(16 * 1024, 2048)
    m['w_down'] = f(inp['w_down'][0]).reshape(16 * 2048, 1024)
    m['gfin_row'] = f(np.asarray(inp['g_final'])[None, :])
    m.update(host_consts())
    return m

INPUT_SPECS = [
    ('x', [SEQ, DM], F32), ('ccol', [128, 8], F32), ('w_ada', [DM, 6144], F32), ('b_ada', [1, 6144], F32),
    ('gmix_col', [128, 8], F32), ('w_in', [DM, 3600], F32), ('bin_row', [1, 1040], F32),
    ('bqk_col', [128, 8], F32), ('bhy_col', [128, 12], F32), ('cqk_w', [128, 8, 3], F32), ('cqk_b', [128, 8], F32),
    ('chy_w', [128, 12, 3], F32), ('chy_b', [128, 12], F32), ('mnorm_g', [1, 512], F32),
    ('hy_w1', [33, 64], F32), ('hy_b1c', [64, 1], F32), ('hy_w2', [64, 64], F32), ('hy_b2c', [64, 1], F32),
    ('hy_w3', [64, 2048], F32), ('hy_frc', [64, 1], F32), ('hy_del_col', [128, 16], F32),
    ('hy_bias_col', [128, 8], F32), ('hnorm_col', [128, 4], F32), ('w_out', [DM, DM], F32),
    ('gffn_row', [1, DM], F32), ('w_router', [DM, 16], F32), ('w_gate', [16 * 1024, 2048], F32),
    ('w_up', [16 * 1024, 2048], F32), ('w_down', [16 * 2048, 1024], F32), ('gfin_row', [1, DM], F32),
    ('ident_bf', [128, 128], BF16), ('ident_f', [128, 128], F32), ('triU', [128, 128], F32),
    ('triL', [128, 128], F32), ('striU', [128, 128], F32), ('F1', [64, 128], BF16),
    ('GrT', [128, 8192], BF16), ('GiT', [128, 8192], BF16), ('GiNT', [128, 8192], BF16),
    ('Rc1', [128, 256], BF16), ('Rc2', [128, 256], BF16), ('LrT', [64, 4096], BF16), ('LiNT', [64, 4096], BF16),
    ('zT', [33, SEQ], F32), ('zrT', [33, SEQ], F32), ('t_row', [1, SEQ], F32), ('tr_row', [1, SEQ], F32),
]


class Ctx:
    pass


def build(stop_after=None, debug=False):
    nc = bass.Bass("TRN2", target_bir_lowering=False)
    es = ExitStack()
    S = Sched(nc, es)
    C = Ctx()
    C.nc, C.S, C.es = nc, S, es
    C.debug = debug
    D = {}
    for name, shape, dt in INPUT_SPECS:
        D[name] = nc.dram_tensor(name, shape, dt, kind="ExternalInput").ap()
    D['out'] = nc.dram_tensor("out", [SEQ, DM], F32, kind="ExternalOutput").ap()

    def scratch(name, shape, dt):
        D[name] = nc.dram_tensor(name, shape, dt, kind="Internal").ap()
    scratch('QTd', [512, SEQ], BF16)
    scratch('KTd', [512, SEQ], BF16)
    scratch('Vd', [SEQ, 512], BF16)
    scratch('Od', [SEQ, 512], BF16)
    scratch('X1d', [512, SEQ], F32)
    scratch('X2d', [512, SEQ], F32)
    scratch('Zd', [512, SEQ], BF16)
    scratch('Z1d', [512, SEQ], BF16)
    scratch('Kd0', [512, 2 * SEQ], BF16)
    scratch('Kd1', [512, 2 * SEQ], BF16)
    scratch('Ycv', [512, SEQ], BF16)
    scratch('Yt', [DM, SEQ], BF16)
    scratch('HFd', [SEQ, DM], BF16)
    scratch('acc', [SEQ, DM], F32)
    C.D = D
    C.dbg = {}

    def dbg_out(name, shape, dt=F32):
        t = nc.dram_tensor("dbg_" + name, shape, dt, kind="ExternalOutput").ap()
        C.dbg[name] = t
        return t
    C.dbg_out = dbg_out

    cnt = [0]

    def sb(name, shape, dt, st=None):
        cnt[0] += 1
        return (st or es).enter_context(nc.sbuf_tensor(f"s{cnt[0]}_{name}", shape, dt))

    def pst(name, shape, dt, st=None):
        cnt[0] += 1
        return (st or es).enter_context(nc.psum_tensor(f"p{cnt[0]}_{name}", shape, dt))
    C.sb, C.pst = sb, pst

    P = Ctx()
    C.P = P
    P.ident_bf = sb("ident_bf", [128, 128], BF16)
    P.ident_f = sb("ident_f", [128, 128], F32)
    P.ones_f = sb("ones_f", [128, 128], F32)
    P.modcol = sb("modcol", [128, 48], F32)
    P.A1col = sb("A1col", [128, 8], F32)
    P.gt1rep = sb("gt1rep", [128, DM], F32)
    P.gt2rep = sb("gt2rep", [128, DM], F32)
    P.A2rep = sb("A2rep", [128, DM], F32)
    P.B2rep = sb("B2rep", [128, DM], F32)
    S.dma(P.ident_bf[:, :], D['ident_bf'][:, :])
    S.dma(P.ident_f[:, :], D['ident_f'][:, :])
    S.memset(P.ones_f[:, :], 1.0)

    phases = [phase_mod, phase_norm_proj, phase_mlstm, phase_hyena, phase_outproj, phase_route, phase_experts, phase_final]
    for ph in phases:
        ph(C)
        if stop_after == ph.__name__:
            break
    outs = [D['out'][:, :]] + [t for t in C.dbg.values()]
    S.finish(outs)
    S.emit()
    return nc, C


def phase_mod(C):
    S, D, P, sb, pst = C.S, C.D, C.P, C.sb, C.pst
    with S.scope() as ph:
        wada = [sb(f"wada{i}", [128, 8, 512], F32, ph) for i in range(2)]
        modrow = sb("modrow", [1, 6144], F32, ph)
        badar = sb("badar", [1, 6144], F32, ph)
        ccol = sb("ccol_sb", [128, 8], F32, ph)
        gmixc = sb("gmixc", [128, 8], F32, ph)
        gffn_rep = sb("gffn_rep", [128, DM], F32, ph)
        sc2rep = sb("sc2rep", [128, DM], F32, ph)
        ps = pst("ps_mod", [128, 512], F32, ph)
        psc = pst("ps_modc", [128, 512], F32, ph)
        psr = [pst(f"ps_modr{i}", [128, 512], F32, ph) for i in range(2)]
        S.dma(badar[:, :], D['b_ada'][:, :])
        S.dma(ccol[:, :], D['ccol'][:, :])
        S.dma(gmixc[:, :], D['gmix_col'][:, :])
        S.dma(gffn_rep[:, :], D['gffn_row'][0:1, :].partition_broadcast(128))
        wsrc = D['w_ada'].rearrange("(k p) c -> p k c", p=128)
        for blk in range(12):
            buf = wada[blk % 2]
            S.dma(buf[:, :, :], wsrc[:, :, blk * 512:(blk + 1) * 512])
            for k in range(8):
                S.mm(ps[0:1, :], ccol[:, k:k + 1], buf[:, k, :], start=(k == 0), stop=(k == 7))
            S.tt(modrow[0:1, blk * 512:(blk + 1) * 512], ps[0:1, :], badar[0:1, blk * 512:(blk + 1) * 512], ALU.add)
        for oc in range(48):
            S.mm(psc[:, oc:oc + 1], modrow[0:1, oc * 128:(oc + 1) * 128], P.ones_f[0:1, 0:1])
        S.copy(P.modcol[:, :], psc[:, 0:48])
        S.stt(P.A1col[:, :], P.modcol[:, 8:16], 1.0, gmixc[:, :], ALU.add, ALU.mult)
        n = 0
        for (dst, j) in ((P.gt1rep, 2), (P.gt2rep, 5), (sc2rep, 4), (P.B2rep, 3)):
            for h in range(2):
                pr = psr[n % 2]
                n += 1
                S.mm(pr[:, :], P.ones_f[0:1, 0:128], modrow[0:1, j * 1024 + h * 512: j * 1024 + (h + 1) * 512])
                S.copy(dst[:, h * 512:(h + 1) * 512], pr[:, :], eng=('act' if n % 2 else 'dve'))
        S.stt(P.A2rep[:, :], sc2rep[:, :], 1.0, gffn_rep[:, :], ALU.add, ALU.mult)
        if C.debug:
            d = C.dbg_out('modcol', [128, 48])
            S.dma(d[:, :], P.modcol[:, :])
            d = C.dbg_out('gt1rep', [128, DM])
            S.dma(d[:, :], P.gt1rep[:, :])


def phase_norm_proj(C):
    S, D, P, sb, pst = C.S, C.D, C.P, C.sb, C.pst
    P.Gt = sb("Gt", [128, NT, 16], F32)
    with S.scope() as ph:
        hT = sb("hT", [128, 8, SEQ], BF16, ph)
        with S.scope() as p1:
            xb = [sb(f"xb{i}", [128, DM], F32, p1) for i in range(2)]
            xn = [sb(f"xn{i}", [128, DM], BF16, p1) for i in range(2)]
            junk = sb("junk", [128, DM], BF16, p1)
            ss = sb("ss", [128, NT], F32, p1)
            rs = sb("rs", [128, NT], F32, p1)
            psT = [pst(f"psT{i}", [128, DM], BF16, p1) for i in range(2)]
            for j in range(NT):
                xt = xb[j % 2]
                S.dma(xt[:, :], D['x'][j * 128:(j + 1) * 128, :])
                S.act(junk[:, :], xt[:, :], AF.Square, accum_out=ss[:, j:j + 1])
                S.act(rs[:, j:j + 1], ss[:, j:j + 1], AF.Ln, scale=1.0 / DM, bias=EPS)
                S.act(rs[:, j:j + 1], rs[:, j:j + 1], AF.Exp, scale=-0.5)
                S.act(xn[j % 2][:, :], xt[:, :], AF.Copy, scale=rs[:, j:j + 1])
                pt = psT[j % 2]
                for k in range(8):
                    S.transpose(pt[:, k * 128:(k + 1) * 128], xn[j % 2][:, k * 128:(k + 1) * 128], P.ident_bf[:, :])
                for k in range(8):
                    S.act(hT[:, k, j * 128:(j + 1) * 128], pt[:, k * 128:(k + 1) * 128], AF.Identity,
                          scale=P.A1col[:, k:k + 1], bias=P.modcol[:, k:k + 1])
        if C.debug:
            d = C.dbg_out('hT', [128, 8, SEQ], BF16)
            for k in range(8):
                S.dma(d[:, k, :], hT[:, k, :])
        with S.scope() as p2:
            wvo = sb("wvo", [128, 8, 1040], BF16, p2)
            brow = sb("brow", [128, 1040], F32, p2)
            vt = [sb(f"vt{i}", [128, 512], BF16, p2) for i in range(2)]
            ot = [sb(f"ot{i}", [128, 512], BF16, p2) for i in range(2)]
            otf = [sb(f"otf{i}", [128, 512], F32, p2) for i in range(2)]
            psV = [pst(f"psV{i}", [128, 512], F32, p2) for i in range(2)]
            psO = [pst(f"psO{i}", [128, 512], F32, p2) for i in range(2)]
            psG = [pst(f"psG{i}", [128, 512], F32, p2) for i in range(2)]
            wsrc = D['w_in'].rearrange("(k p) c -> p k c", p=128)
            S.dma(wvo[:, :, 0:512], wsrc[:, :, 1024:1536], q='pool')
            S.dma(wvo[:, :, 512:1040], wsrc[:, :, 1536:2064], q='pool')
            S.dma(brow[:, :], D['bin_row'][0:1, :].partition_broadcast(128))
            for j in range(NT):
                a = j % 2
                for (pp, c0, c1) in ((psV[a], 0, 512), (psO[a], 512, 1024), (psG[a], 1024, 1040)):
                    for k in range(8):
                        S.mm(pp[:, 0:c1 - c0], hT[:, k, j * 128:(j + 1) * 128], wvo[:, k, c0:c1], start=(k == 0), stop=(k == 7))
                S.tt(vt[a][:, :], psV[a][:, :], brow[:, 0:512], ALU.add)
                S.dma(D['Vd'][j * 128:(j + 1) * 128, :], vt[a][:, :])
                S.tt(otf[a][:, :], psO[a][:, :], brow[:, 512:1024], ALU.add)
                S.act(ot[a][:, :], otf[a][:, :], AF.Sigmoid)
                S.dma(D['Od'][j * 128:(j + 1) * 128, :], ot[a][:, :])
                S.tt(P.Gt[:, j, :], psG[a][:, 0:16], brow[:, 1024:1040], ALU.add)
        if C.debug:
            d = C.dbg_out('Gt', [128, NT, 16])
            S.dma(d[:, :, :], P.Gt[:, :, :])
        with S.scope() as p3:
            wch = [sb(f"wch{i}", [128, 8, 128], BF16, p3) for i in range(2)]
            pre = [sb(f"pre{i}", [128, SEQ + 2], F32, p3) for i in range(2)]
            t0s = [sb(f"cv_t0{i}", [128, 2048], F32, p3) for i in range(2)]
            t2s = [sb(f"cv_t2{i}", [128, 2048], F32, p3) for i in range(2)]
            t3 = [[sb(f"cv_t3{q}{i}", [128, 2048], F32, p3) for i in range(2)] for q in range(2)]
            ob = [[sb(f"cv_ob{q}{i}", [128, 2048], BF16, p3) for i in range(2)] for q in range(2)]
            pending = [None]
            bqk = sb("bqk", [128, 8], F32, p3)
            bhy = sb("bhy", [128, 12], F32, p3)
            cqw = sb("cqw", [128, 8, 3], F32, p3)
            cqb = sb("cqb", [128, 8], F32, p3)
            chw = sb("chw", [128, 12, 3], F32, p3)
            chb = sb("chb", [128, 12], F32, p3)
            psF = [pst(f"psF{i}", [128, 512], F32, p3) for i in range(8)]
            for (tile_, nm) in ((bqk, 'bqk_col'), (bhy, 'bhy_col'), (cqb, 'cqk_b'), (chb, 'chy_b')):
                S.dma(tile_[:, :], D[nm][:, :])
            S.dma(cqw[:, :, :], D['cqk_w'][:, :, :])
            S.dma(chw[:, :, :], D['chy_w'][:, :, :])
            for i in range(2):
                S.memset(pre[i][:, 0:1], 0.0)
                S.memset(pre[i][:, SEQ + 1:SEQ + 2], 0.0)
            wsrc = D['w_in'].rearrange("(k p) c -> p k c", p=128)
            chunks = []
            for cc in range(8):
                chunks.append(('qk', cc, cc * 128))
            for i in range(12):
                chunks.append(('hy', i, 2064 + i * 128))
            nps = 0
            for n, (kind, ci, col0) in enumerate(chunks):
                wc = wch[n % 2]
                pr = pre[n % 2]
                S.dma(wc[:, :, :], wsrc[:, :, col0:col0 + 128], q='pool')
                bcol = bqk[:, ci:ci + 1] if kind == 'qk' else bhy[:, ci:ci + 1]
                cw = cqw if kind == 'qk' else chw
                cb = cqb if kind == 'qk' else chb
                for tb in range(8):
                    pp = psF[nps % 8]
                    nps += 1
                    for k in range(8):
                        S.mm(pp[:, :], wc[:, k, :], hT[:, k, tb * 512:(tb + 1) * 512], start=(k == 0), stop=(k == 7))
                    if tb % 2 == 0:
                        S.act(pr[:, 1 + tb * 512:1 + (tb + 1) * 512], pp[:, :], AF.Identity, bias=bcol)
                    else:
                        S.ts(pr[:, 1 + tb * 512:1 + (tb + 1) * 512], pp[:, :], bcol, ALU.add)
                par = n % 2
                for hh in range(2):
                    o0 = hh * 2048
                    S.act(t0s[hh][:, :], pr[:, o0:o0 + 2048], AF.Identity, scale=cw[:, ci, 0:1], bias=cb[:, ci:ci + 1])
                    S.act(t2s[hh][:, :], pr[:, o0 + 2:o0 + 2050], AF.Identity, scale=cw[:, ci, 2:3])
                for hh in range(2):
                    o0 = hh * 2048
                    S.stt(t0s[hh][:, :], pr[:, o0 + 1:o0 + 2049], cw[:, ci, 1:2], t0s[hh][:, :], ALU.mult, ALU.add)
                for hh in range(2):
                    if kind == 'hy' and ci >= 8:
                        S.tt(ob[par][hh][:, :], t0s[hh][:, :], t2s[hh][:, :], ALU.add, eng='pool')
                    else:
                        S.tt(t3[par][hh][:, :], t0s[hh][:, :], t2s[hh][:, :], ALU.add, eng='pool')
                if pending[0] is not None:
                    pending[0]()

                def fin(kind=kind, ci=ci, par=par):
                    for hh in range(2):
                        o0 = hh * 2048
                        if kind == 'qk':
                            S.act(ob[par][hh][:, :], t3[par][hh][:, :], AF.Silu)
                            dst = D['QTd'] if ci < 4 else D['KTd']
                            S.dma(dst[(ci % 4) * 128:(ci % 4 + 1) * 128, o0:o0 + 2048], ob[par][hh][:, :])
                        elif ci < 8:
                            dst = D['X1d'] if ci < 4 else D['X2d']
                            S.dma(dst[(ci % 4) * 128:(ci % 4 + 1) * 128, o0:o0 + 2048], t3[par][hh][:, :])
                        else:
                            S.dma(D['Zd'][(ci - 8) * 128:(ci - 7) * 128, o0:o0 + 2048], ob[par][hh][:, :])
                pending[0] = fin
            pending[0]()
    if C.debug:
        for nm, shp, dt in (('QTd', [512, SEQ], BF16), ('KTd', [512, SEQ], BF16), ('Vd', [SEQ, 512], BF16),
                            ('Od', [SEQ, 512], BF16), ('X1d', [512, SEQ], F32), ('Zd', [512, SEQ], BF16)):
            d = C.dbg_out(nm, shp, dt)
            with S.scope() as pd:
                if shp[0] == 512:
                    tmp = sb("dbgtmp_" + nm, [128, 4, SEQ], dt, pd)
                    S.dma(tmp[:, :, :], D[nm].rearrange("(a p) n -> p a n", p=128))
                    S.dma(d.rearrange("(a p) n -> p a n", p=128), tmp[:, :, :])
                else:
                    tmp = sb("dbgtmp_" + nm, [128, NT, 512], dt, pd)
                    S.dma(tmp[:, :, :], D[nm].rearrange("(a p) n -> p a n", p=128))
                    S.dma(d.rearrange("(a p) n -> p a n", p=128), tmp[:, :, :])


def phase_mlstm(C):
    S, D, P, sb, pst = C.S, C.D, C.P, C.sb, C.pst
    Gt = P.Gt
    with S.scope() as ph:
        triU = sb("triU", [128, 128], F32, ph)
        triL = sb("triL", [128, 128], F32, ph)
        LF = sb("LF", [128, 2, NT, 4], F32, ph)
        II = sb("II", [128, 2, NT, 4], F32, ph)
        Bc = sb("Bc", [128, 2, NT, 4], F32, ph)
        Wp = sb("Wp", [128, 2, NT, 4], F32, ph)
        ENB = sb("ENB", [128, 2, NT, 4], F32, ph)
        EG = sb("EG", [128, 2, NT, 4], F32, ph)
        zcol = sb("zcol", [128, 1], F32, ph)
        S.dma(triU[:, :], D['triU'][:, :])
        S.dma(triL[:, :], D['triL'][:, :])
        S.memset(zcol[:, :], 0.0)
        with S.scope() as pp:
            psB = pst("psB", [128, 512], F32, pp)
            psGs = pst("psGs", [128, 512], F32, pp)
            for d in range(2):
                S.act(LF[:, d, :, :], Gt[:, :, d * 8 + 4:d * 8 + 8], AF.Exp, scale=-1.0)
                S.copy(II[:, d, :, :], Gt[:, :, d * 8:d * 8 + 4])
            fl = lambda t: t[:, :, :, :].rearrange("p d c e -> p (d c e)")
            S.act(fl(LF), fl(LF), AF.Ln, bias=1.0)
            S.ts(fl(LF), fl(LF), -1.0, ALU.mult)
            S.mm(psB[:, 0:128], triU[:, :], fl(LF)[:, 0:128])
            S.mm(psB[:, 128:256], triL[:, :], fl(LF)[:, 128:256])
            S.mm(psGs[:, 0:256], P.ones_f[:, :], fl(LF))
            S.copy(fl(Bc), psB[:, 0:256])
            S.act(fl(EG), psGs[:, 0:256], AF.Exp)
            S.tt(fl(Wp), fl(II), fl(Bc), ALU.subtract)
            S.act(fl(Wp), fl(Wp), AF.Exp, bias=math.log(128.0 ** -0.5))
            S.act(fl(ENB), fl(Bc), AF.Exp, scale=-1.0)
        if C.debug:
            for nm, t in (('Bc', Bc), ('Wp', Wp), ('EG', EG)):
                d_ = C.dbg_out(nm, [128, 2, NT, 4])
                S.dma(d_[:, :, :, :], t[:, :, :, :])
        for hd in range(4):
            with S.scope() as hs:
                qT = sb("qT", [128, SEQ], BF16, hs)
                kT = sb("kT", [128, SEQ], BF16, hs)
                vaug = sb("vaug", [128, NT, 129], BF16, hs)
                osg = sb("osg", [128, NT, 128], BF16, hs)
                ktm = sb("ktm", [128, NT, 128], BF16, hs)
                Hs = sb("Hs", [128, NT, 128], F32, hs)
                sq = sb("sq", [128, NT, 128], F32, hs)
                gO = sb("gO", [128, NT, 128], BF16, hs)
                ym = sb("ym", [128, NT, 128], BF16, hs)
                ymT = sb("ymT", [128, SEQ], BF16, hs)
                mng = sb("mng", [128, 128], F32, hs)
                STw = [sb(f"STw{i}", [128, 128], BF16, hs) for i in range(2)]
                vw = [sb(f"vw{i}", [128, 129], BF16, hs) for i in range(2)]
                Tst = sb("Tst", [128, 129], F32, hs)
                Tst2 = sb("Tst2", [128, 129], F32, hs)
                Cbfd = [[sb(f"Cbf{d_}{i}", [128, 129], BF16, hs) for i in range(2)] for d_ in range(2)]
                sm = [sb(f"sm{i}", [128, 4], F32, hs) for i in range(2)]
                ssq = sb("ssq", [128, NT], F32, hs)
                pS = [pst(f"pS{i}", [128, 512], F32, hs) for i in range(2)]
                pO = [pst(f"pO{i}", [128, 512], F32, hs) for i in range(2)]
                pC = [pst(f"pC{i}", [128, 512], F32, hs) for i in range(2)]
                pK = pst("pK", [128, 1024], BF16, hs)
                S.dma(qT[:, :], D['QTd'][hd * 128:(hd + 1) * 128, :])
                S.dma(kT[:, :], D['KTd'][hd * 128:(hd + 1) * 128, :])
                S.dma(vaug[:, :, 0:128], D['Vd'][:, hd * 128:(hd + 1) * 128].rearrange("(c p) d -> p c d", p=128))
                S.memset(vaug[:, :, 128:129], 1.0)
                S.dma(osg[:, :, :], D['Od'][:, hd * 128:(hd + 1) * 128].rearrange("(c p) d -> p c d", p=128))
                S.dma(mng[:, :], D['mnorm_g'][0:1, hd * 128:(hd + 1) * 128].partition_broadcast(128))
                for c0 in range(0, NT, 8):
                    for c in range(c0, c0 + 8):
                        S.transpose(pK[:, (c - c0) * 128:(c - c0 + 1) * 128], kT[:, c * 128:(c + 1) * 128], P.ident_bf[:, :])
                    S.copy(ktm[:, c0:c0 + 8, :].rearrange("p c d -> p (c d)"), pK[:, :], eng=('act' if (c0 // 8) % 2 else 'dve'))
                n = 0
                Tsts = [Tst, Tst2]
                prevc = [None, None]
                visited = set()
                for d in range(2):
                    S.memset(Tsts[d][:, :], 0.0)
                    S.memset(Cbfd[d][0][:, :], 0.0)
                nd = [0, 0]
                for i in range(NT):
                    for d in range(2):
                        c = i if d == 0 else NT - 1 - i
                        mask = triU if d == 0 else triL
                        a = n % 2
                        n += 1
                        b_ = nd[d] % 2
                        nd[d] += 1
                        cs = slice(c * 128, (c + 1) * 128)
                        wcol = Wp[:, d, c, hd:hd + 1]
                        S.mm(pS[a][:, 0:128], kT[:, cs], qT[:, cs])
                        S.stt(STw[a][:, :], pS[a][:, 0:128], wcol, mask[:, :], ALU.mult, ALU.mult)
                        S.act(vw[a][:, :], vaug[:, c, :], AF.Copy, scale=wcol)
                        S.mm(pO[a][:, 0:129], STw[a][:, :], vaug[:, c, :], start=True, stop=False)
                        S.mm(pO[a][:, 0:129], qT[:, cs], Cbfd[d][b_][:, :], start=False, stop=True)
                        S.mm(pC[a][:, 0:129], ktm[:, c, :], vw[a][:, :])
                        enb = ENB[:, d, c, hd:hd + 1]
                        s_ = sm[a]
                        S.ts(s_[:, 0:1], pO[a][:, 128:129], enb, ALU.max)
                        S.stt(s_[:, 2:3], pO[a][:, 128:129], -1.0, s_[:, 0:1], ALU.mult, ALU.max)
                        S.recip(s_[:, 3:4], s_[:, 2:3])
                        if c not in visited:
                            visited.add(c)
                            S.act(Hs[:, c, :], pO[a][:, 0:128], AF.Copy, scale=s_[:, 3:4])
                        else:
                            S.stt(Hs[:, c, :], pO[a][:, 0:128], s_[:, 3:4], Hs[:, c, :], ALU.mult, ALU.add)
                        egp = zcol[:, 0:1] if prevc[d] is None else EG[:, d, prevc[d], hd:hd + 1]
                        S.stt(Tsts[d][:, :], Tsts[d][:, :], egp, pC[a][:, 0:129], ALU.mult, ALU.add)
                        S.act(Cbfd[d][(b_ + 1) % 2][:, :], Tsts[d][:, :], AF.Copy, scale=EG[:, d, c, hd:hd + 1])
                        prevc[d] = c
                S.act(sq[:, :, :], Hs[:, :, :], AF.Square)
                S.reduce(ssq[:, :], sq[:, :, :], ALU.add)
                S.ts(ssq[:, :], ssq[:, :], 1.0 / 128.0, ALU.mult, s2=EPS, op1=ALU.add)
                S.act(ssq[:, :], ssq[:, :], AF.Sqrt)
                S.recip(ssq[:, :], ssq[:, :])
                S.tt(gO[:, :, :], osg[:, :, :], mng[:, :].unsqueeze(1).to_broadcast([128, NT, 128]), ALU.mult)
                for c in range(NT):
                    S.stt(ym[:, c, :], Hs[:, c, :], ssq[:, c:c + 1], gO[:, c, :], ALU.mult, ALU.mult)
                for c0 in range(0, NT, 8):
                    for c in range(c0, c0 + 8):
                        S.transpose(pK[:, (c - c0) * 128:(c - c0 + 1) * 128], ym[:, c, :], P.ident_bf[:, :])
                    S.copy(ymT[:, c0 * 128:(c0 + 8) * 128], pK[:, :], eng=('act' if (c0 // 8) % 2 else 'dve'))
                S.dma(D['Yt'][hd * 128:(hd + 1) * 128, :], ymT[:, :])
    if C.debug:
        d_ = C.dbg_out('Yt_m', [512, SEQ], BF16)
        with S.scope() as pd:
            tmp = sb("dbgtmp_ytm", [128, 4, SEQ], BF16, pd)
            S.dma(tmp[:, :, :], D['Yt'][0:512, :].rearrange("(a p) n -> p a n", p=128))
            S.dma(d_.rearrange("(a p) n -> p a n", p=128), tmp[:, :, :])


def phase_hyena(C):
    S, D, P, sb, pst = C.S, C.D, C.P, C.sb, C.pst
    PI = math.pi
    with S.scope() as pa:
        hid2T = sb("hid2T", [64, SEQ], F32, pa)
        hid2rT = sb("hid2rT", [64, SEQ], F32, pa)
        frc = sb("frc", [64, 1], F32, pa)
        frb1 = sb("frb1", [64, 1], F32, pa)
        frb2 = sb("frb2", [64, 1], F32, pa)
        with S.scope() as p1:
            zT = sb("zT", [33, SEQ], F32, p1)
            zrT = sb("zrT", [33, SEQ], F32, p1)
            hid1 = sb("hid1", [64, SEQ], F32, p1)
            w1 = sb("hw1", [33, 64], F32, p1)
            w2 = sb("hw2", [64, 64], F32, p1)
            b1c = sb("b1c", [64, 1], F32, p1)
            b2c = sb("b2c", [64, 1], F32, p1)
            arg = [sb(f"harg{i}", [64, 512], F32, p1) for i in range(2)]
            m1 = [sb(f"hm1{i}", [64, 512], F32, p1) for i in range(2)]
            m2 = [sb(f"hm2{i}", [64, 512], F32, p1) for i in range(2)]
            psM = [pst(f"psM{i}", [128, 512], F32, p1) for i in range(2)]
            S.dma(zT[:, :], D['zT'][:, :])
            S.dma(zrT[:, :], D['zrT'][:, :])
            S.dma(w1[:, :], D['hy_w1'][:, :])
            S.dma(w2[:, :], D['hy_w2'][:, :])
            S.dma(b1c[:, :], D['hy_b1c'][:, :])
            S.dma(b2c[:, :], D['hy_b2c'][:, :])
            S.dma(frc[:, :], D['hy_frc'][:, :])
            S.tt(frb1[:, :], frc[:, :], b1c[:, :], ALU.mult)
            S.tt(frb2[:, :], frc[:, :], b2c[:, :], ALU.mult)
            n = 0

            def sin_layer(ps, frb, dst):
                nonlocal n
                a = n % 2
                n += 1
                S.ts(arg[a][:, :], ps, frc[:, 0:1], ALU.mult, s2=frb[:, 0:1], op1=ALU.add)
                S.ts(m1[a][:, :], arg[a][:, :], PI, ALU.is_gt, s2=-2.0 * PI, op1=ALU.mult)
                S.ts(m2[a][:, :], arg[a][:, :], -PI, ALU.is_lt, s2=2.0 * PI, op1=ALU.mult)
                S.tt(arg[a][:, :], arg[a][:, :], m1[a][:, :], ALU.add)
                S.tt(arg[a][:, :], arg[a][:, :], m2[a][:, :], ALU.add)
                S.act(dst, arg[a][:, :], AF.Sin)
            for (zs, hdst) in ((zT, hid2T), (zrT, hid2rT)):
                for blk in range(8):
                    ps = psM[blk % 2]
                    S.mm(ps[0:64, :], w1[:, :], zs[:, blk * 512:(blk + 1) * 512])
                    sin_layer(ps[0:64, :], frb1, hid1[:, blk * 512:(blk + 1) * 512])
                for blk in range(8):
                    ps = psM[blk % 2]
                    S.mm(ps[0:64, :], w2[:, :], hid1[:, blk * 512:(blk + 1) * 512])
                    sin_layer(ps[0:64, :], frb2, hdst[:, blk * 512:(blk + 1) * 512])
        with S.scope() as p2:
            w3 = sb("hw3", [64, 2048], F32, p2)
            trow = sb("trow", [128, SEQ], F32, p2)
            trrow = sb("trrow", [128, SEQ], F32, p2)
            ndel = sb("ndel", [128, 16], F32, p2)
            hbias = sb("hbias", [128, 8], F32, p2)
            kT = [sb(f"kTb{i}", [128, 2 * SEQ], BF16, p2) for i in range(2)]
            win = [sb(f"win{i}", [128, 512], F32, p2) for i in range(2)]
            k0t = sb("k0t", [128, 4], F32, p2)
            psK_ = [pst(f"psKf{i}", [128, 512], F32, p2) for i in range(3)]
            ps0 = pst("psK0", [128, 512], F32, p2)
            S.dma(w3[:, :], D['hy_w3'][:, :])
            S.dma(trow[:, :], D['t_row'][0:1, :].partition_broadcast(128))
            S.dma(trrow[:, :], D['tr_row'][0:1, :].partition_broadcast(128))
            S.dma(ndel[:, :], D['hy_del_col'][:, :])
            S.dma(hbias[:, :], D['hy_bias_col'][:, :])
            S.stt(ndel[:, :], ndel[:, :], -1.0, ndel[:, :], ALU.mult, ALU.max)
            S.ts(ndel[:, :], ndel[:, :], -1.0, ALU.mult)
            nb = 0
            nk = 0
            for o in range(2):
                for g in range(4):
                    kt = kT[nk % 2]
                    nk += 1
                    for d in range(2):
                        hs = hid2T if d == 0 else hid2rT
                        tr = trow if d == 0 else trrow
                        c0 = (o * 2 + d) * 512 + g * 128
                        di = o * 8 + d * 4 + g
                        for blk in range(8):
                            ps = psK_[nb % 3]
                            wn = win[nb % 2]
                            nb += 1
                            S.mm(ps[:, :], w3[:, c0:c0 + 128], hs[:, blk * 512:(blk + 1) * 512])
                            S.act(wn[:, :], tr[:, blk * 512:(blk + 1) * 512], AF.Exp, scale=ndel[:, di:di + 1])
                            S.stt(kt[:, d * SEQ + blk * 512:d * SEQ + (blk + 1) * 512], wn[:, :], 0.05, ps[:, :], ALU.add, ALU.mult)
                    S.memset(kt[:, SEQ:SEQ + 1], 0.0)
                    cf = (o * 2 + 0) * 512 + g * 128
                    cb = (o * 2 + 1) * 512 + g * 128
                    S.mm(ps0[:, 0:1], w3[:, cf:cf + 128], hid2T[:, 0:1])
                    S.mm(ps0[:, 1:2], w3[:, cb:cb + 128], hid2T[:, 0:1])
                    S.copy(k0t[:, 0:2], ps0[:, 0:2])
                    S.tt(k0t[:, 2:3], k0t[:, 0:1], k0t[:, 1:2], ALU.add)
                    S.ts(k0t[:, 3:4], k0t[:, 2:3], 1.05, ALU.mult)
                    S.tt(kt[:, 0:1], k0t[:, 3:4], hbias[:, o * 4 + g:o * 4 + g + 1], ALU.add)
                    dst = D['Kd0'] if o == 0 else D['Kd1']
                    S.dma(dst[g * 128:(g + 1) * 128, :], kt[:, :])
    if C.debug:
        d_ = C.dbg_out('Kd0', [512, 2 * SEQ], BF16)
        with S.scope() as pd:
            tmp = sb("dbgtmp_kd", [128, 4, 2 * SEQ], BF16, pd)
            S.dma(tmp[:, :, :], D['Kd0'].rearrange("(a p) n -> p a n", p=128))
            S.dma(d_.rearrange("(a p) n -> p a n", p=128), tmp[:, :, :])
    with S.scope() as pb:
        T = Ctx()
        T.F1 = sb("F1", [64, 128], BF16, pb)
        T.GrT = sb("GrT", [128, 64, 128], BF16, pb)
        T.GiT = sb("GiT", [128, 64, 128], BF16, pb)
        T.GiNT = sb("GiNT", [128, 64, 128], BF16, pb)
        T.Rc1 = sb("Rc1", [128, 256], BF16, pb)
        T.Rc2 = sb("Rc2", [128, 256], BF16, pb)
        T.LrT = sb("LrT", [64, 128, 32], BF16, pb)
        T.LiNT = sb("LiNT", [64, 128, 32], BF16, pb)
        hnorm = sb("hnorm", [128, 4], F32, pb)
        S.dma(T.F1[:, :], D['F1'][:, :])
        for nm in ('GrT', 'GiT', 'GiNT'):
            S.dma(getattr(T, nm)[:, :, :], D[nm].rearrange("p (k m) -> p k m", k=64))
        S.dma(T.Rc1[:, :], D['Rc1'][:, :])
        S.dma(T.Rc2[:, :], D['Rc2'][:, :])
        S.dma(T.LrT[:, :, :], D['LrT'].rearrange("p (n m) -> p n m", n=128))
        S.dma(T.LiNT[:, :, :], D['LiNT'].rearrange("p (n m) -> p n m", n=128))
        S.dma(hnorm[:, :], D['hnorm_col'][:, :])
        for o in range(2):
            zsrc = D['Zd'] if o == 0 else D['Z1d']
            kd = D['Kd0'] if o == 0 else D['Kd1']
            with S.scope() as pw:
                W = Ctx()
                W.X = sb("fX", [64, 64, 128], BF16, pw)
                W.AT = sb("fAT", [128, 64, 2, 64], BF16, pw)
                W.Kf = sb("fKf", [128, 64, 2, 64], BF16, pw)
                W.V = sb("fV", [128, 2, 64, 64], BF16, pw)
                W.Wt = sb("fWt", [64, 64, 2, 128], BF16, pw)
                W.Ysb = sb("fY", [32, 64, 128], BF16, pw)
                W.tm = [[sb(f"ftm{i}{j}", [128, 4, 64], F32, pw) for j in range(4)] for i in range(2)]
                W.ring = [pst(f"psR{i}", [128, 512], F32, pw) for i in range(8)]
                W.nr = 0
                W.ne = 0
                for b8 in range(8):
                    ch0 = b8 * 64
                    fft_batch(C, T, W, zsrc[ch0:ch0 + 64, :], kd[ch0:ch0 + 64, :], D['Ycv'][ch0:ch0 + 64, :])
            with S.scope() as pg:
                ysb = [sb(f"gy{i}", [128, SEQ], BF16, pg) for i in range(2)]
                xsb = [sb(f"gx{i}", [128, SEQ], F32, pg) for i in range(2)]
                zo = [sb(f"gz{i}", [128, SEQ], BF16, pg) for i in range(2)]
                xsrc = D['X1d'] if o == 0 else D['X2d']
                if o == 1:
                    z2 = sb("gz2", [128, SEQ], F32, pg)
                    sq = [sb(f"gsq{i}", [128, 512], F32, pg) for i in range(2)]
                    rr = [sb(f"grr{i}", [128, 512], F32, pg) for i in range(2)]
                    psN = [pst(f"psN{i}", [128, 512], F32, pg) for i in range(2)]
                for g in range(4):
                    a = g % 2
                    S.dma(ysb[a][:, :], D['Ycv'][g * 128:(g + 1) * 128, :])
                    S.dma(xsb[a][:, :], xsrc[g * 128:(g + 1) * 128, :])
                    if o == 0:
                        S.tt(zo[a][:, :], xsb[a][:, :], ysb[a][:, :], ALU.mult)
                        S.dma(D['Z1d'][g * 128:(g + 1) * 128, :], zo[a][:, :])
                    else:
                        S.tt(z2[:, :], xsb[a][:, :], ysb[a][:, :], ALU.mult)
                        for blk in range(8):
                            bs = slice(blk * 512, (blk + 1) * 512)
                            q_ = blk % 2
                            S.act(sq[q_][:, :], z2[:, bs], AF.Square)
                            S.mm(psN[q_][:, :], P.ones_f[:, :], sq[q_][:, :])
                            S.ts(rr[q_][:, :], psN[q_][:, :], 1.0 / 128.0, ALU.mult, s2=EPS, op1=ALU.add)
                            S.act(rr[q_][:, :], rr[q_][:, :], AF.Sqrt)
                            S.recip(rr[q_][:, :], rr[q_][:, :])
                            S.stt(zo[a][:, bs], z2[:, bs], hnorm[:, g:g + 1], rr[q_][:, :], ALU.mult, ALU.mult)
                        S.dma(D['Yt'][512 + g * 128:512 + (g + 1) * 128, :], zo[a][:, :])
    if C.debug:
        d_ = C.dbg_out('Yt_h', [512, SEQ], BF16)
        with S.scope() as pd:
            tmp = sb("dbgtmp_yth", [128, 4, SEQ], BF16, pd)
            S.dma(tmp[:, :, :], D['Yt'][512:1024, :].rearrange("(a p) n -> p a n", p=128))
            S.dma(d_.rearrange("(a p) n -> p a n", p=128), tmp[:, :, :])


def fft_batch(C, T, W, zsrc, kdsrc, ydst):
    S = C.S

    def bank():
        W.nr += 1
        return W.ring[W.nr % 8]

    def ev(dst, src):
        W.ne += 1
        S.copy(dst, src, eng=('act' if W.ne % 2 else 'dve'))

    def forward(kdim, mode):
        for cq in range(16):
            pA = bank()
            for i in range(4):
                ch = cq * 4 + i
                S.mm(pA[:, i * 128:(i + 1) * 128], W.X[0:kdim, ch, :], T.F1[0:kdim, :])
            ev(W.AT[:, cq * 4:(cq + 1) * 4, :, :].rearrange("p c r k -> p (c r k)"), pA[:, :])
        for kb in range(16):
            pU = bank()
            for i in range(4):
                k1 = kb * 4 + i
                ur = pU[:, i * 128:i * 128 + 64]
                ui = pU[:, i * 128 + 64:(i + 1) * 128]
                Ar = W.AT[:, :, 0, k1]
                Ai = W.AT[:, :, 1, k1]
                S.mm(ur, T.GrT[:, k1, :], Ar, start=True, stop=False)
                S.mm(ur, T.GiNT[:, k1, :], Ai, start=False, stop=True)
                S.mm(ui, T.GiT[:, k1, :], Ar, start=True, stop=False)
                S.mm(ui, T.GrT[:, k1, :], Ai, start=False, stop=True)
            if mode == 'kernel':
                ev(W.Kf[:, kb * 4:(kb + 1) * 4, :, :].rearrange("p k r c -> p (k r c)"), pU[:, :])
            else:
                pv = pU[:, :].rearrange("p (k r c) -> p k r c", k=4, r=2)
                Ur = pv[:, :, 0, :]
                Ui = pv[:, :, 1, :]
                Kr = W.Kf[:, kb * 4:(kb + 1) * 4, 0, :]
                Ki = W.Kf[:, kb * 4:(kb + 1) * 4, 1, :]
                t = W.tm[kb % 2]
                S.tt(t[0][:, :, :], Ur, Kr, ALU.mult)
                S.tt(t[1][:, :, :], Ui, Ki, ALU.mult)
                S.tt(t[2][:, :, :], Ur, Ki, ALU.mult)
                S.tt(t[3][:, :, :], Ui, Kr, ALU.mult)
                S.tt(W.V[:, 0, kb * 4:(kb + 1) * 4, :], t[0][:, :, :], t[1][:, :, :], ALU.subtract, eng='pool')
                S.tt(W.V[:, 1, kb * 4:(kb + 1) * 4, :], t[2][:, :, :], t[3][:, :, :], ALU.add, eng='pool')

    S.dma(W.X[:, :, :], kdsrc.rearrange("c (a n) -> a c n", n=128))
    forward(64, 'kernel')
    S.dma(W.X[0:32, :, :], zsrc.rearrange("c (a n) -> a c n", n=128))
    forward(32, 'data')
    for cp in range(32):
        pW = bank()
        for i in range(2):
            ch = cp * 2 + i
            o_ = pW[0:64, i * 256:(i + 1) * 256]
            S.mm(o_, W.V[:, 0, :, ch], T.Rc1[:, :], start=True, stop=False)
            S.mm(o_, W.V[:, 1, :, ch], T.Rc2[:, :], start=False, stop=True)
        ev(W.Wt[:, cp * 2:(cp + 1) * 2, :, :].rearrange("p c r n -> p (c r n)"), pW[0:64, :])
    for nb in range(16):
        pY = bank()
        for i in range(8):
            n2 = nb * 8 + i
            o_ = pY[0:32, i * 64:(i + 1) * 64]
            S.mm(o_, T.LrT[:, n2, :], W.Wt[:, :, 0, n2], start=True, stop=False)
            S.mm(o_, T.LiNT[:, n2, :], W.Wt[:, :, 1, n2], start=False, stop=True)
        ev(W.Ysb[:, :, nb * 8:(nb + 1) * 8], pY[0:32, :].rearrange("p (n c) -> p c n", n=8))
    S.dma(ydst.rearrange("c (a n) -> a c n", n=128), W.Ysb[:, :, :])


def phase_outproj(C):
    S, D, P, sb, pst, nc = C.S, C.D, C.P, C.sb, C.pst, C.nc
    P.IDX = sb("IDX", [128, 16, 4], I32)
    P.GW = sb("GW", [128, 16, 4], F32)
    with S.scope() as ph:
        AFF = sb("AFF", [128, NT, 16], F32, ph)
        AFFT = sb("AFFT", [16, SEQ], F32, ph)
        with S.scope() as p1:
            ytT = sb("ytT", [128, 8, SEQ], BF16, p1)
            wout = sb("wout", [128, 8, DM], BF16, p1)
            wr = sb("wr", [128, 8, 16], F32, p1)
            xb = [sb(f"oxb{i}", [128, DM], F32, p1) for i in range(2)]
            x1 = [sb(f"ox1{i}", [128, DM], F32, p1) for i in range(2)]
            tmp = [sb(f"otmp{i}", [128, DM], F32, p1) for i in range(2)]
            hf = [sb(f"ohf{i}", [128, DM], F32, p1) for i in range(2)]
            hfb = [sb(f"ohfb{i}", [128, DM], BF16, p1) for i in range(2)]
            hfT = [sb(f"ohfT{i}", [128, 8, 128], F32, p1) for i in range(2)]
            junk = sb("ojunk", [128, DM], BF16, p1)
            sm = sb("osm", [128, NT, 8], F32, p1)
            ex = [sb(f"oex{i}", [128, 16], F32, p1) for i in range(2)]
            psM = [[pst(f"psMo{i}{h}", [128, 512], F32, p1) for h in range(2)] for i in range(2)]
            psT = [pst(f"psTo{i}", [128, 512], F32, p1) for i in range(2)]
            psR = pst("psR", [128, 512], F32, p1)
            psAT = pst("psAT", [128, 512], F32, p1)
            for k in range(8):
                S.dma(ytT[:, k, :], D['Yt'][k * 128:(k + 1) * 128, :])
            wsrc = D['w_out'].rearrange("(k p) c -> p k c", p=128)
            S.dma(wout[:, :, 0:512], wsrc[:, :, 0:512], q='pool')
            S.dma(wout[:, :, 512:1024], wsrc[:, :, 512:1024], q='pool')
            S.dma(wr[:, :, :], D['w_router'].rearrange("(k p) e -> p k e", p=128))
            def stage_a(j):
                a = j % 2
                xt = xb[a]
                S.dma(xt[:, :], D['x'][j * 128:(j + 1) * 128, :])
                for h in range(2):
                    for k in range(8):
                        S.mm(psM[a][h][:, :], ytT[:, k, j * 128:(j + 1) * 128], wout[:, k, h * 512:(h + 1) * 512],
                             start=(k == 0), stop=(k == 7))
                for h in range(2):
                    S.tt(tmp[a][:, h * 512:(h + 1) * 512], psM[a][h][:, :], P.gt1rep[:, h * 512:(h + 1) * 512], ALU.mult)
                S.tt(x1[a][:, :], tmp[a][:, :], xt[:, :], ALU.add)
                S.dma(D['acc'][j * 128:(j + 1) * 128, :], x1[a][:, :])
                ss = sm[:, j, 0:1]
                rs = sm[:, j, 1:2]
                S.act(junk[:, :], x1[a][:, :], AF.Square, accum_out=ss)
                S.act(rs, ss, AF.Ln, scale=1.0 / DM, bias=EPS)
                S.act(rs, rs, AF.Exp, scale=-0.5)
                S.stt(hf[a][:, :], x1[a][:, :], rs, P.A2rep[:, :], ALU.mult, ALU.mult)
                S.tt(hf[a][:, :], hf[a][:, :], P.B2rep[:, :], ALU.add)
                S.act(hfb[a][:, :], hf[a][:, :], AF.Copy)
                S.dma(D['HFd'][j * 128:(j + 1) * 128, :], hfb[a][:, :])

            def stage_b(j):
                a = j % 2
                for k in range(8):
                    S.transpose(psT[k // 4][:, (k % 4) * 128:(k % 4 + 1) * 128], hf[a][:, k * 128:(k + 1) * 128], P.ident_f[:, :])
                S.copy(hfT[a][:, 0:4, :].rearrange("p k t -> p (k t)"), psT[0][:, :], eng='act')
                S.copy(hfT[a][:, 4:8, :].rearrange("p k t -> p (k t)"), psT[1][:, :], eng='dve')
                for k in range(8):
                    S.mm(psR[:, 0:16], hfT[a][:, k, :], wr[:, k, :], start=(k == 0), stop=(k == 7))
                mx = sm[:, j, 2:3]
                se = sm[:, j, 3:4]
                S.reduce(mx, psR[:, 0:16], ALU.max)
                S.ts(mx, mx, -1.0, ALU.mult)
                S.act(ex[a][:, :], psR[:, 0:16], AF.Exp, bias=mx, accum_out=se)
                S.recip(se, se)
                S.ts(AFF[:, j, :], ex[a][:, :], se, ALU.mult)
                S.transpose(psAT[0:16, 0:128], AFF[:, j, :], P.ident_f[:, :])
                S.copy(AFFT[:, j * 128:(j + 1) * 128], psAT[0:16, 0:128], eng='act')
            for step in range(NT + 1):
                if step < NT:
                    stage_a(step)
                if step >= 1:
                    stage_b(step - 1)
            if C.debug:
                pass
        if C.debug:
            d_ = C.dbg_out('AFF', [128, NT, 16])
            S.dma(d_[:, :, :], AFF[:, :, :])
        with S.scope() as p2:
            junkA = sb("junkA", [16, SEQ], F32, p2)
            bs = sb("bis", [16, 8], F32, p2)
            THR = sb("THR", [128, 16], F32, p2)
            throw = sb("throw", [1, 16], F32, p2)
            SEL = sb("SEL", [128, NT, 16], F32, p2)
            POS = sb("POS", [128, NT, 16], F32, p2)
            selcum = sb("selcum", [128, 16], F32, p2)
            striU = sb("striU", [128, 128], F32, p2)
            COORD = sb("COORD", [128, NT, 16, 4], BF16, p2)
            jf = sb("jf", [128, NT], F32, p2)
            pf = sb("pf", [128, 1], F32, p2)
            iot = sb("iot", [128, 512], F32, p2)
            OH = [sb(f"OH{i}", [128, 512], BF16, p2) for i in range(3)]
            r4 = [sb(f"r4{i}", [128, 8], F32, p2) for i in range(2)]
            psI = [pst(f"psI{i}", [128, 512], F32, p2) for i in range(4)]
            psP = [pst(f"psP{i}", [128, 512], F32, p2) for i in range(2)]
            psX = pst("psX", [128, 512], F32, p2)
            psB2 = pst("psB2", [128, 512], F32, p2)
            lo, hi, mid, cnt, ge, d1, d2 = [bs[:, i:i + 1] for i in range(7)]
            S.dma(striU[:, :], D['striU'][:, :])
            S.memset(lo, 0.0)
            S.memset(hi, 1.0)
            for it in range(30):
                S.tt(mid, lo, hi, ALU.add)
                S.ts(mid, mid, 0.5, ALU.mult)
                S.ts(junkA[:, :], AFFT[:, :], mid, ALU.is_ge, s2=0.0, op1=ALU.add, accum_out=cnt)
                S.ts(ge, cnt, 511.5, ALU.is_gt)
                S.tt(d1, mid, lo, ALU.subtract)
                S.tt(d2, hi, mid, ALU.subtract)
                S.stt(lo, d1, ge, lo, ALU.mult, ALU.add)
                S.stt(hi, d2, ge, mid, ALU.mult, ALU.add)
            S.transpose(psX[0:1, 0:16], lo, P.ident_f[0:16, 0:16])
            S.copy(throw[:, :], psX[0:1, 0:16])
            S.mm(psB2[:, 0:16], P.ones_f[0:1, 0:128], throw[0:1, :])
            S.copy(THR[:, :], psB2[:, 0:16])
            S.tt(SEL[:, :, :], AFF[:, :, :], THR[:, :].unsqueeze(1).to_broadcast([128, NT, 16]), ALU.is_ge)
            S.memset(selcum[:, :], 0.0)
            for j in range(NT):
                pp = psP[j % 2]
                S.mm(pp[:, 0:16], striU[:, :], SEL[:, j, :], start=True, stop=False)
                S.mm(pp[:, 0:16], P.ones_f[:, :], selcum[:, :], start=False, stop=True)
                S.copy(POS[:, j, :], pp[:, 0:16], eng='act')
                S.tt(selcum[:, :], selcum[:, :], SEL[:, j, :], ALU.add)
            S.op('pool', lambda e: e.iota(jf[:, :], [[1, NT]], base=0, channel_multiplier=0, allow_small_or_imprecise_dtypes=True), [], [jf[:, :]])
            S.op('pool', lambda e: e.iota(pf[:, :], [[1, 1]], base=0, channel_multiplier=1, allow_small_or_imprecise_dtypes=True), [], [pf[:, :]])
            S.op('pool', lambda e: e.iota(iot[:, :], [[1, 512]], base=0, channel_multiplier=0, allow_small_or_imprecise_dtypes=True), [], [iot[:, :]])
            S.copy(COORD[:, :, :, 0], jf[:, :].unsqueeze(2).to_broadcast([128, NT, 16]))
            S.copy(COORD[:, :, :, 1], pf[:, 0:1].unsqueeze(2).to_broadcast([128, NT, 16]))
            S.copy(COORD[:, :, :, 2], AFF[:, :, :])
            S.tt(COORD[:, :, :, 3], AFF[:, :, :], COORD[:, :, :, 2], ALU.subtract)
            n = 0
            for e_ in range(16):
                for j in range(NT):
                    oh = OH[n % 3]
                    n += 1
                    S.ts(oh[:, :], iot[:, :], POS[:, j, e_:e_ + 1], ALU.is_equal, s2=SEL[:, j, e_:e_ + 1], op1=ALU.mult)
                    for sc in range(4):
                        S.mm(psI[sc][:, 0:4], oh[:, sc * 128:(sc + 1) * 128], COORD[:, j, e_, :], start=(j == 0), stop=(j == NT - 1))
                for sc in range(4):
                    r = r4[(e_ * 4 + sc) % 2]
                    S.copy(r[:, 0:4], psI[sc][:, 0:4], eng='act')
                    S.stt(r[:, 4:5], r[:, 0:1], 128.0, r[:, 1:2], ALU.mult, ALU.add)
                    S.copy(P.IDX[:, e_, sc:sc + 1], r[:, 4:5])
                    S.tt(P.GW[:, e_, sc:sc + 1], r[:, 2:3], r[:, 3:4], ALU.add)
        if C.debug:
            d_ = C.dbg_out('IDX', [128, 16, 4], I32)
            S.dma(d_[:, :, :], P.IDX[:, :, :])
            d_ = C.dbg_out('GW', [128, 16, 4])
            S.dma(d_[:, :, :], P.GW[:, :, :])


def phase_route(C):
    pass


def phase_experts(C):
    S, D, P, sb, pst, nc = C.S, C.D, C.P, C.sb, C.pst, C.nc
    with S.scope() as ph:
        stg = [sb(f"stg{i}", [128, 8, 512], F32, ph) for i in range(4)]
        wb = [sb(f"wb{i}", [128, 8, 512], BF16, ph) for i in range(8)]
        xs = sb("xs", [128, 4, DM], BF16, ph)
        xsT = sb("xsT", [128, 8, 512], BF16, ph)
        hidT = sb("hidT", [128, 16, 512], BF16, ph)
        sg = [sb(f"sg{i}", [128, 512], F32, ph) for i in range(2)]
        ysb = [sb(f"ysb{i}", [128, DM], F32, ph) for i in range(4)]
        psT = pst("psTe", [128, 1024], BF16, ph)
        psG = [pst(f"psGe{i}", [128, 512], F32, ph) for i in range(2)]
        psU = pst("psUe", [128, 512], F32, ph)
        psY = [pst(f"psYe{i}", [128, 512], F32, ph) for i in range(4)]
        pieces = []
        for e_ in range(16):
            for fb in range(4):
                for nm in ('w_gate', 'w_up'):
                    pieces.append(D[nm][e_ * 1024:(e_ + 1) * 1024, fb * 512:(fb + 1) * 512].rearrange("(k p) c -> p k c", p=128))
            for dh in range(2):
                for fh in range(2):
                    r0 = e_ * 2048 + fh * 1024
                    pieces.append(D['w_down'][r0:r0 + 1024, dh * 512:(dh + 1) * 512].rearrange("(k p) c -> p k c", p=128))
        issued = [0]
        cast_eng = ['act', 'dve']
        LOOK = 5

        def issue_upto(n):
            while issued[0] < min(n, len(pieces)):
                i = issued[0]
                s_ = stg[i % 4]
                S.dma(s_[:, :, :], pieces[i])
                S.copy(wb[i % 8][:, :, :], s_[:, :, :], eng=cast_eng[i % 2])
                issued[0] += 1
        pc = [0]

        def next_piece():
            i = pc[0]
            issue_upto(i + 1 + LOOK)
            pc[0] += 1
            return wb[i % 8]
        ng = 0
        for e_ in range(16):
            for st in range(4):
                idx_ap = P.IDX[:, e_, st:st + 1]
                S.op('pool', lambda e, st=st, idx_ap=idx_ap: e.indirect_dma_start(
                    out=xs[:, st, :], out_offset=None, in_=D['HFd'][:, :],
                    in_offset=bass.IndirectOffsetOnAxis(ap=idx_ap, axis=0)),
                    [D['HFd'][:, :], idx_ap], [xs[:, st, :]], dma=True)
            for st in range(4):
                for k in range(8):
                    S.transpose(psT[:, k * 128:(k + 1) * 128], xs[:, st, k * 128:(k + 1) * 128], P.ident_bf[:, :])
                S.copy(xsT[:, :, st * 128:(st + 1) * 128], psT[:, :].rearrange("p (k t) -> p k t", k=8), eng=('act' if st % 2 else 'dve'))
            for fb in range(4):
                wg = next_piece()
                wu = next_piece()
                for fc in range(4):
                    pg = psG[ng % 2]
                    sgt = sg[ng % 2]
                    ng += 1
                    for k in range(8):
                        S.mm(pg[:, :], wg[:, k, fc * 128:(fc + 1) * 128], xsT[:, k, :], start=(k == 0), stop=(k == 7))
                    for k in range(8):
                        S.mm(psU[:, :], wu[:, k, fc * 128:(fc + 1) * 128], xsT[:, k, :], start=(k == 0), stop=(k == 7))
                    S.act(sgt[:, :], pg[:, :], AF.Silu)
                    S.tt(hidT[:, fb * 4 + fc, :], sgt[:, :], psU[:, :], ALU.mult)
            for dh in range(2):
                for fh in range(2):
                    wd = next_piece()
                    for st in range(4):
                        for f8 in range(8):
                            S.mm(psY[st][:, :], hidT[:, fh * 8 + f8, st * 128:(st + 1) * 128], wd[:, f8, :],
                                 start=(fh == 0 and f8 == 0), stop=(fh == 1 and f8 == 7))
                for st in range(4):
                    S.stt(ysb[st][:, dh * 512:(dh + 1) * 512], psY[st][:, :], P.GW[:, e_, st:st + 1],
                          P.gt2rep[:, dh * 512:(dh + 1) * 512], ALU.mult, ALU.mult)
            for st in range(4):
                idx_ap = P.IDX[:, e_, st:st + 1]
                S.op('pool', lambda e, st=st, idx_ap=idx_ap: e.indirect_dma_start(
                    out=D['acc'][:, :], out_offset=bass.IndirectOffsetOnAxis(ap=idx_ap, axis=0),
                    in_=ysb[st][:, :], in_offset=None, compute_op=ALU.add),
                    [ysb[st][:, :], idx_ap, D['acc'][:, :]], [D['acc'][:, :]], dma=True)


def phase_final(C):
    S, D, P, sb, pst = C.S, C.D, C.P, C.sb, C.pst
    with S.scope() as ph:
        gfin = sb("gfin", [128, DM], F32, ph)
        xb = [sb(f"fxb{i}", [128, DM], F32, ph) for i in range(2)]
        ob = [sb(f"fob{i}", [128, DM], F32, ph) for i in range(2)]
        junk = sb("fjunk", [128, DM], BF16, ph)
        sm = sb("fsm", [128, NT, 2], F32, ph)
        S.dma(gfin[:, :], D['gfin_row'][0:1, :].partition_broadcast(128))
        for j in range(NT):
            a = j % 2
            S.dma(xb[a][:, :], D['acc'][j * 128:(j + 1) * 128, :])
            ss = sm[:, j, 0:1]
            rs = sm[:, j, 1:2]
            S.act(junk[:, :], xb[a][:, :], AF.Square, accum_out=ss)
            S.act(rs, ss, AF.Ln, scale=1.0 / DM, bias=EPS)
            S.act(rs, rs, AF.Exp, scale=-0.5)
            S.stt(ob[a][:, :], xb[a][:, :], rs, gfin[:, :], ALU.mult, ALU.mult)
            S.dma(D['out'][j * 128:(j + 1) * 128, :], ob[a][:, :])


_PROG = {}


def kernel(**inputs):
    if 'nc' not in _PROG:
        _PROG['nc'] = build()[0]
    nc = _PROG['nc']
    B = inputs['x'].shape[0]
    in_maps = [layout_inputs(inputs, b) for b in range(B)]
    res = run_bass_kernel_spmd(nc, in_maps, core_ids=list(range(B)))
    out = np.stack([np.asarray(r["out"], dtype=np.float32) for r in res.results], axis=0)
    return out
```

```python
import math
import numpy as np
import ml_dtypes
import concourse.bass as bass
import concourse.mybir as mybir
from concourse.bass_utils import run_bass_kernel_spmd
from contextlib import ExitStack

F32 = mybir.dt.float32
BF16 = mybir.dt.bfloat16
I32 = mybir.dt.int32
AF = mybir.ActivationFunctionType
ALU = mybir.AluOpType
AX = mybir.AxisListType

SEQ = 4096
DM = 1024
NT = 32
EPS = 1e-6
EMIT_UNTIL = [None]
COMPUTE = ('pe', 'act', 'dve', 'pool')
SELF_SYNC = {'act': True, 'dve': True, 'pool': True, 'pe': False}
NDMA_SEMS = 6


def ap_box(ap):
    t = ap.tensor
    name = t.name
    dims = list(ap.ap)
    off = int(ap.offset)
    sp = str(ap.space() if callable(ap.space) else ap.space)
    is_dram = 'DRAM' in sp.upper() or 'HBM' in sp.upper() or type(t).__name__.startswith('DRAM') or type(t).__name__.startswith('Dram')
    if is_dram:
        lo = off
        hi = off
        for (st, cnt) in dims:
            st = int(st); cnt = int(cnt)
            if st >= 0:
                hi += st * (cnt - 1)
            else:
                lo += st * (cnt - 1)
        return (name, 0, 1, lo, hi + 1)
    if 'PSUM' in sp.upper() or type(t).__name__.startswith('PSum'):
        return (name, 0, 128, 0, 1 << 30)
    p0 = int(ap.start_partition())
    pc = int(dims[0][1])
    lo = off
    hi = off
    for (st, cnt) in dims[1:]:
        st = int(st); cnt = int(cnt)
        if st >= 0:
            hi += st * (cnt - 1)
        else:
            lo += st * (cnt - 1)
    return (name, p0, p0 + pc, lo, hi + 1)


class Op:
    __slots__ = ('stream', 'fn', 'deps', 'is_dma', 'signal', 'semval', 'dma_slot', 'idx', 'extra_waits')


class Sched:
    def __init__(self, nc, es):
        self.nc = nc
        self.es = es
        self.ops = []
        self.track = {}
        self.eng = {'pe': nc.tensor, 'act': nc.scalar, 'dve': nc.vector, 'pool': nc.gpsimd, 'sp': nc.sync}
        self.sem = {s: es.enter_context(nc.semaphore('sem_' + s)) for s in COMPUTE}
        self.dma_sems = {}
        for s in ('sp', 'pool', 'act'):
            self.dma_sems[s] = [es.enter_context(nc.semaphore(f'dsem_{s}{i}')) for i in range(NDMA_SEMS)]
        self.dma_count = {'sp': 0, 'pool': 0, 'act': 0}
        self.last_dma_ops = {'sp': [], 'pool': [], 'act': []}

    def _deps(self, boxes_r, boxes_w, idx, stream, is_dma):
        deps = set()
        for (boxes, is_w) in ((boxes_r, False), (boxes_w, True)):
            for b in boxes:
                lst = self.track.setdefault(b[0], [])
                keep = []
                for ent in lst:
                    eb, eidx, ew = ent
                    ov = not (eb[2] <= b[1] or b[2] <= eb[1] or eb[4] <= b[3] or b[4] <= eb[3])
                    if ov and (is_w or ew):
                        deps.add(eidx)
                    covered = (b[1] <= eb[1] and eb[2] <= b[2] and b[3] <= eb[3] and eb[4] <= b[4])
                    if is_w and covered:
                        continue
                    if (not is_w) and (not ew) and covered and (not is_dma):
                        eo = self.ops[eidx]
                        if eo.stream == stream and not eo.is_dma:
                            continue
                    keep.append(ent)
                keep.append([b, idx, is_w])
                self.track[b[0]] = keep
        deps.discard(idx)
        return deps

    def op(self, stream, fn, reads=(), writes=(), dma=False):
        o = Op()
        o.idx = len(self.ops)
        o.stream = stream
        o.fn = fn
        o.is_dma = dma
        o.signal = False
        o.semval = None
        o.dma_slot = None
        o.extra_waits = []
        br = [ap_box(a) for a in reads if a is not None and not isinstance(a, (int, float))]
        bw = [ap_box(a) for a in writes]
        self.ops.append(o)
        o.deps = self._deps(br, bw, o.idx, stream, dma)
        return o

    def emit(self):
        ops = self.ops
        for o in ops:
            for d in o.deps:
                po = ops[d]
                if po.is_dma:
                    continue
                if po.stream != o.stream or o.is_dma or SELF_SYNC.get(po.stream, True):
                    po.signal = True
        cnt = {s: 0 for s in COMPUTE}
        dcnt = {'sp': 0, 'pool': 0, 'act': 0}
        for o in ops:
            if o.is_dma:
                d = dcnt[o.stream]
                o.dma_slot = (d % NDMA_SEMS, 16 * (d // NDMA_SEMS + 1))
                dcnt[o.stream] = d + 1
            elif o.signal:
                cnt[o.stream] += 1
                o.semval = cnt[o.stream]
        waited = {s: {} for s in self.eng}
        nw = 0
        for o in ops:
            e = self.eng[o.stream]
            w = waited[o.stream]
            toks = []
            if o.is_dma:
                j, v = o.dma_slot
                if v > 16:
                    toks.append((('d', o.stream, j), self.dma_sems[o.stream][j], v - 16))
            for d in o.deps:
                po = ops[d]
                if po.is_dma:
                    j, v = po.dma_slot
                    toks.append((('d', po.stream, j), self.dma_sems[po.stream][j], v))
                else:
                    if po.stream == o.stream and not o.is_dma and not SELF_SYNC.get(po.stream, True):
                        continue
                    toks.append((('c', po.stream), self.sem[po.stream], po.semval))
            best = {}
            for key, sem, v in toks:
                if v is None:
                    raise RuntimeError('dep on non-signaling op')
                if w.get(key, 0) >= v:
                    continue
                if key not in best or best[key][1] < v:
                    best[key] = (sem, v)
            for key, (sem, v) in best.items():
                e.wait_ge(sem, v)
                w[key] = v
                nw += 1
            ins = o.fn(e)
            if ins is None:
                continue
            if o.is_dma:
                j, v = o.dma_slot
                ins.then_inc(self.dma_sems[o.stream][j], 16)
            elif o.signal:
                ins.then_inc(self.sem[o.stream], 1)
        self.n_waits = nw
        return cnt, dcnt

    def dma(self, out, in_, q='sp', **kw):
        return self.op(q, lambda e: e.dma_start(out=out, in_=in_, **kw), [in_], [out], dma=True)

    def mm(self, out, lhsT, rhs, start=True, stop=True, **kw):
        return self.op('pe', lambda e: e.matmul(out, lhsT, rhs, start=start, stop=stop, **kw),
                       [lhsT, rhs] + ([] if start else [out]), [out])

    def transpose(self, out, in_, ident):
        return self.op('pe', lambda e: e.transpose(out, in_, ident), [in_, ident], [out])

    def act(self, out, in_, func, scale=1.0, bias=0.0, accum_out=None, eng='act'):
        rd = [in_]
        if not isinstance(scale, (int, float)):
            rd.append(scale)
            if func == AF.Copy:
                func = AF.Identity
        if not isinstance(bias, (int, float)):
            rd.append(bias)
            if func == AF.Copy:
                func = AF.Identity
        wr = [out] + ([accum_out] if accum_out is not None else [])
        kw = {}
        if accum_out is not None:
            kw['accum_out'] = accum_out
        return self.op('act', lambda e: e.activation(out=out, in_=in_, func=func, scale=scale, bias=bias, **kw), rd, wr)

    def tt(self, out, in0, in1, op, eng='dve'):
        return self.op(eng, lambda e: e.tensor_tensor(out=out, in0=in0, in1=in1, op=op), [in0, in1], [out])

    def ts(self, out, in0, s1, op0, s2=None, op1=None, eng='dve', accum_out=None):
        rd = [in0]
        if not isinstance(s1, (int, float)):
            rd.append(s1)
        if s2 is not None and not isinstance(s2, (int, float)):
            rd.append(s2)
        kw = {}
        if op1 is not None:
            kw['op1'] = op1
        if accum_out is not None:
            kw['accum_out'] = accum_out
        wr = [out] + ([accum_out] if accum_out is not None else [])
        return self.op(eng, lambda e: e.tensor_scalar(out=out, in0=in0, scalar1=s1, scalar2=s2, op0=op0, **kw), rd, wr)

    def stt(self, out, in0, scalar, in1, op0, op1, eng='dve'):
        rd = [in0, in1]
        if not isinstance(scalar, (int, float)):
            rd.append(scalar)
        return self.op(eng, lambda e: e.scalar_tensor_tensor(out=out, in0=in0, scalar=scalar, in1=in1, op0=op0, op1=op1), rd, [out])

    def copy(self, out, in_, eng='dve'):
        if eng == 'act':
            return self.act(out, in_, AF.Copy)
        return self.op(eng, lambda e: e.tensor_copy(out=out, in_=in_), [in_], [out])

    def memset(self, out, val, eng='dve'):
        return self.op(eng, lambda e: e.memset(out, val), [], [out])

    def reduce(self, out, in_, op, axis=None, eng='dve'):
        axis = axis or AX.X
        return self.op(eng, lambda e: e.tensor_reduce(out=out, in_=in_, op=op, axis=axis), [in_], [out])

    def recip(self, out, in_, eng='dve'):
        return self.op(eng, lambda e: e.reciprocal(out=out, in_=in_), [in_], [out])

    def barrier(self):
        alld = set()
        for lst in self.track.values():
            for ent in lst:
                alld.add(ent[1])
        self.track = {}
        for s_ in ('pe', 'act', 'dve', 'pool', 'sp'):
            o = self.op(s_, lambda e: None, [], [])
            o.deps = set(alld)

    def scope(self):
        return _Scope(self)

    def finish(self, out_aps):
        boxes = [ap_box(a) for a in out_aps]
        o = self.op('sp', lambda e: None, list(out_aps), [])
        return o


class _Scope:
    def __init__(self, S):
        self.S = S
        self.st = ExitStack()

    def __enter__(self):
        self.st.__enter__()
        return self.st

    def __exit__(self, *a):
        self.S.barrier()
        return self.st.__exit__(*a)

_CONSTS = {}


def _bf(a):
    return np.ascontiguousarray(a.astype(np.float32)).astype(ml_dtypes.bfloat16)


def host_consts():
    if _CONSTS:
        return _CONSTS
    c = {}
    c['ident_bf'] = _bf(np.eye(128))
    c['ident_f'] = np.eye(128, dtype=np.float32)
    s = np.arange(128)[:, None]
    t = np.arange(128)[None, :]
    c['triU'] = (s <= t).astype(np.float32)
    c['triL'] = (s >= t).astype(np.float32)
    c['striU'] = (s < t).astype(np.float32)
    N = 8192
    n1 = np.arange(64); k1 = np.arange(64); n2 = np.arange(128); k2 = np.arange(128)
    ang = 2 * np.pi * np.outer(n1, k1) / 64
    c['F1'] = _bf(np.concatenate([np.cos(ang), -np.sin(ang)], 1))
    th = 2 * np.pi * ((n2[:, None, None] * (k1[None, :, None] + 64 * k2[None, None, :])) % N) / N
    c['GrT'] = _bf(np.cos(th).reshape(128, 64 * 128))
    c['GiT'] = _bf((-np.sin(th)).reshape(128, 64 * 128))
    c['GiNT'] = _bf((np.sin(th)).reshape(128, 64 * 128))
    ph = 2 * np.pi * np.outer(k2, n2) / 128
    Rr = np.cos(ph); Ri = np.sin(ph)
    c['Rc1'] = _bf(np.concatenate([Rr, Ri], 1))
    c['Rc2'] = _bf(np.concatenate([-Ri, Rr], 1))
    n1h = np.arange(32)
    thL = 2 * np.pi * (k1[:, None, None] * n1h[None, None, :] / 64 + k1[:, None, None] * n2[None, :, None] / N)
    c['LrT'] = _bf((np.cos(thL) / N).reshape(64, 128 * 32))
    c['LiNT'] = _bf((-np.sin(thL) / N).reshape(64, 128 * 32))
    L = SEQ
    f32 = np.float32
    tt = np.linspace(0.0, 1.0, L, dtype=f32)[:, None]
    w = (f32(2.0 * math.pi) * np.arange(L, dtype=f32)[:, None] / f32(L)).astype(f32)
    bands = np.linspace(1e-4, 15, 16, dtype=f32)[None, :]
    z = np.concatenate([tt, np.cos(bands * w), -np.sin(bands * w)], axis=-1).astype(f32)
    zr = np.concatenate([z[0:1], z[:0:-1]], 0)
    c['zT'] = np.ascontiguousarray(z.T)
    c['zrT'] = np.ascontiguousarray(zr.T)
    trow = tt[:, 0]
    trr = np.concatenate([trow[0:1], trow[:0:-1]])
    c['t_row'] = np.ascontiguousarray(trow[None, :]).astype(f32)
    c['tr_row'] = np.ascontiguousarray(trr[None, :]).astype(f32)
    _CONSTS.update(c)
    return _CONSTS


def colmaj(v, nk):
    return np.ascontiguousarray(np.asarray(v, dtype=np.float32).reshape(nk, 128).T)


def layout_inputs(inp, b):
    m = {}
    f = lambda a: np.ascontiguousarray(np.asarray(a, dtype=np.float32))
    m['x'] = f(inp['x'][b])
    m['ccol'] = colmaj(inp['c'][b], 8)
    m['w_ada'] = f(inp['w_ada'][0])
    m['b_ada'] = f(inp['b_ada'][0][None, :])
    m['gmix_col'] = colmaj(inp['g_mix'][0], 8)
    m['w_in'] = f(inp['w_in'][0])
    bin_ = np.asarray(inp['b_in'][0], dtype=np.float32)
    m['bin_row'] = f(bin_[None, 1024:2064])
    m['bqk_col'] = colmaj(bin_[0:1024], 8)
    m['bhy_col'] = colmaj(bin_[2064:3600], 12)
    cw = np.asarray(inp['conv_qk_w'][0], dtype=np.float32)
    m['cqk_w'] = np.ascontiguousarray(cw.reshape(3, 8, 128).transpose(2, 1, 0))
    m['cqk_b'] = colmaj(inp['conv_qk_b'][0], 8)
    cw = np.asarray(inp['conv_hy_w'][0], dtype=np.float32)
    m['chy_w'] = np.ascontiguousarray(cw.reshape(3, 12, 128).transpose(2, 1, 0))
    m['chy_b'] = colmaj(inp['conv_hy_b'][0], 12)
    m['mnorm_g'] = f(inp['mlstm_norm_g'][0][None, :])
    m['hy_w1'] = f(inp['hy_w1'][0])
    m['hy_b1c'] = f(inp['hy_b1'][0][:, None])
    m['hy_w2'] = f(inp['hy_w2'][0])
    m['hy_b2c'] = f(inp['hy_b2'][0][:, None])
    m['hy_w3'] = f(inp['hy_w3'][0])
    m['hy_frc'] = f(inp['hy_freq'][0][:, None])
    m['hy_del_col'] = colmaj(inp['hy_deltas'][0], 16)
    m['hy_bias_col'] = colmaj(np.asarray(inp['hy_bias'][0]).reshape(-1), 8)
    m['hnorm_col'] = colmaj(inp['hyena_norm_g'][0], 4)
    m['w_out'] = f(inp['w_out'][0])
    m['gffn_row'] = f(inp['g_ffn'][0][None, :])
    m['w_router'] = f(inp['w_router'][0])
    m['w_gate'] = f(inp['w_gate'][0]).reshape(16 * 1024, 2048)
    m['w_up'] = f(inp['w_up'][0]).reshape(16 * 1024, 2048)
    m['w_down'] = f(inp['w_down'][0]).reshape(16 * 2048, 1024)
    m['gfin_row'] = f(np.asarray(inp['g_final'])[None, :])
    m.update(host_consts())
    return m

INPUT_SPECS = [
    ('x', [SEQ, DM], F32), ('ccol', [128, 8], F32), ('w_ada', [DM, 6144], F32), ('b_ada', [1, 6144], F32),
    ('gmix_col', [128, 8], F32), ('w_in', [DM, 3600], F32), ('bin_row', [1, 1040], F32),
    ('bqk_col', [128, 8], F32), ('bhy_col', [128, 12], F32), ('cqk_w', [128, 8, 3], F32), ('cqk_b', [128, 8], F32),
    ('chy_w', [128, 12, 3], F32), ('chy_b', [128, 12], F32), ('mnorm_g', [1, 512], F32),
    ('hy_w1', [33, 64], F32), ('hy_b1c', [64, 1], F32), ('hy_w2', [64, 64], F32), ('hy_b2c', [64, 1], F32),
    ('hy_w3', [64, 2048], F32), ('hy_frc', [64, 1], F32), ('hy_del_col', [128, 16], F32),
    ('hy_bias_col', [128, 8], F32), ('hnorm_col', [128, 4], F32), ('w_out', [DM, DM], F32),
    ('gffn_row', [1, DM], F32), ('w_router', [DM, 16], F32), ('w_gate', [16 * 1024, 2048], F32),
    ('w_up', [16 * 1024, 2048], F32), ('w_down', [16 * 2048, 1024], F32), ('gfin_row', [1, DM], F32),
    ('ident_bf', [128, 128], BF16), ('ident_f', [128, 128], F32), ('triU', [128, 128], F32),
    ('triL', [128, 128], F32), ('striU', [128, 128], F32), ('F1', [64, 128], BF16),
    ('GrT', [128, 8192], BF16), ('GiT', [128, 8192], BF16), ('GiNT', [128, 8192], BF16),
    ('Rc1', [128, 256], BF16), ('Rc2', [128, 256], BF16), ('LrT', [64, 4096], BF16), ('LiNT', [64, 4096], BF16),
    ('zT', [33, SEQ], F32), ('zrT', [33, SEQ], F32), ('t_row', [1, SEQ], F32), ('tr_row', [1, SEQ], F32),
]


class Ctx:
    pass


def build(stop_after=None, debug=False):
    nc = bass.Bass("TRN2", target_bir_lowering=False)
    es = ExitStack()
    S = Sched(nc, es)
    C = Ctx()
    C.nc, C.S, C.es = nc, S, es
    C.debug = debug
    D = {}
    for name, shape, dt in INPUT_SPECS:
        D[name] = nc.dram_tensor(name, shape, dt, kind="ExternalInput").ap()
    D['out'] = nc.dram_tensor("out", [SEQ, DM], F32, kind="ExternalOutput").ap()

    def scratch(name, shape, dt):
        D[name] = nc.dram_tensor(name, shape, dt, kind="Internal").ap()
    scratch('QTd', [512, SEQ], BF16)
    scratch('KTd', [512, SEQ], BF16)
    scratch('Vd', [SEQ, 512], BF16)
    scratch('Od', [SEQ, 512], BF16)
    scratch('X1d', [512, SEQ], F32)
    scratch('X2d', [512, SEQ], F32)
    scratch('Zd', [512, SEQ], BF16)
    scratch('Z1d', [512, SEQ], BF16)
    scratch('Kd0', [512, 2 * SEQ], BF16)
    scratch('Kd1', [512, 2 * SEQ], BF16)
    scratch('Ycv', [512, SEQ], BF16)
    scratch('Yt', [DM, SEQ], BF16)
    scratch('HFd', [SEQ, DM], BF16)
    scratch('acc', [SEQ, DM], F32)
    C.D = D
    C.dbg = {}

    def dbg_out(name, shape, dt=F32):
        t = nc.dram_tensor("dbg_" + name, shape, dt, kind="ExternalOutput").ap()
        C.dbg[name] = t
        return t
    C.dbg_out = dbg_out

    cnt = [0]

    def sb(name, shape, dt, st=None):
        cnt[0] += 1
        return (st or es).enter_context(nc.sbuf_tensor(f"s{cnt[0]}_{name}", shape, dt))

    def pst(name, shape, dt, st=None):
        cnt[0] += 1
        return (st or es).enter_context(nc.psum_tensor(f"p{cnt[0]}_{name}", shape, dt))
    C.sb, C.pst = sb, pst

    P = Ctx()
    C.P = P
    P.ident_bf = sb("ident_bf", [128, 128], BF16)
    P.ident_f = sb("ident_f", [128, 128], F32)
    P.ones_f = sb("ones_f", [128, 128], F32)
    P.modcol = sb("modcol", [128, 48], F32)
    P.A1col = sb("A1col", [128, 8], F32)
    P.gt1rep = sb("gt1rep", [128, DM], F32)
    P.gt2rep = sb("gt2rep", [128, DM], F32)
    P.A2rep = sb("A2rep", [128, DM], F32)
    P.B2rep = sb("B2rep", [128, DM], F32)
    S.dma(P.ident_bf[:, :], D['ident_bf'][:, :])
    S.dma(P.ident_f[:, :], D['ident_f'][:, :])
    S.memset(P.ones_f[:, :], 1.0)

    phases = [phase_mod, phase_norm_proj, phase_mlstm, phase_hyena, phase_outproj, phase_route, phase_experts, phase_final]
    for ph in phases:
        ph(C)
        if stop_after == ph.__name__:
            break
    outs = [D['out'][:, :]] + [t for t in C.dbg.values()]
    S.finish(outs)
    S.emit()
    return nc, C


def phase_mod(C):
    S, D, P, sb, pst = C.S, C.D, C.P, C.sb, C.pst
    with S.scope() as ph:
        wada = [sb(f"wada{i}", [128, 8, 512], F32, ph) for i in range(2)]
        modrow = sb("modrow", [1, 6144], F32, ph)
        badar = sb("badar", [1, 6144], F32, ph)
        ccol = sb("ccol_sb", [128, 8], F32, ph)
        gmixc = sb("gmixc", [128, 8], F32, ph)
        gffn_rep = sb("gffn_rep", [128, DM], F32, ph)
        sc2rep = sb("sc2rep", [128, DM], F32, ph)
        ps = pst("ps_mod", [128, 512], F32, ph)
        psc = pst("ps_modc", [128, 512], F32, ph)
        psr = [pst(f"ps_modr{i}", [128, 512], F32, ph) for i in range(2)]
        S.dma(badar[:, :], D['b_ada'][:, :])
        S.dma(ccol[:, :], D['ccol'][:, :])
        S.dma(gmixc[:, :], D['gmix_col'][:, :])
        S.dma(gffn_rep[:, :], D['gffn_row'][0:1, :].partition_broadcast(128))
        wsrc = D['w_ada'].rearrange("(k p) c -> p k c", p=128)
        for blk in range(12):
            buf = wada[blk % 2]
            S.dma(buf[:, :, :], wsrc[:, :, blk * 512:(blk + 1) * 512])
            for k in range(8):
                S.mm(ps[0:1, :], ccol[:, k:k + 1], buf[:, k, :], start=(k == 0), stop=(k == 7))
            S.tt(modrow[0:1, blk * 512:(blk + 1) * 512], ps[0:1, :], badar[0:1, blk * 512:(blk + 1) * 512], ALU.add)
        for oc in range(48):
            S.mm(psc[:, oc:oc + 1], modrow[0:1, oc * 128:(oc + 1) * 128], P.ones_f[0:1, 0:1])
        S.copy(P.modcol[:, :], psc[:, 0:48])
        S.stt(P.A1col[:, :], P.modcol[:, 8:16], 1.0, gmixc[:, :], ALU.add, ALU.mult)
        n = 0
        for (dst, j) in ((P.gt1rep, 2), (P.gt2rep, 5), (sc2rep, 4), (P.B2rep, 3)):
            for h in range(2):
                pr = psr[n % 2]
                n += 1
                S.mm(pr[:, :], P.ones_f[0:1, 0:128], modrow[0:1, j * 1024 + h * 512: j * 1024 + (h + 1) * 512])
                S.copy(dst[:, h * 512:(h + 1) * 512], pr[:, :], eng=('act' if n % 2 else 'dve'))
        S.stt(P.A2rep[:, :], sc2rep[:, :], 1.0, gffn_rep[:, :], ALU.add, ALU.mult)
        if C.debug:
            d = C.dbg_out('modcol', [128, 48])
            S.dma(d[:, :], P.modcol[:, :])
            d = C.dbg_out('gt1rep', [128, DM])
            S.dma(d[:, :], P.gt1rep[:, :])


def phase_norm_proj(C):
    S, D, P, sb, pst = C.S, C.D, C.P, C.sb, C.pst
    P.Gt = sb("Gt", [128, NT, 16], F32)
    with S.scope() as ph:
        hT = sb("hT", [128, 8, SEQ], BF16, ph)
        with S.scope() as p1:
            xb = [sb(f"xb{i}", [128, DM], F32, p1) for i in range(2)]
            xn = [sb(f"xn{i}", [128, DM], BF16, p1) for i in range(2)]
            junk = sb("junk", [128, DM], BF16, p1)
            ss = sb("ss", [128, NT], F32, p1)
            rs = sb("rs", [128, NT], F32, p1)
            psT = [pst(f"psT{i}", [128, DM], BF16, p1) for i in range(2)]
            for j in range(NT):
                xt = xb[j % 2]
                S.dma(xt[:, :], D['x'][j * 128:(j + 1) * 128, :])
                S.act(junk[:, :], xt[:, :], AF.Square, accum_out=ss[:, j:j + 1])
                S.act(rs[:, j:j + 1], ss[:, j:j + 1], AF.Ln, scale=1.0 / DM, bias=EPS)
                S.act(rs[:, j:j + 1], rs[:, j:j + 1], AF.Exp, scale=-0.5)
                S.act(xn[j % 2][:, :], xt[:, :], AF.Copy, scale=rs[:, j:j + 1])
                pt = psT[j % 2]
                for k in range(8):
                    S.transpose(pt[:, k * 128:(k + 1) * 128], xn[j % 2][:, k * 128:(k + 1) * 128], P.ident_bf[:, :])
                for k in range(8):
                    S.act(hT[:, k, j * 128:(j + 1) * 128], pt[:, k * 128:(k + 1) * 128], AF.Identity,
                          scale=P.A1col[:, k:k + 1], bias=P.modcol[:, k:k + 1])
        if C.debug:
            d = C.dbg_out('hT', [128, 8, SEQ], BF16)
            for k in range(8):
                S.dma(d[:, k, :], hT[:, k, :])
        with S.scope() as p2:
            wvo = sb("wvo", [128, 8, 1040], BF16, p2)
            brow = sb("brow", [128, 1040], F32, p2)
            vt = [sb(f"vt{i}", [128, 512], BF16, p2) for i in range(2)]
            ot = [sb(f"ot{i}", [128, 512], BF16, p2) for i in range(2)]
            otf = [sb(f"otf{i}", [128, 512], F32, p2) for i in range(2)]
            psV = [pst(f"psV{i}", [128, 512], F32, p2) for i in range(2)]
            psO = [pst(f"psO{i}", [128, 512], F32, p2) for i in range(2)]
            psG = [pst(f"psG{i}", [128, 512], F32, p2) for i in range(2)]
            wsrc = D['w_in'].rearrange("(k p) c -> p k c", p=128)
            S.dma(wvo[:, :, 0:512], wsrc[:, :, 1024:1536], q='pool')
            S.dma(wvo[:, :, 512:1040], wsrc[:, :, 1536:2064], q='pool')
            S.dma(brow[:, :], D['bin_row'][0:1, :].partition_broadcast(128))
            for j in range(NT):
                a = j % 2
                for (pp, c0, c1) in ((psV[a], 0, 512), (psO[a], 512, 1024), (psG[a], 1024, 1040)):
                    for k in range(8):
                        S.mm(pp[:, 0:c1 - c0], hT[:, k, j * 128:(j + 1) * 128], wvo[:, k, c0:c1], start=(k == 0), stop=(k == 7))
                S.tt(vt[a][:, :], psV[a][:, :], brow[:, 0:512], ALU.add)
                S.dma(D['Vd'][j * 128:(j + 1) * 128, :], vt[a][:, :])
                S.tt(otf[a][:, :], psO[a][:, :], brow[:, 512:1024], ALU.add)
                S.act(ot[a][:, :], otf[a][:, :], AF.Sigmoid)
                S.dma(D['Od'][j * 128:(j + 1) * 128, :], ot[a][:, :])
                S.tt(P.Gt[:, j, :], psG[a][:, 0:16], brow[:, 1024:1040], ALU.add)
        if C.debug:
            d = C.dbg_out('Gt', [128, NT, 16])
            S.dma(d[:, :, :], P.Gt[:, :, :])
        with S.scope() as p3:
            wch = [sb(f"wch{i}", [128, 8, 128], BF16, p3) for i in range(3)]
            pre = [sb(f"pre{i}", [128, SEQ + 2], F32, p3) for i in range(2)]
            t0s = [sb(f"cv_t0{i}", [128, 2048], F32, p3) for i in range(2)]
            t2s = [sb(f"cv_t2{i}", [128, 2048], F32, p3) for i in range(2)]
            t3 = [[sb(f"cv_t3{q}{i}", [128, 2048], F32, p3) for i in range(2)] for q in range(2)]
            ob = [[sb(f"cv_ob{q}{i}", [128, 2048], BF16, p3) for i in range(2)] for q in range(2)]
            pending = [None]
            bqk = sb("bqk", [128, 8], F32, p3)
            bhy = sb("bhy", [128, 12], F32, p3)
            cqw = sb("cqw", [128, 8, 3], F32, p3)
            cqb = sb("cqb", [128, 8], F32, p3)
            chw = sb("chw", [128, 12, 3], F32, p3)
            chb = sb("chb", [128, 12], F32, p3)
            psF = [pst(f"psF{i}", [128, 512], F32, p3) for i in range(8)]
            for (tile_, nm) in ((bqk, 'bqk_col'), (bhy, 'bhy_col'), (cqb, 'cqk_b'), (chb, 'chy_b')):
                S.dma(tile_[:, :], D[nm][:, :])
            S.dma(cqw[:, :, :], D['cqk_w'][:, :, :])
            S.dma(chw[:, :, :], D['chy_w'][:, :, :])
            for i in range(2):
                S.memset(pre[i][:, 0:1], 0.0)
                S.memset(pre[i][:, SEQ + 1:SEQ + 2], 0.0)
            wsrc = D['w_in'].rearrange("(k p) c -> p k c", p=128)
            chunks = []
            for cc in range(8):
                chunks.append(('qk', cc, cc * 128))
            for i in range(12):
                chunks.append(('hy', i, 2064 + i * 128))
            nps = 0
            for m_ in range(2):
                S.dma(wch[m_ % 3][:, :, :], wsrc[:, :, chunks[m_][2]:chunks[m_][2] + 128], q='pool')
            for n, (kind, ci, col0) in enumerate(chunks):
                wc = wch[n % 3]
                pr = pre[n % 2]
                if n + 2 < len(chunks):
                    S.dma(wch[(n + 2) % 3][:, :, :], wsrc[:, :, chunks[n + 2][2]:chunks[n + 2][2] + 128], q='pool')
                bcol = bqk[:, ci:ci + 1] if kind == 'qk' else bhy[:, ci:ci + 1]
                cw = cqw if kind == 'qk' else chw
                cb = cqb if kind == 'qk' else chb
                for tb in range(8):
                    pp = psF[nps % 8]
                    nps += 1
                    for k in range(8):
                        S.mm(pp[:, :], wc[:, k, :], hT[:, k, tb * 512:(tb + 1) * 512], start=(k == 0), stop=(k == 7))
                    if tb % 2 == 0:
                        S.act(pr[:, 1 + tb * 512:1 + (tb + 1) * 512], pp[:, :], AF.Identity, bias=bcol)
                    else:
                        S.ts(pr[:, 1 + tb * 512:1 + (tb + 1) * 512], pp[:, :], bcol, ALU.add)
                par = n % 2
                for hh in range(2):
                    o0 = hh * 2048
                    S.act(t0s[hh][:, :], pr[:, o0:o0 + 2048], AF.Identity, scale=cw[:, ci, 0:1], bias=cb[:, ci:ci + 1])
                    S.act(t2s[hh][:, :], pr[:, o0 + 2:o0 + 2050], AF.Identity, scale=cw[:, ci, 2:3])
                for hh in range(2):
                    o0 = hh * 2048
                    S.stt(t0s[hh][:, :], pr[:, o0 + 1:o0 + 2049], cw[:, ci, 1:2], t0s[hh][:, :], ALU.mult, ALU.add)
                for hh in range(2):
                    if kind == 'hy' and ci >= 8:
                        S.tt(ob[par][hh][:, :], t0s[hh][:, :], t2s[hh][:, :], ALU.add, eng='pool')
                    else:
                        S.tt(t3[par][hh][:, :], t0s[hh][:, :], t2s[hh][:, :], ALU.add, eng='pool')
                if pending[0] is not None:
                    pending[0]()

                def fin(kind=kind, ci=ci, par=par):
                    for hh in range(2):
                        o0 = hh * 2048
                        if kind == 'qk':
                            S.act(ob[par][hh][:, :], t3[par][hh][:, :], AF.Silu)
                            dst = D['QTd'] if ci < 4 else D['KTd']
                            S.dma(dst[(ci % 4) * 128:(ci % 4 + 1) * 128, o0:o0 + 2048], ob[par][hh][:, :])
                        elif ci < 8:
                            dst = D['X1d'] if ci < 4 else D['X2d']
                            S.dma(dst[(ci % 4) * 128:(ci % 4 + 1) * 128, o0:o0 + 2048], t3[par][hh][:, :])
                        else:
                            S.dma(D['Zd'][(ci - 8) * 128:(ci - 7) * 128, o0:o0 + 2048], ob[par][hh][:, :])
                pending[0] = fin
            pending[0]()
    if C.debug:
        for nm, shp, dt in (('QTd', [512, SEQ], BF16), ('KTd', [512, SEQ], BF16), ('Vd', [SEQ, 512], BF16),
                            ('Od', [SEQ, 512], BF16), ('X1d', [512, SEQ], F32), ('Zd', [512, SEQ], BF16)):
            d = C.dbg_out(nm, shp, dt)
            with S.scope() as pd:
                if shp[0] == 512:
                    tmp = sb("dbgtmp_" + nm, [128, 4, SEQ], dt, pd)
                    S.dma(tmp[:, :, :], D[nm].rearrange("(a p) n -> p a n", p=128))
                    S.dma(d.rearrange("(a p) n -> p a n", p=128), tmp[:, :, :])
                else:
                    tmp = sb("dbgtmp_" + nm, [128, NT, 512], dt, pd)
                    S.dma(tmp[:, :, :], D[nm].rearrange("(a p) n -> p a n", p=128))
                    S.dma(d.rearrange("(a p) n -> p a n", p=128), tmp[:, :, :])


def phase_mlstm(C):
    S, D, P, sb, pst = C.S, C.D, C.P, C.sb, C.pst
    Gt = P.Gt
    with S.scope() as ph:
        triU = sb("triU", [128, 128], F32, ph)
        triL = sb("triL", [128, 128], F32, ph)
        LF = sb("LF", [128, 2, NT, 4], F32, ph)
        II = sb("II", [128, 2, NT, 4], F32, ph)
        Bc = sb("Bc", [128, 2, NT, 4], F32, ph)
        Wp = sb("Wp", [128, 2, NT, 4], F32, ph)
        ENB = sb("ENB", [128, 2, NT, 4], F32, ph)
        EG = sb("EG", [128, 2, NT, 4], F32, ph)
        zcol = sb("zcol", [128, 1], F32, ph)
        S.dma(triU[:, :], D['triU'][:, :])
        S.dma(triL[:, :], D['triL'][:, :])
        S.memset(zcol[:, :], 0.0)
        with S.scope() as pp:
            psB = pst("psB", [128, 512], F32, pp)
            psGs = pst("psGs", [128, 512], F32, pp)
            for d in range(2):
                S.act(LF[:, d, :, :], Gt[:, :, d * 8 + 4:d * 8 + 8], AF.Exp, scale=-1.0)
                S.copy(II[:, d, :, :], Gt[:, :, d * 8:d * 8 + 4])
            fl = lambda t: t[:, :, :, :].rearrange("p d c e -> p (d c e)")
            S.act(fl(LF), fl(LF), AF.Ln, bias=1.0)
            S.ts(fl(LF), fl(LF), -1.0, ALU.mult)
            S.mm(psB[:, 0:128], triU[:, :], fl(LF)[:, 0:128])
            S.mm(psB[:, 128:256], triL[:, :], fl(LF)[:, 128:256])
            S.mm(psGs[:, 0:256], P.ones_f[:, :], fl(LF))
            S.copy(fl(Bc), psB[:, 0:256])
            S.act(fl(EG), psGs[:, 0:256], AF.Exp)
            S.tt(fl(Wp), fl(II), fl(Bc), ALU.subtract)
            S.act(fl(Wp), fl(Wp), AF.Exp, bias=math.log(128.0 ** -0.5))
            S.act(fl(ENB), fl(Bc), AF.Exp, scale=-1.0)
        if C.debug:
            for nm, t in (('Bc', Bc), ('Wp', Wp), ('EG', EG)):
                d_ = C.dbg_out(nm, [128, 2, NT, 4])
                S.dma(d_[:, :, :, :], t[:, :, :, :])
        for hd in range(4):
            with S.scope() as hs:
                qT = sb("qT", [128, SEQ], BF16, hs)
                kT = sb("kT", [128, SEQ], BF16, hs)
                vaug = sb("vaug", [128, NT, 129], BF16, hs)
                osg = sb("osg", [128, NT, 128], BF16, hs)
                ktm = sb("ktm", [128, NT, 128], BF16, hs)
                Hs = sb("Hs", [128, NT, 128], F32, hs)
                sq = sb("sq", [128, NT, 128], F32, hs)
                gO = sb("gO", [128, NT, 128], BF16, hs)
                ym = sb("ym", [128, NT, 128], BF16, hs)
                ymT = sb("ymT", [128, SEQ], BF16, hs)
                mng = sb("mng", [128, 128], F32, hs)
                STw = [sb(f"STw{i}", [128, 128], BF16, hs) for i in range(2)]
                vw = [sb(f"vw{i}", [128, 129], BF16, hs) for i in range(2)]
                Tst = sb("Tst", [128, 129], F32, hs)
                Tst2 = sb("Tst2", [128, 129], F32, hs)
                Cbfd = [[sb(f"Cbf{d_}{i}", [128, 129], BF16, hs) for i in range(2)] for d_ in range(2)]
                sm = [sb(f"sm{i}", [128, 4], F32, hs) for i in range(2)]
                ssq = sb("ssq", [128, NT], F32, hs)
                pS = [pst(f"pS{i}", [128, 512], F32, hs) for i in range(2)]
                pO = [pst(f"pO{i}", [128, 512], F32, hs) for i in range(2)]
                pC = [pst(f"pC{i}", [128, 512], F32, hs) for i in range(2)]
                pK = pst("pK", [128, 1024], BF16, hs)
                S.dma(qT[:, :], D['QTd'][hd * 128:(hd + 1) * 128, :])
                S.dma(kT[:, :], D['KTd'][hd * 128:(hd + 1) * 128, :])
                S.dma(vaug[:, :, 0:128], D['Vd'][:, hd * 128:(hd + 1) * 128].rearrange("(c p) d -> p c d", p=128))
                S.memset(vaug[:, :, 128:129], 1.0)
                S.dma(osg[:, :, :], D['Od'][:, hd * 128:(hd + 1) * 128].rearrange("(c p) d -> p c d", p=128))
                S.dma(mng[:, :], D['mnorm_g'][0:1, hd * 128:(hd + 1) * 128].partition_broadcast(128))
                for c0 in range(0, NT, 8):
                    for c in range(c0, c0 + 8):
                        S.transpose(pK[:, (c - c0) * 128:(c - c0 + 1) * 128], kT[:, c * 128:(c + 1) * 128], P.ident_bf[:, :])
                    S.copy(ktm[:, c0:c0 + 8, :].rearrange("p c d -> p (c d)"), pK[:, :], eng=('act' if (c0 // 8) % 2 else 'dve'))
                n = 0
                Tsts = [Tst, Tst2]
                prevc = [None, None]
                visited = set()
                for d in range(2):
                    S.memset(Tsts[d][:, :], 0.0)
                    S.memset(Cbfd[d][0][:, :], 0.0)
                nd = [0, 0]
                for i in range(NT):
                    for d in range(2):
                        c = i if d == 0 else NT - 1 - i
                        mask = triU if d == 0 else triL
                        a = n % 2
                        n += 1
                        b_ = nd[d] % 2
                        nd[d] += 1
                        cs = slice(c * 128, (c + 1) * 128)
                        wcol = Wp[:, d, c, hd:hd + 1]
                        S.mm(pS[a][:, 0:128], kT[:, cs], qT[:, cs])
                        S.stt(STw[a][:, :], pS[a][:, 0:128], wcol, mask[:, :], ALU.mult, ALU.mult)
                        S.act(vw[a][:, :], vaug[:, c, :], AF.Copy, scale=wcol)
                        S.mm(pO[a][:, 0:129], STw[a][:, :], vaug[:, c, :], start=True, stop=False)
                        S.mm(pO[a][:, 0:129], qT[:, cs], Cbfd[d][b_][:, :], start=False, stop=True)
                        S.mm(pC[a][:, 0:129], ktm[:, c, :], vw[a][:, :])
                        enb = ENB[:, d, c, hd:hd + 1]
                        s_ = sm[a]
                        S.ts(s_[:, 0:1], pO[a][:, 128:129], enb, ALU.max)
                        S.stt(s_[:, 2:3], pO[a][:, 128:129], -1.0, s_[:, 0:1], ALU.mult, ALU.max)
                        S.recip(s_[:, 3:4], s_[:, 2:3])
                        if c not in visited:
                            visited.add(c)
                            S.act(Hs[:, c, :], pO[a][:, 0:128], AF.Copy, scale=s_[:, 3:4])
                        else:
                            S.stt(Hs[:, c, :], pO[a][:, 0:128], s_[:, 3:4], Hs[:, c, :], ALU.mult, ALU.add)
                        egp = zcol[:, 0:1] if prevc[d] is None else EG[:, d, prevc[d], hd:hd + 1]
                        S.stt(Tsts[d][:, :], Tsts[d][:, :], egp, pC[a][:, 0:129], ALU.mult, ALU.add)
                        S.act(Cbfd[d][(b_ + 1) % 2][:, :], Tsts[d][:, :], AF.Copy, scale=EG[:, d, c, hd:hd + 1])
                        prevc[d] = c
                S.act(sq[:, :, :], Hs[:, :, :], AF.Square)
                S.reduce(ssq[:, :], sq[:, :, :], ALU.add)
                S.ts(ssq[:, :], ssq[:, :], 1.0 / 128.0, ALU.mult, s2=EPS, op1=ALU.add)
                S.act(ssq[:, :], ssq[:, :], AF.Sqrt)
                S.recip(ssq[:, :], ssq[:, :])
                S.tt(gO[:, :, :], osg[:, :, :], mng[:, :].unsqueeze(1).to_broadcast([128, NT, 128]), ALU.mult)
                for c in range(NT):
                    S.stt(ym[:, c, :], Hs[:, c, :], ssq[:, c:c + 1], gO[:, c, :], ALU.mult, ALU.mult)
                for c0 in range(0, NT, 8):
                    for c in range(c0, c0 + 8):
                        S.transpose(pK[:, (c - c0) * 128:(c - c0 + 1) * 128], ym[:, c, :], P.ident_bf[:, :])
                    S.copy(ymT[:, c0 * 128:(c0 + 8) * 128], pK[:, :], eng=('act' if (c0 // 8) % 2 else 'dve'))
                S.dma(D['Yt'][hd * 128:(hd + 1) * 128, :], ymT[:, :])
    if C.debug:
        d_ = C.dbg_out('Yt_m', [512, SEQ], BF16)
        with S.scope() as pd:
            tmp = sb("dbgtmp_ytm", [128, 4, SEQ], BF16, pd)
            S.dma(tmp[:, :, :], D['Yt'][0:512, :].rearrange("(a p) n -> p a n", p=128))
            S.dma(d_.rearrange("(a p) n -> p a n", p=128), tmp[:, :, :])


def phase_hyena(C):
    S, D, P, sb, pst = C.S, C.D, C.P, C.sb, C.pst
    PI = math.pi
    with S.scope() as pa:
        hid2T = sb("hid2T", [64, SEQ], F32, pa)
        hid2rT = sb("hid2rT", [64, SEQ], F32, pa)
        frc = sb("frc", [64, 1], F32, pa)
        frb1 = sb("frb1", [64, 1], F32, pa)
        frb2 = sb("frb2", [64, 1], F32, pa)
        with S.scope() as p1:
            zT = sb("zT", [33, SEQ], F32, p1)
            zrT = sb("zrT", [33, SEQ], F32, p1)
            hid1 = sb("hid1", [64, SEQ], F32, p1)
            w1 = sb("hw1", [33, 64], F32, p1)
            w2 = sb("hw2", [64, 64], F32, p1)
            b1c = sb("b1c", [64, 1], F32, p1)
            b2c = sb("b2c", [64, 1], F32, p1)
            arg = [sb(f"harg{i}", [64, 512], F32, p1) for i in range(2)]
            m1 = [sb(f"hm1{i}", [64, 512], F32, p1) for i in range(2)]
            m2 = [sb(f"hm2{i}", [64, 512], F32, p1) for i in range(2)]
            psM = [pst(f"psM{i}", [128, 512], F32, p1) for i in range(2)]
            S.dma(zT[:, :], D['zT'][:, :])
            S.dma(zrT[:, :], D['zrT'][:, :])
            S.dma(w1[:, :], D['hy_w1'][:, :])
            S.dma(w2[:, :], D['hy_w2'][:, :])
            S.dma(b1c[:, :], D['hy_b1c'][:, :])
            S.dma(b2c[:, :], D['hy_b2c'][:, :])
            S.dma(frc[:, :], D['hy_frc'][:, :])
            S.tt(frb1[:, :], frc[:, :], b1c[:, :], ALU.mult)
            S.tt(frb2[:, :], frc[:, :], b2c[:, :], ALU.mult)
            n = 0

            def sin_layer(ps, frb, dst):
                nonlocal n
                a = n % 2
                n += 1
                S.ts(arg[a][:, :], ps, frc[:, 0:1], ALU.mult, s2=frb[:, 0:1], op1=ALU.add)
                S.ts(m1[a][:, :], arg[a][:, :], PI, ALU.is_gt, s2=-2.0 * PI, op1=ALU.mult)
                S.ts(m2[a][:, :], arg[a][:, :], -PI, ALU.is_lt, s2=2.0 * PI, op1=ALU.mult)
                S.tt(arg[a][:, :], arg[a][:, :], m1[a][:, :], ALU.add)
                S.tt(arg[a][:, :], arg[a][:, :], m2[a][:, :], ALU.add)
                S.act(dst, arg[a][:, :], AF.Sin)
            for (zs, hdst) in ((zT, hid2T), (zrT, hid2rT)):
                for blk in range(8):
                    ps = psM[blk % 2]
                    S.mm(ps[0:64, :], w1[:, :], zs[:, blk * 512:(blk + 1) * 512])
                    sin_layer(ps[0:64, :], frb1, hid1[:, blk * 512:(blk + 1) * 512])
                for blk in range(8):
                    ps = psM[blk % 2]
                    S.mm(ps[0:64, :], w2[:, :], hid1[:, blk * 512:(blk + 1) * 512])
                    sin_layer(ps[0:64, :], frb2, hdst[:, blk * 512:(blk + 1) * 512])
        with S.scope() as p2:
            w3 = sb("hw3", [64, 2048], F32, p2)
            trow = sb("trow", [128, SEQ], F32, p2)
            trrow = sb("trrow", [128, SEQ], F32, p2)
            ndel = sb("ndel", [128, 16], F32, p2)
            hbias = sb("hbias", [128, 8], F32, p2)
            kT = [sb(f"kTb{i}", [128, 2 * SEQ], BF16, p2) for i in range(2)]
            win = [sb(f"win{i}", [128, 512], F32, p2) for i in range(2)]
            k0t = sb("k0t", [128, 4], F32, p2)
            psK_ = [pst(f"psKf{i}", [128, 512], F32, p2) for i in range(3)]
            ps0 = pst("psK0", [128, 512], F32, p2)
            S.dma(w3[:, :], D['hy_w3'][:, :])
            S.dma(trow[:, :], D['t_row'][0:1, :].partition_broadcast(128))
            S.dma(trrow[:, :], D['tr_row'][0:1, :].partition_broadcast(128))
            S.dma(ndel[:, :], D['hy_del_col'][:, :])
            S.dma(hbias[:, :], D['hy_bias_col'][:, :])
            S.stt(ndel[:, :], ndel[:, :], -1.0, ndel[:, :], ALU.mult, ALU.max)
            S.ts(ndel[:, :], ndel[:, :], -1.0, ALU.mult)
            nb = 0
            nk = 0
            for o in range(2):
                for g in range(4):
                    kt = kT[nk % 2]
                    nk += 1
                    for d in range(2):
                        hs = hid2T if d == 0 else hid2rT
                        tr = trow if d == 0 else trrow
                        c0 = (o * 2 + d) * 512 + g * 128
                        di = o * 8 + d * 4 + g
                        for blk in range(8):
                            ps = psK_[nb % 3]
                            wn = win[nb % 2]
                            nb += 1
                            S.mm(ps[:, :], w3[:, c0:c0 + 128], hs[:, blk * 512:(blk + 1) * 512])
                            S.act(wn[:, :], tr[:, blk * 512:(blk + 1) * 512], AF.Exp, scale=ndel[:, di:di + 1])
                            S.stt(kt[:, d * SEQ + blk * 512:d * SEQ + (blk + 1) * 512], wn[:, :], 0.05, ps[:, :], ALU.add, ALU.mult)
                    S.memset(kt[:, SEQ:SEQ + 1], 0.0)
                    cf = (o * 2 + 0) * 512 + g * 128
                    cb = (o * 2 + 1) * 512 + g * 128
                    S.mm(ps0[:, 0:1], w3[:, cf:cf + 128], hid2T[:, 0:1])
                    S.mm(ps0[:, 1:2], w3[:, cb:cb + 128], hid2T[:, 0:1])
                    S.copy(k0t[:, 0:2], ps0[:, 0:2])
                    S.tt(k0t[:, 2:3], k0t[:, 0:1], k0t[:, 1:2], ALU.add)
                    S.ts(k0t[:, 3:4], k0t[:, 2:3], 1.05, ALU.mult)
                    S.tt(kt[:, 0:1], k0t[:, 3:4], hbias[:, o * 4 + g:o * 4 + g + 1], ALU.add)
                    dst = D['Kd0'] if o == 0 else D['Kd1']
                    S.dma(dst[g * 128:(g + 1) * 128, :], kt[:, :])
    if C.debug:
        d_ = C.dbg_out('Kd0', [512, 2 * SEQ], BF16)
        with S.scope() as pd:
            tmp = sb("dbgtmp_kd", [128, 4, 2 * SEQ], BF16, pd)
            S.dma(tmp[:, :, :], D['Kd0'].rearrange("(a p) n -> p a n", p=128))
            S.dma(d_.rearrange("(a p) n -> p a n", p=128), tmp[:, :, :])
    with S.scope() as pb:
        T = Ctx()
        T.F1 = sb("F1", [64, 128], BF16, pb)
        T.GrT = sb("GrT", [128, 64, 128], BF16, pb)
        T.GiT = sb("GiT", [128, 64, 128], BF16, pb)
        T.GiNT = sb("GiNT", [128, 64, 128], BF16, pb)
        T.Rc1 = sb("Rc1", [128, 256], BF16, pb)
        T.Rc2 = sb("Rc2", [128, 256], BF16, pb)
        T.LrT = sb("LrT", [64, 128, 32], BF16, pb)
        T.LiNT = sb("LiNT", [64, 128, 32], BF16, pb)
        hnorm = sb("hnorm", [128, 4], F32, pb)
        S.dma(T.F1[:, :], D['F1'][:, :])
        for nm in ('GrT', 'GiT', 'GiNT'):
            S.dma(getattr(T, nm)[:, :, :], D[nm].rearrange("p (k m) -> p k m", k=64))
        S.dma(T.Rc1[:, :], D['Rc1'][:, :])
        S.dma(T.Rc2[:, :], D['Rc2'][:, :])
        S.dma(T.LrT[:, :, :], D['LrT'].rearrange("p (n m) -> p n m", n=128))
        S.dma(T.LiNT[:, :, :], D['LiNT'].rearrange("p (n m) -> p n m", n=128))
        S.dma(hnorm[:, :], D['hnorm_col'][:, :])
        for o in range(2):
            zsrc = D['Zd'] if o == 0 else D['Z1d']
            kd = D['Kd0'] if o == 0 else D['Kd1']
            with S.scope() as pw:
                W = Ctx()
                W.X = sb("fX", [64, 64, 128], BF16, pw)
                W.AT = sb("fAT", [128, 64, 2, 64], BF16, pw)
                W.Kf = sb("fKf", [128, 64, 2, 64], BF16, pw)
                W.V = sb("fV", [128, 2, 64, 64], BF16, pw)
                W.Wt = sb("fWt", [64, 64, 2, 128], BF16, pw)
                W.Ysb = sb("fY", [32, 64, 128], BF16, pw)
                W.tm = [[sb(f"ftm{i}{j}", [128, 4, 64], F32, pw) for j in range(4)] for i in range(2)]
                W.ring = [pst(f"psR{i}", [128, 512], F32, pw) for i in range(8)]
                W.nr = 0
                W.ne = 0
                for b8 in range(8):
                    ch0 = b8 * 64
                    fft_batch(C, T, W, zsrc[ch0:ch0 + 64, :], kd[ch0:ch0 + 64, :], D['Ycv'][ch0:ch0 + 64, :])
            with S.scope() as pg:
                ysb = [sb(f"gy{i}", [128, SEQ], BF16, pg) for i in range(2)]
                xsb = [sb(f"gx{i}", [128, SEQ], F32, pg) for i in range(2)]
                zo = [sb(f"gz{i}", [128, SEQ], BF16, pg) for i in range(2)]
                xsrc = D['X1d'] if o == 0 else D['X2d']
                if o == 1:
                    z2 = sb("gz2", [128, SEQ], F32, pg)
                    sq = [sb(f"gsq{i}", [128, 512], F32, pg) for i in range(2)]
                    rr = [sb(f"grr{i}", [128, 512], F32, pg) for i in range(2)]
                    psN = [pst(f"psN{i}", [128, 512], F32, pg) for i in range(2)]
                for g in range(4):
                    a = g % 2
                    S.dma(ysb[a][:, :], D['Ycv'][g * 128:(g + 1) * 128, :])
                    S.dma(xsb[a][:, :], xsrc[g * 128:(g + 1) * 128, :])
                    if o == 0:
                        S.tt(zo[a][:, :], xsb[a][:, :], ysb[a][:, :], ALU.mult)
                        S.dma(D['Z1d'][g * 128:(g + 1) * 128, :], zo[a][:, :])
                    else:
                        S.tt(z2[:, :], xsb[a][:, :], ysb[a][:, :], ALU.mult)
                        for blk in range(8):
                            bs = slice(blk * 512, (blk + 1) * 512)
                            q_ = blk % 2
                            S.act(sq[q_][:, :], z2[:, bs], AF.Square)
                            S.mm(psN[q_][:, :], P.ones_f[:, :], sq[q_][:, :])
                            S.ts(rr[q_][:, :], psN[q_][:, :], 1.0 / 128.0, ALU.mult, s2=EPS, op1=ALU.add)
                            S.act(rr[q_][:, :], rr[q_][:, :], AF.Sqrt)
                            S.recip(rr[q_][:, :], rr[q_][:, :])
                            S.stt(zo[a][:, bs], z2[:, bs], hnorm[:, g:g + 1], rr[q_][:, :], ALU.mult, ALU.mult)
                        S.dma(D['Yt'][512 + g * 128:512 + (g + 1) * 128, :], zo[a][:, :])
    if C.debug:
        d_ = C.dbg_out('Yt_h', [512, SEQ], BF16)
        with S.scope() as pd:
            tmp = sb("dbgtmp_yth", [128, 4, SEQ], BF16, pd)
            S.dma(tmp[:, :, :], D['Yt'][512:1024, :].rearrange("(a p) n -> p a n", p=128))
            S.dma(d_.rearrange("(a p) n -> p a n", p=128), tmp[:, :, :])


def fft_batch(C, T, W, zsrc, kdsrc, ydst):
    S = C.S

    def bank():
        W.nr += 1
        return W.ring[W.nr % 8]

    def ev(dst, src):
        W.ne += 1
        S.copy(dst, src, eng=('act' if W.ne % 2 else 'dve'))

    def forward(kdim, mode):
        for cq in range(16):
            pA = bank()
            for i in range(4):
                ch = cq * 4 + i
                S.mm(pA[:, i * 128:(i + 1) * 128], W.X[0:kdim, ch, :], T.F1[0:kdim, :])
            ev(W.AT[:, cq * 4:(cq + 1) * 4, :, :].rearrange("p c r k -> p (c r k)"), pA[:, :])
        for kb in range(16):
            pU = bank()
            for i in range(4):
                k1 = kb * 4 + i
                ur = pU[:, i * 128:i * 128 + 64]
                ui = pU[:, i * 128 + 64:(i + 1) * 128]
                Ar = W.AT[:, :, 0, k1]
                Ai = W.AT[:, :, 1, k1]
                S.mm(ur, T.GrT[:, k1, :], Ar, start=True, stop=False)
                S.mm(ur, T.GiNT[:, k1, :], Ai, start=False, stop=True)
                S.mm(ui, T.GiT[:, k1, :], Ar, start=True, stop=False)
                S.mm(ui, T.GrT[:, k1, :], Ai, start=False, stop=True)
            if mode == 'kernel':
                ev(W.Kf[:, kb * 4:(kb + 1) * 4, :, :].rearrange("p k r c -> p (k r c)"), pU[:, :])
            else:
                pv = pU[:, :].rearrange("p (k r c) -> p k r c", k=4, r=2)
                Ur = pv[:, :, 0, :]
                Ui = pv[:, :, 1, :]
                Kr = W.Kf[:, kb * 4:(kb + 1) * 4, 0, :]
                Ki = W.Kf[:, kb * 4:(kb + 1) * 4, 1, :]
                t = W.tm[kb % 2]
                S.tt(t[0][:, :, :], Ur, Kr, ALU.mult)
                S.tt(t[1][:, :, :], Ui, Ki, ALU.mult)
                S.tt(t[2][:, :, :], Ur, Ki, ALU.mult)
                S.tt(t[3][:, :, :], Ui, Kr, ALU.mult)
                S.tt(W.V[:, 0, kb * 4:(kb + 1) * 4, :], t[0][:, :, :], t[1][:, :, :], ALU.subtract, eng='pool')
                S.tt(W.V[:, 1, kb * 4:(kb + 1) * 4, :], t[2][:, :, :], t[3][:, :, :], ALU.add, eng='pool')

    S.dma(W.X[:, :, :], kdsrc.rearrange("c (a n) -> a c n", n=128))
    forward(64, 'kernel')
    S.dma(W.X[0:32, :, :], zsrc.rearrange("c (a n) -> a c n", n=128))
    forward(32, 'data')
    for cp in range(32):
        pW = bank()
        for i in range(2):
            ch = cp * 2 + i
            o_ = pW[0:64, i * 256:(i + 1) * 256]
            S.mm(o_, W.V[:, 0, :, ch], T.Rc1[:, :], start=True, stop=False)
            S.mm(o_, W.V[:, 1, :, ch], T.Rc2[:, :], start=False, stop=True)
        ev(W.Wt[:, cp * 2:(cp + 1) * 2, :, :].rearrange("p c r n -> p (c r n)"), pW[0:64, :])
    for nb in range(16):
        pY = bank()
        for i in range(8):
            n2 = nb * 8 + i
            o_ = pY[0:32, i * 64:(i + 1) * 64]
            S.mm(o_, T.LrT[:, n2, :], W.Wt[:, :, 0, n2], start=True, stop=False)
            S.mm(o_, T.LiNT[:, n2, :], W.Wt[:, :, 1, n2], start=False, stop=True)
        ev(W.Ysb[:, :, nb * 8:(nb + 1) * 8], pY[0:32, :].rearrange("p (n c) -> p c n", n=8))
    S.dma(ydst.rearrange("c (a n) -> a c n", n=128), W.Ysb[:, :, :])


def phase_outproj(C):
    S, D, P, sb, pst, nc = C.S, C.D, C.P, C.sb, C.pst, C.nc
    P.IDX = sb("IDX", [128, 16, 4], I32)
    P.GW = sb("GW", [128, 16, 4], F32)
    with S.scope() as ph:
        AFF = sb("AFF", [128, NT, 16], F32, ph)
        AFFT = sb("AFFT", [16, SEQ], F32, ph)
        with S.scope() as p1:
            ytT = sb("ytT", [128, 8, SEQ], BF16, p1)
            wout = sb("wout", [128, 8, DM], BF16, p1)
            wr = sb("wr", [128, 8, 16], F32, p1)
            xb = [sb(f"oxb{i}", [128, DM], F32, p1) for i in range(2)]
            x1 = [sb(f"ox1{i}", [128, DM], F32, p1) for i in range(3)]
            tmp = [sb(f"otmp{i}", [128, DM], F32, p1) for i in range(2)]
            hf = [sb(f"ohf{i}", [128, DM], F32, p1) for i in range(3)]
            hfb = [sb(f"ohfb{i}", [128, DM], BF16, p1) for i in range(3)]
            hfT = [sb(f"ohfT{i}", [128, 8, 128], F32, p1) for i in range(2)]
            junk = sb("ojunk", [128, DM], BF16, p1)
            sm = sb("osm", [128, NT, 8], F32, p1)
            ex = [sb(f"oex{i}", [128, 16], F32, p1) for i in range(2)]
            psM = [[pst(f"psMo{i}{h}", [128, 512], F32, p1) for h in range(2)] for i in range(2)]
            psT = [pst(f"psTo{i}", [128, 512], F32, p1) for i in range(2)]
            psR = pst("psR", [128, 512], F32, p1)
            psAT = pst("psAT", [128, 512], F32, p1)
            psAT2 = psT[1]
            for k in range(8):
                S.dma(ytT[:, k, :], D['Yt'][k * 128:(k + 1) * 128, :])
            wsrc = D['w_out'].rearrange("(k p) c -> p k c", p=128)
            S.dma(wout[:, :, 0:512], wsrc[:, :, 0:512], q='pool')
            S.dma(wout[:, :, 512:1024], wsrc[:, :, 512:1024], q='pool')
            S.dma(wr[:, :, :], D['w_router'].rearrange("(k p) e -> p k e", p=128))
            def stage_a1(j):
                a = j % 2
                b = j % 3
                xt = xb[a]
                S.dma(xt[:, :], D['x'][j * 128:(j + 1) * 128, :])
                for h in range(2):
                    for k in range(8):
                        S.mm(psM[a][h][:, :], ytT[:, k, j * 128:(j + 1) * 128], wout[:, k, h * 512:(h + 1) * 512],
                             start=(k == 0), stop=(k == 7))
                for h in range(2):
                    S.tt(tmp[a][:, h * 512:(h + 1) * 512], psM[a][h][:, :], P.gt1rep[:, h * 512:(h + 1) * 512], ALU.mult)
                S.tt(x1[b][:, :], tmp[a][:, :], xt[:, :], ALU.add)
                S.dma(D['acc'][j * 128:(j + 1) * 128, :], x1[b][:, :])

            def stage_a2(j):
                b = j % 3
                ss = sm[:, j, 0:1]
                rs = sm[:, j, 1:2]
                S.act(junk[:, :], x1[b][:, :], AF.Square, accum_out=ss)
                S.act(rs, ss, AF.Ln, scale=1.0 / DM, bias=EPS)
                S.act(rs, rs, AF.Exp, scale=-0.5)
                S.stt(hf[b][:, :], x1[b][:, :], rs, P.A2rep[:, :], ALU.mult, ALU.mult)
                S.tt(hf[b][:, :], hf[b][:, :], P.B2rep[:, :], ALU.add)
                S.act(hfb[b][:, :], hf[b][:, :], AF.Copy)
                S.dma(D['HFd'][j * 128:(j + 1) * 128, :], hfb[b][:, :])

            def stage_b1(j):
                a = j % 2
                b = j % 3
                for k in range(8):
                    S.transpose(psT[k // 4][:, (k % 4) * 128:(k % 4 + 1) * 128], hf[b][:, k * 128:(k + 1) * 128], P.ident_f[:, :])
                S.copy(hfT[a][:, 0:4, :].rearrange("p k t -> p (k t)"), psT[0][:, :], eng='act')
                S.copy(hfT[a][:, 4:8, :].rearrange("p k t -> p (k t)"), psT[1][:, :], eng='dve')

            def stage_b2(j):
                a = j % 2
                for k in range(8):
                    S.mm(psR[:, 0:16], hfT[a][:, k, :], wr[:, k, :], start=(k == 0), stop=(k == 7))
                mx = sm[:, j, 2:3]
                se = sm[:, j, 3:4]
                S.reduce(mx, psR[:, 0:16], ALU.max)
                S.ts(mx, mx, -1.0, ALU.mult)
                S.act(ex[a][:, :], psR[:, 0:16], AF.Exp, bias=mx, accum_out=se)
                S.recip(se, se)
                S.ts(AFF[:, j, :], ex[a][:, :], se, ALU.mult)
            for step in range(NT + 2):
                if 0 <= step - 2 < NT:
                    stage_b1(step - 2)
                if step < NT:
                    stage_a1(step)
                if 0 <= step - 1 < NT:
                    stage_a2(step - 1)
                if 0 <= step - 2 < NT:
                    stage_b2(step - 2)
            for j in range(NT):
                pat = psAT if j % 2 == 0 else psAT2
                S.transpose(pat[0:16, 0:128], AFF[:, j, :], P.ident_f[:, :])
                S.copy(AFFT[:, j * 128:(j + 1) * 128], pat[0:16, 0:128], eng=('act' if j % 2 else 'dve'))
            if C.debug:
                pass
        if C.debug:
            d_ = C.dbg_out('AFF', [128, NT, 16])
            S.dma(d_[:, :, :], AFF[:, :, :])
        with S.scope() as p2:
            junkA = sb("junkA", [16, SEQ], F32, p2)
            bs = sb("bis", [16, 8], F32, p2)
            THR = sb("THR", [128, 16], F32, p2)
            throw = sb("throw", [1, 16], F32, p2)
            SEL = sb("SEL", [128, NT, 16], F32, p2)
            POS = sb("POS", [128, NT, 16], F32, p2)
            selcum = sb("selcum", [128, 16], F32, p2)
            striU = sb("striU", [128, 128], F32, p2)
            COORD = sb("COORD", [128, NT, 16, 4], BF16, p2)
            jf = sb("jf", [128, NT], F32, p2)
            pf = sb("pf", [128, 1], F32, p2)
            iot = sb("iot", [128, 512], F32, p2)
            OH = [sb(f"OH{i}", [128, 512], BF16, p2) for i in range(3)]
            r4 = [sb(f"r4{i}", [128, 8], F32, p2) for i in range(2)]
            psI = [pst(f"psI{i}", [128, 512], F32, p2) for i in range(4)]
            psP = [pst(f"psP{i}", [128, 512], F32, p2) for i in range(2)]
            psX = pst("psX", [128, 512], F32, p2)
            psB2 = pst("psB2", [128, 512], F32, p2)
            lo, hi, mid, cnt, ge, d1, d2 = [bs[:, i:i + 1] for i in range(7)]
            S.dma(striU[:, :], D['striU'][:, :])
            S.memset(lo, 0.0)
            S.memset(hi, 1.0)
            for it in range(30):
                S.tt(mid, lo, hi, ALU.add)
                S.ts(mid, mid, 0.5, ALU.mult)
                S.ts(junkA[:, :], AFFT[:, :], mid, ALU.is_ge, s2=0.0, op1=ALU.add, accum_out=cnt)
                S.ts(ge, cnt, 511.5, ALU.is_gt)
                S.tt(d1, mid, lo, ALU.subtract)
                S.tt(d2, hi, mid, ALU.subtract)
                S.stt(lo, d1, ge, lo, ALU.mult, ALU.add)
                S.stt(hi, d2, ge, mid, ALU.mult, ALU.add)
            S.transpose(psX[0:1, 0:16], lo, P.ident_f[0:16, 0:16])
            S.copy(throw[:, :], psX[0:1, 0:16])
            S.mm(psB2[:, 0:16], P.ones_f[0:1, 0:128], throw[0:1, :])
            S.copy(THR[:, :], psB2[:, 0:16])
            S.tt(SEL[:, :, :], AFF[:, :, :], THR[:, :].unsqueeze(1).to_broadcast([128, NT, 16]), ALU.is_ge)
            S.memset(selcum[:, :], 0.0)
            for j in range(NT):
                pp = psP[j % 2]
                S.mm(pp[:, 0:16], striU[:, :], SEL[:, j, :], start=True, stop=False)
                S.mm(pp[:, 0:16], P.ones_f[:, :], selcum[:, :], start=False, stop=True)
                S.copy(POS[:, j, :], pp[:, 0:16], eng='act')
                S.tt(selcum[:, :], selcum[:, :], SEL[:, j, :], ALU.add)
            S.op('pool', lambda e: e.iota(jf[:, :], [[1, NT]], base=0, channel_multiplier=0, allow_small_or_imprecise_dtypes=True), [], [jf[:, :]])
            S.op('pool', lambda e: e.iota(pf[:, :], [[1, 1]], base=0, channel_multiplier=1, allow_small_or_imprecise_dtypes=True), [], [pf[:, :]])
            S.op('pool', lambda e: e.iota(iot[:, :], [[1, 512]], base=0, channel_multiplier=0, allow_small_or_imprecise_dtypes=True), [], [iot[:, :]])
            S.copy(COORD[:, :, :, 0], jf[:, :].unsqueeze(2).to_broadcast([128, NT, 16]))
            S.copy(COORD[:, :, :, 1], pf[:, 0:1].unsqueeze(2).to_broadcast([128, NT, 16]))
            S.copy(COORD[:, :, :, 2], AFF[:, :, :])
            S.tt(COORD[:, :, :, 3], AFF[:, :, :], COORD[:, :, :, 2], ALU.subtract)
            n = 0
            for e_ in range(16):
                for j in range(NT):
                    oh = OH[n % 3]
                    n += 1
                    S.ts(oh[:, :], iot[:, :], POS[:, j, e_:e_ + 1], ALU.is_equal, s2=SEL[:, j, e_:e_ + 1], op1=ALU.mult)
                    for sc in range(4):
                        S.mm(psI[sc][:, 0:4], oh[:, sc * 128:(sc + 1) * 128], COORD[:, j, e_, :], start=(j == 0), stop=(j == NT - 1))
                for sc in range(4):
                    r = r4[(e_ * 4 + sc) % 2]
                    S.copy(r[:, 0:4], psI[sc][:, 0:4], eng='act')
                    S.stt(r[:, 4:5], r[:, 0:1], 128.0, r[:, 1:2], ALU.mult, ALU.add)
                    S.copy(P.IDX[:, e_, sc:sc + 1], r[:, 4:5])
                    S.tt(P.GW[:, e_, sc:sc + 1], r[:, 2:3], r[:, 3:4], ALU.add)
        if C.debug:
            d_ = C.dbg_out('IDX', [128, 16, 4], I32)
            S.dma(d_[:, :, :], P.IDX[:, :, :])
            d_ = C.dbg_out('GW', [128, 16, 4])
            S.dma(d_[:, :, :], P.GW[:, :, :])


def phase_route(C):
    pass


def phase_experts(C):
    S, D, P, sb, pst, nc = C.S, C.D, C.P, C.sb, C.pst, C.nc
    with S.scope() as ph:
        stg = [sb(f"stg{i}", [128, 8, 512], F32, ph) for i in range(4)]
        wb = [sb(f"wb{i}", [128, 8, 512], BF16, ph) for i in range(8)]
        xs = sb("xs", [128, 4, DM], BF16, ph)
        xsT = sb("xsT", [128, 8, 512], BF16, ph)
        hidT = sb("hidT", [128, 16, 512], BF16, ph)
        sg = [sb(f"sg{i}", [128, 512], F32, ph) for i in range(2)]
        ysb = [sb(f"ysb{i}", [128, DM], F32, ph) for i in range(4)]
        psT = pst("psTe", [128, 1024], BF16, ph)
        psG = [pst(f"psGe{i}", [128, 512], F32, ph) for i in range(2)]
        psU = pst("psUe", [128, 512], F32, ph)
        psY = [pst(f"psYe{i}", [128, 512], F32, ph) for i in range(4)]
        pieces = []
        for e_ in range(16):
            for fb in range(4):
                for nm in ('w_gate', 'w_up'):
                    pieces.append(D[nm][e_ * 1024:(e_ + 1) * 1024, fb * 512:(fb + 1) * 512].rearrange("(k p) c -> p k c", p=128))
            for dh in range(2):
                for fh in range(2):
                    r0 = e_ * 2048 + fh * 1024
                    pieces.append(D['w_down'][r0:r0 + 1024, dh * 512:(dh + 1) * 512].rearrange("(k p) c -> p k c", p=128))
        issued = [0]
        cast_eng = ['act', 'dve']
        LOOK = 5

        def issue_upto(n):
            while issued[0] < min(n, len(pieces)):
                i = issued[0]
                s_ = stg[i % 4]
                S.dma(s_[:, :, :], pieces[i])
                S.copy(wb[i % 8][:, :, :], s_[:, :, :], eng=cast_eng[i % 2])
                issued[0] += 1
        pc = [0]

        def next_piece():
            i = pc[0]
            issue_upto(i + 1 + LOOK)
            pc[0] += 1
            return wb[i % 8]
        ng = 0
        for e_ in range(16):
            for st in range(4):
                idx_ap = P.IDX[:, e_, st:st + 1]
                S.op('pool', lambda e, st=st, idx_ap=idx_ap: e.indirect_dma_start(
                    out=xs[:, st, :], out_offset=None, in_=D['HFd'][:, :],
                    in_offset=bass.IndirectOffsetOnAxis(ap=idx_ap, axis=0)),
                    [D['HFd'][:, :], idx_ap], [xs[:, st, :]], dma=True)
            for st in range(4):
                for k in range(8):
                    S.transpose(psT[:, k * 128:(k + 1) * 128], xs[:, st, k * 128:(k + 1) * 128], P.ident_bf[:, :])
                S.copy(xsT[:, :, st * 128:(st + 1) * 128], psT[:, :].rearrange("p (k t) -> p k t", k=8), eng=('act' if st % 2 else 'dve'))
            for fb in range(4):
                wg = next_piece()
                wu = next_piece()
                for fc in range(4):
                    pg = psG[ng % 2]
                    sgt = sg[ng % 2]
                    ng += 1
                    for k in range(8):
                        S.mm(pg[:, :], wg[:, k, fc * 128:(fc + 1) * 128], xsT[:, k, :], start=(k == 0), stop=(k == 7))
                    for k in range(8):
                        S.mm(psU[:, :], wu[:, k, fc * 128:(fc + 1) * 128], xsT[:, k, :], start=(k == 0), stop=(k == 7))
                    S.act(sgt[:, :], pg[:, :], AF.Silu)
                    S.tt(hidT[:, fb * 4 + fc, :], sgt[:, :], psU[:, :], ALU.mult)
            for dh in range(2):
                for fh in range(2):
                    wd = next_piece()
                    for st in range(4):
                        for f8 in range(8):
                            S.mm(psY[st][:, :], hidT[:, fh * 8 + f8, st * 128:(st + 1) * 128], wd[:, f8, :],
                                 start=(fh == 0 and f8 == 0), stop=(fh == 1 and f8 == 7))
                for st in range(4):
                    S.stt(ysb[st][:, dh * 512:(dh + 1) * 512], psY[st][:, :], P.GW[:, e_, st:st + 1],
                          P.gt2rep[:, dh * 512:(dh + 1) * 512], ALU.mult, ALU.mult)
            for st in range(4):
                idx_ap = P.IDX[:, e_, st:st + 1]
                S.op('pool', lambda e, st=st, idx_ap=idx_ap: e.indirect_dma_start(
                    out=D['acc'][:, :], out_offset=bass.IndirectOffsetOnAxis(ap=idx_ap, axis=0),
                    in_=ysb[st][:, :], in_offset=None, compute_op=ALU.add),
                    [ysb[st][:, :], idx_ap, D['acc'][:, :]], [D['acc'][:, :]], dma=True)


def phase_final(C):
    S, D, P, sb, pst = C.S, C.D, C.P, C.sb, C.pst
    with S.scope() as ph:
        gfin = sb("gfin", [128, DM], F32, ph)
        xb = [sb(f"fxb{i}", [128, DM], F32, ph) for i in range(2)]
        ob = [sb(f"fob{i}", [128, DM], F32, ph) for i in range(2)]
        junk = sb("fjunk", [128, DM], BF16, ph)
        sm = sb("fsm", [128, NT, 2], F32, ph)
        S.dma(gfin[:, :], D['gfin_row'][0:1, :].partition_broadcast(128))
        for j in range(NT):
            a = j % 2
            S.dma(xb[a][:, :], D['acc'][j * 128:(j + 1) * 128, :])
            ss = sm[:, j, 0:1]
            rs = sm[:, j, 1:2]
            S.act(junk[:, :], xb[a][:, :], AF.Square, accum_out=ss)
            S.act(rs, ss, AF.Ln, scale=1.0 / DM, bias=EPS)
            S.act(rs, rs, AF.Exp, scale=-0.5)
            S.stt(ob[a][:, :], xb[a][:, :], rs, gfin[:, :], ALU.mult, ALU.mult)
            S.dma(D['out'][j * 128:(j + 1) * 128, :], ob[a][:, :])


_PROG = {}


def kernel(**inputs):
    if 'nc' not in _PROG:
        _PROG['nc'] = build()[0]
    nc = _PROG['nc']
    B = inputs['x'].shape[0]
    in_maps = [layout_inputs(inputs, b) for b in range(B)]
    res = run_bass_kernel_spmd(nc, in_maps, core_ids=list(range(B)))
    out = np.stack([np.asarray(r["out"], dtype=np.float32) for r in res.results], axis=0)
    return out
```

```python
import math
import numpy as np
import ml_dtypes
import concourse.bass as bass
import concourse.mybir as mybir
from concourse.bass_utils import run_bass_kernel_spmd
from contextlib import ExitStack

F32 = mybir.dt.float32
BF16 = mybir.dt.bfloat16
I32 = mybir.dt.int32
AF = mybir.ActivationFunctionType
ALU = mybir.AluOpType
AX = mybir.AxisListType

SEQ = 4096
DM = 1024
NT = 32
EPS = 1e-6
EMIT_UNTIL = [None]
COMPUTE = ('pe', 'act', 'dve', 'pool')
SELF_SYNC = {'act': True, 'dve': True, 'pool': True, 'pe': False}
NDMA_SEMS = 6


def ap_box(ap):
    t = ap.tensor
    name = t.name
    dims = list(ap.ap)
    off = int(ap.offset)
    sp = str(ap.space() if callable(ap.space) else ap.space)
    is_dram = 'DRAM' in sp.upper() or 'HBM' in sp.upper() or type(t).__name__.startswith('DRAM') or type(t).__name__.startswith('Dram')
    if is_dram:
        lo = off
        hi = off
        for (st, cnt) in dims:
            st = int(st); cnt = int(cnt)
            if st >= 0:
                hi += st * (cnt - 1)
            else:
                lo += st * (cnt - 1)
        return (name, 0, 1, lo, hi + 1)
    if 'PSUM' in sp.upper() or type(t).__name__.startswith('PSum'):
        return (name, 0, 128, 0, 1 << 30)
    p0 = int(ap.start_partition())
    pc = int(dims[0][1])
    lo = off
    hi = off
    for (st, cnt) in dims[1:]:
        st = int(st); cnt = int(cnt)
        if st >= 0:
            hi += st * (cnt - 1)
        else:
            lo += st * (cnt - 1)
    return (name, p0, p0 + pc, lo, hi + 1)


class Op:
    __slots__ = ('stream', 'fn', 'deps', 'is_dma', 'signal', 'semval', 'dma_slot', 'idx', 'extra_waits')


class Sched:
    def __init__(self, nc, es):
        self.nc = nc
        self.es = es
        self.ops = []
        self.track = {}
        self.eng = {'pe': nc.tensor, 'act': nc.scalar, 'dve': nc.vector, 'pool': nc.gpsimd, 'sp': nc.sync}
        self.sem = {s: es.enter_context(nc.semaphore('sem_' + s)) for s in COMPUTE}
        self.dma_sems = {}
        for s in ('sp', 'pool', 'act'):
            self.dma_sems[s] = [es.enter_context(nc.semaphore(f'dsem_{s}{i}')) for i in range(NDMA_SEMS)]
        self.dma_count = {'sp': 0, 'pool': 0, 'act': 0}
        self.last_dma_ops = {'sp': [], 'pool': [], 'act': []}

    def _deps(self, boxes_r, boxes_w, idx, stream, is_dma):
        deps = set()
        for (boxes, is_w) in ((boxes_r, False), (boxes_w, True)):
            for b in boxes:
                lst = self.track.setdefault(b[0], [])
                keep = []
                for ent in lst:
                    eb, eidx, ew = ent
                    ov = not (eb[2] <= b[1] or b[2] <= eb[1] or eb[4] <= b[3] or b[4] <= eb[3])
                    if ov and (is_w or ew):
                        deps.add(eidx)
                    covered = (b[1] <= eb[1] and eb[2] <= b[2] and b[3] <= eb[3] and eb[4] <= b[4])
                    if is_w and covered:
                        continue
                    if (not is_w) and (not ew) and covered and (not is_dma):
                        eo = self.ops[eidx]
                        if eo.stream == stream and not eo.is_dma:
                            continue
                    keep.append(ent)
                keep.append([b, idx, is_w])
                self.track[b[0]] = keep
        deps.discard(idx)
        return deps

    def op(self, stream, fn, reads=(), writes=(), dma=False):
        o = Op()
        o.idx = len(self.ops)
        o.stream = stream
        o.fn = fn
        o.is_dma = dma
        o.signal = False
        o.semval = None
        o.dma_slot = None
        o.extra_waits = []
        br = [ap_box(a) for a in reads if a is not None and not isinstance(a, (int, float))]
        bw = [ap_box(a) for a in writes]
        self.ops.append(o)
        o.deps = self._deps(br, bw, o.idx, stream, dma)
        return o

    def emit(self):
        ops = self.ops
        for o in ops:
            for d in o.deps:
                po = ops[d]
                if po.is_dma:
                    continue
                if po.stream != o.stream or o.is_dma or SELF_SYNC.get(po.stream, True):
                    po.signal = True
        cnt = {s: 0 for s in COMPUTE}
        dcnt = {'sp': 0, 'pool': 0, 'act': 0}
        for o in ops:
            if o.is_dma:
                d = dcnt[o.stream]
                o.dma_slot = (d % NDMA_SEMS, 16 * (d // NDMA_SEMS + 1))
                dcnt[o.stream] = d + 1
            elif o.signal:
                cnt[o.stream] += 1
                o.semval = cnt[o.stream]
        waited = {s: {} for s in self.eng}
        nw = 0
        for o in ops:
            e = self.eng[o.stream]
            w = waited[o.stream]
            toks = []
            if o.is_dma:
                j, v = o.dma_slot
                if v > 16:
                    toks.append((('d', o.stream, j), self.dma_sems[o.stream][j], v - 16))
            for d in o.deps:
                po = ops[d]
                if po.is_dma:
                    j, v = po.dma_slot
                    toks.append((('d', po.stream, j), self.dma_sems[po.stream][j], v))
                else:
                    if po.stream == o.stream and not o.is_dma and not SELF_SYNC.get(po.stream, True):
                        continue
                    toks.append((('c', po.stream), self.sem[po.stream], po.semval))
            best = {}
            for key, sem, v in toks:
                if v is None:
                    raise RuntimeError('dep on non-signaling op')
                if w.get(key, 0) >= v:
                    continue
                if key not in best or best[key][1] < v:
                    best[key] = (sem, v)
            for key, (sem, v) in best.items():
                e.wait_ge(sem, v)
                w[key] = v
                nw += 1
            ins = o.fn(e)
            if ins is None:
                continue
            if o.is_dma:
                j, v = o.dma_slot
                ins.then_inc(self.dma_sems[o.stream][j], 16)
            elif o.signal:
                ins.then_inc(self.sem[o.stream], 1)
        self.n_waits = nw
        return cnt, dcnt

    def dma(self, out, in_, q='sp', **kw):
        return self.op(q, lambda e: e.dma_start(out=out, in_=in_, **kw), [in_], [out], dma=True)

    def mm(self, out, lhsT, rhs, start=True, stop=True, **kw):
        return self.op('pe', lambda e: e.matmul(out, lhsT, rhs, start=start, stop=stop, **kw),
                       [lhsT, rhs] + ([] if start else [out]), [out])

    def transpose(self, out, in_, ident):
        return self.op('pe', lambda e: e.transpose(out, in_, ident), [in_, ident], [out])

    def act(self, out, in_, func, scale=1.0, bias=0.0, accum_out=None, eng='act'):
        rd = [in_]
        if not isinstance(scale, (int, float)):
            rd.append(scale)
            if func == AF.Copy:
                func = AF.Identity
        if not isinstance(bias, (int, float)):
            rd.append(bias)
            if func == AF.Copy:
                func = AF.Identity
        wr = [out] + ([accum_out] if accum_out is not None else [])
        kw = {}
        if accum_out is not None:
            kw['accum_out'] = accum_out
        return self.op('act', lambda e: e.activation(out=out, in_=in_, func=func, scale=scale, bias=bias, **kw), rd, wr)

    def tt(self, out, in0, in1, op, eng='dve'):
        return self.op(eng, lambda e: e.tensor_tensor(out=out, in0=in0, in1=in1, op=op), [in0, in1], [out])

    def ts(self, out, in0, s1, op0, s2=None, op1=None, eng='dve', accum_out=None):
        rd = [in0]
        if not isinstance(s1, (int, float)):
            rd.append(s1)
        if s2 is not None and not isinstance(s2, (int, float)):
            rd.append(s2)
        kw = {}
        if op1 is not None:
            kw['op1'] = op1
        if accum_out is not None:
            kw['accum_out'] = accum_out
        wr = [out] + ([accum_out] if accum_out is not None else [])
        return self.op(eng, lambda e: e.tensor_scalar(out=out, in0=in0, scalar1=s1, scalar2=s2, op0=op0, **kw), rd, wr)

    def stt(self, out, in0, scalar, in1, op0, op1, eng='dve'):
        rd = [in0, in1]
        if not isinstance(scalar, (int, float)):
            rd.append(scalar)
        return self.op(eng, lambda e: e.scalar_tensor_tensor(out=out, in0=in0, scalar=scalar, in1=in1, op0=op0, op1=op1), rd, [out])

    def copy(self, out, in_, eng='dve'):
        if eng == 'act':
            return self.act(out, in_, AF.Copy)
        return self.op(eng, lambda e: e.tensor_copy(out=out, in_=in_), [in_], [out])

    def memset(self, out, val, eng='dve'):
        return self.op(eng, lambda e: e.memset(out, val), [], [out])

    def reduce(self, out, in_, op, axis=None, eng='dve'):
        axis = axis or AX.X
        return self.op(eng, lambda e: e.tensor_reduce(out=out, in_=in_, op=op, axis=axis), [in_], [out])

    def recip(self, out, in_, eng='dve'):
        return self.op(eng, lambda e: e.reciprocal(out=out, in_=in_), [in_], [out])

    def barrier(self):
        alld = set()
        for lst in self.track.values():
            for ent in lst:
                alld.add(ent[1])
        self.track = {}
        for s_ in ('pe', 'act', 'dve', 'pool', 'sp'):
            o = self.op(s_, lambda e: None, [], [])
            o.deps = set(alld)

    def scope(self):
        return _Scope(self)

    def finish(self, out_aps):
        boxes = [ap_box(a) for a in out_aps]
        o = self.op('sp', lambda e: None, list(out_aps), [])
        return o


class _Scope:
    def __init__(self, S):
        self.S = S
        self.st = ExitStack()

    def __enter__(self):
        self.st.__enter__()
        return self.st

    def __exit__(self, *a):
        self.S.barrier()
        return self.st.__exit__(*a)

_CONSTS = {}


def _bf(a):
    return np.ascontiguousarray(a.astype(np.float32)).astype(ml_dtypes.bfloat16)


def host_consts():
    if _CONSTS:
        return _CONSTS
    c = {}
    c['ident_bf'] = _bf(np.eye(128))
    c['ident_f'] = np.eye(128, dtype=np.float32)
    s = np.arange(128)[:, None]
    t = np.arange(128)[None, :]
    c['triU'] = (s <= t).astype(np.float32)
    c['triL'] = (s >= t).astype(np.float32)
    c['striU'] = (s < t).astype(np.float32)
    N = 8192
    n1 = np.arange(64); k1 = np.arange(64); n2 = np.arange(128); k2 = np.arange(128)
    ang = 2 * np.pi * np.outer(n1, k1) / 64
    c['F1'] = _bf(np.concatenate([np.cos(ang), -np.sin(ang)], 1))
    th = 2 * np.pi * ((n2[:, None, None] * (k1[None, :, None] + 64 * k2[None, None, :])) % N) / N
    c['GrT'] = _bf(np.cos(th).reshape(128, 64 * 128))
    c['GiT'] = _bf((-np.sin(th)).reshape(128, 64 * 128))
    c['GiNT'] = _bf((np.sin(th)).reshape(128, 64 * 128))
    ph = 2 * np.pi * np.outer(k2, n2) / 128
    Rr = np.cos(ph); Ri = np.sin(ph)
    c['Rc1'] = _bf(np.concatenate([Rr, Ri], 1))
    c['Rc2'] = _bf(np.concatenate([-Ri, Rr], 1))
    n1h = np.arange(32)
    thL = 2 * np.pi * (k1[:, None, None] * n1h[None, None, :] / 64 + k1[:, None, None] * n2[None, :, None] / N)
    c['LrT'] = _bf((np.cos(thL) / N).reshape(64, 128 * 32))
    c['LiNT'] = _bf((-np.sin(thL) / N).reshape(64, 128 * 32))
    L = SEQ
    f32 = np.float32
    tt = np.linspace(0.0, 1.0, L, dtype=f32)[:, None]
    w = (f32(2.0 * math.pi) * np.arange(L, dtype=f32)[:, None] / f32(L)).astype(f32)
    bands = np.linspace(1e-4, 15, 16, dtype=f32)[None, :]
    z = np.concatenate([tt, np.cos(bands * w), -np.sin(bands * w)], axis=-1).astype(f32)
    zr = np.concatenate([z[0:1], z[:0:-1]], 0)
    c['zT'] = np.ascontiguousarray(z.T)
    c['zrT'] = np.ascontiguousarray(zr.T)
    trow = tt[:, 0]
    trr = np.concatenate([trow[0:1], trow[:0:-1]])
    c['t_row'] = np.ascontiguousarray(trow[None, :]).astype(f32)
    c['tr_row'] = np.ascontiguousarray(trr[None, :]).astype(f32)
    _CONSTS.update(c)
    return _CONSTS


def colmaj(v, nk):
    return np.ascontiguousarray(np.asarray(v, dtype=np.float32).reshape(nk, 128).T)


def layout_inputs(inp, b):
    m = {}
    f = lambda a: np.ascontiguousarray(np.asarray(a, dtype=np.float32))
    m['x'] = f(inp['x'][b])
    m['ccol'] = colmaj(inp['c'][b], 8)
    m['w_ada'] = f(inp['w_ada'][0])
    m['b_ada'] = f(inp['b_ada'][0][None, :])
    m['gmix_col'] = colmaj(inp['g_mix'][0], 8)
    m['w_in'] = f(inp['w_in'][0])
    bin_ = np.asarray(inp['b_in'][0], dtype=np.float32)
    m['bin_row'] = f(bin_[None, 1024:2064])
    m['bqk_col'] = colmaj(bin_[0:1024], 8)
    m['bhy_col'] = colmaj(bin_[2064:3600], 12)
    cw = np.asarray(inp['conv_qk_w'][0], dtype=np.float32)
    m['cqk_w'] = np.ascontiguousarray(cw.reshape(3, 8, 128).transpose(2, 1, 0))
    m['cqk_b'] = colmaj(inp['conv_qk_b'][0], 8)
    cw = np.asarray(inp['conv_hy_w'][0], dtype=np.float32)
    m['chy_w'] = np.ascontiguousarray(cw.reshape(3, 12, 128).transpose(2, 1, 0))
    m['chy_b'] = colmaj(inp['conv_hy_b'][0], 12)
    m['mnorm_g'] = f(inp['mlstm_norm_g'][0][None, :])
    m['hy_w1'] = f(inp['hy_w1'][0])
    m['hy_b1c'] = f(inp['hy_b1'][0][:, None])
    m['hy_w2'] = f(inp['hy_w2'][0])
    m['hy_b2c'] = f(inp['hy_b2'][0][:, None])
    m['hy_w3'] = f(inp['hy_w3'][0])
    m['hy_frc'] = f(inp['hy_freq'][0][:, None])
    m['hy_del_col'] = colmaj(inp['hy_deltas'][0], 16)
    m['hy_bias_col'] = colmaj(np.asarray(inp['hy_bias'][0]).reshape(-1), 8)
    m['hnorm_col'] = colmaj(inp['hyena_norm_g'][0], 4)
    m['w_out'] = f(inp['w_out'][0])
    m['gffn_row'] = f(inp['g_ffn'][0][None, :])
    m['w_router'] = f(inp['w_router'][0])
    m['w_gate'] = f(inp['w_gate'][0]).reshape(16 * 1024, 2048)
    m['w_up'] = f(inp['w_up'][0]).reshape(16 * 1024, 2048)
    m['w_down'] = f(inp['w_down'][0]).reshape(16 * 2048, 1024)
    m['gfin_row'] = f(np.asarray(inp['g_final'])[None, :])
    m.update(host_consts())
    return m

INPUT_SPECS = [
    ('x', [SEQ, DM], F32), ('ccol', [128, 8], F32), ('w_ada', [DM, 6144], F32), ('b_ada', [1, 6144], F32),
    ('gmix_col', [128, 8], F32), ('w_in', [DM, 3600], F32), ('bin_row', [1, 1040], F32),
    ('bqk_col', [128, 8], F32), ('bhy_col', [128, 12], F32), ('cqk_w', [128, 8, 3], F32), ('cqk_b', [128, 8], F32),
    ('chy_w', [128, 12, 3], F32), ('chy_b', [128, 12], F32), ('mnorm_g', [1, 512], F32),
    ('hy_w1', [33, 64], F32), ('hy_b1c', [64, 1], F32), ('hy_w2', [64, 64], F32), ('hy_b2c', [64, 1], F32),
    ('hy_w3', [64, 2048], F32), ('hy_frc', [64, 1], F32), ('hy_del_col', [128, 16], F32),
    ('hy_bias_col', [128, 8], F32), ('hnorm_col', [128, 4], F32), ('w_out', [DM, DM], F32),
    ('gffn_row', [1, DM], F32), ('w_router', [DM, 16], F32), ('w_gate', [16 * 1024, 2048], F32),
    ('w_up', [16 * 1024, 2048], F32), ('w_down', [16 * 2048, 1024], F32), ('gfin_row', [1, DM], F32),
    ('ident_bf', [128, 128], BF16), ('ident_f', [128, 128], F32), ('triU', [128, 128], F32),
    ('triL', [128, 128], F32), ('striU', [128, 128], F32), ('F1', [64, 128], BF16),
    ('GrT', [128, 8192], BF16), ('GiT', [128, 8192], BF16), ('GiNT', [128, 8192], BF16),
    ('Rc1', [128, 256], BF16), ('Rc2', [128, 256], BF16), ('LrT', [64, 4096], BF16), ('LiNT', [64, 4096], BF16),
    ('zT', [33, SEQ], F32), ('zrT', [33, SEQ], F32), ('t_row', [1, SEQ], F32), ('tr_row', [1, SEQ], F32),
]


class Ctx:
    pass


def build(stop_after=None, debug=False):
    nc = bass.Bass("TRN2", target_bir_lowering=False)
    es = ExitStack()
    S = Sched(nc, es)
    C = Ctx()
    C.nc, C.S, C.es = nc, S, es
    C.debug = debug
    D = {}
    for name, shape, dt in INPUT_SPECS:
        D[name] = nc.dram_tensor(name, shape, dt, kind="ExternalInput").ap()
    D['out'] = nc.dram_tensor("out", [SEQ, DM], F32, kind="ExternalOutput").ap()

    def scratch(name, shape, dt):
        D[name] = nc.dram_tensor(name, shape, dt, kind="Internal").ap()
    scratch('QTd', [512, SEQ], BF16)
    scratch('KTd', [512, SEQ], BF16)
    scratch('Vd', [SEQ, 512], BF16)
    scratch('Od', [SEQ, 512], BF16)
    scratch('X1d', [512, SEQ], F32)
    scratch('X2d', [512, SEQ], F32)
    scratch('Zd', [512, SEQ], BF16)
    scratch('Z1d', [512, SEQ], BF16)
    scratch('Kd0', [512, 2 * SEQ], BF16)
    scratch('Kd1', [512, 2 * SEQ], BF16)
    scratch('Ycv', [512, SEQ], BF16)
    scratch('Yt', [DM, SEQ], BF16)
    scratch('HFd', [SEQ, DM], BF16)
    scratch('acc', [SEQ, DM], F32)
    C.D = D
    C.dbg = {}

    def dbg_out(name, shape, dt=F32):
        t = nc.dram_tensor("dbg_" + name, shape, dt, kind="ExternalOutput").ap()
        C.dbg[name] = t
        return t
    C.dbg_out = dbg_out

    cnt = [0]

    def sb(name, shape, dt, st=None):
        cnt[0] += 1
        return (st or es).enter_context(nc.sbuf_tensor(f"s{cnt[0]}_{name}", shape, dt))

    def pst(name, shape, dt, st=None):
        cnt[0] += 1
        return (st or es).enter_context(nc.psum_tensor(f"p{cnt[0]}_{name}", shape, dt))
    C.sb, C.pst = sb, pst

    P = Ctx()
    C.P = P
    P.ident_bf = sb("ident_bf", [128, 128], BF16)
    P.ident_f = sb("ident_f", [128, 128], F32)
    P.ones_f = sb("ones_f", [128, 128], F32)
    P.modcol = sb("modcol", [128, 48], F32)
    P.A1col = sb("A1col", [128, 8], F32)
    P.gt1rep = sb("gt1rep", [128, DM], F32)
    P.gt2rep = sb("gt2rep", [128, DM], F32)
    P.A2rep = sb("A2rep", [128, DM], F32)
    P.B2rep = sb("B2rep", [128, DM], F32)
    S.dma(P.ident_bf[:, :], D['ident_bf'][:, :])
    S.dma(P.ident_f[:, :], D['ident_f'][:, :])
    S.memset(P.ones_f[:, :], 1.0)

    phases = [phase_mod, phase_norm_proj, phase_mlstm, phase_hyena, phase_outproj, phase_route, phase_experts, phase_final]
    for ph in phases:
        ph(C)
        if stop_after == ph.__name__:
            break
    outs = [D['out'][:, :]] + [t for t in C.dbg.values()]
    S.finish(outs)
    S.emit()
    return nc, C


def phase_mod(C):
    S, D, P, sb, pst = C.S, C.D, C.P, C.sb, C.pst
    with S.scope() as ph:
        wada = [sb(f"wada{i}", [128, 8, 512], F32, ph) for i in range(2)]
        modrow = sb("modrow", [1, 6144], F32, ph)
        badar = sb("badar", [1, 6144], F32, ph)
        ccol = sb("ccol_sb", [128, 8], F32, ph)
        gmixc = sb("gmixc", [128, 8], F32, ph)
        gffn_rep = sb("gffn_rep", [128, DM], F32, ph)
        sc2rep = sb("sc2rep", [128, DM], F32, ph)
        ps = pst("ps_mod", [128, 512], F32, ph)
        psc = pst("ps_modc", [128, 512], F32, ph)
        psr = [pst(f"ps_modr{i}", [128, 512], F32, ph) for i in range(2)]
        S.dma(badar[:, :], D['b_ada'][:, :])
        S.dma(ccol[:, :], D['ccol'][:, :])
        S.dma(gmixc[:, :], D['gmix_col'][:, :])
        S.dma(gffn_rep[:, :], D['gffn_row'][0:1, :].partition_broadcast(128))
        wsrc = D['w_ada'].rearrange("(k p) c -> p k c", p=128)
        for blk in range(12):
            buf = wada[blk % 2]
            S.dma(buf[:, :, :], wsrc[:, :, blk * 512:(blk + 1) * 512])
            for k in range(8):
                S.mm(ps[0:1, :], ccol[:, k:k + 1], buf[:, k, :], start=(k == 0), stop=(k == 7))
            S.tt(modrow[0:1, blk * 512:(blk + 1) * 512], ps[0:1, :], badar[0:1, blk * 512:(blk + 1) * 512], ALU.add)
        for oc in range(48):
            S.mm(psc[:, oc:oc + 1], modrow[0:1, oc * 128:(oc + 1) * 128], P.ones_f[0:1, 0:1])
        S.copy(P.modcol[:, :], psc[:, 0:48])
        S.stt(P.A1col[:, :], P.modcol[:, 8:16], 1.0, gmixc[:, :], ALU.add, ALU.mult)
        n = 0
        for (dst, j) in ((P.gt1rep, 2), (P.gt2rep, 5), (sc2rep, 4), (P.B2rep, 3)):
            for h in range(2):
                pr = psr[n % 2]
                n += 1
                S.mm(pr[:, :], P.ones_f[0:1, 0:128], modrow[0:1, j * 1024 + h * 512: j * 1024 + (h + 1) * 512])
                S.copy(dst[:, h * 512:(h + 1) * 512], pr[:, :], eng=('act' if n % 2 else 'dve'))
        S.stt(P.A2rep[:, :], sc2rep[:, :], 1.0, gffn_rep[:, :], ALU.add, ALU.mult)
        if C.debug:
            d = C.dbg_out('modcol', [128, 48])
            S.dma(d[:, :], P.modcol[:, :])
            d = C.dbg_out('gt1rep', [128, DM])
            S.dma(d[:, :], P.gt1rep[:, :])


def phase_norm_proj(C):
    S, D, P, sb, pst = C.S, C.D, C.P, C.sb, C.pst
    P.Gt = sb("Gt", [128, NT, 16], F32)
    with S.scope() as ph:
        hT = sb("hT", [128, 8, SEQ], BF16, ph)
        with S.scope() as p1:
            xb = [sb(f"xb{i}", [128, DM], F32, p1) for i in range(2)]
            xn = [sb(f"xn{i}", [128, DM], BF16, p1) for i in range(2)]
            junk = sb("junk", [128, DM], BF16, p1)
            ss = sb("ss", [128, NT], F32, p1)
            rs = sb("rs", [128, NT], F32, p1)
            psT = [pst(f"psT{i}", [128, DM], BF16, p1) for i in range(2)]
            for j in range(NT):
                xt = xb[j % 2]
                S.dma(xt[:, :], D['x'][j * 128:(j + 1) * 128, :])
                S.act(junk[:, :], xt[:, :], AF.Square, accum_out=ss[:, j:j + 1])
                S.act(rs[:, j:j + 1], ss[:, j:j + 1], AF.Ln, scale=1.0 / DM, bias=EPS)
                S.act(rs[:, j:j + 1], rs[:, j:j + 1], AF.Exp, scale=-0.5)
                S.act(xn[j % 2][:, :], xt[:, :], AF.Copy, scale=rs[:, j:j + 1])
                pt = psT[j % 2]
                for k in range(8):
                    S.transpose(pt[:, k * 128:(k + 1) * 128], xn[j % 2][:, k * 128:(k + 1) * 128], P.ident_bf[:, :])
                for k in range(8):
                    S.act(hT[:, k, j * 128:(j + 1) * 128], pt[:, k * 128:(k + 1) * 128], AF.Identity,
                          scale=P.A1col[:, k:k + 1], bias=P.modcol[:, k:k + 1])
        if C.debug:
            d = C.dbg_out('hT', [128, 8, SEQ], BF16)
            for k in range(8):
                S.dma(d[:, k, :], hT[:, k, :])
        with S.scope() as p2:
            wvo = sb("wvo", [128, 8, 1040], BF16, p2)
            brow = sb("brow", [128, 1040], F32, p2)
            vt = [sb(f"vt{i}", [128, 512], BF16, p2) for i in range(2)]
            ot = [sb(f"ot{i}", [128, 512], BF16, p2) for i in range(2)]
            otf = [sb(f"otf{i}", [128, 512], F32, p2) for i in range(2)]
            psV = [pst(f"psV{i}", [128, 512], F32, p2) for i in range(2)]
            psO = [pst(f"psO{i}", [128, 512], F32, p2) for i in range(2)]
            psG = [pst(f"psG{i}", [128, 512], F32, p2) for i in range(2)]
            wsrc = D['w_in'].rearrange("(k p) c -> p k c", p=128)
            S.dma(wvo[:, :, 0:512], wsrc[:, :, 1024:1536], q='pool')
            S.dma(wvo[:, :, 512:1040], wsrc[:, :, 1536:2064], q='pool')
            S.dma(brow[:, :], D['bin_row'][0:1, :].partition_broadcast(128))
            for j in range(NT):
                a = j % 2
                for (pp, c0, c1) in ((psV[a], 0, 512), (psO[a], 512, 1024), (psG[a], 1024, 1040)):
                    for k in range(8):
                        S.mm(pp[:, 0:c1 - c0], hT[:, k, j * 128:(j + 1) * 128], wvo[:, k, c0:c1], start=(k == 0), stop=(k == 7))
                S.tt(vt[a][:, :], psV[a][:, :], brow[:, 0:512], ALU.add)
                S.dma(D['Vd'][j * 128:(j + 1) * 128, :], vt[a][:, :])
                S.tt(otf[a][:, :], psO[a][:, :], brow[:, 512:1024], ALU.add)
                S.act(ot[a][:, :], otf[a][:, :], AF.Sigmoid)
                S.dma(D['Od'][j * 128:(j + 1) * 128, :], ot[a][:, :])
                S.tt(P.Gt[:, j, :], psG[a][:, 0:16], brow[:, 1024:1040], ALU.add)
        if C.debug:
            d = C.dbg_out('Gt', [128, NT, 16])
            S.dma(d[:, :, :], P.Gt[:, :, :])
        with S.scope() as p3:
            wch = [sb(f"wch{i}", [128, 8, 128], BF16, p3) for i in range(3)]
            pre = [sb(f"pre{i}", [128, SEQ + 2], F32, p3) for i in range(2)]
            t0s = [sb(f"cv_t0{i}", [128, 2048], F32, p3) for i in range(2)]
            t2s = [sb(f"cv_t2{i}", [128, 2048], F32, p3) for i in range(2)]
            t3 = [[sb(f"cv_t3{q}{i}", [128, 2048], F32, p3) for i in range(2)] for q in range(2)]
            ob = [[sb(f"cv_ob{q}{i}", [128, 2048], BF16, p3) for i in range(2)] for q in range(2)]
            pending = [None]
            bqk = sb("bqk", [128, 8], F32, p3)
            bhy = sb("bhy", [128, 12], F32, p3)
            cqw = sb("cqw", [128, 8, 3], F32, p3)
            cqb = sb("cqb", [128, 8], F32, p3)
            chw = sb("chw", [128, 12, 3], F32, p3)
            chb = sb("chb", [128, 12], F32, p3)
            psF = [pst(f"psF{i}", [128, 512], F32, p3) for i in range(8)]
            for (tile_, nm) in ((bqk, 'bqk_col'), (bhy, 'bhy_col'), (cqb, 'cqk_b'), (chb, 'chy_b')):
                S.dma(tile_[:, :], D[nm][:, :])
            S.dma(cqw[:, :, :], D['cqk_w'][:, :, :])
            S.dma(chw[:, :, :], D['chy_w'][:, :, :])
            for i in range(2):
                S.memset(pre[i][:, 0:1], 0.0)
                S.memset(pre[i][:, SEQ + 1:SEQ + 2], 0.0)
            wsrc = D['w_in'].rearrange("(k p) c -> p k c", p=128)
            chunks = []
            for cc in range(8):
                chunks.append(('qk', cc, cc * 128))
            for i in range(12):
                chunks.append(('hy', i, 2064 + i * 128))
            nps = 0
            for m_ in range(2):
                S.dma(wch[m_ % 3][:, :, :], wsrc[:, :, chunks[m_][2]:chunks[m_][2] + 128], q='pool')
            for n, (kind, ci, col0) in enumerate(chunks):
                wc = wch[n % 3]
                pr = pre[n % 2]
                if n + 2 < len(chunks):
                    S.dma(wch[(n + 2) % 3][:, :, :], wsrc[:, :, chunks[n + 2][2]:chunks[n + 2][2] + 128], q='pool')
                bcol = bqk[:, ci:ci + 1] if kind == 'qk' else bhy[:, ci:ci + 1]
                cw = cqw if kind == 'qk' else chw
                cb = cqb if kind == 'qk' else chb
                for tb in range(8):
                    pp = psF[nps % 8]
                    nps += 1
                    for k in range(8):
                        S.mm(pp[:, :], wc[:, k, :], hT[:, k, tb * 512:(tb + 1) * 512], start=(k == 0), stop=(k == 7))
                    if tb % 2 == 0:
                        S.act(pr[:, 1 + tb * 512:1 + (tb + 1) * 512], pp[:, :], AF.Identity, bias=bcol)
                    else:
                        S.ts(pr[:, 1 + tb * 512:1 + (tb + 1) * 512], pp[:, :], bcol, ALU.add)
                par = n % 2
                for hh in range(2):
                    o0 = hh * 2048
                    S.act(t0s[hh][:, :], pr[:, o0:o0 + 2048], AF.Identity, scale=cw[:, ci, 0:1], bias=cb[:, ci:ci + 1])
                    S.act(t2s[hh][:, :], pr[:, o0 + 2:o0 + 2050], AF.Identity, scale=cw[:, ci, 2:3])
                for hh in range(2):
                    o0 = hh * 2048
                    S.stt(t0s[hh][:, :], pr[:, o0 + 1:o0 + 2049], cw[:, ci, 1:2], t0s[hh][:, :], ALU.mult, ALU.add)
                for hh in range(2):
                    if kind == 'hy' and ci >= 8:
                        S.tt(ob[par][hh][:, :], t0s[hh][:, :], t2s[hh][:, :], ALU.add, eng='pool')
                    else:
                        S.tt(t3[par][hh][:, :], t0s[hh][:, :], t2s[hh][:, :], ALU.add, eng='pool')
                if pending[0] is not None:
                    pending[0]()

                def fin(kind=kind, ci=ci, par=par):
                    for hh in range(2):
                        o0 = hh * 2048
                        if kind == 'qk':
                            S.act(ob[par][hh][:, :], t3[par][hh][:, :], AF.Silu)
                            dst = D['QTd'] if ci < 4 else D['KTd']
                            S.dma(dst[(ci % 4) * 128:(ci % 4 + 1) * 128, o0:o0 + 2048], ob[par][hh][:, :])
                        elif ci < 8:
                            dst = D['X1d'] if ci < 4 else D['X2d']
                            S.dma(dst[(ci % 4) * 128:(ci % 4 + 1) * 128, o0:o0 + 2048], t3[par][hh][:, :])
                        else:
                            S.dma(D['Zd'][(ci - 8) * 128:(ci - 7) * 128, o0:o0 + 2048], ob[par][hh][:, :])
                pending[0] = fin
            pending[0]()
    if C.debug:
        for nm, shp, dt in (('QTd', [512, SEQ], BF16), ('KTd', [512, SEQ], BF16), ('Vd', [SEQ, 512], BF16),
                            ('Od', [SEQ, 512], BF16), ('X1d', [512, SEQ], F32), ('Zd', [512, SEQ], BF16)):
            d = C.dbg_out(nm, shp, dt)
            with S.scope() as pd:
                if shp[0] == 512:
                    tmp = sb("dbgtmp_" + nm, [128, 4, SEQ], dt, pd)
                    S.dma(tmp[:, :, :], D[nm].rearrange("(a p) n -> p a n", p=128))
                    S.dma(d.rearrange("(a p) n -> p a n", p=128), tmp[:, :, :])
                else:
                    tmp = sb("dbgtmp_" + nm, [128, NT, 512], dt, pd)
                    S.dma(tmp[:, :, :], D[nm].rearrange("(a p) n -> p a n", p=128))
                    S.dma(d.rearrange("(a p) n -> p a n", p=128), tmp[:, :, :])


def phase_mlstm(C):
    S, D, P, sb, pst = C.S, C.D, C.P, C.sb, C.pst
    Gt = P.Gt
    with S.scope() as ph:
        triU = sb("triU", [128, 128], F32, ph)
        triL = sb("triL", [128, 128], F32, ph)
        LF = sb("LF", [128, 2, NT, 4], F32, ph)
        II = sb("II", [128, 2, NT, 4], F32, ph)
        Bc = sb("Bc", [128, 2, NT, 4], F32, ph)
        Wp = sb("Wp", [128, 2, NT, 4], F32, ph)
        ENB = sb("ENB", [128, 2, NT, 4], F32, ph)
        EG = sb("EG", [128, 2, NT, 4], F32, ph)
        zcol = sb("zcol", [128, 1], F32, ph)
        S.dma(triU[:, :], D['triU'][:, :])
        S.dma(triL[:, :], D['triL'][:, :])
        S.memset(zcol[:, :], 0.0)
        with S.scope() as pp:
            psB = pst("psB", [128, 512], F32, pp)
            psGs = pst("psGs", [128, 512], F32, pp)
            for d in range(2):
                S.act(LF[:, d, :, :], Gt[:, :, d * 8 + 4:d * 8 + 8], AF.Exp, scale=-1.0)
                S.copy(II[:, d, :, :], Gt[:, :, d * 8:d * 8 + 4])
            fl = lambda t: t[:, :, :, :].rearrange("p d c e -> p (d c e)")
            S.act(fl(LF), fl(LF), AF.Ln, bias=1.0)
            S.ts(fl(LF), fl(LF), -1.0, ALU.mult)
            S.mm(psB[:, 0:128], triU[:, :], fl(LF)[:, 0:128])
            S.mm(psB[:, 128:256], triL[:, :], fl(LF)[:, 128:256])
            S.mm(psGs[:, 0:256], P.ones_f[:, :], fl(LF))
            S.copy(fl(Bc), psB[:, 0:256])
            S.act(fl(EG), psGs[:, 0:256], AF.Exp)
            S.tt(fl(Wp), fl(II), fl(Bc), ALU.subtract)
            S.act(fl(Wp), fl(Wp), AF.Exp, bias=math.log(128.0 ** -0.5))
            S.act(fl(ENB), fl(Bc), AF.Exp, scale=-1.0)
        if C.debug:
            for nm, t in (('Bc', Bc), ('Wp', Wp), ('EG', EG)):
                d_ = C.dbg_out(nm, [128, 2, NT, 4])
                S.dma(d_[:, :, :, :], t[:, :, :, :])
        for hd in range(4):
            with S.scope() as hs:
                qT = sb("qT", [128, SEQ], BF16, hs)
                kT = sb("kT", [128, SEQ], BF16, hs)
                vaug = sb("vaug", [128, NT, 129], BF16, hs)
                osg = sb("osg", [128, NT, 128], BF16, hs)
                ktm = sb("ktm", [128, NT, 128], BF16, hs)
                Hs = sb("Hs", [128, NT, 128], F32, hs)
                sq = sb("sq", [128, NT, 128], F32, hs)
                gO = sb("gO", [128, NT, 128], BF16, hs)
                ym = sb("ym", [128, NT, 128], BF16, hs)
                ymT = sb("ymT", [128, SEQ], BF16, hs)
                mng = sb("mng", [128, 128], F32, hs)
                STw = [sb(f"STw{i}", [128, 128], BF16, hs) for i in range(4)]
                vw = [sb(f"vw{i}", [128, 129], BF16, hs) for i in range(4)]
                Tst = sb("Tst", [128, 129], F32, hs)
                Tst2 = sb("Tst2", [128, 129], F32, hs)
                Cbfd = [[sb(f"Cbf{d_}{i}", [128, 129], BF16, hs) for i in range(2)] for d_ in range(2)]
                sm = [sb(f"sm{i}", [128, 4], F32, hs) for i in range(2)]
                ssq = sb("ssq", [128, NT], F32, hs)
                pS = [pst(f"pS{i}", [128, 512], F32, hs) for i in range(2)]
                pO = [pst(f"pO{i}", [128, 512], F32, hs) for i in range(2)]
                pC = [pst(f"pC{i}", [128, 512], F32, hs) for i in range(2)]
                pK = pst("pK", [128, 1024], BF16, hs)
                S.dma(qT[:, :], D['QTd'][hd * 128:(hd + 1) * 128, :])
                S.dma(kT[:, :], D['KTd'][hd * 128:(hd + 1) * 128, :])
                S.dma(vaug[:, :, 0:128], D['Vd'][:, hd * 128:(hd + 1) * 128].rearrange("(c p) d -> p c d", p=128))
                S.memset(vaug[:, :, 128:129], 1.0)
                S.dma(osg[:, :, :], D['Od'][:, hd * 128:(hd + 1) * 128].rearrange("(c p) d -> p c d", p=128))
                S.dma(mng[:, :], D['mnorm_g'][0:1, hd * 128:(hd + 1) * 128].partition_broadcast(128))
                for c0 in range(0, NT, 8):
                    for c in range(c0, c0 + 8):
                        S.transpose(pK[:, (c - c0) * 128:(c - c0 + 1) * 128], kT[:, c * 128:(c + 1) * 128], P.ident_bf[:, :])
                    S.copy(ktm[:, c0:c0 + 8, :].rearrange("p c d -> p (c d)"), pK[:, :], eng=('act' if (c0 // 8) % 2 else 'dve'))
                n = 0
                Tsts = [Tst, Tst2]
                prevc = [None, None]
                visited = set()
                for d in range(2):
                    S.memset(Tsts[d][:, :], 0.0)
                    S.memset(Cbfd[d][0][:, :], 0.0)
                nd = [0, 0]
                steps = []
                for i in range(NT):
                    for d in range(2):
                        steps.append((d, i if d == 0 else NT - 1 - i))

                def phase1(n):
                    d, c = steps[n]
                    cs = slice(c * 128, (c + 1) * 128)
                    wcol = Wp[:, d, c, hd:hd + 1]
                    mask = triU if d == 0 else triL
                    S.mm(pS[n % 2][:, 0:128], kT[:, cs], qT[:, cs])
                    S.stt(STw[n % 4][:, :], pS[n % 2][:, 0:128], wcol, mask[:, :], ALU.mult, ALU.mult)
                    S.act(vw[n % 4][:, :], vaug[:, c, :], AF.Copy, scale=wcol)

                def phase2(n):
                    d, c = steps[n]
                    a = n % 2
                    b_ = nd[d] % 2
                    nd[d] += 1
                    cs = slice(c * 128, (c + 1) * 128)
                    S.mm(pO[a][:, 0:129], STw[n % 4][:, :], vaug[:, c, :], start=True, stop=False)
                    S.mm(pO[a][:, 0:129], qT[:, cs], Cbfd[d][b_][:, :], start=False, stop=True)
                    S.mm(pC[a][:, 0:129], ktm[:, c, :], vw[n % 4][:, :])
                    enb = ENB[:, d, c, hd:hd + 1]
                    s_ = sm[a]
                    S.ts(s_[:, 0:1], pO[a][:, 128:129], enb, ALU.max)
                    S.stt(s_[:, 2:3], pO[a][:, 128:129], -1.0, s_[:, 0:1], ALU.mult, ALU.max)
                    S.recip(s_[:, 3:4], s_[:, 2:3])
                    if c not in visited:
                        visited.add(c)
                        S.act(Hs[:, c, :], pO[a][:, 0:128], AF.Copy, scale=s_[:, 3:4])
                    else:
                        S.stt(Hs[:, c, :], pO[a][:, 0:128], s_[:, 3:4], Hs[:, c, :], ALU.mult, ALU.add)
                    egp = zcol[:, 0:1] if prevc[d] is None else EG[:, d, prevc[d], hd:hd + 1]
                    S.stt(Tsts[d][:, :], Tsts[d][:, :], egp, pC[a][:, 0:129], ALU.mult, ALU.add)
                    S.act(Cbfd[d][(b_ + 1) % 2][:, :], Tsts[d][:, :], AF.Copy, scale=EG[:, d, c, hd:hd + 1])
                    prevc[d] = c
                phase1(0)
                for n_ in range(len(steps)):
                    if n_ + 1 < len(steps):
                        phase1(n_ + 1)
                    phase2(n_)
                S.act(sq[:, :, :], Hs[:, :, :], AF.Square)
                S.reduce(ssq[:, :], sq[:, :, :], ALU.add)
                S.ts(ssq[:, :], ssq[:, :], 1.0 / 128.0, ALU.mult, s2=EPS, op1=ALU.add)
                S.act(ssq[:, :], ssq[:, :], AF.Sqrt)
                S.recip(ssq[:, :], ssq[:, :])
                S.tt(gO[:, :, :], osg[:, :, :], mng[:, :].unsqueeze(1).to_broadcast([128, NT, 128]), ALU.mult)
                for c in range(NT):
                    S.stt(ym[:, c, :], Hs[:, c, :], ssq[:, c:c + 1], gO[:, c, :], ALU.mult, ALU.mult)
                for c0 in range(0, NT, 8):
                    for c in range(c0, c0 + 8):
                        S.transpose(pK[:, (c - c0) * 128:(c - c0 + 1) * 128], ym[:, c, :], P.ident_bf[:, :])
                    S.copy(ymT[:, c0 * 128:(c0 + 8) * 128], pK[:, :], eng=('act' if (c0 // 8) % 2 else 'dve'))
                S.dma(D['Yt'][hd * 128:(hd + 1) * 128, :], ymT[:, :])
    if C.debug:
        d_ = C.dbg_out('Yt_m', [512, SEQ], BF16)
        with S.scope() as pd:
            tmp = sb("dbgtmp_ytm", [128, 4, SEQ], BF16, pd)
            S.dma(tmp[:, :, :], D['Yt'][0:512, :].rearrange("(a p) n -> p a n", p=128))
            S.dma(d_.rearrange("(a p) n -> p a n", p=128), tmp[:, :, :])


def phase_hyena(C):
    S, D, P, sb, pst = C.S, C.D, C.P, C.sb, C.pst
    PI = math.pi
    with S.scope() as pa:
        hid2T = sb("hid2T", [64, SEQ], F32, pa)
        hid2rT = sb("hid2rT", [64, SEQ], F32, pa)
        frc = sb("frc", [64, 1], F32, pa)
        frb1 = sb("frb1", [64, 1], F32, pa)
        frb2 = sb("frb2", [64, 1], F32, pa)
        with S.scope() as p1:
            zT = sb("zT", [33, SEQ], F32, p1)
            zrT = sb("zrT", [33, SEQ], F32, p1)
            hid1 = sb("hid1", [64, SEQ], F32, p1)
            w1 = sb("hw1", [33, 64], F32, p1)
            w2 = sb("hw2", [64, 64], F32, p1)
            b1c = sb("b1c", [64, 1], F32, p1)
            b2c = sb("b2c", [64, 1], F32, p1)
            arg = [sb(f"harg{i}", [64, 512], F32, p1) for i in range(2)]
            m1 = [sb(f"hm1{i}", [64, 512], F32, p1) for i in range(2)]
            m2 = [sb(f"hm2{i}", [64, 512], F32, p1) for i in range(2)]
            psM = [pst(f"psM{i}", [128, 512], F32, p1) for i in range(2)]
            S.dma(zT[:, :], D['zT'][:, :])
            S.dma(zrT[:, :], D['zrT'][:, :])
            S.dma(w1[:, :], D['hy_w1'][:, :])
            S.dma(w2[:, :], D['hy_w2'][:, :])
            S.dma(b1c[:, :], D['hy_b1c'][:, :])
            S.dma(b2c[:, :], D['hy_b2c'][:, :])
            S.dma(frc[:, :], D['hy_frc'][:, :])
            S.tt(frb1[:, :], frc[:, :], b1c[:, :], ALU.mult)
            S.tt(frb2[:, :], frc[:, :], b2c[:, :], ALU.mult)
            n = 0

            def sin_layer(ps, frb, dst):
                nonlocal n
                a = n % 2
                n += 1
                S.ts(arg[a][:, :], ps, frc[:, 0:1], ALU.mult, s2=frb[:, 0:1], op1=ALU.add)
                S.ts(m1[a][:, :], arg[a][:, :], PI, ALU.is_gt, s2=-2.0 * PI, op1=ALU.mult)
                S.ts(m2[a][:, :], arg[a][:, :], -PI, ALU.is_lt, s2=2.0 * PI, op1=ALU.mult)
                S.tt(arg[a][:, :], arg[a][:, :], m1[a][:, :], ALU.add)
                S.tt(arg[a][:, :], arg[a][:, :], m2[a][:, :], ALU.add)
                S.act(dst, arg[a][:, :], AF.Sin)
            for (zs, hdst) in ((zT, hid2T), (zrT, hid2rT)):
                for blk in range(8):
                    ps = psM[blk % 2]
                    S.mm(ps[0:64, :], w1[:, :], zs[:, blk * 512:(blk + 1) * 512])
                    sin_layer(ps[0:64, :], frb1, hid1[:, blk * 512:(blk + 1) * 512])
                for blk in range(8):
                    ps = psM[blk % 2]
                    S.mm(ps[0:64, :], w2[:, :], hid1[:, blk * 512:(blk + 1) * 512])
                    sin_layer(ps[0:64, :], frb2, hdst[:, blk * 512:(blk + 1) * 512])
        with S.scope() as p2:
            w3 = sb("hw3", [64, 2048], F32, p2)
            trow = sb("trow", [128, SEQ], F32, p2)
            trrow = sb("trrow", [128, SEQ], F32, p2)
            ndel = sb("ndel", [128, 16], F32, p2)
            hbias = sb("hbias", [128, 8], F32, p2)
            kT = [sb(f"kTb{i}", [128, 2 * SEQ], BF16, p2) for i in range(2)]
            win = [sb(f"win{i}", [128, 512], F32, p2) for i in range(2)]
            k0t = sb("k0t", [128, 4], F32, p2)
            psK_ = [pst(f"psKf{i}", [128, 512], F32, p2) for i in range(3)]
            ps0 = pst("psK0", [128, 512], F32, p2)
            S.dma(w3[:, :], D['hy_w3'][:, :])
            S.dma(trow[:, :], D['t_row'][0:1, :].partition_broadcast(128))
            S.dma(trrow[:, :], D['tr_row'][0:1, :].partition_broadcast(128))
            S.dma(ndel[:, :], D['hy_del_col'][:, :])
            S.dma(hbias[:, :], D['hy_bias_col'][:, :])
            S.stt(ndel[:, :], ndel[:, :], -1.0, ndel[:, :], ALU.mult, ALU.max)
            S.ts(ndel[:, :], ndel[:, :], -1.0, ALU.mult)
            nb = 0
            nk = 0
            for o in range(2):
                for g in range(4):
                    kt = kT[nk % 2]
                    nk += 1
                    for d in range(2):
                        hs = hid2T if d == 0 else hid2rT
                        tr = trow if d == 0 else trrow
                        c0 = (o * 2 + d) * 512 + g * 128
                        di = o * 8 + d * 4 + g
                        for blk in range(8):
                            ps = psK_[nb % 3]
                            wn = win[nb % 2]
                            nb += 1
                            S.mm(ps[:, :], w3[:, c0:c0 + 128], hs[:, blk * 512:(blk + 1) * 512])
                            S.act(wn[:, :], tr[:, blk * 512:(blk + 1) * 512], AF.Exp, scale=ndel[:, di:di + 1])
                            S.stt(kt[:, d * SEQ + blk * 512:d * SEQ + (blk + 1) * 512], wn[:, :], 0.05, ps[:, :], ALU.add, ALU.mult)
                    S.memset(kt[:, SEQ:SEQ + 1], 0.0)
                    cf = (o * 2 + 0) * 512 + g * 128
                    cb = (o * 2 + 1) * 512 + g * 128
                    S.mm(ps0[:, 0:1], w3[:, cf:cf + 128], hid2T[:, 0:1])
                    S.mm(ps0[:, 1:2], w3[:, cb:cb + 128], hid2T[:, 0:1])
                    S.copy(k0t[:, 0:2], ps0[:, 0:2])
                    S.tt(k0t[:, 2:3], k0t[:, 0:1], k0t[:, 1:2], ALU.add)
                    S.ts(k0t[:, 3:4], k0t[:, 2:3], 1.05, ALU.mult)
                    S.tt(kt[:, 0:1], k0t[:, 3:4], hbias[:, o * 4 + g:o * 4 + g + 1], ALU.add)
                    dst = D['Kd0'] if o == 0 else D['Kd1']
                    S.dma(dst[g * 128:(g + 1) * 128, :], kt[:, :])
    if C.debug:
        d_ = C.dbg_out('Kd0', [512, 2 * SEQ], BF16)
        with S.scope() as pd:
            tmp = sb("dbgtmp_kd", [128, 4, 2 * SEQ], BF16, pd)
            S.dma(tmp[:, :, :], D['Kd0'].rearrange("(a p) n -> p a n", p=128))
            S.dma(d_.rearrange("(a p) n -> p a n", p=128), tmp[:, :, :])
    with S.scope() as pb:
        T = Ctx()
        T.F1 = sb("F1", [64, 128], BF16, pb)
        T.GrT = sb("GrT", [128, 64, 128], BF16, pb)
        T.GiT = sb("GiT", [128, 64, 128], BF16, pb)
        T.Rc1 = sb("Rc1", [128, 256], BF16, pb)
        T.Rc2 = sb("Rc2", [128, 256], BF16, pb)
        T.LrT = sb("LrT", [64, 128, 32], BF16, pb)
        T.LiNT = sb("LiNT", [64, 128, 32], BF16, pb)
        hnorm = sb("hnorm", [128, 4], F32, pb)
        S.dma(T.F1[:, :], D['F1'][:, :])
        for nm in ('GrT', 'GiT'):
            S.dma(getattr(T, nm)[:, :, :], D[nm].rearrange("p (k m) -> p k m", k=64))
        S.dma(T.Rc1[:, :], D['Rc1'][:, :])
        S.dma(T.Rc2[:, :], D['Rc2'][:, :])
        S.dma(T.LrT[:, :, :], D['LrT'].rearrange("p (n m) -> p n m", n=128))
        S.dma(T.LiNT[:, :, :], D['LiNT'].rearrange("p (n m) -> p n m", n=128))
        S.dma(hnorm[:, :], D['hnorm_col'][:, :])
        for o in range(2):
            zsrc = D['Zd'] if o == 0 else D['Z1d']
            kd = D['Kd0'] if o == 0 else D['Kd1']
            with S.scope() as pw:
                W = Ctx()
                W.X = sb("fX", [64, 64, 128], BF16, pw)
                W.AT = sb("fAT", [128, 64, 2, 64], BF16, pw)
                W.ATn = sb("fATn", [128, 64, 2, 64], BF16, pw)
                W.Kf = sb("fKf", [128, 64, 2, 64], BF16, pw)
                W.V = sb("fV", [128, 2, 64, 64], BF16, pw)
                W.Wt = sb("fWt", [64, 64, 2, 128], BF16, pw)
                W.Ysb = sb("fY", [32, 64, 128], BF16, pw)
                W.tm = [[sb(f"ftm{i}{j}", [128, 4, 64], F32, pw) for j in range(4)] for i in range(2)]
                W.ring = [pst(f"psR{i}", [128, 512], F32, pw) for i in range(8)]
                W.nr = 0
                W.ne = 0
                for b8 in range(8):
                    ch0 = b8 * 64
                    fft_batch(C, T, W, zsrc[ch0:ch0 + 64, :], kd[ch0:ch0 + 64, :], D['Ycv'][ch0:ch0 + 64, :])
            with S.scope() as pg:
                ysb = [sb(f"gy{i}", [128, SEQ], BF16, pg) for i in range(2)]
                xsb = [sb(f"gx{i}", [128, SEQ], F32, pg) for i in range(2)]
                zo = [sb(f"gz{i}", [128, SEQ], BF16, pg) for i in range(2)]
                xsrc = D['X1d'] if o == 0 else D['X2d']
                if o == 1:
                    z2 = sb("gz2", [128, SEQ], F32, pg)
                    sq = [sb(f"gsq{i}", [128, 512], F32, pg) for i in range(2)]
                    rr = [sb(f"grr{i}", [128, 512], F32, pg) for i in range(2)]
                    psN = [pst(f"psN{i}", [128, 512], F32, pg) for i in range(2)]
                for g in range(4):
                    a = g % 2
                    S.dma(ysb[a][:, :], D['Ycv'][g * 128:(g + 1) * 128, :])
                    S.dma(xsb[a][:, :], xsrc[g * 128:(g + 1) * 128, :])
                    if o == 0:
                        S.tt(zo[a][:, :], xsb[a][:, :], ysb[a][:, :], ALU.mult)
                        S.dma(D['Z1d'][g * 128:(g + 1) * 128, :], zo[a][:, :])
                    else:
                        S.tt(z2[:, :], xsb[a][:, :], ysb[a][:, :], ALU.mult)
                        for blk in range(8):
                            bs = slice(blk * 512, (blk + 1) * 512)
                            q_ = blk % 2
                            S.act(sq[q_][:, :], z2[:, bs], AF.Square)
                            S.mm(psN[q_][:, :], P.ones_f[:, :], sq[q_][:, :])
                            S.ts(rr[q_][:, :], psN[q_][:, :], 1.0 / 128.0, ALU.mult, s2=EPS, op1=ALU.add)
                            S.act(rr[q_][:, :], rr[q_][:, :], AF.Sqrt)
                            S.recip(rr[q_][:, :], rr[q_][:, :])
                            S.stt(zo[a][:, bs], z2[:, bs], hnorm[:, g:g + 1], rr[q_][:, :], ALU.mult, ALU.mult)
                        S.dma(D['Yt'][512 + g * 128:512 + (g + 1) * 128, :], zo[a][:, :])
    if C.debug:
        d_ = C.dbg_out('Yt_h', [512, SEQ], BF16)
        with S.scope() as pd:
            tmp = sb("dbgtmp_yth", [128, 4, SEQ], BF16, pd)
            S.dma(tmp[:, :, :], D['Yt'][512:1024, :].rearrange("(a p) n -> p a n", p=128))
            S.dma(d_.rearrange("(a p) n -> p a n", p=128), tmp[:, :, :])


def fft_batch(C, T, W, zsrc, kdsrc, ydst):
    S = C.S

    def bank():
        W.nr += 1
        return W.ring[W.nr % 8]

    def ev(dst, src):
        W.ne += 1
        S.copy(dst, src, eng=('act' if W.ne % 2 else 'dve'))

    def forward(kdim, mode):
        for cq in range(16):
            pA = bank()
            for i in range(4):
                ch = cq * 4 + i
                S.mm(pA[:, i * 128:(i + 1) * 128], W.X[0:kdim, ch, :], T.F1[0:kdim, :])
            ev(W.AT[:, cq * 4:(cq + 1) * 4, :, :].rearrange("p c r k -> p (c r k)"), pA[:, :])
            src = pA[:, :].rearrange("p (c r k) -> p c r k", c=4, r=2)
            S.act(W.ATn[:, cq * 4:(cq + 1) * 4, 0, :], src[:, :, 1, :], AF.Copy, scale=-1.0)
            S.copy(W.ATn[:, cq * 4:(cq + 1) * 4, 1, :], src[:, :, 0, :])
        for kb in range(16):
            pU = bank()
            for i in range(4):
                k1 = kb * 4 + i
                o_ = pU[:, i * 128:(i + 1) * 128]
                S.mm(o_, T.GrT[:, k1, :], W.AT[:, :, :, k1].rearrange("p c r -> p r c"), start=True, stop=False)
                S.mm(o_, T.GiT[:, k1, :], W.ATn[:, :, :, k1].rearrange("p c r -> p r c"), start=False, stop=True)
            if mode == 'kernel':
                ev(W.Kf[:, kb * 4:(kb + 1) * 4, :, :].rearrange("p k r c -> p (k r c)"), pU[:, :])
            else:
                pv = pU[:, :].rearrange("p (k r c) -> p k r c", k=4, r=2)
                Ur = pv[:, :, 0, :]
                Ui = pv[:, :, 1, :]
                Kr = W.Kf[:, kb * 4:(kb + 1) * 4, 0, :]
                Ki = W.Kf[:, kb * 4:(kb + 1) * 4, 1, :]
                t = W.tm[kb % 2]
                S.tt(t[0][:, :, :], Ur, Kr, ALU.mult)
                S.tt(t[1][:, :, :], Ui, Ki, ALU.mult)
                S.tt(t[2][:, :, :], Ur, Ki, ALU.mult)
                S.tt(t[3][:, :, :], Ui, Kr, ALU.mult)
                S.tt(W.V[:, 0, kb * 4:(kb + 1) * 4, :], t[0][:, :, :], t[1][:, :, :], ALU.subtract, eng='pool')
                S.tt(W.V[:, 1, kb * 4:(kb + 1) * 4, :], t[2][:, :, :], t[3][:, :, :], ALU.add, eng='pool')

    S.dma(W.X[:, :, :], kdsrc.rearrange("c (a n) -> a c n", n=128))
    forward(64, 'kernel')
    S.dma(W.X[0:32, :, :], zsrc.rearrange("c (a n) -> a c n", n=128))
    forward(32, 'data')
    for cp in range(32):
        pW = bank()
        for i in range(2):
            ch = cp * 2 + i
            o_ = pW[0:64, i * 256:(i + 1) * 256]
            S.mm(o_, W.V[:, 0, :, ch], T.Rc1[:, :], start=True, stop=False)
            S.mm(o_, W.V[:, 1, :, ch], T.Rc2[:, :], start=False, stop=True)
        ev(W.Wt[:, cp * 2:(cp + 1) * 2, :, :].rearrange("p c r n -> p (c r n)"), pW[0:64, :])
    for nb in range(16):
        pY = bank()
        for i in range(8):
            n2 = nb * 8 + i
            o_ = pY[0:32, i * 64:(i + 1) * 64]
            S.mm(o_, T.LrT[:, n2, :], W.Wt[:, :, 0, n2], start=True, stop=False)
            S.mm(o_, T.LiNT[:, n2, :], W.Wt[:, :, 1, n2], start=False, stop=True)
        ev(W.Ysb[:, :, nb * 8:(nb + 1) * 8], pY[0:32, :].rearrange("p (n c) -> p c n", n=8))
    S.dma(ydst.rearrange("c (a n) -> a c n", n=128), W.Ysb[:, :, :])


def phase_outproj(C):
    S, D, P, sb, pst, nc = C.S, C.D, C.P, C.sb, C.pst, C.nc
    P.IDX = sb("IDX", [128, 16, 4], I32)
    P.GW = sb("GW", [128, 16, 4], F32)
    with S.scope() as ph:
        AFF = sb("AFF", [128, NT, 16], F32, ph)
        AFFT = sb("AFFT", [16, SEQ], F32, ph)
        with S.scope() as p1:
            ytT = sb("ytT", [128, 8, SEQ], BF16, p1)
            wout = sb("wout", [128, 8, DM], BF16, p1)
            wr = sb("wr", [128, 8, 16], F32, p1)
            xb = [sb(f"oxb{i}", [128, DM], F32, p1) for i in range(2)]
            x1 = [sb(f"ox1{i}", [128, DM], F32, p1) for i in range(3)]
            tmp = [sb(f"otmp{i}", [128, DM], F32, p1) for i in range(2)]
            hf = [sb(f"ohf{i}", [128, DM], F32, p1) for i in range(3)]
            hfb = [sb(f"ohfb{i}", [128, DM], BF16, p1) for i in range(3)]
            hfT = [sb(f"ohfT{i}", [128, 8, 128], F32, p1) for i in range(2)]
            junk = sb("ojunk", [128, DM], BF16, p1)
            sm = sb("osm", [128, NT, 8], F32, p1)
            ex = [sb(f"oex{i}", [128, 16], F32, p1) for i in range(2)]
            psM = [[pst(f"psMo{i}{h}", [128, 512], F32, p1) for h in range(2)] for i in range(2)]
            psT = [pst(f"psTo{i}", [128, 512], F32, p1) for i in range(2)]
            psR = pst("psR", [128, 512], F32, p1)
            psAT = pst("psAT", [128, 512], F32, p1)
            psAT2 = psT[1]
            for k in range(8):
                S.dma(ytT[:, k, :], D['Yt'][k * 128:(k + 1) * 128, :])
            wsrc = D['w_out'].rearrange("(k p) c -> p k c", p=128)
            S.dma(wout[:, :, 0:512], wsrc[:, :, 0:512], q='pool')
            S.dma(wout[:, :, 512:1024], wsrc[:, :, 512:1024], q='pool')
            S.dma(wr[:, :, :], D['w_router'].rearrange("(k p) e -> p k e", p=128))
            def stage_a1(j):
                a = j % 2
                b = j % 3
                xt = xb[a]
                S.dma(xt[:, :], D['x'][j * 128:(j + 1) * 128, :])
                for h in range(2):
                    for k in range(8):
                        S.mm(psM[a][h][:, :], ytT[:, k, j * 128:(j + 1) * 128], wout[:, k, h * 512:(h + 1) * 512],
                             start=(k == 0), stop=(k == 7))
                for h in range(2):
                    S.tt(tmp[a][:, h * 512:(h + 1) * 512], psM[a][h][:, :], P.gt1rep[:, h * 512:(h + 1) * 512], ALU.mult)
                S.tt(x1[b][:, :], tmp[a][:, :], xt[:, :], ALU.add)
                S.dma(D['acc'][j * 128:(j + 1) * 128, :], x1[b][:, :])

            def stage_a2(j):
                b = j % 3
                ss = sm[:, j, 0:1]
                rs = sm[:, j, 1:2]
                S.act(junk[:, :], x1[b][:, :], AF.Square, accum_out=ss)
                S.act(rs, ss, AF.Ln, scale=1.0 / DM, bias=EPS)
                S.act(rs, rs, AF.Exp, scale=-0.5)
                S.stt(hf[b][:, :], x1[b][:, :], rs, P.A2rep[:, :], ALU.mult, ALU.mult)
                S.tt(hf[b][:, :], hf[b][:, :], P.B2rep[:, :], ALU.add)
                S.act(hfb[b][:, :], hf[b][:, :], AF.Copy)
                S.dma(D['HFd'][j * 128:(j + 1) * 128, :], hfb[b][:, :])

            def stage_b1(j):
                a = j % 2
                b = j % 3
                for k in range(8):
                    S.transpose(psT[k // 4][:, (k % 4) * 128:(k % 4 + 1) * 128], hf[b][:, k * 128:(k + 1) * 128], P.ident_f[:, :])
                S.copy(hfT[a][:, 0:4, :].rearrange("p k t -> p (k t)"), psT[0][:, :], eng='act')
                S.copy(hfT[a][:, 4:8, :].rearrange("p k t -> p (k t)"), psT[1][:, :], eng='dve')

            def stage_b2(j):
                a = j % 2
                for k in range(8):
                    S.mm(psR[:, 0:16], hfT[a][:, k, :], wr[:, k, :], start=(k == 0), stop=(k == 7))
                mx = sm[:, j, 2:3]
                se = sm[:, j, 3:4]
                S.reduce(mx, psR[:, 0:16], ALU.max)
                S.ts(mx, mx, -1.0, ALU.mult)
                S.act(ex[a][:, :], psR[:, 0:16], AF.Exp, bias=mx, accum_out=se)
                S.recip(se, se)
                S.ts(AFF[:, j, :], ex[a][:, :], se, ALU.mult)
            for step in range(NT + 2):
                if 0 <= step - 2 < NT:
                    stage_b1(step - 2)
                if step < NT:
                    stage_a1(step)
                if 0 <= step - 1 < NT:
                    stage_a2(step - 1)
                if 0 <= step - 2 < NT:
                    stage_b2(step - 2)
            for j in range(NT):
                pat = psAT if j % 2 == 0 else psAT2
                S.transpose(pat[0:16, 0:128], AFF[:, j, :], P.ident_f[:, :])
                S.copy(AFFT[:, j * 128:(j + 1) * 128], pat[0:16, 0:128], eng=('act' if j % 2 else 'dve'))
            if C.debug:
                pass
        if C.debug:
            d_ = C.dbg_out('AFF', [128, NT, 16])
            S.dma(d_[:, :, :], AFF[:, :, :])
        with S.scope() as p2:
            junkA = sb("junkA", [16, SEQ], F32, p2)
            bs = sb("bis", [16, 8], F32, p2)
            THR = sb("THR", [128, 16], F32, p2)
            throw = sb("throw", [1, 16], F32, p2)
            SEL = sb("SEL", [128, NT, 16], F32, p2)
            POS = sb("POS", [128, NT, 16], F32, p2)
            selcum = sb("selcum", [128, 16], F32, p2)
            striU = sb("striU", [128, 128], F32, p2)
            COORD = sb("COORD", [128, NT, 16, 4], BF16, p2)
            jf = sb("jf", [128, NT], F32, p2)
            pf = sb("pf", [128, 1], F32, p2)
            iot = sb("iot", [128, 512], F32, p2)
            OH = [sb(f"OH{i}", [128, 512], BF16, p2) for i in range(3)]
            r4 = [sb(f"r4{i}", [128, 8], F32, p2) for i in range(2)]
            psI = [pst(f"psI{i}", [128, 512], F32, p2) for i in range(4)]
            psP = [pst(f"psP{i}", [128, 512], F32, p2) for i in range(2)]
            psX = pst("psX", [128, 512], F32, p2)
            psB2 = pst("psB2", [128, 512], F32, p2)
            lo, hi, mid, cnt, ge, d1, d2 = [bs[:, i:i + 1] for i in range(7)]
            S.dma(striU[:, :], D['striU'][:, :])
            S.memset(lo, 0.0)
            S.memset(hi, 1.0)
            for it in range(30):
                S.tt(mid, lo, hi, ALU.add)
                S.ts(mid, mid, 0.5, ALU.mult)
                S.ts(junkA[:, :], AFFT[:, :], mid, ALU.is_ge, s2=0.0, op1=ALU.add, accum_out=cnt)
                S.ts(ge, cnt, 511.5, ALU.is_gt)
                S.tt(d1, mid, lo, ALU.subtract)
                S.tt(d2, hi, mid, ALU.subtract)
                S.stt(lo, d1, ge, lo, ALU.mult, ALU.add)
                S.stt(hi, d2, ge, mid, ALU.mult, ALU.add)
            S.transpose(psX[0:1, 0:16], lo, P.ident_f[0:16, 0:16])
            S.copy(throw[:, :], psX[0:1, 0:16])
            S.mm(psB2[:, 0:16], P.ones_f[0:1, 0:128], throw[0:1, :])
            S.copy(THR[:, :], psB2[:, 0:16])
            S.tt(SEL[:, :, :], AFF[:, :, :], THR[:, :].unsqueeze(1).to_broadcast([128, NT, 16]), ALU.is_ge)
            S.memset(selcum[:, :], 0.0)
            for j in range(NT):
                pp = psP[j % 2]
                S.mm(pp[:, 0:16], striU[:, :], SEL[:, j, :], start=True, stop=False)
                S.mm(pp[:, 0:16], P.ones_f[:, :], selcum[:, :], start=False, stop=True)
                S.copy(POS[:, j, :], pp[:, 0:16], eng='act')
                S.tt(selcum[:, :], selcum[:, :], SEL[:, j, :], ALU.add)
            S.op('pool', lambda e: e.iota(jf[:, :], [[1, NT]], base=0, channel_multiplier=0, allow_small_or_imprecise_dtypes=True), [], [jf[:, :]])
            S.op('pool', lambda e: e.iota(pf[:, :], [[1, 1]], base=0, channel_multiplier=1, allow_small_or_imprecise_dtypes=True), [], [pf[:, :]])
            S.op('pool', lambda e: e.iota(iot[:, :], [[1, 512]], base=0, channel_multiplier=0, allow_small_or_imprecise_dtypes=True), [], [iot[:, :]])
            S.copy(COORD[:, :, :, 0], jf[:, :].unsqueeze(2).to_broadcast([128, NT, 16]))
            S.copy(COORD[:, :, :, 1], pf[:, 0:1].unsqueeze(2).to_broadcast([128, NT, 16]))
            S.copy(COORD[:, :, :, 2], AFF[:, :, :])
            S.tt(COORD[:, :, :, 3], AFF[:, :, :], COORD[:, :, :, 2], ALU.subtract)
            n = 0
            for e_ in range(16):
                for j in range(NT):
                    oh = OH[n % 3]
                    n += 1
                    S.ts(oh[:, :], iot[:, :], POS[:, j, e_:e_ + 1], ALU.is_equal, s2=SEL[:, j, e_:e_ + 1], op1=ALU.mult)
                    for sc in range(4):
                        S.mm(psI[sc][:, 0:4], oh[:, sc * 128:(sc + 1) * 128], COORD[:, j, e_, :], start=(j == 0), stop=(j == NT - 1))
                for sc in range(4):
                    r = r4[(e_ * 4 + sc) % 2]
                    S.copy(r[:, 0:4], psI[sc][:, 0:4], eng='act')
                    S.stt(r[:, 4:5], r[:, 0:1], 128.0, r[:, 1:2], ALU.mult, ALU.add)
                    S.copy(P.IDX[:, e_, sc:sc + 1], r[:, 4:5])
                    S.tt(P.GW[:, e_, sc:sc + 1], r[:, 2:3], r[:, 3:4], ALU.add)
        if C.debug:
            d_ = C.dbg_out('IDX', [128, 16, 4], I32)
            S.dma(d_[:, :, :], P.IDX[:, :, :])
            d_ = C.dbg_out('GW', [128, 16, 4])
            S.dma(d_[:, :, :], P.GW[:, :, :])


def phase_route(C):
    pass


def phase_experts(C):
    S, D, P, sb, pst, nc = C.S, C.D, C.P, C.sb, C.pst, C.nc
    with S.scope() as ph:
        stg = [sb(f"stg{i}", [128, 8, 512], F32, ph) for i in range(4)]
        wb = [sb(f"wb{i}", [128, 8, 512], BF16, ph) for i in range(8)]
        xs = sb("xs", [128, 4, DM], BF16, ph)
        xsT = sb("xsT", [128, 8, 512], BF16, ph)
        hidT = sb("hidT", [128, 16, 512], BF16, ph)
        sg = [sb(f"sg{i}", [128, 512], F32, ph) for i in range(2)]
        ysb = [sb(f"ysb{i}", [128, DM], F32, ph) for i in range(4)]
        psT = pst("psTe", [128, 1024], BF16, ph)
        psG = [pst(f"psGe{i}", [128, 512], F32, ph) for i in range(2)]
        psU = pst("psUe", [128, 512], F32, ph)
        psY = [pst(f"psYe{i}", [128, 512], F32, ph) for i in range(4)]
        pieces = []
        for e_ in range(16):
            for fb in range(4):
                for nm in ('w_gate', 'w_up'):
                    pieces.append(D[nm][e_ * 1024:(e_ + 1) * 1024, fb * 512:(fb + 1) * 512].rearrange("(k p) c -> p k c", p=128))
            for dh in range(2):
                for fh in range(2):
                    r0 = e_ * 2048 + fh * 1024
                    pieces.append(D['w_down'][r0:r0 + 1024, dh * 512:(dh + 1) * 512].rearrange("(k p) c -> p k c", p=128))
        issued = [0]
        cast_eng = ['act', 'dve']
        LOOK = 5

        def issue_upto(n):
            while issued[0] < min(n, len(pieces)):
                i = issued[0]
                s_ = stg[i % 4]
                S.dma(s_[:, :, :], pieces[i])
                S.copy(wb[i % 8][:, :, :], s_[:, :, :], eng=cast_eng[i % 2])
                issued[0] += 1
        pc = [0]

        def next_piece():
            i = pc[0]
            issue_upto(i + 1 + LOOK)
            pc[0] += 1
            return wb[i % 8]
        ng = 0
        for e_ in range(16):
            for st in range(4):
                idx_ap = P.IDX[:, e_, st:st + 1]
                S.op('pool', lambda e, st=st, idx_ap=idx_ap: e.indirect_dma_start(
                    out=xs[:, st, :], out_offset=None, in_=D['HFd'][:, :],
                    in_offset=bass.IndirectOffsetOnAxis(ap=idx_ap, axis=0)),
                    [D['HFd'][:, :], idx_ap], [xs[:, st, :]], dma=True)
            for st in range(4):
                for k in range(8):
                    S.transpose(psT[:, k * 128:(k + 1) * 128], xs[:, st, k * 128:(k + 1) * 128], P.ident_bf[:, :])
                S.copy(xsT[:, :, st * 128:(st + 1) * 128], psT[:, :].rearrange("p (k t) -> p k t", k=8), eng=('act' if st % 2 else 'dve'))
            for fb in range(4):
                wg = next_piece()
                wu = next_piece()
                for fc in range(4):
                    pg = psG[ng % 2]
                    sgt = sg[ng % 2]
                    ng += 1
                    for k in range(8):
                        S.mm(pg[:, :], wg[:, k, fc * 128:(fc + 1) * 128], xsT[:, k, :], start=(k == 0), stop=(k == 7))
                    for k in range(8):
                        S.mm(psU[:, :], wu[:, k, fc * 128:(fc + 1) * 128], xsT[:, k, :], start=(k == 0), stop=(k == 7))
                    S.act(sgt[:, :], pg[:, :], AF.Silu)
                    S.tt(hidT[:, fb * 4 + fc, :], sgt[:, :], psU[:, :], ALU.mult)
            for dh in range(2):
                for fh in range(2):
                    wd = next_piece()
                    for st in range(4):
                        for f8 in range(8):
                            S.mm(psY[st][:, :], hidT[:, fh * 8 + f8, st * 128:(st + 1) * 128], wd[:, f8, :],
                                 start=(fh == 0 and f8 == 0), stop=(fh == 1 and f8 == 7))
                for st in range(4):
                    S.stt(ysb[st][:, dh * 512:(dh + 1) * 512], psY[st][:, :], P.GW[:, e_, st:st + 1],
                          P.gt2rep[:, dh * 512:(dh + 1) * 512], ALU.mult, ALU.mult)
            for st in range(4):
                idx_ap = P.IDX[:, e_, st:st + 1]
                S.op('pool', lambda e, st=st, idx_ap=idx_ap: e.indirect_dma_start(
                    out=D['acc'][:, :], out_offset=bass.IndirectOffsetOnAxis(ap=idx_ap, axis=0),
                    in_=ysb[st][:, :], in_offset=None, compute_op=ALU.add),
                    [ysb[st][:, :], idx_ap, D['acc'][:, :]], [D['acc'][:, :]], dma=True)


def phase_final(C):
    S, D, P, sb, pst = C.S, C.D, C.P, C.sb, C.pst
    with S.scope() as ph:
        gfin = sb("gfin", [128, DM], F32, ph)
        xb = [sb(f"fxb{i}", [128, DM], F32, ph) for i in range(2)]
        ob = [sb(f"fob{i}", [128, DM], F32, ph) for i in range(2)]
        junk = sb("fjunk", [128, DM], BF16, ph)
        sm = sb("fsm", [128, NT, 2], F32, ph)
        S.dma(gfin[:, :], D['gfin_row'][0:1, :].partition_broadcast(128))
        for j in range(NT):
            a = j % 2
            S.dma(xb[a][:, :], D['acc'][j * 128:(j + 1) * 128, :])
            ss = sm[:, j, 0:1]
            rs = sm[:, j, 1:2]
            S.act(junk[:, :], xb[a][:, :], AF.Square, accum_out=ss)
            S.act(rs, ss, AF.Ln, scale=1.0 / DM, bias=EPS)
            S.act(rs, rs, AF.Exp, scale=-0.5)
            S.stt(ob[a][:, :], xb[a][:, :], rs, gfin[:, :], ALU.mult, ALU.mult)
            S.dma(D['out'][j * 128:(j + 1) * 128, :], ob[a][:, :])


_PROG = {}


def kernel(**inputs):
    if 'nc' not in _PROG:
        _PROG['nc'] = build()[0]
    nc = _PROG['nc']
    B = inputs['x'].shape[0]
    in_maps = [layout_inputs(inputs, b) for b in range(B)]
    res = run_bass_kernel_spmd(nc, in_maps, core_ids=list(range(B)))
    out = np.stack([np.asarray(r["out"], dtype=np.float32) for r in res.results], axis=0)
    return out
```

```python
import math
import numpy as np
import ml_dtypes
import concourse.bass as bass
import concourse.mybir as mybir
from concourse.bass_utils import run_bass_kernel_spmd
from contextlib import ExitStack

F32 = mybir.dt.float32
BF16 = mybir.dt.bfloat16
I32 = mybir.dt.int32
AF = mybir.ActivationFunctionType
ALU = mybir.AluOpType
AX = mybir.AxisListType

SEQ = 4096
DM = 1024
NT = 32
EPS = 1e-6
EMIT_UNTIL = [None]
COMPUTE = ('pe', 'act', 'dve', 'pool')
SELF_SYNC = {'act': True, 'dve': True, 'pool': True, 'pe': False}
NDMA_SEMS = 6


def ap_box(ap):
    t = ap.tensor
    name = t.name
    dims = list(ap.ap)
    off = int(ap.offset)
    sp = str(ap.space() if callable(ap.space) else ap.space)
    is_dram = 'DRAM' in sp.upper() or 'HBM' in sp.upper() or type(t).__name__.startswith('DRAM') or type(t).__name__.startswith('Dram')
    if is_dram:
        lo = off
        hi = off
        for (st, cnt) in dims:
            st = int(st); cnt = int(cnt)
            if st >= 0:
                hi += st * (cnt - 1)
            else:
                lo += st * (cnt - 1)
        return (name, 0, 1, lo, hi + 1)
    if 'PSUM' in sp.upper() or type(t).__name__.startswith('PSum'):
        return (name, 0, 128, 0, 1 << 30)
    p0 = int(ap.start_partition())
    pc = int(dims[0][1])
    lo = off
    hi = off
    for (st, cnt) in dims[1:]:
        st = int(st); cnt = int(cnt)
        if st >= 0:
            hi += st * (cnt - 1)
        else:
            lo += st * (cnt - 1)
    return (name, p0, p0 + pc, lo, hi + 1)


class Op:
    __slots__ = ('stream', 'fn', 'deps', 'is_dma', 'signal', 'semval', 'dma_slot', 'idx', 'extra_waits')


class Sched:
    def __init__(self, nc, es):
        self.nc = nc
        self.es = es
        self.ops = []
        self.track = {}
        self.eng = {'pe': nc.tensor, 'act': nc.scalar, 'dve': nc.vector, 'pool': nc.gpsimd, 'sp': nc.sync}
        self.sem = {s: es.enter_context(nc.semaphore('sem_' + s)) for s in COMPUTE}
        self.dma_sems = {}
        for s in ('sp', 'pool', 'act'):
            self.dma_sems[s] = [es.enter_context(nc.semaphore(f'dsem_{s}{i}')) for i in range(NDMA_SEMS)]
        self.dma_count = {'sp': 0, 'pool': 0, 'act': 0}
        self.last_dma_ops = {'sp': [], 'pool': [], 'act': []}

    def _deps(self, boxes_r, boxes_w, idx, stream, is_dma):
        deps = set()
        for (boxes, is_w) in ((boxes_r, False), (boxes_w, True)):
            for b in boxes:
                lst = self.track.setdefault(b[0], [])
                keep = []
                for ent in lst:
                    eb, eidx, ew = ent
                    ov = not (eb[2] <= b[1] or b[2] <= eb[1] or eb[4] <= b[3] or b[4] <= eb[3])
                    if ov and (is_w or ew):
                        deps.add(eidx)
                    covered = (b[1] <= eb[1] and eb[2] <= b[2] and b[3] <= eb[3] and eb[4] <= b[4])
                    if is_w and covered:
                        continue
                    if (not is_w) and (not ew) and covered and (not is_dma):
                        eo = self.ops[eidx]
                        if eo.stream == stream and not eo.is_dma:
                            continue
                    keep.append(ent)
                keep.append([b, idx, is_w])
                self.track[b[0]] = keep
        deps.discard(idx)
        return deps

    def op(self, stream, fn, reads=(), writes=(), dma=False):
        o = Op()
        o.idx = len(self.ops)
        o.stream = stream
        o.fn = fn
        o.is_dma = dma
        o.signal = False
        o.semval = None
        o.dma_slot = None
        o.extra_waits = []
        br = [ap_box(a) for a in reads if a is not None and not isinstance(a, (int, float))]
        bw = [ap_box(a) for a in writes]
        self.ops.append(o)
        o.deps = self._deps(br, bw, o.idx, stream, dma)
        return o

    def emit(self):
        ops = self.ops
        for o in ops:
            for d in o.deps:
                po = ops[d]
                if po.is_dma:
                    continue
                if po.stream != o.stream or o.is_dma or SELF_SYNC.get(po.stream, True):
                    po.signal = True
        cnt = {s: 0 for s in COMPUTE}
        dcnt = {'sp': 0, 'pool': 0, 'act': 0}
        for o in ops:
            if o.is_dma:
                d = dcnt[o.stream]
                o.dma_slot = (d % NDMA_SEMS, 16 * (d // NDMA_SEMS + 1))
                dcnt[o.stream] = d + 1
            elif o.signal:
                cnt[o.stream] += 1
                o.semval = cnt[o.stream]
        waited = {s: {} for s in self.eng}
        nw = 0
        for o in ops:
            e = self.eng[o.stream]
            w = waited[o.stream]
            toks = []
            if o.is_dma:
                j, v = o.dma_slot
                if v > 16:
                    toks.append((('d', o.stream, j), self.dma_sems[o.stream][j], v - 16))
            for d in o.deps:
                po = ops[d]
                if po.is_dma:
                    j, v = po.dma_slot
                    toks.append((('d', po.stream, j), self.dma_sems[po.stream][j], v))
                else:
                    if po.stream == o.stream and not o.is_dma and not SELF_SYNC.get(po.stream, True):
                        continue
                    toks.append((('c', po.stream), self.sem[po.stream], po.semval))
            best = {}
            for key, sem, v in toks:
                if v is None:
                    raise RuntimeError('dep on non-signaling op')
                if w.get(key, 0) >= v:
                    continue
                if key not in best or best[key][1] < v:
                    best[key] = (sem, v)
            for key, (sem, v) in best.items():
                e.wait_ge(sem, v)
                w[key] = v
                nw += 1
            ins = o.fn(e)
            if ins is None:
                continue
            if o.is_dma:
                j, v = o.dma_slot
                ins.then_inc(self.dma_sems[o.stream][j], 16)
            elif o.signal:
                ins.then_inc(self.sem[o.stream], 1)
        self.n_waits = nw
        return cnt, dcnt

    def dma(self, out, in_, q='sp', **kw):
        return self.op(q, lambda e: e.dma_start(out=out, in_=in_, **kw), [in_], [out], dma=True)

    def mm(self, out, lhsT, rhs, start=True, stop=True, **kw):
        return self.op('pe', lambda e: e.matmul(out, lhsT, rhs, start=start, stop=stop, **kw),
                       [lhsT, rhs] + ([] if start else [out]), [out])

    def transpose(self, out, in_, ident):
        return self.op('pe', lambda e: e.transpose(out, in_, ident), [in_, ident], [out])

    def act(self, out, in_, func, scale=1.0, bias=0.0, accum_out=None, eng='act'):
        rd = [in_]
        if not isinstance(scale, (int, float)):
            rd.append(scale)
            if func == AF.Copy:
                func = AF.Identity
        if not isinstance(bias, (int, float)):
            rd.append(bias)
            if func == AF.Copy:
                func = AF.Identity
        wr = [out] + ([accum_out] if accum_out is not None else [])
        kw = {}
        if accum_out is not None:
            kw['accum_out'] = accum_out
        return self.op('act', lambda e: e.activation(out=out, in_=in_, func=func, scale=scale, bias=bias, **kw), rd, wr)

    def tt(self, out, in0, in1, op, eng='dve'):
        return self.op(eng, lambda e: e.tensor_tensor(out=out, in0=in0, in1=in1, op=op), [in0, in1], [out])

    def ts(self, out, in0, s1, op0, s2=None, op1=None, eng='dve', accum_out=None):
        rd = [in0]
        if not isinstance(s1, (int, float)):
            rd.append(s1)
        if s2 is not None and not isinstance(s2, (int, float)):
            rd.append(s2)
        kw = {}
        if op1 is not None:
            kw['op1'] = op1
        if accum_out is not None:
            kw['accum_out'] = accum_out
        wr = [out] + ([accum_out] if accum_out is not None else [])
        return self.op(eng, lambda e: e.tensor_scalar(out=out, in0=in0, scalar1=s1, scalar2=s2, op0=op0, **kw), rd, wr)

    def stt(self, out, in0, scalar, in1, op0, op1, eng='dve'):
        rd = [in0, in1]
        if not isinstance(scalar, (int, float)):
            rd.append(scalar)
        return self.op(eng, lambda e: e.scalar_tensor_tensor(out=out, in0=in0, scalar=scalar, in1=in1, op0=op0, op1=op1), rd, [out])

    def copy(self, out, in_, eng='dve'):
        if eng == 'act':
            return self.act(out, in_, AF.Copy)
        return self.op(eng, lambda e: e.tensor_copy(out=out, in_=in_), [in_], [out])

    def memset(self, out, val, eng='dve'):
        return self.op(eng, lambda e: e.memset(out, val), [], [out])

    def reduce(self, out, in_, op, axis=None, eng='dve'):
        axis = axis or AX.X
        return self.op(eng, lambda e: e.tensor_reduce(out=out, in_=in_, op=op, axis=axis), [in_], [out])

    def recip(self, out, in_, eng='dve'):
        return self.op(eng, lambda e: e.reciprocal(out=out, in_=in_), [in_], [out])

    def barrier(self):
        alld = set()
        for lst in self.track.values():
            for ent in lst:
                alld.add(ent[1])
        self.track = {}
        for s_ in ('pe', 'act', 'dve', 'pool', 'sp'):
            o = self.op(s_, lambda e: None, [], [])
            o.deps = set(alld)

    def scope(self):
        return _Scope(self)

    def finish(self, out_aps):
        boxes = [ap_box(a) for a in out_aps]
        o = self.op('sp', lambda e: None, list(out_aps), [])
        return o


class _Scope:
    def __init__(self, S):
        self.S = S
        self.st = ExitStack()

    def __enter__(self):
        self.st.__enter__()
        return self.st

    def __exit__(self, *a):
        self.S.barrier()
        return self.st.__exit__(*a)

_CONSTS = {}


def _bf(a):
    return np.ascontiguousarray(a.astype(np.float32)).astype(ml_dtypes.bfloat16)


def host_consts():
    if _CONSTS:
        return _CONSTS
    c = {}
    c['ident_bf'] = _bf(np.eye(128))
    c['ident_f'] = np.eye(128, dtype=np.float32)
    s = np.arange(128)[:, None]
    t = np.arange(128)[None, :]
    c['triU'] = (s <= t).astype(np.float32)
    c['triL'] = (s >= t).astype(np.float32)
    c['striU'] = (s < t).astype(np.float32)
    N = 8192
    n1 = np.arange(64); k1 = np.arange(64); n2 = np.arange(128); k2 = np.arange(128)
    ang = 2 * np.pi * np.outer(n1, k1) / 64
    c['F1'] = _bf(np.concatenate([np.cos(ang), -np.sin(ang)], 1))
    th = 2 * np.pi * ((n2[:, None, None] * (k1[None, :, None] + 64 * k2[None, None, :])) % N) / N
    c['GrT'] = _bf(np.cos(th).reshape(128, 64 * 128))
    c['GiT'] = _bf((-np.sin(th)).reshape(128, 64 * 128))
    c['GiNT'] = _bf((np.sin(th)).reshape(128, 64 * 128))
    ph = 2 * np.pi * np.outer(k2, n2) / 128
    Rr = np.cos(ph); Ri = np.sin(ph)
    c['Rc1'] = _bf(np.concatenate([Rr, Ri], 1))
    c['Rc2'] = _bf(np.concatenate([-Ri, Rr], 1))
    n1h = np.arange(32)
    thL = 2 * np.pi * (k1[:, None, None] * n1h[None, None, :] / 64 + k1[:, None, None] * n2[None, :, None] / N)
    c['LrT'] = _bf((np.cos(thL) / N).reshape(64, 128 * 32))
    c['LiNT'] = _bf((-np.sin(thL) / N).reshape(64, 128 * 32))
    L = SEQ
    f32 = np.float32
    tt = np.linspace(0.0, 1.0, L, dtype=f32)[:, None]
    w = (f32(2.0 * math.pi) * np.arange(L, dtype=f32)[:, None] / f32(L)).astype(f32)
    bands = np.linspace(1e-4, 15, 16, dtype=f32)[None, :]
    z = np.concatenate([tt, np.cos(bands * w), -np.sin(bands * w)], axis=-1).astype(f32)
    zr = np.concatenate([z[0:1], z[:0:-1]], 0)
    c['zT'] = np.ascontiguousarray(z.T)
    c['zrT'] = np.ascontiguousarray(zr.T)
    trow = tt[:, 0]
    trr = np.concatenate([trow[0:1], trow[:0:-1]])
    c['t_row'] = np.ascontiguousarray(trow[None, :]).astype(f32)
    c['tr_row'] = np.ascontiguousarray(trr[None, :]).astype(f32)
    _CONSTS.update(c)
    return _CONSTS


def colmaj(v, nk):
    return np.ascontiguousarray(np.asarray(v, dtype=np.float32).reshape(nk, 128).T)


def layout_inputs(inp, b):
    m = {}
    f = lambda a: np.ascontiguousarray(np.asarray(a, dtype=np.float32))
    m['x'] = f(inp['x'][b])
    m['ccol'] = colmaj(inp['c'][b], 8)
    m['w_ada'] = f(inp['w_ada'][0])
    m['b_ada'] = f(inp['b_ada'][0][None, :])
    m['gmix_col'] = colmaj(inp['g_mix'][0], 8)
    m['w_in'] = f(inp['w_in'][0])
    bin_ = np.asarray(inp['b_in'][0], dtype=np.float32)
    m['bin_row'] = f(bin_[None, 1024:2064])
    m['bqk_col'] = colmaj(bin_[0:1024], 8)
    m['bhy_col'] = colmaj(bin_[2064:3600], 12)
    cw = np.asarray(inp['conv_qk_w'][0], dtype=np.float32)
    m['cqk_w'] = np.ascontiguousarray(cw.reshape(3, 8, 128).transpose(2, 1, 0))
    m['cqk_b'] = colmaj(inp['conv_qk_b'][0], 8)
    cw = np.asarray(inp['conv_hy_w'][0], dtype=np.float32)
    m['chy_w'] = np.ascontiguousarray(cw.reshape(3, 12, 128).transpose(2, 1, 0))
    m['chy_b'] = colmaj(inp['conv_hy_b'][0], 12)
    m['mnorm_g'] = f(inp['mlstm_norm_g'][0][None, :])
    m['hy_w1'] = f(inp['hy_w1'][0])
    m['hy_b1c'] = f(inp['hy_b1'][0][:, None])
    m['hy_w2'] = f(inp['hy_w2'][0])
    m['hy_b2c'] = f(inp['hy_b2'][0][:, None])
    m['hy_w3'] = f(inp['hy_w3'][0])
    m['hy_frc'] = f(inp['hy_freq'][0][:, None])
    m['hy_del_col'] = colmaj(inp['hy_deltas'][0], 16)
    m['hy_bias_col'] = colmaj(np.asarray(inp['hy_bias'][0]).reshape(-1), 8)
    m['hnorm_col'] = colmaj(inp['hyena_norm_g'][0], 4)
    m['w_out'] = f(inp['w_out'][0])
    m['gffn_row'] = f(inp['g_ffn'][0][None, :])
    m['w_router'] = f(inp['w_router'][0])
    m['w_gate'] = f(inp['w_gate'][0]).reshape(16 * 1024, 2048)
    m['w_up'] = f(inp['w_up'][0]).reshape(16 * 1024, 2048)
    m['w_down'] = f(inp['w_down'][0]).reshape(16 * 2048, 1024)
    m['gfin_row'] = f(np.asarray(inp['g_final'])[None, :])
    m.update(host_consts())
    return m

INPUT_SPECS = [
    ('x', [SEQ, DM], F32), ('ccol', [128, 8], F32), ('w_ada', [DM, 6144], F32), ('b_ada', [1, 6144], F32),
    ('gmix_col', [128, 8], F32), ('w_in', [DM, 3600], F32), ('bin_row', [1, 1040], F32),
    ('bqk_col', [128, 8], F32), ('bhy_col', [128, 12], F32), ('cqk_w', [128, 8, 3], F32), ('cqk_b', [128, 8], F32),
    ('chy_w', [128, 12, 3], F32), ('chy_b', [128, 12], F32), ('mnorm_g', [1, 512], F32),
    ('hy_w1', [33, 64], F32), ('hy_b1c', [64, 1], F32), ('hy_w2', [64, 64], F32), ('hy_b2c', [64, 1], F32),
    ('hy_w3', [64, 2048], F32), ('hy_frc', [64, 1], F32), ('hy_del_col', [128, 16], F32),
    ('hy_bias_col', [128, 8], F32), ('hnorm_col', [128, 4], F32), ('w_out', [DM, DM], F32),
    ('gffn_row', [1, DM], F32), ('w_router', [DM, 16], F32), ('w_gate', [16 * 1024, 2048], F32),
    ('w_up', [16 * 1024, 2048], F32), ('w_down', [16 * 2048, 1024], F32), ('gfin_row', [1, DM], F32),
    ('ident_bf', [128, 128], BF16), ('ident_f', [128, 128], F32), ('triU', [128, 128], F32),
    ('triL', [128, 128], F32), ('striU', [128, 128], F32), ('F1', [64, 128], BF16),
    ('GrT', [128, 8192], BF16), ('GiT', [128, 8192], BF16), ('GiNT', [128, 8192], BF16),
    ('Rc1', [128, 256], BF16), ('Rc2', [128, 256], BF16), ('LrT', [64, 4096], BF16), ('LiNT', [64, 4096], BF16),
    ('zT', [33, SEQ], F32), ('zrT', [33, SEQ], F32), ('t_row', [1, SEQ], F32), ('tr_row', [1, SEQ], F32),
]


class Ctx:
    pass


def build(stop_after=None, debug=False):
    nc = bass.Bass("TRN2", target_bir_lowering=False)
    es = ExitStack()
    S = Sched(nc, es)
    C = Ctx()
    C.nc, C.S, C.es = nc, S, es
    C.debug = debug
    D = {}
    for name, shape, dt in INPUT_SPECS:
        D[name] = nc.dram_tensor(name, shape, dt, kind="ExternalInput").ap()
    D['out'] = nc.dram_tensor("out", [SEQ, DM], F32, kind="ExternalOutput").ap()

    def scratch(name, shape, dt):
        D[name] = nc.dram_tensor(name, shape, dt, kind="Internal").ap()
    scratch('QTd', [512, SEQ], BF16)
    scratch('KTd', [512, SEQ], BF16)
    scratch('Vd', [SEQ, 512], BF16)
    scratch('Od', [SEQ, 512], BF16)
    scratch('X1d', [512, SEQ], F32)
    scratch('X2d', [512, SEQ], F32)
    scratch('Zd', [512, SEQ], BF16)
    scratch('Z1d', [512, SEQ], BF16)
    scratch('Kd0', [512, 2 * SEQ], BF16)
    scratch('Kd1', [512, 2 * SEQ], BF16)
    scratch('Ycv', [512, SEQ], BF16)
    scratch('Yt', [DM, SEQ], BF16)
    scratch('HFd', [SEQ, DM], BF16)
    scratch('acc', [SEQ, DM], F32)
    C.D = D
    C.dbg = {}

    def dbg_out(name, shape, dt=F32):
        t = nc.dram_tensor("dbg_" + name, shape, dt, kind="ExternalOutput").ap()
        C.dbg[name] = t
        return t
    C.dbg_out = dbg_out

    cnt = [0]

    def sb(name, shape, dt, st=None):
        cnt[0] += 1
        return (st or es).enter_context(nc.sbuf_tensor(f"s{cnt[0]}_{name}", shape, dt))

    def pst(name, shape, dt, st=None):
        cnt[0] += 1
        return (st or es).enter_context(nc.psum_tensor(f"p{cnt[0]}_{name}", shape, dt))
    C.sb, C.pst = sb, pst

    P = Ctx()
    C.P = P
    P.ident_bf = sb("ident_bf", [128, 128], BF16)
    P.ident_f = sb("ident_f", [128, 128], F32)
    P.ones_f = sb("ones_f", [128, 128], F32)
    P.modcol = sb("modcol", [128, 48], F32)
    P.A1col = sb("A1col", [128, 8], F32)
    P.gt1rep = sb("gt1rep", [128, DM], F32)
    P.gt2rep = sb("gt2rep", [128, DM], F32)
    P.A2rep = sb("A2rep", [128, DM], F32)
    P.B2rep = sb("B2rep", [128, DM], F32)
    S.dma(P.ident_bf[:, :], D['ident_bf'][:, :])
    S.dma(P.ident_f[:, :], D['ident_f'][:, :])
    S.memset(P.ones_f[:, :], 1.0)

    phases = [phase_mod, phase_norm_proj, phase_mlstm, phase_hyena, phase_outproj, phase_route, phase_experts, phase_final]
    for ph in phases:
        ph(C)
        if stop_after == ph.__name__:
            break
    outs = [D['out'][:, :]] + [t for t in C.dbg.values()]
    S.finish(outs)
    S.emit()
    return nc, C


def phase_mod(C):
    S, D, P, sb, pst = C.S, C.D, C.P, C.sb, C.pst
    with S.scope() as ph:
        wada = [sb(f"wada{i}", [128, 8, 512], F32, ph) for i in range(2)]
        modrow = sb("modrow", [1, 6144], F32, ph)
        badar = sb("badar", [1, 6144], F32, ph)
        ccol = sb("ccol_sb", [128, 8], F32, ph)
        gmixc = sb("gmixc", [128, 8], F32, ph)
        gffn_rep = sb("gffn_rep", [128, DM], F32, ph)
        sc2rep = sb("sc2rep", [128, DM], F32, ph)
        ps = pst("ps_mod", [128, 512], F32, ph)
        psc = pst("ps_modc", [128, 512], F32, ph)
        psr = [pst(f"ps_modr{i}", [128, 512], F32, ph) for i in range(2)]
        S.dma(badar[:, :], D['b_ada'][:, :])
        S.dma(ccol[:, :], D['ccol'][:, :])
        S.dma(gmixc[:, :], D['gmix_col'][:, :])
        S.dma(gffn_rep[:, :], D['gffn_row'][0:1, :].partition_broadcast(128))
        wsrc = D['w_ada'].rearrange("(k p) c -> p k c", p=128)
        for blk in range(12):
            buf = wada[blk % 2]
            S.dma(buf[:, :, :], wsrc[:, :, blk * 512:(blk + 1) * 512])
            for k in range(8):
                S.mm(ps[0:1, :], ccol[:, k:k + 1], buf[:, k, :], start=(k == 0), stop=(k == 7))
            S.tt(modrow[0:1, blk * 512:(blk + 1) * 512], ps[0:1, :], badar[0:1, blk * 512:(blk + 1) * 512], ALU.add)
        for oc in range(48):
            S.mm(psc[:, oc:oc + 1], modrow[0:1, oc * 128:(oc + 1) * 128], P.ones_f[0:1, 0:1])
        S.copy(P.modcol[:, :], psc[:, 0:48])
        S.stt(P.A1col[:, :], P.modcol[:, 8:16], 1.0, gmixc[:, :], ALU.add, ALU.mult)
        n = 0
        for (dst, j) in ((P.gt1rep, 2), (P.gt2rep, 5), (sc2rep, 4), (P.B2rep, 3)):
            for h in range(2):
                pr = psr[n % 2]
                n += 1
                S.mm(pr[:, :], P.ones_f[0:1, 0:128], modrow[0:1, j * 1024 + h * 512: j * 1024 + (h + 1) * 512])
                S.copy(dst[:, h * 512:(h + 1) * 512], pr[:, :], eng=('act' if n % 2 else 'dve'))
        S.stt(P.A2rep[:, :], sc2rep[:, :], 1.0, gffn_rep[:, :], ALU.add, ALU.mult)
        if C.debug:
            d = C.dbg_out('modcol', [128, 48])
            S.dma(d[:, :], P.modcol[:, :])
            d = C.dbg_out('gt1rep', [128, DM])
            S.dma(d[:, :], P.gt1rep[:, :])


def phase_norm_proj(C):
    S, D, P, sb, pst = C.S, C.D, C.P, C.sb, C.pst
    P.Gt = sb("Gt", [128, NT, 16], F32)
    with S.scope() as ph:
        hT = sb("hT", [128, 8, SEQ], BF16, ph)
        with S.scope() as p1:
            xb = [sb(f"xb{i}", [128, DM], F32, p1) for i in range(2)]
            xn = [sb(f"xn{i}", [128, DM], BF16, p1) for i in range(2)]
            junk = sb("junk", [128, DM], BF16, p1)
            ss = sb("ss", [128, NT], F32, p1)
            rs = sb("rs", [128, NT], F32, p1)
            psT = [pst(f"psT{i}", [128, DM], BF16, p1) for i in range(2)]
            for j in range(NT):
                xt = xb[j % 2]
                S.dma(xt[:, :], D['x'][j * 128:(j + 1) * 128, :])
                S.act(junk[:, :], xt[:, :], AF.Square, accum_out=ss[:, j:j + 1])
                S.act(rs[:, j:j + 1], ss[:, j:j + 1], AF.Ln, scale=1.0 / DM, bias=EPS)
                S.act(rs[:, j:j + 1], rs[:, j:j + 1], AF.Exp, scale=-0.5)
                S.act(xn[j % 2][:, :], xt[:, :], AF.Copy, scale=rs[:, j:j + 1])
                pt = psT[j % 2]
                for k in range(8):
                    S.transpose(pt[:, k * 128:(k + 1) * 128], xn[j % 2][:, k * 128:(k + 1) * 128], P.ident_bf[:, :])
                for k in range(8):
                    S.act(hT[:, k, j * 128:(j + 1) * 128], pt[:, k * 128:(k + 1) * 128], AF.Identity,
                          scale=P.A1col[:, k:k + 1], bias=P.modcol[:, k:k + 1])
        if C.debug:
            d = C.dbg_out('hT', [128, 8, SEQ], BF16)
            for k in range(8):
                S.dma(d[:, k, :], hT[:, k, :])
        with S.scope() as p2:
            wvo = sb("wvo", [128, 8, 1040], BF16, p2)
            brow = sb("brow", [128, 1040], F32, p2)
            vt = [sb(f"vt{i}", [128, 512], BF16, p2) for i in range(2)]
            ot = [sb(f"ot{i}", [128, 512], BF16, p2) for i in range(2)]
            otf = [sb(f"otf{i}", [128, 512], F32, p2) for i in range(2)]
            psV = [pst(f"psV{i}", [128, 512], F32, p2) for i in range(2)]
            psO = [pst(f"psO{i}", [128, 512], F32, p2) for i in range(2)]
            psG = [pst(f"psG{i}", [128, 512], F32, p2) for i in range(2)]
            wsrc = D['w_in'].rearrange("(k p) c -> p k c", p=128)
            S.dma(wvo[:, :, 0:512], wsrc[:, :, 1024:1536], q='pool')
            S.dma(wvo[:, :, 512:1040], wsrc[:, :, 1536:2064], q='pool')
            S.dma(brow[:, :], D['bin_row'][0:1, :].partition_broadcast(128))
            for j in range(NT):
                a = j % 2
                for (pp, c0, c1) in ((psV[a], 0, 512), (psO[a], 512, 1024), (psG[a], 1024, 1040)):
                    for k in range(8):
                        S.mm(pp[:, 0:c1 - c0], hT[:, k, j * 128:(j + 1) * 128], wvo[:, k, c0:c1], start=(k == 0), stop=(k == 7))
                S.tt(vt[a][:, :], psV[a][:, :], brow[:, 0:512], ALU.add)
                S.dma(D['Vd'][j * 128:(j + 1) * 128, :], vt[a][:, :])
                S.tt(otf[a][:, :], psO[a][:, :], brow[:, 512:1024], ALU.add)
                S.act(ot[a][:, :], otf[a][:, :], AF.Sigmoid)
                S.dma(D['Od'][j * 128:(j + 1) * 128, :], ot[a][:, :])
                S.tt(P.Gt[:, j, :], psG[a][:, 0:16], brow[:, 1024:1040], ALU.add)
        if C.debug:
            d = C.dbg_out('Gt', [128, NT, 16])
            S.dma(d[:, :, :], P.Gt[:, :, :])
        with S.scope() as p3:
            wch = [sb(f"wch{i}", [128, 8, 128], BF16, p3) for i in range(3)]
            pre = [sb(f"pre{i}", [128, SEQ + 2], F32, p3) for i in range(2)]
            t0s = [sb(f"cv_t0{i}", [128, 2048], F32, p3) for i in range(2)]
            t2s = [sb(f"cv_t2{i}", [128, 2048], F32, p3) for i in range(2)]
            t3 = [[sb(f"cv_t3{q}{i}", [128, 2048], F32, p3) for i in range(2)] for q in range(2)]
            ob = [[sb(f"cv_ob{q}{i}", [128, 2048], BF16, p3) for i in range(2)] for q in range(2)]
            pending = [None]
            bqk = sb("bqk", [128, 8], F32, p3)
            bhy = sb("bhy", [128, 12], F32, p3)
            cqw = sb("cqw", [128, 8, 3], F32, p3)
            cqb = sb("cqb", [128, 8], F32, p3)
            chw = sb("chw", [128, 12, 3], F32, p3)
            chb = sb("chb", [128, 12], F32, p3)
            psF = [pst(f"psF{i}", [128, 512], F32, p3) for i in range(8)]
            for (tile_, nm) in ((bqk, 'bqk_col'), (bhy, 'bhy_col'), (cqb, 'cqk_b'), (chb, 'chy_b')):
                S.dma(tile_[:, :], D[nm][:, :])
            S.dma(cqw[:, :, :], D['cqk_w'][:, :, :])
            S.dma(chw[:, :, :], D['chy_w'][:, :, :])
            for i in range(2):
                S.memset(pre[i][:, 0:1], 0.0)
                S.memset(pre[i][:, SEQ + 1:SEQ + 2], 0.0)
            wsrc = D['w_in'].rearrange("(k p) c -> p k c", p=128)
            chunks = []
            for cc in range(8):
                chunks.append(('qk', cc, cc * 128))
            for i in range(12):
                chunks.append(('hy', i, 2064 + i * 128))
            nps = 0
            for m_ in range(2):
                S.dma(wch[m_ % 3][:, :, :], wsrc[:, :, chunks[m_][2]:chunks[m_][2] + 128], q='pool')
            for n, (kind, ci, col0) in enumerate(chunks):
                wc = wch[n % 3]
                pr = pre[n % 2]
                if n + 2 < len(chunks):
                    S.dma(wch[(n + 2) % 3][:, :, :], wsrc[:, :, chunks[n + 2][2]:chunks[n + 2][2] + 128], q='pool')
                bcol = bqk[:, ci:ci + 1] if kind == 'qk' else bhy[:, ci:ci + 1]
                cw = cqw if kind == 'qk' else chw
                cb = cqb if kind == 'qk' else chb
                for tb in range(8):
                    pp = psF[nps % 8]
                    nps += 1
                    for k in range(8):
                        S.mm(pp[:, :], wc[:, k, :], hT[:, k, tb * 512:(tb + 1) * 512], start=(k == 0), stop=(k == 7))
                    if tb % 2 == 0:
                        S.act(pr[:, 1 + tb * 512:1 + (tb + 1) * 512], pp[:, :], AF.Identity, bias=bcol)
                    else:
                        S.ts(pr[:, 1 + tb * 512:1 + (tb + 1) * 512], pp[:, :], bcol, ALU.add)
                par = n % 2
                for hh in range(2):
                    o0 = hh * 2048
                    S.act(t0s[hh][:, :], pr[:, o0:o0 + 2048], AF.Identity, scale=cw[:, ci, 0:1], bias=cb[:, ci:ci + 1])
                    S.act(t2s[hh][:, :], pr[:, o0 + 2:o0 + 2050], AF.Identity, scale=cw[:, ci, 2:3])
                for hh in range(2):
                    o0 = hh * 2048
                    S.stt(t0s[hh][:, :], pr[:, o0 + 1:o0 + 2049], cw[:, ci, 1:2], t0s[hh][:, :], ALU.mult, ALU.add)
                for hh in range(2):
                    if kind == 'hy' and ci >= 8:
                        S.tt(ob[par][hh][:, :], t0s[hh][:, :], t2s[hh][:, :], ALU.add, eng='pool')
                    else:
                        S.tt(t3[par][hh][:, :], t0s[hh][:, :], t2s[hh][:, :], ALU.add, eng='pool')
                if pending[0] is not None:
                    pending[0]()

                def fin(kind=kind, ci=ci, par=par):
                    for hh in range(2):
                        o0 = hh * 2048
                        if kind == 'qk':
                            S.act(ob[par][hh][:, :], t3[par][hh][:, :], AF.Silu)
                            dst = D['QTd'] if ci < 4 else D['KTd']
                            S.dma(dst[(ci % 4) * 128:(ci % 4 + 1) * 128, o0:o0 + 2048], ob[par][hh][:, :])
                        elif ci < 8:
                            dst = D['X1d'] if ci < 4 else D['X2d']
                            S.dma(dst[(ci % 4) * 128:(ci % 4 + 1) * 128, o0:o0 + 2048], t3[par][hh][:, :])
                        else:
                            S.dma(D['Zd'][(ci - 8) * 128:(ci - 7) * 128, o0:o0 + 2048], ob[par][hh][:, :])
                pending[0] = fin
            pending[0]()
    if C.debug:
        for nm, shp, dt in (('QTd', [512, SEQ], BF16), ('KTd', [512, SEQ], BF16), ('Vd', [SEQ, 512], BF16),
                            ('Od', [SEQ, 512], BF16), ('X1d', [512, SEQ], F32), ('Zd', [512, SEQ], BF16)):
            d = C.dbg_out(nm, shp, dt)
            with S.scope() as pd:
                if shp[0] == 512:
                    tmp = sb("dbgtmp_" + nm, [128, 4, SEQ], dt, pd)
                    S.dma(tmp[:, :, :], D[nm].rearrange("(a p) n -> p a n", p=128))
                    S.dma(d.rearrange("(a p) n -> p a n", p=128), tmp[:, :, :])
                else:
                    tmp = sb("dbgtmp_" + nm, [128, NT, 512], dt, pd)
                    S.dma(tmp[:, :, :], D[nm].rearrange("(a p) n -> p a n", p=128))
                    S.dma(d.rearrange("(a p) n -> p a n", p=128), tmp[:, :, :])


def phase_mlstm(C):
    S, D, P, sb, pst = C.S, C.D, C.P, C.sb, C.pst
    Gt = P.Gt
    with S.scope() as ph:
        triU = sb("triU", [128, 128], F32, ph)
        triL = sb("triL", [128, 128], F32, ph)
        LF = sb("LF", [128, 2, NT, 4], F32, ph)
        II = sb("II", [128, 2, NT, 4], F32, ph)
        Bc = sb("Bc", [128, 2, NT, 4], F32, ph)
        Wp = sb("Wp", [128, 2, NT, 4], F32, ph)
        ENB = sb("ENB", [128, 2, NT, 4], F32, ph)
        EG = sb("EG", [128, 2, NT, 4], F32, ph)
        zcol = sb("zcol", [128, 1], F32, ph)
        S.dma(triU[:, :], D['triU'][:, :])
        S.dma(triL[:, :], D['triL'][:, :])
        S.memset(zcol[:, :], 0.0)
        with S.scope() as pp:
            psB = pst("psB", [128, 512], F32, pp)
            psGs = pst("psGs", [128, 512], F32, pp)
            for d in range(2):
                S.act(LF[:, d, :, :], Gt[:, :, d * 8 + 4:d * 8 + 8], AF.Exp, scale=-1.0)
                S.copy(II[:, d, :, :], Gt[:, :, d * 8:d * 8 + 4])
            fl = lambda t: t[:, :, :, :].rearrange("p d c e -> p (d c e)")
            S.act(fl(LF), fl(LF), AF.Ln, bias=1.0)
            S.ts(fl(LF), fl(LF), -1.0, ALU.mult)
            S.mm(psB[:, 0:128], triU[:, :], fl(LF)[:, 0:128])
            S.mm(psB[:, 128:256], triL[:, :], fl(LF)[:, 128:256])
            S.mm(psGs[:, 0:256], P.ones_f[:, :], fl(LF))
            S.copy(fl(Bc), psB[:, 0:256])
            S.act(fl(EG), psGs[:, 0:256], AF.Exp)
            S.tt(fl(Wp), fl(II), fl(Bc), ALU.subtract)
            S.act(fl(Wp), fl(Wp), AF.Exp, bias=math.log(128.0 ** -0.5))
            S.act(fl(ENB), fl(Bc), AF.Exp, scale=-1.0)
        if C.debug:
            for nm, t in (('Bc', Bc), ('Wp', Wp), ('EG', EG)):
                d_ = C.dbg_out(nm, [128, 2, NT, 4])
                S.dma(d_[:, :, :, :], t[:, :, :, :])
        for hd in range(4):
            with S.scope() as hs:
                qT = sb("qT", [128, SEQ], BF16, hs)
                kT = sb("kT", [128, SEQ], BF16, hs)
                vaug = sb("vaug", [128, NT, 129], BF16, hs)
                osg = sb("osg", [128, NT, 128], BF16, hs)
                ktm = sb("ktm", [128, NT, 128], BF16, hs)
                Hs = sb("Hs", [128, NT, 128], F32, hs)
                sq = sb("sq", [128, NT, 128], F32, hs)
                gO = sb("gO", [128, NT, 128], BF16, hs)
                ym = sb("ym", [128, NT, 128], BF16, hs)
                ymT = sb("ymT", [128, SEQ], BF16, hs)
                mng = sb("mng", [128, 128], F32, hs)
                STw = [sb(f"STw{i}", [128, 128], BF16, hs) for i in range(4)]
                vw = [sb(f"vw{i}", [128, 129], BF16, hs) for i in range(4)]
                Tst = sb("Tst", [128, 129], F32, hs)
                Tst2 = sb("Tst2", [128, 129], F32, hs)
                Cbfd = [[sb(f"Cbf{d_}{i}", [128, 129], BF16, hs) for i in range(2)] for d_ in range(2)]
                sm = [sb(f"sm{i}", [128, 4], F32, hs) for i in range(2)]
                ssq = sb("ssq", [128, NT], F32, hs)
                pS = [pst(f"pS{i}", [128, 512], F32, hs) for i in range(2)]
                pO = [pst(f"pO{i}", [128, 512], F32, hs) for i in range(2)]
                pC = [pst(f"pC{i}", [128, 512], F32, hs) for i in range(2)]
                pK = pst("pK", [128, 1024], BF16, hs)
                S.dma(qT[:, :], D['QTd'][hd * 128:(hd + 1) * 128, :])
                S.dma(kT[:, :], D['KTd'][hd * 128:(hd + 1) * 128, :])
                S.dma(vaug[:, :, 0:128], D['Vd'][:, hd * 128:(hd + 1) * 128].rearrange("(c p) d -> p c d", p=128))
                S.memset(vaug[:, :, 128:129], 1.0)
                S.dma(osg[:, :, :], D['Od'][:, hd * 128:(hd + 1) * 128].rearrange("(c p) d -> p c d", p=128))
                S.dma(mng[:, :], D['mnorm_g'][0:1, hd * 128:(hd + 1) * 128].partition_broadcast(128))
                for c0 in range(0, NT, 8):
                    for c in range(c0, c0 + 8):
                        S.transpose(pK[:, (c - c0) * 128:(c - c0 + 1) * 128], kT[:, c * 128:(c + 1) * 128], P.ident_bf[:, :])
                    S.copy(ktm[:, c0:c0 + 8, :].rearrange("p c d -> p (c d)"), pK[:, :], eng=('act' if (c0 // 8) % 2 else 'dve'))
                n = 0
                Tsts = [Tst, Tst2]
                prevc = [None, None]
                visited = set()
                for d in range(2):
                    S.memset(Tsts[d][:, :], 0.0)
                    S.memset(Cbfd[d][0][:, :], 0.0)
                nd = [0, 0]
                steps = []
                for i in range(NT):
                    for d in range(2):
                        steps.append((d, i if d == 0 else NT - 1 - i))

                def phase1(n):
                    d, c = steps[n]
                    cs = slice(c * 128, (c + 1) * 128)
                    wcol = Wp[:, d, c, hd:hd + 1]
                    mask = triU if d == 0 else triL
                    S.mm(pS[n % 2][:, 0:128], kT[:, cs], qT[:, cs])
                    S.stt(STw[n % 4][:, :], pS[n % 2][:, 0:128], wcol, mask[:, :], ALU.mult, ALU.mult)
                    S.act(vw[n % 4][:, :], vaug[:, c, :], AF.Copy, scale=wcol)

                def phase2(n):
                    d, c = steps[n]
                    a = n % 2
                    b_ = nd[d] % 2
                    nd[d] += 1
                    cs = slice(c * 128, (c + 1) * 128)
                    S.mm(pO[a][:, 0:129], STw[n % 4][:, :], vaug[:, c, :], start=True, stop=False)
                    S.mm(pO[a][:, 0:129], qT[:, cs], Cbfd[d][b_][:, :], start=False, stop=True)
                    S.mm(pC[a][:, 0:129], ktm[:, c, :], vw[n % 4][:, :])
                    enb = ENB[:, d, c, hd:hd + 1]
                    s_ = sm[a]
                    S.ts(s_[:, 0:1], pO[a][:, 128:129], enb, ALU.max)
                    S.stt(s_[:, 2:3], pO[a][:, 128:129], -1.0, s_[:, 0:1], ALU.mult, ALU.max)
                    S.recip(s_[:, 3:4], s_[:, 2:3])
                    if c not in visited:
                        visited.add(c)
                        S.act(Hs[:, c, :], pO[a][:, 0:128], AF.Copy, scale=s_[:, 3:4])
                    else:
                        S.stt(Hs[:, c, :], pO[a][:, 0:128], s_[:, 3:4], Hs[:, c, :], ALU.mult, ALU.add)
                    egp = zcol[:, 0:1] if prevc[d] is None else EG[:, d, prevc[d], hd:hd + 1]
                    S.stt(Tsts[d][:, :], Tsts[d][:, :], egp, pC[a][:, 0:129], ALU.mult, ALU.add)
                    S.act(Cbfd[d][(b_ + 1) % 2][:, :], Tsts[d][:, :], AF.Copy, scale=EG[:, d, c, hd:hd + 1])
                    prevc[d] = c
                phase1(0)
                for n_ in range(len(steps)):
                    if n_ + 1 < len(steps):
                        phase1(n_ + 1)
                    phase2(n_)
                S.act(sq[:, :, :], Hs[:, :, :], AF.Square)
                S.reduce(ssq[:, :], sq[:, :, :], ALU.add)
                S.ts(ssq[:, :], ssq[:, :], 1.0 / 128.0, ALU.mult, s2=EPS, op1=ALU.add)
                S.act(ssq[:, :], ssq[:, :], AF.Sqrt)
                S.recip(ssq[:, :], ssq[:, :])
                S.tt(gO[:, :, :], osg[:, :, :], mng[:, :].unsqueeze(1).to_broadcast([128, NT, 128]), ALU.mult)
                for c in range(NT):
                    S.stt(ym[:, c, :], Hs[:, c, :], ssq[:, c:c + 1], gO[:, c, :], ALU.mult, ALU.mult)
                for c0 in range(0, NT, 8):
                    for c in range(c0, c0 + 8):
                        S.transpose(pK[:, (c - c0) * 128:(c - c0 + 1) * 128], ym[:, c, :], P.ident_bf[:, :])
                    S.copy(ymT[:, c0 * 128:(c0 + 8) * 128], pK[:, :], eng=('act' if (c0 // 8) % 2 else 'dve'))
                S.dma(D['Yt'][hd * 128:(hd + 1) * 128, :], ymT[:, :])
    if C.debug:
        d_ = C.dbg_out('Yt_m', [512, SEQ], BF16)
        with S.scope() as pd:
            tmp = sb("dbgtmp_ytm", [128, 4, SEQ], BF16, pd)
            S.dma(tmp[:, :, :], D['Yt'][0:512, :].rearrange("(a p) n -> p a n", p=128))
            S.dma(d_.rearrange("(a p) n -> p a n", p=128), tmp[:, :, :])


def phase_hyena(C):
    S, D, P, sb, pst = C.S, C.D, C.P, C.sb, C.pst
    PI = math.pi
    with S.scope() as pa:
        hid2T = sb("hid2T", [64, SEQ], F32, pa)
        hid2rT = sb("hid2rT", [64, SEQ], F32, pa)
        frc = sb("frc", [64, 1], F32, pa)
        frb1 = sb("frb1", [64, 1], F32, pa)
        frb2 = sb("frb2", [64, 1], F32, pa)
        with S.scope() as p1:
            zT = sb("zT", [33, SEQ], F32, p1)
            zrT = sb("zrT", [33, SEQ], F32, p1)
            hid1 = sb("hid1", [64, SEQ], F32, p1)
            w1 = sb("hw1", [33, 64], F32, p1)
            w2 = sb("hw2", [64, 64], F32, p1)
            b1c = sb("b1c", [64, 1], F32, p1)
            b2c = sb("b2c", [64, 1], F32, p1)
            arg = [sb(f"harg{i}", [64, 512], F32, p1) for i in range(2)]
            m1 = [sb(f"hm1{i}", [64, 512], F32, p1) for i in range(2)]
            m2 = [sb(f"hm2{i}", [64, 512], F32, p1) for i in range(2)]
            psM = [pst(f"psM{i}", [128, 512], F32, p1) for i in range(2)]
            S.dma(zT[:, :], D['zT'][:, :])
            S.dma(zrT[:, :], D['zrT'][:, :])
            S.dma(w1[:, :], D['hy_w1'][:, :])
            S.dma(w2[:, :], D['hy_w2'][:, :])
            S.dma(b1c[:, :], D['hy_b1c'][:, :])
            S.dma(b2c[:, :], D['hy_b2c'][:, :])
            S.dma(frc[:, :], D['hy_frc'][:, :])
            S.tt(frb1[:, :], frc[:, :], b1c[:, :], ALU.mult)
            S.tt(frb2[:, :], frc[:, :], b2c[:, :], ALU.mult)
            n = 0

            def sin_layer(ps, frb, dst):
                nonlocal n
                a = n % 2
                n += 1
                S.ts(arg[a][:, :], ps, frc[:, 0:1], ALU.mult, s2=frb[:, 0:1], op1=ALU.add)
                S.ts(m1[a][:, :], arg[a][:, :], PI, ALU.is_gt, s2=-2.0 * PI, op1=ALU.mult)
                S.ts(m2[a][:, :], arg[a][:, :], -PI, ALU.is_lt, s2=2.0 * PI, op1=ALU.mult)
                S.tt(arg[a][:, :], arg[a][:, :], m1[a][:, :], ALU.add)
                S.tt(arg[a][:, :], arg[a][:, :], m2[a][:, :], ALU.add)
                S.act(dst, arg[a][:, :], AF.Sin)
            for (zs, hdst) in ((zT, hid2T), (zrT, hid2rT)):
                for blk in range(8):
                    ps = psM[blk % 2]
                    S.mm(ps[0:64, :], w1[:, :], zs[:, blk * 512:(blk + 1) * 512])
                    sin_layer(ps[0:64, :], frb1, hid1[:, blk * 512:(blk + 1) * 512])
                for blk in range(8):
                    ps = psM[blk % 2]
                    S.mm(ps[0:64, :], w2[:, :], hid1[:, blk * 512:(blk + 1) * 512])
                    sin_layer(ps[0:64, :], frb2, hdst[:, blk * 512:(blk + 1) * 512])
        with S.scope() as p2:
            w3 = sb("hw3", [64, 2048], F32, p2)
            trow = sb("trow", [128, SEQ], F32, p2)
            trrow = sb("trrow", [128, SEQ], F32, p2)
            ndel = sb("ndel", [128, 16], F32, p2)
            hbias = sb("hbias", [128, 8], F32, p2)
            kT = [sb(f"kTb{i}", [128, 2 * SEQ], BF16, p2) for i in range(2)]
            win = [sb(f"win{i}", [128, 512], F32, p2) for i in range(2)]
            k0t = sb("k0t", [128, 4], F32, p2)
            psK_ = [pst(f"psKf{i}", [128, 512], F32, p2) for i in range(3)]
            ps0 = pst("psK0", [128, 512], F32, p2)
            S.dma(w3[:, :], D['hy_w3'][:, :])
            S.dma(trow[:, :], D['t_row'][0:1, :].partition_broadcast(128))
            S.dma(trrow[:, :], D['tr_row'][0:1, :].partition_broadcast(128))
            S.dma(ndel[:, :], D['hy_del_col'][:, :])
            S.dma(hbias[:, :], D['hy_bias_col'][:, :])
            S.stt(ndel[:, :], ndel[:, :], -1.0, ndel[:, :], ALU.mult, ALU.max)
            S.ts(ndel[:, :], ndel[:, :], -1.0, ALU.mult)
            nb = 0
            nk = 0
            for o in range(2):
                for g in range(4):
                    kt = kT[nk % 2]
                    nk += 1
                    for d in range(2):
                        hs = hid2T if d == 0 else hid2rT
                        tr = trow if d == 0 else trrow
                        c0 = (o * 2 + d) * 512 + g * 128
                        di = o * 8 + d * 4 + g
                        for blk in range(8):
                            ps = psK_[nb % 3]
                            wn = win[nb % 2]
                            nb += 1
                            S.mm(ps[:, :], w3[:, c0:c0 + 128], hs[:, blk * 512:(blk + 1) * 512])
                            S.act(wn[:, :], tr[:, blk * 512:(blk + 1) * 512], AF.Exp, scale=ndel[:, di:di + 1])
                            S.stt(kt[:, d * SEQ + blk * 512:d * SEQ + (blk + 1) * 512], wn[:, :], 0.05, ps[:, :], ALU.add, ALU.mult)
                    S.memset(kt[:, SEQ:SEQ + 1], 0.0)
                    cf = (o * 2 + 0) * 512 + g * 128
                    cb = (o * 2 + 1) * 512 + g * 128
                    S.mm(ps0[:, 0:1], w3[:, cf:cf + 128], hid2T[:, 0:1])
                    S.mm(ps0[:, 1:2], w3[:, cb:cb + 128], hid2T[:, 0:1])
                    S.copy(k0t[:, 0:2], ps0[:, 0:2])
                    S.tt(k0t[:, 2:3], k0t[:, 0:1], k0t[:, 1:2], ALU.add)
                    S.ts(k0t[:, 3:4], k0t[:, 2:3], 1.05, ALU.mult)
                    S.tt(kt[:, 0:1], k0t[:, 3:4], hbias[:, o * 4 + g:o * 4 + g + 1], ALU.add)
                    dst = D['Kd0'] if o == 0 else D['Kd1']
                    S.dma(dst[g * 128:(g + 1) * 128, :], kt[:, :])
    if C.debug:
        d_ = C.dbg_out('Kd0', [512, 2 * SEQ], BF16)
        with S.scope() as pd:
            tmp = sb("dbgtmp_kd", [128, 4, 2 * SEQ], BF16, pd)
            S.dma(tmp[:, :, :], D['Kd0'].rearrange("(a p) n -> p a n", p=128))
            S.dma(d_.rearrange("(a p) n -> p a n", p=128), tmp[:, :, :])
    with S.scope() as pb:
        T = Ctx()
        T.F1 = sb("F1", [64, 128], BF16, pb)
        T.GrT = sb("GrT", [128, 64, 128], BF16, pb)
        T.GiT = sb("GiT", [128, 64, 128], BF16, pb)
        T.Rc1 = sb("Rc1", [128, 256], BF16, pb)
        T.Rc2 = sb("Rc2", [128, 256], BF16, pb)
        T.LrT = sb("LrT", [64, 128, 32], BF16, pb)
        T.LiNT = sb("LiNT", [64, 128, 32], BF16, pb)
        hnorm = sb("hnorm", [128, 4], F32, pb)
        S.dma(T.F1[:, :], D['F1'][:, :])
        for nm in ('GrT', 'GiT'):
            S.dma(getattr(T, nm)[:, :, :], D[nm].rearrange("p (k m) -> p k m", k=64))
        S.dma(T.Rc1[:, :], D['Rc1'][:, :])
        S.dma(T.Rc2[:, :], D['Rc2'][:, :])
        S.dma(T.LrT[:, :, :], D['LrT'].rearrange("p (n m) -> p n m", n=128))
        S.dma(T.LiNT[:, :, :], D['LiNT'].rearrange("p (n m) -> p n m", n=128))
        S.dma(hnorm[:, :], D['hnorm_col'][:, :])
        for o in range(2):
            zsrc = D['Zd'] if o == 0 else D['Z1d']
            kd = D['Kd0'] if o == 0 else D['Kd1']
            with S.scope() as pw:
                W = Ctx()
                W.X = sb("fX", [64, 64, 128], BF16, pw)
                W.AT = sb("fAT", [128, 64, 2, 64], BF16, pw)
                W.ATn = sb("fATn", [128, 64, 2, 64], BF16, pw)
                W.Kf = sb("fKf", [128, 64, 2, 64], BF16, pw)
                W.V = sb("fV", [128, 2, 64, 64], BF16, pw)
                W.Wt = sb("fWt", [64, 64, 2, 128], BF16, pw)
                W.Ysb = sb("fY", [32, 64, 128], BF16, pw)
                W.tm = [[sb(f"ftm{i}{j}", [128, 4, 64], F32, pw) for j in range(4)] for i in range(2)]
                W.ring = [pst(f"psR{i}", [128, 512], F32, pw) for i in range(8)]
                W.nr = 0
                W.ne = 0
                W.pref = False
                for b8 in range(8):
                    ch0 = b8 * 64
                    fft_batch(C, T, W, zsrc[ch0:ch0 + 64, :], kd[ch0:ch0 + 64, :], D['Ycv'][ch0:ch0 + 64, :],
                              kd_next=(kd[ch0 + 64:ch0 + 128, :] if b8 < 7 else None))
            with S.scope() as pg:
                ysb = [sb(f"gy{i}", [128, SEQ], BF16, pg) for i in range(2)]
                xsb = [sb(f"gx{i}", [128, SEQ], F32, pg) for i in range(2)]
                zo = [sb(f"gz{i}", [128, SEQ], BF16, pg) for i in range(2)]
                xsrc = D['X1d'] if o == 0 else D['X2d']
                if o == 1:
                    z2 = sb("gz2", [128, SEQ], F32, pg)
                    sq = [sb(f"gsq{i}", [128, 512], F32, pg) for i in range(2)]
                    rr = [sb(f"grr{i}", [128, 512], F32, pg) for i in range(2)]
                    psN = [pst(f"psN{i}", [128, 512], F32, pg) for i in range(2)]
                for g in range(4):
                    a = g % 2
                    S.dma(ysb[a][:, :], D['Ycv'][g * 128:(g + 1) * 128, :])
                    S.dma(xsb[a][:, :], xsrc[g * 128:(g + 1) * 128, :])
                    if o == 0:
                        S.tt(zo[a][:, :], xsb[a][:, :], ysb[a][:, :], ALU.mult)
                        S.dma(D['Z1d'][g * 128:(g + 1) * 128, :], zo[a][:, :])
                    else:
                        S.tt(z2[:, :], xsb[a][:, :], ysb[a][:, :], ALU.mult)
                        for blk in range(8):
                            bs = slice(blk * 512, (blk + 1) * 512)
                            q_ = blk % 2
                            S.act(sq[q_][:, :], z2[:, bs], AF.Square)
                            S.mm(psN[q_][:, :], P.ones_f[:, :], sq[q_][:, :])
                            S.ts(rr[q_][:, :], psN[q_][:, :], 1.0 / 128.0, ALU.mult, s2=EPS, op1=ALU.add)
                            S.act(rr[q_][:, :], rr[q_][:, :], AF.Sqrt)
                            S.recip(rr[q_][:, :], rr[q_][:, :])
                            S.stt(zo[a][:, bs], z2[:, bs], hnorm[:, g:g + 1], rr[q_][:, :], ALU.mult, ALU.mult)
                        S.dma(D['Yt'][512 + g * 128:512 + (g + 1) * 128, :], zo[a][:, :])
    if C.debug:
        d_ = C.dbg_out('Yt_h', [512, SEQ], BF16)
        with S.scope() as pd:
            tmp = sb("dbgtmp_yth", [128, 4, SEQ], BF16, pd)
            S.dma(tmp[:, :, :], D['Yt'][512:1024, :].rearrange("(a p) n -> p a n", p=128))
            S.dma(d_.rearrange("(a p) n -> p a n", p=128), tmp[:, :, :])


def fft_batch(C, T, W, zsrc, kdsrc, ydst, kd_next=None):
    S = C.S

    def bank():
        W.nr += 1
        return W.ring[W.nr % 8]

    def ev(dst, src):
        W.ne += 1
        S.copy(dst, src, eng=('act' if W.ne % 2 else 'dve'))

    def forward_d(kdim):
        for cq in range(16):
            pA = bank()
            for i in range(4):
                ch = cq * 4 + i
                S.mm(pA[:, i * 128:(i + 1) * 128], W.X[0:kdim, ch, :], T.F1[0:kdim, :])
            ev(W.AT[:, cq * 4:(cq + 1) * 4, :, :].rearrange("p c r k -> p (c r k)"), pA[:, :])
            src = pA[:, :].rearrange("p (c r k) -> p c r k", c=4, r=2)
            S.act(W.ATn[:, cq * 4:(cq + 1) * 4, 0, :], src[:, :, 1, :], AF.Copy, scale=-1.0)
            S.copy(W.ATn[:, cq * 4:(cq + 1) * 4, 1, :], src[:, :, 0, :])

    def forward_s(mode):
        for kb in range(16):
            pU = bank()
            for i in range(4):
                k1 = kb * 4 + i
                o_ = pU[:, i * 128:(i + 1) * 128]
                S.mm(o_, T.GrT[:, k1, :], W.AT[:, :, :, k1].rearrange("p c r -> p r c"), start=True, stop=False)
                S.mm(o_, T.GiT[:, k1, :], W.ATn[:, :, :, k1].rearrange("p c r -> p r c"), start=False, stop=True)
            if mode == 'kernel':
                ev(W.Kf[:, kb * 4:(kb + 1) * 4, :, :].rearrange("p k r c -> p (k r c)"), pU[:, :])
            else:
                pv = pU[:, :].rearrange("p (k r c) -> p k r c", k=4, r=2)
                Ur = pv[:, :, 0, :]
                Ui = pv[:, :, 1, :]
                Kr = W.Kf[:, kb * 4:(kb + 1) * 4, 0, :]
                Ki = W.Kf[:, kb * 4:(kb + 1) * 4, 1, :]
                t = W.tm[kb % 2]
                S.tt(t[0][:, :, :], Ur, Kr, ALU.mult)
                S.tt(t[1][:, :, :], Ui, Ki, ALU.mult)
                S.tt(t[2][:, :, :], Ur, Ki, ALU.mult)
                S.tt(t[3][:, :, :], Ui, Kr, ALU.mult)
                S.tt(W.V[:, 0, kb * 4:(kb + 1) * 4, :], t[0][:, :, :], t[1][:, :, :], ALU.subtract, eng='pool')
                S.tt(W.V[:, 1, kb * 4:(kb + 1) * 4, :], t[2][:, :, :], t[3][:, :, :], ALU.add, eng='pool')

    if not W.pref:
        S.dma(W.X[:, :, :], kdsrc.rearrange("c (a n) -> a c n", n=128))
    forward_d(64)
    forward_s('kernel')
    S.dma(W.X[0:32, :, :], zsrc.rearrange("c (a n) -> a c n", n=128))
    forward_d(32)
    W.pref = False
    if kd_next is not None:
        S.dma(W.X[:, :, :], kd_next.rearrange("c (a n) -> a c n", n=128))
        W.pref = True
    forward_s('data')
    for cp in range(32):
        pW = bank()
        for i in range(2):
            ch = cp * 2 + i
            o_ = pW[0:64, i * 256:(i + 1) * 256]
            S.mm(o_, W.V[:, 0, :, ch], T.Rc1[:, :], start=True, stop=False)
            S.mm(o_, W.V[:, 1, :, ch], T.Rc2[:, :], start=False, stop=True)
        ev(W.Wt[:, cp * 2:(cp + 1) * 2, :, :].rearrange("p c r n -> p (c r n)"), pW[0:64, :])
    for nb in range(16):
        pY = bank()
        for i in range(8):
            n2 = nb * 8 + i
            o_ = pY[0:32, i * 64:(i + 1) * 64]
            S.mm(o_, T.LrT[:, n2, :], W.Wt[:, :, 0, n2], start=True, stop=False)
            S.mm(o_, T.LiNT[:, n2, :], W.Wt[:, :, 1, n2], start=False, stop=True)
        ev(W.Ysb[:, :, nb * 8:(nb + 1) * 8], pY[0:32, :].rearrange("p (n c) -> p c n", n=8))
    S.dma(ydst.rearrange("c (a n) -> a c n", n=128), W.Ysb[:, :, :])


def phase_outproj(C):
    S, D, P, sb, pst, nc = C.S, C.D, C.P, C.sb, C.pst, C.nc
    P.IDX = sb("IDX", [128, 16, 4], I32)
    P.GW = sb("GW", [128, 16, 4], F32)
    with S.scope() as ph:
        AFF = sb("AFF", [128, NT, 16], F32, ph)
        AFFT = sb("AFFT", [16, SEQ], F32, ph)
        with S.scope() as p1:
            ytT = sb("ytT", [128, 8, SEQ], BF16, p1)
            wout = sb("wout", [128, 8, DM], BF16, p1)
            wr = sb("wr", [128, 8, 16], F32, p1)
            xb = [sb(f"oxb{i}", [128, DM], F32, p1) for i in range(2)]
            x1 = [sb(f"ox1{i}", [128, DM], F32, p1) for i in range(3)]
            tmp = [sb(f"otmp{i}", [128, DM], F32, p1) for i in range(2)]
            hf = [sb(f"ohf{i}", [128, DM], F32, p1) for i in range(3)]
            hfb = [sb(f"ohfb{i}", [128, DM], BF16, p1) for i in range(3)]
            hfT = [sb(f"ohfT{i}", [128, 8, 128], F32, p1) for i in range(2)]
            junk = sb("ojunk", [128, DM], BF16, p1)
            sm = sb("osm", [128, NT, 8], F32, p1)
            ex = [sb(f"oex{i}", [128, 16], F32, p1) for i in range(2)]
            psM = [[pst(f"psMo{i}{h}", [128, 512], F32, p1) for h in range(2)] for i in range(2)]
            psT = [pst(f"psTo{i}", [128, 512], F32, p1) for i in range(2)]
            psR = pst("psR", [128, 512], F32, p1)
            psAT = pst("psAT", [128, 512], F32, p1)
            psAT2 = psT[1]
            for k in range(8):
                S.dma(ytT[:, k, :], D['Yt'][k * 128:(k + 1) * 128, :])
            wsrc = D['w_out'].rearrange("(k p) c -> p k c", p=128)
            S.dma(wout[:, :, 0:512], wsrc[:, :, 0:512], q='pool')
            S.dma(wout[:, :, 512:1024], wsrc[:, :, 512:1024], q='pool')
            S.dma(wr[:, :, :], D['w_router'].rearrange("(k p) e -> p k e", p=128))
            def stage_a1(j):
                a = j % 2
                b = j % 3
                xt = xb[a]
                S.dma(xt[:, :], D['x'][j * 128:(j + 1) * 128, :])
                for h in range(2):
                    for k in range(8):
                        S.mm(psM[a][h][:, :], ytT[:, k, j * 128:(j + 1) * 128], wout[:, k, h * 512:(h + 1) * 512],
                             start=(k == 0), stop=(k == 7))
                for h in range(2):
                    S.tt(tmp[a][:, h * 512:(h + 1) * 512], psM[a][h][:, :], P.gt1rep[:, h * 512:(h + 1) * 512], ALU.mult)
                S.tt(x1[b][:, :], tmp[a][:, :], xt[:, :], ALU.add)
                S.dma(D['acc'][j * 128:(j + 1) * 128, :], x1[b][:, :])

            def stage_a2(j):
                b = j % 3
                ss = sm[:, j, 0:1]
                rs = sm[:, j, 1:2]
                S.act(junk[:, :], x1[b][:, :], AF.Square, accum_out=ss)
                S.act(rs, ss, AF.Ln, scale=1.0 / DM, bias=EPS)
                S.act(rs, rs, AF.Exp, scale=-0.5)
                S.stt(hf[b][:, :], x1[b][:, :], rs, P.A2rep[:, :], ALU.mult, ALU.mult)
                S.tt(hf[b][:, :], hf[b][:, :], P.B2rep[:, :], ALU.add)
                S.act(hfb[b][:, :], hf[b][:, :], AF.Copy)
                S.dma(D['HFd'][j * 128:(j + 1) * 128, :], hfb[b][:, :])

            def stage_b1(j):
                a = j % 2
                b = j % 3
                for k in range(8):
                    S.transpose(psT[k // 4][:, (k % 4) * 128:(k % 4 + 1) * 128], hf[b][:, k * 128:(k + 1) * 128], P.ident_f[:, :])
                S.copy(hfT[a][:, 0:4, :].rearrange("p k t -> p (k t)"), psT[0][:, :], eng='act')
                S.copy(hfT[a][:, 4:8, :].rearrange("p k t -> p (k t)"), psT[1][:, :], eng='dve')

            def stage_b2(j):
                a = j % 2
                for k in range(8):
                    S.mm(psR[:, 0:16], hfT[a][:, k, :], wr[:, k, :], start=(k == 0), stop=(k == 7))
                mx = sm[:, j, 2:3]
                se = sm[:, j, 3:4]
                S.reduce(mx, psR[:, 0:16], ALU.max)
                S.ts(mx, mx, -1.0, ALU.mult)
                S.act(ex[a][:, :], psR[:, 0:16], AF.Exp, bias=mx, accum_out=se)
                S.recip(se, se)
                S.ts(AFF[:, j, :], ex[a][:, :], se, ALU.mult)
            for step in range(NT + 2):
                if 0 <= step - 2 < NT:
                    stage_b1(step - 2)
                if step < NT:
                    stage_a1(step)
                if 0 <= step - 1 < NT:
                    stage_a2(step - 1)
                if 0 <= step - 2 < NT:
                    stage_b2(step - 2)
            for j in range(NT):
                pat = psAT if j % 2 == 0 else psAT2
                S.transpose(pat[0:16, 0:128], AFF[:, j, :], P.ident_f[:, :])
                S.copy(AFFT[:, j * 128:(j + 1) * 128], pat[0:16, 0:128], eng=('act' if j % 2 else 'dve'))
            if C.debug:
                pass
        if C.debug:
            d_ = C.dbg_out('AFF', [128, NT, 16])
            S.dma(d_[:, :, :], AFF[:, :, :])
        with S.scope() as p2:
            junkA = sb("junkA", [16, SEQ], F32, p2)
            bs = sb("bis", [16, 8], F32, p2)
            THR = sb("THR", [128, 16], F32, p2)
            throw = sb("throw", [1, 16], F32, p2)
            SEL = sb("SEL", [128, NT, 16], F32, p2)
            POS = sb("POS", [128, NT, 16], F32, p2)
            selcum = sb("selcum", [128, 16], F32, p2)
            striU = sb("striU", [128, 128], F32, p2)
            COORD = sb("COORD", [128, NT, 16, 4], BF16, p2)
            jf = sb("jf", [128, NT], F32, p2)
            pf = sb("pf", [128, 1], F32, p2)
            iot = sb("iot", [128, 512], F32, p2)
            OH = [sb(f"OH{i}", [128, 512], BF16, p2) for i in range(3)]
            r4 = [sb(f"r4{i}", [128, 8], F32, p2) for i in range(2)]
            psI = [pst(f"psI{i}", [128, 512], F32, p2) for i in range(4)]
            psP = [pst(f"psP{i}", [128, 512], F32, p2) for i in range(2)]
            psX = pst("psX", [128, 512], F32, p2)
            psB2 = pst("psB2", [128, 512], F32, p2)
            lo, hi, mid, cnt, ge, d1, d2 = [bs[:, i:i + 1] for i in range(7)]
            S.dma(striU[:, :], D['striU'][:, :])
            S.memset(lo, 0.0)
            S.memset(hi, 1.0)
            for it in range(30):
                S.tt(mid, lo, hi, ALU.add)
                S.ts(mid, mid, 0.5, ALU.mult)
                S.ts(junkA[:, :], AFFT[:, :], mid, ALU.is_ge, s2=0.0, op1=ALU.add, accum_out=cnt)
                S.ts(ge, cnt, 511.5, ALU.is_gt)
                S.tt(d1, mid, lo, ALU.subtract)
                S.tt(d2, hi, mid, ALU.subtract)
                S.stt(lo, d1, ge, lo, ALU.mult, ALU.add)
                S.stt(hi, d2, ge, mid, ALU.mult, ALU.add)
            S.transpose(psX[0:1, 0:16], lo, P.ident_f[0:16, 0:16])
            S.copy(throw[:, :], psX[0:1, 0:16])
            S.mm(psB2[:, 0:16], P.ones_f[0:1, 0:128], throw[0:1, :])
            S.copy(THR[:, :], psB2[:, 0:16])
            S.tt(SEL[:, :, :], AFF[:, :, :], THR[:, :].unsqueeze(1).to_broadcast([128, NT, 16]), ALU.is_ge)
            S.memset(selcum[:, :], 0.0)
            for j in range(NT):
                pp = psP[j % 2]
                S.mm(pp[:, 0:16], striU[:, :], SEL[:, j, :], start=True, stop=False)
                S.mm(pp[:, 0:16], P.ones_f[:, :], selcum[:, :], start=False, stop=True)
                S.copy(POS[:, j, :], pp[:, 0:16], eng='act')
                S.tt(selcum[:, :], selcum[:, :], SEL[:, j, :], ALU.add)
            S.op('pool', lambda e: e.iota(jf[:, :], [[1, NT]], base=0, channel_multiplier=0, allow_small_or_imprecise_dtypes=True), [], [jf[:, :]])
            S.op('pool', lambda e: e.iota(pf[:, :], [[1, 1]], base=0, channel_multiplier=1, allow_small_or_imprecise_dtypes=True), [], [pf[:, :]])
            S.op('pool', lambda e: e.iota(iot[:, :], [[1, 512]], base=0, channel_multiplier=0, allow_small_or_imprecise_dtypes=True), [], [iot[:, :]])
            S.copy(COORD[:, :, :, 0], jf[:, :].unsqueeze(2).to_broadcast([128, NT, 16]))
            S.copy(COORD[:, :, :, 1], pf[:, 0:1].unsqueeze(2).to_broadcast([128, NT, 16]))
            S.copy(COORD[:, :, :, 2], AFF[:, :, :])
            S.tt(COORD[:, :, :, 3], AFF[:, :, :], COORD[:, :, :, 2], ALU.subtract)
            n = 0
            for e_ in range(16):
                for j in range(NT):
                    oh = OH[n % 3]
                    n += 1
                    S.ts(oh[:, :], iot[:, :], POS[:, j, e_:e_ + 1], ALU.is_equal, s2=SEL[:, j, e_:e_ + 1], op1=ALU.mult)
                    for sc in range(4):
                        S.mm(psI[sc][:, 0:4], oh[:, sc * 128:(sc + 1) * 128], COORD[:, j, e_, :], start=(j == 0), stop=(j == NT - 1))
                for sc in range(4):
                    r = r4[(e_ * 4 + sc) % 2]
                    S.copy(r[:, 0:4], psI[sc][:, 0:4], eng='act')
                    S.stt(r[:, 4:5], r[:, 0:1], 128.0, r[:, 1:2], ALU.mult, ALU.add)
                    S.copy(P.IDX[:, e_, sc:sc + 1], r[:, 4:5])
                    S.tt(P.GW[:, e_, sc:sc + 1], r[:, 2:3], r[:, 3:4], ALU.add)
        if C.debug:
            d_ = C.dbg_out('IDX', [128, 16, 4], I32)
            S.dma(d_[:, :, :], P.IDX[:, :, :])
            d_ = C.dbg_out('GW', [128, 16, 4])
            S.dma(d_[:, :, :], P.GW[:, :, :])


def phase_route(C):
    pass


def phase_experts(C):
    S, D, P, sb, pst, nc = C.S, C.D, C.P, C.sb, C.pst, C.nc
    with S.scope() as ph:
        stg = [sb(f"stg{i}", [128, 8, 512], F32, ph) for i in range(4)]
        wb = [sb(f"wb{i}", [128, 8, 512], BF16, ph) for i in range(8)]
        xs = sb("xs", [128, 4, DM], BF16, ph)
        xsT = sb("xsT", [128, 8, 512], BF16, ph)
        hidT = sb("hidT", [128, 16, 512], BF16, ph)
        sg = [sb(f"sg{i}", [128, 512], F32, ph) for i in range(2)]
        ysb = [sb(f"ysb{i}", [128, DM], F32, ph) for i in range(4)]
        psT = pst("psTe", [128, 1024], BF16, ph)
        psG = [pst(f"psGe{i}", [128, 512], F32, ph) for i in range(2)]
        psU = pst("psUe", [128, 512], F32, ph)
        psY = [pst(f"psYe{i}", [128, 512], F32, ph) for i in range(4)]
        pieces = []
        for e_ in range(16):
            for fb in range(4):
                for nm in ('w_gate', 'w_up'):
                    pieces.append(D[nm][e_ * 1024:(e_ + 1) * 1024, fb * 512:(fb + 1) * 512].rearrange("(k p) c -> p k c", p=128))
            for dh in range(2):
                for fh in range(2):
                    r0 = e_ * 2048 + fh * 1024
                    pieces.append(D['w_down'][r0:r0 + 1024, dh * 512:(dh + 1) * 512].rearrange("(k p) c -> p k c", p=128))
        issued = [0]
        cast_eng = ['act', 'dve']
        LOOK = 5

        def issue_upto(n):
            while issued[0] < min(n, len(pieces)):
                i = issued[0]
                s_ = stg[i % 4]
                S.dma(s_[:, :, :], pieces[i])
                S.copy(wb[i % 8][:, :, :], s_[:, :, :], eng=cast_eng[i % 2])
                issued[0] += 1
        pc = [0]

        def next_piece():
            i = pc[0]
            issue_upto(i + 1 + LOOK)
            pc[0] += 1
            return wb[i % 8]
        ng = 0
        for e_ in range(16):
            for st in range(4):
                idx_ap = P.IDX[:, e_, st:st + 1]
                S.op('pool', lambda e, st=st, idx_ap=idx_ap: e.indirect_dma_start(
                    out=xs[:, st, :], out_offset=None, in_=D['HFd'][:, :],
                    in_offset=bass.IndirectOffsetOnAxis(ap=idx_ap, axis=0)),
                    [D['HFd'][:, :], idx_ap], [xs[:, st, :]], dma=True)
            for st in range(4):
                for k in range(8):
                    S.transpose(psT[:, k * 128:(k + 1) * 128], xs[:, st, k * 128:(k + 1) * 128], P.ident_bf[:, :])
                S.copy(xsT[:, :, st * 128:(st + 1) * 128], psT[:, :].rearrange("p (k t) -> p k t", k=8), eng=('act' if st % 2 else 'dve'))
            for fb in range(4):
                wg = next_piece()
                wu = next_piece()
                for fc in range(4):
                    pg = psG[ng % 2]
                    sgt = sg[ng % 2]
                    ng += 1
                    for k in range(8):
                        S.mm(pg[:, :], wg[:, k, fc * 128:(fc + 1) * 128], xsT[:, k, :], start=(k == 0), stop=(k == 7))
                    for k in range(8):
                        S.mm(psU[:, :], wu[:, k, fc * 128:(fc + 1) * 128], xsT[:, k, :], start=(k == 0), stop=(k == 7))
                    S.act(sgt[:, :], pg[:, :], AF.Silu)
                    S.tt(hidT[:, fb * 4 + fc, :], sgt[:, :], psU[:, :], ALU.mult)
            for dh in range(2):
                for fh in range(2):
                    wd = next_piece()
                    for st in range(4):
                        for f8 in range(8):
                            S.mm(psY[st][:, :], hidT[:, fh * 8 + f8, st * 128:(st + 1) * 128], wd[:, f8, :],
                                 start=(fh == 0 and f8 == 0), stop=(fh == 1 and f8 == 7))
                for st in range(4):
                    S.stt(ysb[st][:, dh * 512:(dh + 1) * 512], psY[st][:, :], P.GW[:, e_, st:st + 1],
                          P.gt2rep[:, dh * 512:(dh + 1) * 512], ALU.mult, ALU.mult)
            for st in range(4):
                idx_ap = P.IDX[:, e_, st:st + 1]
                S.op('pool', lambda e, st=st, idx_ap=idx_ap: e.indirect_dma_start(
                    out=D['acc'][:, :], out_offset=bass.IndirectOffsetOnAxis(ap=idx_ap, axis=0),
                    in_=ysb[st][:, :], in_offset=None, compute_op=ALU.add),
                    [ysb[st][:, :], idx_ap, D['acc'][:, :]], [D['acc'][:, :]], dma=True)


def phase_final(C):
    S, D, P, sb, pst = C.S, C.D, C.P, C.sb, C.pst
    with S.scope() as ph:
        gfin = sb("gfin", [128, DM], F32, ph)
        xb = [sb(f"fxb{i}", [128, DM], F32, ph) for i in range(2)]
        ob = [sb(f"fob{i}", [128, DM], F32, ph) for i in range(2)]
        junk = sb("fjunk", [128, DM], BF16, ph)
        sm = sb("fsm", [128, NT, 2], F32, ph)
        S.dma(gfin[:, :], D['gfin_row'][0:1, :].partition_broadcast(128))
        for j in range(NT):
            a = j % 2
            S.dma(xb[a][:, :], D['acc'][j * 128:(j + 1) * 128, :])
            ss = sm[:, j, 0:1]
            rs = sm[:, j, 1:2]
            S.act(junk[:, :], xb[a][:, :], AF.Square, accum_out=ss)
            S.act(rs, ss, AF.Ln, scale=1.0 / DM, bias=EPS)
            S.act(rs, rs, AF.Exp, scale=-0.5)
            S.stt(ob[a][:, :], xb[a][:, :], rs, gfin[:, :], ALU.mult, ALU.mult)
            S.dma(D['out'][j * 128:(j + 1) * 128, :], ob[a][:, :])


_PROG = {}


def kernel(**inputs):
    if 'nc' not in _PROG:
        _PROG['nc'] = build()[0]
    nc = _PROG['nc']
    B = inputs['x'].shape[0]
    in_maps = [layout_inputs(inputs, b) for b in range(B)]
    res = run_bass_kernel_spmd(nc, in_maps, core_ids=list(range(B)))
    out = np.stack([np.asarray(r["out"], dtype=np.float32) for r in res.results], axis=0)
    return out
```

```python
import math
import numpy as np
import ml_dtypes
import concourse.bass as bass
import concourse.mybir as mybir
from concourse.bass_utils import run_bass_kernel_spmd
from contextlib import ExitStack

F32 = mybir.dt.float32
BF16 = mybir.dt.bfloat16
I32 = mybir.dt.int32
AF = mybir.ActivationFunctionType
ALU = mybir.AluOpType
AX = mybir.AxisListType

SEQ = 4096
DM = 1024
NT = 32
EPS = 1e-6
EMIT_UNTIL = [None]
COMPUTE = ('pe', 'act', 'dve', 'pool')
SELF_SYNC = {'act': True, 'dve': True, 'pool': True, 'pe': False}
NDMA_SEMS = 6


def ap_box(ap):
    t = ap.tensor
    name = t.name
    dims = list(ap.ap)
    off = int(ap.offset)
    sp = str(ap.space() if callable(ap.space) else ap.space)
    is_dram = 'DRAM' in sp.upper() or 'HBM' in sp.upper() or type(t).__name__.startswith('DRAM') or type(t).__name__.startswith('Dram')
    if is_dram:
        lo = off
        hi = off
        for (st, cnt) in dims:
            st = int(st); cnt = int(cnt)
            if st >= 0:
                hi += st * (cnt - 1)
            else:
                lo += st * (cnt - 1)
        return (name, 0, 1, lo, hi + 1)
    if 'PSUM' in sp.upper() or type(t).__name__.startswith('PSum'):
        return (name, 0, 128, 0, 1 << 30)
    p0 = int(ap.start_partition())
    pc = int(dims[0][1])
    lo = off
    hi = off
    for (st, cnt) in dims[1:]:
        st = int(st); cnt = int(cnt)
        if st >= 0:
            hi += st * (cnt - 1)
        else:
            lo += st * (cnt - 1)
    return (name, p0, p0 + pc, lo, hi + 1)


class Op:
    __slots__ = ('stream', 'fn', 'deps', 'is_dma', 'signal', 'semval', 'dma_slot', 'idx', 'extra_waits')


class Sched:
    def __init__(self, nc, es):
        self.nc = nc
        self.es = es
        self.ops = []
        self.track = {}
        self.eng = {'pe': nc.tensor, 'act': nc.scalar, 'dve': nc.vector, 'pool': nc.gpsimd, 'sp': nc.sync}
        self.sem = {s: es.enter_context(nc.semaphore('sem_' + s)) for s in COMPUTE}
        self.dma_sems = {}
        for s in ('sp', 'pool', 'act'):
            self.dma_sems[s] = [es.enter_context(nc.semaphore(f'dsem_{s}{i}')) for i in range(NDMA_SEMS)]
        self.dma_count = {'sp': 0, 'pool': 0, 'act': 0}
        self.last_dma_ops = {'sp': [], 'pool': [], 'act': []}

    def _deps(self, boxes_r, boxes_w, idx, stream, is_dma):
        deps = set()
        for (boxes, is_w) in ((boxes_r, False), (boxes_w, True)):
            for b in boxes:
                lst = self.track.setdefault(b[0], [])
                keep = []
                for ent in lst:
                    eb, eidx, ew = ent
                    ov = not (eb[2] <= b[1] or b[2] <= eb[1] or eb[4] <= b[3] or b[4] <= eb[3])
                    if ov and (is_w or ew):
                        deps.add(eidx)
                    covered = (b[1] <= eb[1] and eb[2] <= b[2] and b[3] <= eb[3] and eb[4] <= b[4])
                    if is_w and covered:
                        continue
                    if (not is_w) and (not ew) and covered and (not is_dma):
                        eo = self.ops[eidx]
                        if eo.stream == stream and not eo.is_dma:
                            continue
                    keep.append(ent)
                keep.append([b, idx, is_w])
                self.track[b[0]] = keep
        deps.discard(idx)
        return deps

    def op(self, stream, fn, reads=(), writes=(), dma=False):
        o = Op()
        o.idx = len(self.ops)
        o.stream = stream
        o.fn = fn
        o.is_dma = dma
        o.signal = False
        o.semval = None
        o.dma_slot = None
        o.extra_waits = []
        br = [ap_box(a) for a in reads if a is not None and not isinstance(a, (int, float))]
        bw = [ap_box(a) for a in writes]
        self.ops.append(o)
        o.deps = self._deps(br, bw, o.idx, stream, dma)
        return o

    def emit(self):
        ops = self.ops
        for o in ops:
            for d in o.deps:
                po = ops[d]
                if po.is_dma:
                    continue
                if po.stream != o.stream or o.is_dma or SELF_SYNC.get(po.stream, True):
                    po.signal = True
        cnt = {s: 0 for s in COMPUTE}
        dcnt = {'sp': 0, 'pool': 0, 'act': 0}
        for o in ops:
            if o.is_dma:
                d = dcnt[o.stream]
                o.dma_slot = (d % NDMA_SEMS, 16 * (d // NDMA_SEMS + 1))
                dcnt[o.stream] = d + 1
            elif o.signal:
                cnt[o.stream] += 1
                o.semval = cnt[o.stream]
        waited = {s: {} for s in self.eng}
        nw = 0
        for o in ops:
            e = self.eng[o.stream]
            w = waited[o.stream]
            toks = []
            if o.is_dma:
                j, v = o.dma_slot
                if v > 16:
                    toks.append((('d', o.stream, j), self.dma_sems[o.stream][j], v - 16))
            for d in o.deps:
                po = ops[d]
                if po.is_dma:
                    j, v = po.dma_slot
                    toks.append((('d', po.stream, j), self.dma_sems[po.stream][j], v))
                else:
                    if po.stream == o.stream and not o.is_dma and not SELF_SYNC.get(po.stream, True):
                        continue
                    toks.append((('c', po.stream), self.sem[po.stream], po.semval))
            best = {}
            for key, sem, v in toks:
                if v is None:
                    raise RuntimeError('dep on non-signaling op')
                if w.get(key, 0) >= v:
                    continue
                if key not in best or best[key][1] < v:
                    best[key] = (sem, v)
            for key, (sem, v) in best.items():
                e.wait_ge(sem, v)
                w[key] = v
                nw += 1
            ins = o.fn(e)
            if ins is None:
                continue
            if o.is_dma:
                j, v = o.dma_slot
                ins.then_inc(self.dma_sems[o.stream][j], 16)
            elif o.signal:
                ins.then_inc(self.sem[o.stream], 1)
        self.n_waits = nw
        return cnt, dcnt

    def dma(self, out, in_, q='sp', **kw):
        return self.op(q, lambda e: e.dma_start(out=out, in_=in_, **kw), [in_], [out], dma=True)

    def mm(self, out, lhsT, rhs, start=True, stop=True, **kw):
        return self.op('pe', lambda e: e.matmul(out, lhsT, rhs, start=start, stop=stop, **kw),
                       [lhsT, rhs] + ([] if start else [out]), [out])

    def transpose(self, out, in_, ident):
        return self.op('pe', lambda e: e.transpose(out, in_, ident), [in_, ident], [out])

    def act(self, out, in_, func, scale=1.0, bias=0.0, accum_out=None, eng='act'):
        rd = [in_]
        if not isinstance(scale, (int, float)):
            rd.append(scale)
            if func == AF.Copy:
                func = AF.Identity
        if not isinstance(bias, (int, float)):
            rd.append(bias)
            if func == AF.Copy:
                func = AF.Identity
        wr = [out] + ([accum_out] if accum_out is not None else [])
        kw = {}
        if accum_out is not None:
            kw['accum_out'] = accum_out
        return self.op('act', lambda e: e.activation(out=out, in_=in_, func=func, scale=scale, bias=bias, **kw), rd, wr)

    def tt(self, out, in0, in1, op, eng='dve'):
        return self.op(eng, lambda e: e.tensor_tensor(out=out, in0=in0, in1=in1, op=op), [in0, in1], [out])

    def ts(self, out, in0, s1, op0, s2=None, op1=None, eng='dve', accum_out=None):
        rd = [in0]
        if not isinstance(s1, (int, float)):
            rd.append(s1)
        if s2 is not None and not isinstance(s2, (int, float)):
            rd.append(s2)
        kw = {}
        if op1 is not None:
            kw['op1'] = op1
        if accum_out is not None:
            kw['accum_out'] = accum_out
        wr = [out] + ([accum_out] if accum_out is not None else [])
        return self.op(eng, lambda e: e.tensor_scalar(out=out, in0=in0, scalar1=s1, scalar2=s2, op0=op0, **kw), rd, wr)

    def stt(self, out, in0, scalar, in1, op0, op1, eng='dve'):
        rd = [in0, in1]
        if not isinstance(scalar, (int, float)):
            rd.append(scalar)
        return self.op(eng, lambda e: e.scalar_tensor_tensor(out=out, in0=in0, scalar=scalar, in1=in1, op0=op0, op1=op1), rd, [out])

    def copy(self, out, in_, eng='dve'):
        if eng == 'act':
            return self.act(out, in_, AF.Copy)
        return self.op(eng, lambda e: e.tensor_copy(out=out, in_=in_), [in_], [out])

    def memset(self, out, val, eng='dve'):
        return self.op(eng, lambda e: e.memset(out, val), [], [out])

    def reduce(self, out, in_, op, axis=None, eng='dve'):
        axis = axis or AX.X
        return self.op(eng, lambda e: e.tensor_reduce(out=out, in_=in_, op=op, axis=axis), [in_], [out])

    def recip(self, out, in_, eng='dve'):
        return self.op(eng, lambda e: e.reciprocal(out=out, in_=in_), [in_], [out])

    def barrier(self):
        alld = set()
        for lst in self.track.values():
            for ent in lst:
                alld.add(ent[1])
        self.track = {}
        for s_ in ('pe', 'act', 'dve', 'pool', 'sp'):
            o = self.op(s_, lambda e: None, [], [])
            o.deps = set(alld)

    def scope(self):
        return _Scope(self)

    def finish(self, out_aps):
        boxes = [ap_box(a) for a in out_aps]
        o = self.op('sp', lambda e: None, list(out_aps), [])
        return o


class _Scope:
    def __init__(self, S):
        self.S = S
        self.st = ExitStack()

    def __enter__(self):
        self.st.__enter__()
        return self.st

    def __exit__(self, *a):
        self.S.barrier()
        return self.st.__exit__(*a)

_CONSTS = {}


def _bf(a):
    return np.ascontiguousarray(a.astype(np.float32)).astype(ml_dtypes.bfloat16)


def host_consts():
    if _CONSTS:
        return _CONSTS
    c = {}
    c['ident_bf'] = _bf(np.eye(128))
    c['ident_f'] = np.eye(128, dtype=np.float32)
    s = np.arange(128)[:, None]
    t = np.arange(128)[None, :]
    c['triU'] = (s <= t).astype(np.float32)
    c['triL'] = (s >= t).astype(np.float32)
    c['striU'] = (s < t).astype(np.float32)
    N = 8192
    n1 = np.arange(64); k1 = np.arange(64); n2 = np.arange(128); k2 = np.arange(128)
    ang = 2 * np.pi * np.outer(n1, k1) / 64
    c['F1'] = _bf(np.concatenate([np.cos(ang), -np.sin(ang)], 1))
    th = 2 * np.pi * ((n2[:, None, None] * (k1[None, :, None] + 64 * k2[None, None, :])) % N) / N
    c['GrT'] = _bf(np.cos(th).reshape(128, 64 * 128))
    c['GiT'] = _bf((-np.sin(th)).reshape(128, 64 * 128))
    c['GiNT'] = _bf((np.sin(th)).reshape(128, 64 * 128))
    ph = 2 * np.pi * np.outer(k2, n2) / 128
    Rr = np.cos(ph); Ri = np.sin(ph)
    c['Rc1'] = _bf(np.concatenate([Rr, Ri], 1))
    c['Rc2'] = _bf(np.concatenate([-Ri, Rr], 1))
    n1h = np.arange(32)
    thL = 2 * np.pi * (k1[:, None, None] * n1h[None, None, :] / 64 + k1[:, None, None] * n2[None, :, None] / N)
    c['LrT'] = _bf((np.cos(thL) / N).reshape(64, 128 * 32))
    c['LiNT'] = _bf((-np.sin(thL) / N).reshape(64, 128 * 32))
    L = SEQ
    f32 = np.float32
    tt = np.linspace(0.0, 1.0, L, dtype=f32)[:, None]
    w = (f32(2.0 * math.pi) * np.arange(L, dtype=f32)[:, None] / f32(L)).astype(f32)
    bands = np.linspace(1e-4, 15, 16, dtype=f32)[None, :]
    z = np.concatenate([tt, np.cos(bands * w), -np.sin(bands * w)], axis=-1).astype(f32)
    zr = np.concatenate([z[0:1], z[:0:-1]], 0)
    c['zT'] = np.ascontiguousarray(z.T)
    c['zrT'] = np.ascontiguousarray(zr.T)
    trow = tt[:, 0]
    trr = np.concatenate([trow[0:1], trow[:0:-1]])
    c['t_row'] = np.ascontiguousarray(trow[None, :]).astype(f32)
    c['tr_row'] = np.ascontiguousarray(trr[None, :]).astype(f32)
    _CONSTS.update(c)
    return _CONSTS


def colmaj(v, nk):
    return np.ascontiguousarray(np.asarray(v, dtype=np.float32).reshape(nk, 128).T)


def layout_inputs(inp, b):
    m = {}
    f = lambda a: np.ascontiguousarray(np.asarray(a, dtype=np.float32))
    m['x'] = f(inp['x'][b])
    m['ccol'] = colmaj(inp['c'][b], 8)
    m['w_ada'] = f(inp['w_ada'][0])
    m['b_ada'] = f(inp['b_ada'][0][None, :])
    m['gmix_col'] = colmaj(inp['g_mix'][0], 8)
    m['w_in'] = f(inp['w_in'][0])
    bin_ = np.asarray(inp['b_in'][0], dtype=np.float32)
    m['bin_row'] = f(bin_[None, 1024:2064])
    m['bqk_col'] = colmaj(bin_[0:1024], 8)
    m['bhy_col'] = colmaj(bin_[2064:3600], 12)
    cw = np.asarray(inp['conv_qk_w'][0], dtype=np.float32)
    m['cqk_w'] = np.ascontiguousarray(cw.reshape(3, 8, 128).transpose(2, 1, 0))
    m['cqk_b'] = colmaj(inp['conv_qk_b'][0], 8)
    cw = np.asarray(inp['conv_hy_w'][0], dtype=np.float32)
    m['chy_w'] = np.ascontiguousarray(cw.reshape(3, 12, 128).transpose(2, 1, 0))
    m['chy_b'] = colmaj(inp['conv_hy_b'][0], 12)
    m['mnorm_g'] = f(inp['mlstm_norm_g'][0][None, :])
    m['hy_w1'] = f(inp['hy_w1'][0])
    m['hy_b1c'] = f(inp['hy_b1'][0][:, None])
    m['hy_w2'] = f(inp['hy_w2'][0])
    m['hy_b2c'] = f(inp['hy_b2'][0][:, None])
    m['hy_w3'] = f(inp['hy_w3'][0])
    m['hy_frc'] = f(inp['hy_freq'][0][:, None])
    m['hy_del_col'] = colmaj(inp['hy_deltas'][0], 16)
    m['hy_bias_col'] = colmaj(np.asarray(inp['hy_bias'][0]).reshape(-1), 8)
    m['hnorm_col'] = colmaj(inp['hyena_norm_g'][0], 4)
    m['w_out'] = f(inp['w_out'][0])
    m['gffn_row'] = f(inp['g_ffn'][0][None, :])
    m['w_router'] = f(inp['w_router'][0])
    m['w_gate'] = f(inp['w_gate'][0]).reshape(16 * 1024, 2048)
    m['w_up'] = f(inp['w_up'][0]).reshape(16 * 1024, 2048)
    m['w_down'] = f(inp['w_down'][0]).reshape(16 * 2048, 1024)
    m['gfin_row'] = f(np.asarray(inp['g_final'])[None, :])
    m.update(host_consts())
    return m

INPUT_SPECS = [
    ('x', [SEQ, DM], F32), ('ccol', [128, 8], F32), ('w_ada', [DM, 6144], F32), ('b_ada', [1, 6144], F32),
    ('gmix_col', [128, 8], F32), ('w_in', [DM, 3600], F32), ('bin_row', [1, 1040], F32),
    ('bqk_col', [128, 8], F32), ('bhy_col', [128, 12], F32), ('cqk_w', [128, 8, 3], F32), ('cqk_b', [128, 8], F32),
    ('chy_w', [128, 12, 3], F32), ('chy_b', [128, 12], F32), ('mnorm_g', [1, 512], F32),
    ('hy_w1', [33, 64], F32), ('hy_b1c', [64, 1], F32), ('hy_w2', [64, 64], F32), ('hy_b2c', [64, 1], F32),
    ('hy_w3', [64, 2048], F32), ('hy_frc', [64, 1], F32), ('hy_del_col', [128, 16], F32),
    ('hy_bias_col', [128, 8], F32), ('hnorm_col', [128, 4], F32), ('w_out', [DM, DM], F32),
    ('gffn_row', [1, DM], F32), ('w_router', [DM, 16], F32), ('w_gate', [16 * 1024, 2048], F32),
    ('w_up', [16 * 1024, 2048], F32), ('w_down', [16 * 2048, 1024], F32), ('gfin_row', [1, DM], F32),
    ('ident_bf', [128, 128], BF16), ('ident_f', [128, 128], F32), ('triU', [128, 128], F32),
    ('triL', [128, 128], F32), ('striU', [128, 128], F32), ('F1', [64, 128], BF16),
    ('GrT', [128, 8192], BF16), ('GiT', [128, 8192], BF16), ('GiNT', [128, 8192], BF16),
    ('Rc1', [128, 256], BF16), ('Rc2', [128, 256], BF16), ('LrT', [64, 4096], BF16), ('LiNT', [64, 4096], BF16),
    ('zT', [33, SEQ], F32), ('zrT', [33, SEQ], F32), ('t_row', [1, SEQ], F32), ('tr_row', [1, SEQ], F32),
]


class Ctx:
    pass


def build(stop_after=None, debug=False):
    nc = bass.Bass("TRN2", target_bir_lowering=False)
    es = ExitStack()
    S = Sched(nc, es)
    C = Ctx()
    C.nc, C.S, C.es = nc, S, es
    C.debug = debug
    D = {}
    for name, shape, dt in INPUT_SPECS:
        D[name] = nc.dram_tensor(name, shape, dt, kind="ExternalInput").ap()
    D['out'] = nc.dram_tensor("out", [SEQ, DM], F32, kind="ExternalOutput").ap()

    def scratch(name, shape, dt):
        D[name] = nc.dram_tensor(name, shape, dt, kind="Internal").ap()
    scratch('QTd', [512, SEQ], BF16)
    scratch('KTd', [512, SEQ], BF16)
    scratch('Vd', [SEQ, 512], BF16)
    scratch('Od', [SEQ, 512], BF16)
    scratch('X1d', [512, SEQ], F32)
    scratch('X2d', [512, SEQ], F32)
    scratch('Zd', [512, SEQ], BF16)
    scratch('Z1d', [512, SEQ], BF16)
    scratch('Kd0', [512, 2 * SEQ], BF16)
    scratch('Kd1', [512, 2 * SEQ], BF16)
    scratch('Ycv', [512, SEQ], BF16)
    scratch('Yt', [DM, SEQ], BF16)
    scratch('HFd', [SEQ, DM], BF16)
    scratch('acc', [SEQ, DM], F32)
    C.D = D
    C.dbg = {}

    def dbg_out(name, shape, dt=F32):
        t = nc.dram_tensor("dbg_" + name, shape, dt, kind="ExternalOutput").ap()
        C.dbg[name] = t
        return t
    C.dbg_out = dbg_out

    cnt = [0]

    def sb(name, shape, dt, st=None):
        cnt[0] += 1
        return (st or es).enter_context(nc.sbuf_tensor(f"s{cnt[0]}_{name}", shape, dt))

    def pst(name, shape, dt, st=None):
        cnt[0] += 1
        return (st or es).enter_context(nc.psum_tensor(f"p{cnt[0]}_{name}", shape, dt))
    C.sb, C.pst = sb, pst

    P = Ctx()
    C.P = P
    P.ident_bf = sb("ident_bf", [128, 128], BF16)
    P.ident_f = sb("ident_f", [128, 128], F32)
    P.ones_f = sb("ones_f", [128, 128], F32)
    P.modcol = sb("modcol", [128, 48], F32)
    P.A1col = sb("A1col", [128, 8], F32)
    P.gt1rep = sb("gt1rep", [128, DM], F32)
    P.gt2rep = sb("gt2rep", [128, DM], F32)
    P.A2rep = sb("A2rep", [128, DM], F32)
    P.B2rep = sb("B2rep", [128, DM], F32)
    S.dma(P.ident_bf[:, :], D['ident_bf'][:, :])
    S.dma(P.ident_f[:, :], D['ident_f'][:, :])
    S.memset(P.ones_f[:, :], 1.0)

    phases = [phase_mod, phase_norm_proj, phase_mlstm, phase_hyena, phase_outproj, phase_route, phase_experts, phase_final]
    for ph in phases:
        ph(C)
        if stop_after == ph.__name__:
            break
    outs = [D['out'][:, :]] + [t for t in C.dbg.values()]
    S.finish(outs)
    S.emit()
    return nc, C


def phase_mod(C):
    S, D, P, sb, pst = C.S, C.D, C.P, C.sb, C.pst
    with S.scope() as ph:
        wada = [sb(f"wada{i}", [128, 8, 512], F32, ph) for i in range(2)]
        modrow = sb("modrow", [1, 6144], F32, ph)
        badar = sb("badar", [1, 6144], F32, ph)
        ccol = sb("ccol_sb", [128, 8], F32, ph)
        gmixc = sb("gmixc", [128, 8], F32, ph)
        gffn_rep = sb("gffn_rep", [128, DM], F32, ph)
        sc2rep = sb("sc2rep", [128, DM], F32, ph)
        ps = pst("ps_mod", [128, 512], F32, ph)
        psc = pst("ps_modc", [128, 512], F32, ph)
        psr = [pst(f"ps_modr{i}", [128, 512], F32, ph) for i in range(2)]
        S.dma(badar[:, :], D['b_ada'][:, :])
        S.dma(ccol[:, :], D['ccol'][:, :])
        S.dma(gmixc[:, :], D['gmix_col'][:, :])
        S.dma(gffn_rep[:, :], D['gffn_row'][0:1, :].partition_broadcast(128))
        wsrc = D['w_ada'].rearrange("(k p) c -> p k c", p=128)
        for blk in range(12):
            buf = wada[blk % 2]
            S.dma(buf[:, :, :], wsrc[:, :, blk * 512:(blk + 1) * 512])
            for k in range(8):
                S.mm(ps[0:1, :], ccol[:, k:k + 1], buf[:, k, :], start=(k == 0), stop=(k == 7))
            S.tt(modrow[0:1, blk * 512:(blk + 1) * 512], ps[0:1, :], badar[0:1, blk * 512:(blk + 1) * 512], ALU.add)
        for oc in range(48):
            S.mm(psc[:, oc:oc + 1], modrow[0:1, oc * 128:(oc + 1) * 128], P.ones_f[0:1, 0:1])
        S.copy(P.modcol[:, :], psc[:, 0:48])
        S.stt(P.A1col[:, :], P.modcol[:, 8:16], 1.0, gmixc[:, :], ALU.add, ALU.mult)
        n = 0
        for (dst, j) in ((P.gt1rep, 2), (P.gt2rep, 5), (sc2rep, 4), (P.B2rep, 3)):
            for h in range(2):
                pr = psr[n % 2]
                n += 1
                S.mm(pr[:, :], P.ones_f[0:1, 0:128], modrow[0:1, j * 1024 + h * 512: j * 1024 + (h + 1) * 512])
                S.copy(dst[:, h * 512:(h + 1) * 512], pr[:, :], eng=('act' if n % 2 else 'dve'))
        S.stt(P.A2rep[:, :], sc2rep[:, :], 1.0, gffn_rep[:, :], ALU.add, ALU.mult)
        if C.debug:
            d = C.dbg_out('modcol', [128, 48])
            S.dma(d[:, :], P.modcol[:, :])
            d = C.dbg_out('gt1rep', [128, DM])
            S.dma(d[:, :], P.gt1rep[:, :])


def phase_norm_proj(C):
    S, D, P, sb, pst = C.S, C.D, C.P, C.sb, C.pst
    P.Gt = sb("Gt", [128, NT, 16], F32)
    with S.scope() as ph:
        hT = sb("hT", [128, 8, SEQ], BF16, ph)
        with S.scope() as p1:
            xb = [sb(f"xb{i}", [128, DM], F32, p1) for i in range(2)]
            xn = [sb(f"xn{i}", [128, DM], BF16, p1) for i in range(2)]
            junk = sb("junk", [128, DM], BF16, p1)
            ss = sb("ss", [128, NT], F32, p1)
            rs = sb("rs", [128, NT], F32, p1)
            psT = [pst(f"psT{i}", [128, DM], BF16, p1) for i in range(2)]
            for j in range(NT):
                xt = xb[j % 2]
                S.dma(xt[:, :], D['x'][j * 128:(j + 1) * 128, :])
                S.act(junk[:, :], xt[:, :], AF.Square, accum_out=ss[:, j:j + 1])
                S.act(rs[:, j:j + 1], ss[:, j:j + 1], AF.Ln, scale=1.0 / DM, bias=EPS)
                S.act(rs[:, j:j + 1], rs[:, j:j + 1], AF.Exp, scale=-0.5)
                S.act(xn[j % 2][:, :], xt[:, :], AF.Copy, scale=rs[:, j:j + 1])
                pt = psT[j % 2]
                for k in range(8):
                    S.transpose(pt[:, k * 128:(k + 1) * 128], xn[j % 2][:, k * 128:(k + 1) * 128], P.ident_bf[:, :])
                for k in range(8):
                    S.act(hT[:, k, j * 128:(j + 1) * 128], pt[:, k * 128:(k + 1) * 128], AF.Identity,
                          scale=P.A1col[:, k:k + 1], bias=P.modcol[:, k:k + 1])
        if C.debug:
            d = C.dbg_out('hT', [128, 8, SEQ], BF16)
            for k in range(8):
                S.dma(d[:, k, :], hT[:, k, :])
        with S.scope() as p2:
            wvo = sb("wvo", [128, 8, 1040], BF16, p2)
            brow = sb("brow", [128, 1040], F32, p2)
            vt = [sb(f"vt{i}", [128, 512], BF16, p2) for i in range(2)]
            ot = [sb(f"ot{i}", [128, 512], BF16, p2) for i in range(2)]
            otf = [sb(f"otf{i}", [128, 512], F32, p2) for i in range(2)]
            psV = [pst(f"psV{i}", [128, 512], F32, p2) for i in range(2)]
            psO = [pst(f"psO{i}", [128, 512], F32, p2) for i in range(2)]
            psG = [pst(f"psG{i}", [128, 512], F32, p2) for i in range(2)]
            wsrc = D['w_in'].rearrange("(k p) c -> p k c", p=128)
            S.dma(wvo[:, :, 0:512], wsrc[:, :, 1024:1536], q='pool')
            S.dma(wvo[:, :, 512:1040], wsrc[:, :, 1536:2064], q='pool')
            S.dma(brow[:, :], D['bin_row'][0:1, :].partition_broadcast(128))
            for j in range(NT):
                a = j % 2
                for (pp, c0, c1) in ((psV[a], 0, 512), (psO[a], 512, 1024), (psG[a], 1024, 1040)):
                    for k in range(8):
                        S.mm(pp[:, 0:c1 - c0], hT[:, k, j * 128:(j + 1) * 128], wvo[:, k, c0:c1], start=(k == 0), stop=(k == 7))
                S.tt(vt[a][:, :], psV[a][:, :], brow[:, 0:512], ALU.add)
                S.dma(D['Vd'][j * 128:(j + 1) * 128, :], vt[a][:, :])
                S.tt(otf[a][:, :], psO[a][:, :], brow[:, 512:1024], ALU.add)
                S.act(ot[a][:, :], otf[a][:, :], AF.Sigmoid)
                S.dma(D['Od'][j * 128:(j + 1) * 128, :], ot[a][:, :])
                S.tt(P.Gt[:, j, :], psG[a][:, 0:16], brow[:, 1024:1040], ALU.add)
        if C.debug:
            d = C.dbg_out('Gt', [128, NT, 16])
            S.dma(d[:, :, :], P.Gt[:, :, :])
        with S.scope() as p3:
            wch = [sb(f"wch{i}", [128, 8, 128], BF16, p3) for i in range(3)]
            pre = [sb(f"pre{i}", [128, SEQ + 2], F32, p3) for i in range(2)]
            t0s = [sb(f"cv_t0{i}", [128, 2048], F32, p3) for i in range(2)]
            t2s = [sb(f"cv_t2{i}", [128, 2048], F32, p3) for i in range(2)]
            t3 = [[sb(f"cv_t3{q}{i}", [128, 2048], F32, p3) for i in range(2)] for q in range(2)]
            ob = [[sb(f"cv_ob{q}{i}", [128, 2048], BF16, p3) for i in range(2)] for q in range(2)]
            pending = [None]
            bqk = sb("bqk", [128, 8], F32, p3)
            bhy = sb("bhy", [128, 12], F32, p3)
            cqw = sb("cqw", [128, 8, 3], F32, p3)
            cqb = sb("cqb", [128, 8], F32, p3)
            chw = sb("chw", [128, 12, 3], F32, p3)
            chb = sb("chb", [128, 12], F32, p3)
            psF = [pst(f"psF{i}", [128, 512], F32, p3) for i in range(8)]
            for (tile_, nm) in ((bqk, 'bqk_col'), (bhy, 'bhy_col'), (cqb, 'cqk_b'), (chb, 'chy_b')):
                S.dma(tile_[:, :], D[nm][:, :])
            S.dma(cqw[:, :, :], D['cqk_w'][:, :, :])
            S.dma(chw[:, :, :], D['chy_w'][:, :, :])
            for i in range(2):
                S.memset(pre[i][:, 0:1], 0.0)
                S.memset(pre[i][:, SEQ + 1:SEQ + 2], 0.0)
            wsrc = D['w_in'].rearrange("(k p) c -> p k c", p=128)
            chunks = []
            for cc in range(8):
                chunks.append(('qk', cc, cc * 128))
            for i in range(12):
                chunks.append(('hy', i, 2064 + i * 128))
            nps = 0
            for m_ in range(2):
                S.dma(wch[m_ % 3][:, :, :], wsrc[:, :, chunks[m_][2]:chunks[m_][2] + 128], q='pool')
            for n, (kind, ci, col0) in enumerate(chunks):
                wc = wch[n % 3]
                pr = pre[n % 2]
                if n + 2 < len(chunks):
                    S.dma(wch[(n + 2) % 3][:, :, :], wsrc[:, :, chunks[n + 2][2]:chunks[n + 2][2] + 128], q='pool')
                bcol = bqk[:, ci:ci + 1] if kind == 'qk' else bhy[:, ci:ci + 1]
                cw = cqw if kind == 'qk' else chw
                cb = cqb if kind == 'qk' else chb
                for tb in range(8):
                    pp = psF[nps % 8]
                    nps += 1
                    for k in range(8):
                        S.mm(pp[:, :], wc[:, k, :], hT[:, k, tb * 512:(tb + 1) * 512], start=(k == 0), stop=(k == 7))
                    if tb % 2 == 0:
                        S.act(pr[:, 1 + tb * 512:1 + (tb + 1) * 512], pp[:, :], AF.Identity, bias=bcol)
                    else:
                        S.ts(pr[:, 1 + tb * 512:1 + (tb + 1) * 512], pp[:, :], bcol, ALU.add)
                par = n % 2
                for hh in range(2):
                    o0 = hh * 2048
                    S.act(t0s[hh][:, :], pr[:, o0:o0 + 2048], AF.Identity, scale=cw[:, ci, 0:1], bias=cb[:, ci:ci + 1])
                    S.act(t2s[hh][:, :], pr[:, o0 + 2:o0 + 2050], AF.Identity, scale=cw[:, ci, 2:3])
                for hh in range(2):
                    o0 = hh * 2048
                    S.stt(t0s[hh][:, :], pr[:, o0 + 1:o0 + 2049], cw[:, ci, 1:2], t0s[hh][:, :], ALU.mult, ALU.add)
                for hh in range(2):
                    if kind == 'hy' and ci >= 8:
                        S.tt(ob[par][hh][:, :], t0s[hh][:, :], t2s[hh][:, :], ALU.add, eng='pool')
                    else:
                        S.tt(t3[par][hh][:, :], t0s[hh][:, :], t2s[hh][:, :], ALU.add, eng='pool')
                if pending[0] is not None:
                    pending[0]()

                def fin(kind=kind, ci=ci, par=par):
                    for hh in range(2):
                        o0 = hh * 2048
                        if kind == 'qk':
                            S.act(ob[par][hh][:, :], t3[par][hh][:, :], AF.Silu)
                            dst = D['QTd'] if ci < 4 else D['KTd']
                            S.dma(dst[(ci % 4) * 128:(ci % 4 + 1) * 128, o0:o0 + 2048], ob[par][hh][:, :])
                        elif ci < 8:
                            dst = D['X1d'] if ci < 4 else D['X2d']
                            S.dma(dst[(ci % 4) * 128:(ci % 4 + 1) * 128, o0:o0 + 2048], t3[par][hh][:, :])
                        else:
                            S.dma(D['Zd'][(ci - 8) * 128:(ci - 7) * 128, o0:o0 + 2048], ob[par][hh][:, :])
                pending[0] = fin
            pending[0]()
    if C.debug:
        for nm, shp, dt in (('QTd', [512, SEQ], BF16), ('KTd', [512, SEQ], BF16), ('Vd', [SEQ, 512], BF16),
                            ('Od', [SEQ, 512], BF16), ('X1d', [512, SEQ], F32), ('Zd', [512, SEQ], BF16)):
            d = C.dbg_out(nm, shp, dt)
            with S.scope() as pd:
                if shp[0] == 512:
                    tmp = sb("dbgtmp_" + nm, [128, 4, SEQ], dt, pd)
                    S.dma(tmp[:, :, :], D[nm].rearrange("(a p) n -> p a n", p=128))
                    S.dma(d.rearrange("(a p) n -> p a n", p=128), tmp[:, :, :])
                else:
                    tmp = sb("dbgtmp_" + nm, [128, NT, 512], dt, pd)
                    S.dma(tmp[:, :, :], D[nm].rearrange("(a p) n -> p a n", p=128))
                    S.dma(d.rearrange("(a p) n -> p a n", p=128), tmp[:, :, :])


def phase_mlstm(C):
    S, D, P, sb, pst = C.S, C.D, C.P, C.sb, C.pst
    Gt = P.Gt
    with S.scope() as ph:
        triU = sb("triU", [128, 128], F32, ph)
        triL = sb("triL", [128, 128], F32, ph)
        LF = sb("LF", [128, 2, NT, 4], F32, ph)
        II = sb("II", [128, 2, NT, 4], F32, ph)
        Bc = sb("Bc", [128, 2, NT, 4], F32, ph)
        Wp = sb("Wp", [128, 2, NT, 4], F32, ph)
        ENB = sb("ENB", [128, 2, NT, 4], F32, ph)
        EG = sb("EG", [128, 2, NT, 4], F32, ph)
        zcol = sb("zcol", [128, 1], F32, ph)
        S.dma(triU[:, :], D['triU'][:, :])
        S.dma(triL[:, :], D['triL'][:, :])
        S.memset(zcol[:, :], 0.0)
        with S.scope() as pp:
            psB = pst("psB", [128, 512], F32, pp)
            psGs = pst("psGs", [128, 512], F32, pp)
            for d in range(2):
                S.act(LF[:, d, :, :], Gt[:, :, d * 8 + 4:d * 8 + 8], AF.Exp, scale=-1.0)
                S.copy(II[:, d, :, :], Gt[:, :, d * 8:d * 8 + 4])
            fl = lambda t: t[:, :, :, :].rearrange("p d c e -> p (d c e)")
            S.act(fl(LF), fl(LF), AF.Ln, bias=1.0)
            S.ts(fl(LF), fl(LF), -1.0, ALU.mult)
            S.mm(psB[:, 0:128], triU[:, :], fl(LF)[:, 0:128])
            S.mm(psB[:, 128:256], triL[:, :], fl(LF)[:, 128:256])
            S.mm(psGs[:, 0:256], P.ones_f[:, :], fl(LF))
            S.copy(fl(Bc), psB[:, 0:256])
            S.act(fl(EG), psGs[:, 0:256], AF.Exp)
            S.tt(fl(Wp), fl(II), fl(Bc), ALU.subtract)
            S.act(fl(Wp), fl(Wp), AF.Exp, bias=math.log(128.0 ** -0.5))
            S.act(fl(ENB), fl(Bc), AF.Exp, scale=-1.0)
        if C.debug:
            for nm, t in (('Bc', Bc), ('Wp', Wp), ('EG', EG)):
                d_ = C.dbg_out(nm, [128, 2, NT, 4])
                S.dma(d_[:, :, :, :], t[:, :, :, :])
        for hd in range(4):
            with S.scope() as hs:
                qT = sb("qT", [128, SEQ], BF16, hs)
                kT = sb("kT", [128, SEQ], BF16, hs)
                vaug = sb("vaug", [128, NT, 129], BF16, hs)
                osg = sb("osg", [128, NT, 128], BF16, hs)
                ktm = sb("ktm", [128, NT, 128], BF16, hs)
                Hs = sb("Hs", [128, NT, 128], F32, hs)
                sq = sb("sq", [128, NT, 128], F32, hs)
                gO = sb("gO", [128, NT, 128], BF16, hs)
                ym = sb("ym", [128, NT, 128], BF16, hs)
                ymT = sb("ymT", [128, SEQ], BF16, hs)
                mng = sb("mng", [128, 128], F32, hs)
                STw = [sb(f"STw{i}", [128, 128], BF16, hs) for i in range(4)]
                vw = [sb(f"vw{i}", [128, 129], BF16, hs) for i in range(4)]
                Tst = sb("Tst", [128, 129], F32, hs)
                Tst2 = sb("Tst2", [128, 129], F32, hs)
                Cbfd = [[sb(f"Cbf{d_}{i}", [128, 129], BF16, hs) for i in range(2)] for d_ in range(2)]
                sm = [sb(f"sm{i}", [128, 4], F32, hs) for i in range(2)]
                ssq = sb("ssq", [128, NT], F32, hs)
                pS = [pst(f"pS{i}", [128, 512], F32, hs) for i in range(2)]
                pO = [pst(f"pO{i}", [128, 512], F32, hs) for i in range(2)]
                pC = [pst(f"pC{i}", [128, 512], F32, hs) for i in range(2)]
                pK = pst("pK", [128, 1024], BF16, hs)
                S.dma(qT[:, :], D['QTd'][hd * 128:(hd + 1) * 128, :])
                S.dma(kT[:, :], D['KTd'][hd * 128:(hd + 1) * 128, :])
                S.dma(vaug[:, :, 0:128], D['Vd'][:, hd * 128:(hd + 1) * 128].rearrange("(c p) d -> p c d", p=128))
                S.memset(vaug[:, :, 128:129], 1.0)
                S.dma(osg[:, :, :], D['Od'][:, hd * 128:(hd + 1) * 128].rearrange("(c p) d -> p c d", p=128))
                S.dma(mng[:, :], D['mnorm_g'][0:1, hd * 128:(hd + 1) * 128].partition_broadcast(128))
                for c0 in range(0, NT, 8):
                    for c in range(c0, c0 + 8):
                        S.transpose(pK[:, (c - c0) * 128:(c - c0 + 1) * 128], kT[:, c * 128:(c + 1) * 128], P.ident_bf[:, :])
                    S.copy(ktm[:, c0:c0 + 8, :].rearrange("p c d -> p (c d)"), pK[:, :], eng=('act' if (c0 // 8) % 2 else 'dve'))
                n = 0
                Tsts = [Tst, Tst2]
                prevc = [None, None]
                visited = set()
                for d in range(2):
                    S.memset(Tsts[d][:, :], 0.0)
                    S.memset(Cbfd[d][0][:, :], 0.0)
                nd = [0, 0]
                steps = []
                for i in range(NT):
                    for d in range(2):
                        steps.append((d, i if d == 0 else NT - 1 - i))

                def phase1(n):
                    d, c = steps[n]
                    cs = slice(c * 128, (c + 1) * 128)
                    wcol = Wp[:, d, c, hd:hd + 1]
                    mask = triU if d == 0 else triL
                    S.mm(pS[n % 2][:, 0:128], kT[:, cs], qT[:, cs])
                    S.stt(STw[n % 4][:, :], pS[n % 2][:, 0:128], wcol, mask[:, :], ALU.mult, ALU.mult)
                    S.act(vw[n % 4][:, :], vaug[:, c, :], AF.Copy, scale=wcol)

                def phase2(n):
                    d, c = steps[n]
                    a = n % 2
                    b_ = nd[d] % 2
                    nd[d] += 1
                    cs = slice(c * 128, (c + 1) * 128)
                    S.mm(pO[a][:, 0:129], STw[n % 4][:, :], vaug[:, c, :], start=True, stop=False)
                    S.mm(pO[a][:, 0:129], qT[:, cs], Cbfd[d][b_][:, :], start=False, stop=True)
                    S.mm(pC[a][:, 0:129], ktm[:, c, :], vw[n % 4][:, :])
                    enb = ENB[:, d, c, hd:hd + 1]
                    s_ = sm[a]
                    S.ts(s_[:, 0:1], pO[a][:, 128:129], enb, ALU.max)
                    S.stt(s_[:, 2:3], pO[a][:, 128:129], -1.0, s_[:, 0:1], ALU.mult, ALU.max)
                    S.recip(s_[:, 3:4], s_[:, 2:3])
                    if c not in visited:
                        visited.add(c)
                        S.act(Hs[:, c, :], pO[a][:, 0:128], AF.Copy, scale=s_[:, 3:4])
                    else:
                        S.stt(Hs[:, c, :], pO[a][:, 0:128], s_[:, 3:4], Hs[:, c, :], ALU.mult, ALU.add)
                    egp = zcol[:, 0:1] if prevc[d] is None else EG[:, d, prevc[d], hd:hd + 1]
                    S.stt(Tsts[d][:, :], Tsts[d][:, :], egp, pC[a][:, 0:129], ALU.mult, ALU.add)
                    S.act(Cbfd[d][(b_ + 1) % 2][:, :], Tsts[d][:, :], AF.Copy, scale=EG[:, d, c, hd:hd + 1])
                    prevc[d] = c
                phase1(0)
                for n_ in range(len(steps)):
                    if n_ + 1 < len(steps):
                        phase1(n_ + 1)
                    phase2(n_)
                S.act(sq[:, :, :], Hs[:, :, :], AF.Square)
                S.reduce(ssq[:, :], sq[:, :, :], ALU.add)
                S.ts(ssq[:, :], ssq[:, :], 1.0 / 128.0, ALU.mult, s2=EPS, op1=ALU.add)
                S.act(ssq[:, :], ssq[:, :], AF.Sqrt)
                S.recip(ssq[:, :], ssq[:, :])
                S.tt(gO[:, :, :], osg[:, :, :], mng[:, :].unsqueeze(1).to_broadcast([128, NT, 128]), ALU.mult)
                for c in range(NT):
                    S.stt(ym[:, c, :], Hs[:, c, :], ssq[:, c:c + 1], gO[:, c, :], ALU.mult, ALU.mult)
                for c0 in range(0, NT, 8):
                    for c in range(c0, c0 + 8):
                        S.transpose(pK[:, (c - c0) * 128:(c - c0 + 1) * 128], ym[:, c, :], P.ident_bf[:, :])
                    S.copy(ymT[:, c0 * 128:(c0 + 8) * 128], pK[:, :], eng=('act' if (c0 // 8) % 2 else 'dve'))
                S.dma(D['Yt'][hd * 128:(hd + 1) * 128, :], ymT[:, :])
    if C.debug:
        d_ = C.dbg_out('Yt_m', [512, SEQ], BF16)
        with S.scope() as pd:
            tmp = sb("dbgtmp_ytm", [128, 4, SEQ], BF16, pd)
            S.dma(tmp[:, :, :], D['Yt'][0:512, :].rearrange("(a p) n -> p a n", p=128))
            S.dma(d_.rearrange("(a p) n -> p a n", p=128), tmp[:, :, :])


def phase_hyena(C):
    S, D, P, sb, pst = C.S, C.D, C.P, C.sb, C.pst
    PI = math.pi
    with S.scope() as pa:
        hid2T = sb("hid2T", [64, SEQ], F32, pa)
        hid2rT = sb("hid2rT", [64, SEQ], F32, pa)
        frc = sb("frc", [64, 1], F32, pa)
        frb1 = sb("frb1", [64, 1], F32, pa)
        frb2 = sb("frb2", [64, 1], F32, pa)
        with S.scope() as p1:
            zT = sb("zT", [33, SEQ], F32, p1)
            zrT = sb("zrT", [33, SEQ], F32, p1)
            hid1 = sb("hid1", [64, SEQ], F32, p1)
            w1 = sb("hw1", [33, 64], F32, p1)
            w2 = sb("hw2", [64, 64], F32, p1)
            b1c = sb("b1c", [64, 1], F32, p1)
            b2c = sb("b2c", [64, 1], F32, p1)
            arg = [sb(f"harg{i}", [64, 512], F32, p1) for i in range(2)]
            m1 = [sb(f"hm1{i}", [64, 512], F32, p1) for i in range(2)]
            m2 = [sb(f"hm2{i}", [64, 512], F32, p1) for i in range(2)]
            psM = [pst(f"psM{i}", [128, 512], F32, p1) for i in range(2)]
            S.dma(zT[:, :], D['zT'][:, :])
            S.dma(zrT[:, :], D['zrT'][:, :])
            S.dma(w1[:, :], D['hy_w1'][:, :])
            S.dma(w2[:, :], D['hy_w2'][:, :])
            S.dma(b1c[:, :], D['hy_b1c'][:, :])
            S.dma(b2c[:, :], D['hy_b2c'][:, :])
            S.dma(frc[:, :], D['hy_frc'][:, :])
            S.tt(frb1[:, :], frc[:, :], b1c[:, :], ALU.mult)
            S.tt(frb2[:, :], frc[:, :], b2c[:, :], ALU.mult)
            n = 0

            def sin_layer(ps, frb, dst):
                nonlocal n
                a = n % 2
                n += 1
                S.ts(arg[a][:, :], ps, frc[:, 0:1], ALU.mult, s2=frb[:, 0:1], op1=ALU.add)
                S.ts(m1[a][:, :], arg[a][:, :], PI, ALU.is_gt, s2=-2.0 * PI, op1=ALU.mult)
                S.ts(m2[a][:, :], arg[a][:, :], -PI, ALU.is_lt, s2=2.0 * PI, op1=ALU.mult)
                S.tt(arg[a][:, :], arg[a][:, :], m1[a][:, :], ALU.add)
                S.tt(arg[a][:, :], arg[a][:, :], m2[a][:, :], ALU.add)
                S.act(dst, arg[a][:, :], AF.Sin)
            for (zs, hdst) in ((zT, hid2T), (zrT, hid2rT)):
                for blk in range(8):
                    ps = psM[blk % 2]
                    S.mm(ps[0:64, :], w1[:, :], zs[:, blk * 512:(blk + 1) * 512])
                    sin_layer(ps[0:64, :], frb1, hid1[:, blk * 512:(blk + 1) * 512])
                for blk in range(8):
                    ps = psM[blk % 2]
                    S.mm(ps[0:64, :], w2[:, :], hid1[:, blk * 512:(blk + 1) * 512])
                    sin_layer(ps[0:64, :], frb2, hdst[:, blk * 512:(blk + 1) * 512])
        with S.scope() as p2:
            w3 = sb("hw3", [64, 2048], F32, p2)
            trow = sb("trow", [128, SEQ], F32, p2)
            trrow = sb("trrow", [128, SEQ], F32, p2)
            ndel = sb("ndel", [128, 16], F32, p2)
            hbias = sb("hbias", [128, 8], F32, p2)
            kT = [sb(f"kTb{i}", [128, 2 * SEQ], BF16, p2) for i in range(2)]
            win = [sb(f"win{i}", [128, 512], F32, p2) for i in range(2)]
            k0t = sb("k0t", [128, 4], F32, p2)
            psK_ = [pst(f"psKf{i}", [128, 512], F32, p2) for i in range(3)]
            ps0 = pst("psK0", [128, 512], F32, p2)
            S.dma(w3[:, :], D['hy_w3'][:, :])
            S.dma(trow[:, :], D['t_row'][0:1, :].partition_broadcast(128))
            S.dma(trrow[:, :], D['tr_row'][0:1, :].partition_broadcast(128))
            S.dma(ndel[:, :], D['hy_del_col'][:, :])
            S.dma(hbias[:, :], D['hy_bias_col'][:, :])
            S.stt(ndel[:, :], ndel[:, :], -1.0, ndel[:, :], ALU.mult, ALU.max)
            S.ts(ndel[:, :], ndel[:, :], -1.0, ALU.mult)
            nb = 0
            nk = 0
            for o in range(2):
                for g in range(4):
                    kt = kT[nk % 2]
                    nk += 1
                    for d in range(2):
                        hs = hid2T if d == 0 else hid2rT
                        tr = trow if d == 0 else trrow
                        c0 = (o * 2 + d) * 512 + g * 128
                        di = o * 8 + d * 4 + g
                        for blk in range(8):
                            ps = psK_[nb % 3]
                            wn = win[nb % 2]
                            nb += 1
                            S.mm(ps[:, :], w3[:, c0:c0 + 128], hs[:, blk * 512:(blk + 1) * 512])
                            S.act(wn[:, :], tr[:, blk * 512:(blk + 1) * 512], AF.Exp, scale=ndel[:, di:di + 1])
                            S.stt(kt[:, d * SEQ + blk * 512:d * SEQ + (blk + 1) * 512], wn[:, :], 0.05, ps[:, :], ALU.add, ALU.mult)
                    S.memset(kt[:, SEQ:SEQ + 1], 0.0)
                    cf = (o * 2 + 0) * 512 + g * 128
                    cb = (o * 2 + 1) * 512 + g * 128
                    S.mm(ps0[:, 0:1], w3[:, cf:cf + 128], hid2T[:, 0:1])
                    S.mm(ps0[:, 1:2], w3[:, cb:cb + 128], hid2T[:, 0:1])
                    S.copy(k0t[:, 0:2], ps0[:, 0:2])
                    S.tt(k0t[:, 2:3], k0t[:, 0:1], k0t[:, 1:2], ALU.add)
                    S.ts(k0t[:, 3:4], k0t[:, 2:3], 1.05, ALU.mult)
                    S.tt(kt[:, 0:1], k0t[:, 3:4], hbias[:, o * 4 + g:o * 4 + g + 1], ALU.add)
                    dst = D['Kd0'] if o == 0 else D['Kd1']
                    S.dma(dst[g * 128:(g + 1) * 128, :], kt[:, :])
    if C.debug:
        d_ = C.dbg_out('Kd0', [512, 2 * SEQ], BF16)
        with S.scope() as pd:
            tmp = sb("dbgtmp_kd", [128, 4, 2 * SEQ], BF16, pd)
            S.dma(tmp[:, :, :], D['Kd0'].rearrange("(a p) n -> p a n", p=128))
            S.dma(d_.rearrange("(a p) n -> p a n", p=128), tmp[:, :, :])
    with S.scope() as pb:
        T = Ctx()
        T.F1 = sb("F1", [64, 128], BF16, pb)
        T.GrT = sb("GrT", [128, 64, 128], BF16, pb)
        T.GiT = sb("GiT", [128, 64, 128], BF16, pb)
        T.Rc1 = sb("Rc1", [128, 256], BF16, pb)
        T.Rc2 = sb("Rc2", [128, 256], BF16, pb)
        T.LrT = sb("LrT", [64, 128, 32], BF16, pb)
        T.LiNT = sb("LiNT", [64, 128, 32], BF16, pb)
        hnorm = sb("hnorm", [128, 4], F32, pb)
        S.dma(T.F1[:, :], D['F1'][:, :])
        for nm in ('GrT', 'GiT'):
            S.dma(getattr(T, nm)[:, :, :], D[nm].rearrange("p (k m) -> p k m", k=64))
        S.dma(T.Rc1[:, :], D['Rc1'][:, :])
        S.dma(T.Rc2[:, :], D['Rc2'][:, :])
        S.dma(T.LrT[:, :, :], D['LrT'].rearrange("p (n m) -> p n m", n=128))
        S.dma(T.LiNT[:, :, :], D['LiNT'].rearrange("p (n m) -> p n m", n=128))
        S.dma(hnorm[:, :], D['hnorm_col'][:, :])
        for o in range(2):
            zsrc = D['Zd'] if o == 0 else D['Z1d']
            kd = D['Kd0'] if o == 0 else D['Kd1']
            with S.scope() as pw:
                W = Ctx()
                W.X = sb("fX", [64, 64, 128], BF16, pw)
                W.AT = sb("fAT", [128, 64, 2, 64], BF16, pw)
                W.ATn = sb("fATn", [128, 64, 2, 64], BF16, pw)
                W.Kf = sb("fKf", [128, 64, 2, 64], BF16, pw)
                W.V = sb("fV", [128, 2, 64, 64], BF16, pw)
                W.Wt = sb("fWt", [64, 64, 2, 128], BF16, pw)
                W.Ysb = sb("fY", [32, 64, 128], BF16, pw)
                W.tm = [[sb(f"ftm{i}{j}", [128, 4, 64], F32, pw) for j in range(4)] for i in range(2)]
                W.ring = [pst(f"psR{i}", [128, 512], F32, pw) for i in range(8)]
                W.nr = 0
                W.ne = 0
                W.pref = False
                for b8 in range(8):
                    ch0 = b8 * 64
                    fft_batch(C, T, W, zsrc[ch0:ch0 + 64, :], kd[ch0:ch0 + 64, :], D['Ycv'][ch0:ch0 + 64, :],
                              kd_next=(kd[ch0 + 64:ch0 + 128, :] if b8 < 7 else None))
            with S.scope() as pg:
                ysb = [sb(f"gy{i}", [128, SEQ], BF16, pg) for i in range(2)]
                xsb = [sb(f"gx{i}", [128, SEQ], F32, pg) for i in range(2)]
                zo = [sb(f"gz{i}", [128, SEQ], BF16, pg) for i in range(2)]
                xsrc = D['X1d'] if o == 0 else D['X2d']
                if o == 1:
                    z2 = sb("gz2", [128, SEQ], F32, pg)
                    sq = [sb(f"gsq{i}", [128, 512], F32, pg) for i in range(2)]
                    rr = [sb(f"grr{i}", [128, 512], F32, pg) for i in range(2)]
                    psN = [pst(f"psN{i}", [128, 512], F32, pg) for i in range(2)]
                for g in range(4):
                    a = g % 2
                    S.dma(ysb[a][:, :], D['Ycv'][g * 128:(g + 1) * 128, :])
                    S.dma(xsb[a][:, :], xsrc[g * 128:(g + 1) * 128, :])
                    if o == 0:
                        S.tt(zo[a][:, :], xsb[a][:, :], ysb[a][:, :], ALU.mult)
                        S.dma(D['Z1d'][g * 128:(g + 1) * 128, :], zo[a][:, :])
                    else:
                        S.tt(z2[:, :], xsb[a][:, :], ysb[a][:, :], ALU.mult)
                        for blk in range(8):
                            bs = slice(blk * 512, (blk + 1) * 512)
                            q_ = blk % 2
                            S.tt(sq[q_][:, :], z2[:, bs], z2[:, bs], ALU.mult)
                            S.mm(psN[q_][:, :], P.ones_f[:, :], sq[q_][:, :])
                            S.act(rr[q_][:, :], psN[q_][:, :], AF.Ln, scale=1.0 / 128.0, bias=EPS)
                            S.act(rr[q_][:, :], rr[q_][:, :], AF.Exp, scale=-0.5)
                            S.stt(zo[a][:, bs], z2[:, bs], hnorm[:, g:g + 1], rr[q_][:, :], ALU.mult, ALU.mult)
                        S.dma(D['Yt'][512 + g * 128:512 + (g + 1) * 128, :], zo[a][:, :])
    if C.debug:
        d_ = C.dbg_out('Yt_h', [512, SEQ], BF16)
        with S.scope() as pd:
            tmp = sb("dbgtmp_yth", [128, 4, SEQ], BF16, pd)
            S.dma(tmp[:, :, :], D['Yt'][512:1024, :].rearrange("(a p) n -> p a n", p=128))
            S.dma(d_.rearrange("(a p) n -> p a n", p=128), tmp[:, :, :])


def fft_batch(C, T, W, zsrc, kdsrc, ydst, kd_next=None):
    S = C.S

    def bank():
        W.nr += 1
        return W.ring[W.nr % 8]

    def ev(dst, src):
        W.ne += 1
        S.copy(dst, src, eng=('act' if W.ne % 2 else 'dve'))

    def forward_d(kdim):
        for cq in range(16):
            pA = bank()
            for i in range(4):
                ch = cq * 4 + i
                S.mm(pA[:, i * 128:(i + 1) * 128], W.X[0:kdim, ch, :], T.F1[0:kdim, :])
            ev(W.AT[:, cq * 4:(cq + 1) * 4, :, :].rearrange("p c r k -> p (c r k)"), pA[:, :])
            src = pA[:, :].rearrange("p (c r k) -> p c r k", c=4, r=2)
            S.act(W.ATn[:, cq * 4:(cq + 1) * 4, 0, :], src[:, :, 1, :], AF.Copy, scale=-1.0)
            S.copy(W.ATn[:, cq * 4:(cq + 1) * 4, 1, :], src[:, :, 0, :])

    def forward_s(mode):
        for kb in range(16):
            pU = bank()
            for i in range(4):
                k1 = kb * 4 + i
                o_ = pU[:, i * 128:(i + 1) * 128]
                S.mm(o_, T.GrT[:, k1, :], W.AT[:, :, :, k1].rearrange("p c r -> p r c"), start=True, stop=False)
                S.mm(o_, T.GiT[:, k1, :], W.ATn[:, :, :, k1].rearrange("p c r -> p r c"), start=False, stop=True)
            if mode == 'kernel':
                ev(W.Kf[:, kb * 4:(kb + 1) * 4, :, :].rearrange("p k r c -> p (k r c)"), pU[:, :])
            else:
                pv = pU[:, :].rearrange("p (k r c) -> p k r c", k=4, r=2)
                Ur = pv[:, :, 0, :]
                Ui = pv[:, :, 1, :]
                Kr = W.Kf[:, kb * 4:(kb + 1) * 4, 0, :]
                Ki = W.Kf[:, kb * 4:(kb + 1) * 4, 1, :]
                t = W.tm[kb % 2]
                S.tt(t[0][:, :, :], Ur, Kr, ALU.mult)
                S.tt(t[1][:, :, :], Ui, Ki, ALU.mult)
                S.tt(t[2][:, :, :], Ur, Ki, ALU.mult)
                S.tt(t[3][:, :, :], Ui, Kr, ALU.mult)
                S.tt(W.V[:, 0, kb * 4:(kb + 1) * 4, :], t[0][:, :, :], t[1][:, :, :], ALU.subtract, eng='pool')
                S.tt(W.V[:, 1, kb * 4:(kb + 1) * 4, :], t[2][:, :, :], t[3][:, :, :], ALU.add, eng='pool')

    if not W.pref:
        S.dma(W.X[:, :, :], kdsrc.rearrange("c (a n) -> a c n", n=128))
    forward_d(64)
    forward_s('kernel')
    S.dma(W.X[0:32, :, :], zsrc.rearrange("c (a n) -> a c n", n=128))
    forward_d(32)
    W.pref = False
    if kd_next is not None:
        S.dma(W.X[:, :, :], kd_next.rearrange("c (a n) -> a c n", n=128))
        W.pref = True
    forward_s('data')
    for cp in range(32):
        pW = bank()
        for i in range(2):
            ch = cp * 2 + i
            o_ = pW[0:64, i * 256:(i + 1) * 256]
            S.mm(o_, W.V[:, 0, :, ch], T.Rc1[:, :], start=True, stop=False)
            S.mm(o_, W.V[:, 1, :, ch], T.Rc2[:, :], start=False, stop=True)
        ev(W.Wt[:, cp * 2:(cp + 1) * 2, :, :].rearrange("p c r n -> p (c r n)"), pW[0:64, :])
    for nb in range(16):
        pY = bank()
        for i in range(8):
            n2 = nb * 8 + i
            o_ = pY[0:32, i * 64:(i + 1) * 64]
            S.mm(o_, T.LrT[:, n2, :], W.Wt[:, :, 0, n2], start=True, stop=False)
            S.mm(o_, T.LiNT[:, n2, :], W.Wt[:, :, 1, n2], start=False, stop=True)
        ev(W.Ysb[:, :, nb * 8:(nb + 1) * 8], pY[0:32, :].rearrange("p (n c) -> p c n", n=8))
    S.dma(ydst.rearrange("c (a n) -> a c n", n=128), W.Ysb[:, :, :])


def phase_outproj(C):
    S, D, P, sb, pst, nc = C.S, C.D, C.P, C.sb, C.pst, C.nc
    P.IDX = sb("IDX", [128, 16, 4], I32)
    P.GW = sb("GW", [128, 16, 4], F32)
    with S.scope() as ph:
        AFF = sb("AFF", [128, NT, 16], F32, ph)
        AFFT = sb("AFFT", [16, SEQ], F32, ph)
        with S.scope() as p1:
            ytT = sb("ytT", [128, 8, SEQ], BF16, p1)
            wout = sb("wout", [128, 8, DM], BF16, p1)
            wr = sb("wr", [128, 8, 16], F32, p1)
            xb = [sb(f"oxb{i}", [128, DM], F32, p1) for i in range(2)]
            x1 = [sb(f"ox1{i}", [128, DM], F32, p1) for i in range(3)]
            tmp = [sb(f"otmp{i}", [128, DM], F32, p1) for i in range(2)]
            hf = [sb(f"ohf{i}", [128, DM], F32, p1) for i in range(3)]
            hfb = [sb(f"ohfb{i}", [128, DM], BF16, p1) for i in range(3)]
            hfT = [sb(f"ohfT{i}", [128, 8, 128], F32, p1) for i in range(2)]
            junk = sb("ojunk", [128, DM], BF16, p1)
            sm = sb("osm", [128, NT, 8], F32, p1)
            ex = [sb(f"oex{i}", [128, 16], F32, p1) for i in range(2)]
            psM = [[pst(f"psMo{i}{h}", [128, 512], F32, p1) for h in range(2)] for i in range(2)]
            psT = [pst(f"psTo{i}", [128, 512], F32, p1) for i in range(2)]
            psR = pst("psR", [128, 512], F32, p1)
            psAT = pst("psAT", [128, 512], F32, p1)
            psAT2 = psT[1]
            for k in range(8):
                S.dma(ytT[:, k, :], D['Yt'][k * 128:(k + 1) * 128, :])
            wsrc = D['w_out'].rearrange("(k p) c -> p k c", p=128)
            S.dma(wout[:, :, 0:512], wsrc[:, :, 0:512], q='pool')
            S.dma(wout[:, :, 512:1024], wsrc[:, :, 512:1024], q='pool')
            S.dma(wr[:, :, :], D['w_router'].rearrange("(k p) e -> p k e", p=128))
            def stage_a1(j):
                a = j % 2
                b = j % 3
                xt = xb[a]
                S.dma(xt[:, :], D['x'][j * 128:(j + 1) * 128, :])
                for h in range(2):
                    for k in range(8):
                        S.mm(psM[a][h][:, :], ytT[:, k, j * 128:(j + 1) * 128], wout[:, k, h * 512:(h + 1) * 512],
                             start=(k == 0), stop=(k == 7))
                for h in range(2):
                    S.tt(tmp[a][:, h * 512:(h + 1) * 512], psM[a][h][:, :], P.gt1rep[:, h * 512:(h + 1) * 512], ALU.mult)
                S.tt(x1[b][:, :], tmp[a][:, :], xt[:, :], ALU.add)
                S.dma(D['acc'][j * 128:(j + 1) * 128, :], x1[b][:, :])

            def stage_a2(j):
                b = j % 3
                ss = sm[:, j, 0:1]
                rs = sm[:, j, 1:2]
                S.act(junk[:, :], x1[b][:, :], AF.Square, accum_out=ss)
                S.act(rs, ss, AF.Ln, scale=1.0 / DM, bias=EPS)
                S.act(rs, rs, AF.Exp, scale=-0.5)
                S.stt(hf[b][:, :], x1[b][:, :], rs, P.A2rep[:, :], ALU.mult, ALU.mult)
                S.tt(hf[b][:, :], hf[b][:, :], P.B2rep[:, :], ALU.add)
                S.act(hfb[b][:, :], hf[b][:, :], AF.Copy)
                S.dma(D['HFd'][j * 128:(j + 1) * 128, :], hfb[b][:, :])

            def stage_b1(j):
                a = j % 2
                b = j % 3
                for k in range(8):
                    S.transpose(psT[k // 4][:, (k % 4) * 128:(k % 4 + 1) * 128], hf[b][:, k * 128:(k + 1) * 128], P.ident_f[:, :])
                S.copy(hfT[a][:, 0:4, :].rearrange("p k t -> p (k t)"), psT[0][:, :], eng='act')
                S.copy(hfT[a][:, 4:8, :].rearrange("p k t -> p (k t)"), psT[1][:, :], eng='dve')

            def stage_b2(j):
                a = j % 2
                for k in range(8):
                    S.mm(psR[:, 0:16], hfT[a][:, k, :], wr[:, k, :], start=(k == 0), stop=(k == 7))
                mx = sm[:, j, 2:3]
                se = sm[:, j, 3:4]
                S.reduce(mx, psR[:, 0:16], ALU.max)
                S.ts(mx, mx, -1.0, ALU.mult)
                S.act(ex[a][:, :], psR[:, 0:16], AF.Exp, bias=mx, accum_out=se)
                S.recip(se, se)
                S.ts(AFF[:, j, :], ex[a][:, :], se, ALU.mult)
            for step in range(NT + 2):
                if 0 <= step - 2 < NT:
                    stage_b1(step - 2)
                if step < NT:
                    stage_a1(step)
                if 0 <= step - 1 < NT:
                    stage_a2(step - 1)
                if 0 <= step - 2 < NT:
                    stage_b2(step - 2)
            for j in range(NT):
                pat = psAT if j % 2 == 0 else psAT2
                S.transpose(pat[0:16, 0:128], AFF[:, j, :], P.ident_f[:, :])
                S.copy(AFFT[:, j * 128:(j + 1) * 128], pat[0:16, 0:128], eng=('act' if j % 2 else 'dve'))
            if C.debug:
                pass
        if C.debug:
            d_ = C.dbg_out('AFF', [128, NT, 16])
            S.dma(d_[:, :, :], AFF[:, :, :])
        with S.scope() as p2:
            junkA = sb("junkA", [16, SEQ], F32, p2)
            bs = sb("bis", [16, 8], F32, p2)
            THR = sb("THR", [128, 16], F32, p2)
            throw = sb("throw", [1, 16], F32, p2)
            SEL = sb("SEL", [128, NT, 16], F32, p2)
            POS = sb("POS", [128, NT, 16], F32, p2)
            selcum = sb("selcum", [128, 16], F32, p2)
            striU = sb("striU", [128, 128], F32, p2)
            COORD = sb("COORD", [128, NT, 16, 4], BF16, p2)
            jf = sb("jf", [128, NT], F32, p2)
            pf = sb("pf", [128, 1], F32, p2)
            iot = sb("iot", [128, 512], mybir.dt.float16, p2)
            OH = [sb(f"OH{i}", [128, 512], BF16, p2) for i in range(3)]
            r4 = [sb(f"r4{i}", [128, 8], F32, p2) for i in range(2)]
            psI = [pst(f"psI{i}", [128, 512], F32, p2) for i in range(4)]
            psP = [pst(f"psP{i}", [128, 512], F32, p2) for i in range(2)]
            psX = pst("psX", [128, 512], F32, p2)
            psB2 = pst("psB2", [128, 512], F32, p2)
            lo, hi, mid, cnt, ge, d1, d2 = [bs[:, i:i + 1] for i in range(7)]
            S.dma(striU[:, :], D['striU'][:, :])
            S.memset(lo, 0.0)
            S.memset(hi, 1.0)
            for it in range(30):
                S.tt(mid, lo, hi, ALU.add)
                S.ts(mid, mid, 0.5, ALU.mult)
                S.ts(junkA[:, :], AFFT[:, :], mid, ALU.is_ge, s2=0.0, op1=ALU.add, accum_out=cnt)
                S.ts(ge, cnt, 511.5, ALU.is_gt)
                S.tt(d1, mid, lo, ALU.subtract)
                S.tt(d2, hi, mid, ALU.subtract)
                S.stt(lo, d1, ge, lo, ALU.mult, ALU.add)
                S.stt(hi, d2, ge, mid, ALU.mult, ALU.add)
            S.transpose(psX[0:1, 0:16], lo, P.ident_f[0:16, 0:16])
            S.copy(throw[:, :], psX[0:1, 0:16])
            S.mm(psB2[:, 0:16], P.ones_f[0:1, 0:128], throw[0:1, :])
            S.copy(THR[:, :], psB2[:, 0:16])
            S.tt(SEL[:, :, :], AFF[:, :, :], THR[:, :].unsqueeze(1).to_broadcast([128, NT, 16]), ALU.is_ge)
            S.memset(selcum[:, :], 0.0)
            for j in range(NT):
                pp = psP[j % 2]
                S.mm(pp[:, 0:16], striU[:, :], SEL[:, j, :], start=True, stop=False)
                S.mm(pp[:, 0:16], P.ones_f[:, :], selcum[:, :], start=False, stop=True)
                S.copy(POS[:, j, :], pp[:, 0:16], eng='act')
                S.tt(selcum[:, :], selcum[:, :], SEL[:, j, :], ALU.add)
            S.op('pool', lambda e: e.iota(jf[:, :], [[1, NT]], base=0, channel_multiplier=0, allow_small_or_imprecise_dtypes=True), [], [jf[:, :]])
            S.op('pool', lambda e: e.iota(pf[:, :], [[1, 1]], base=0, channel_multiplier=1, allow_small_or_imprecise_dtypes=True), [], [pf[:, :]])
            S.op('pool', lambda e: e.iota(iot[:, :], [[1, 512]], base=0, channel_multiplier=0, allow_small_or_imprecise_dtypes=True), [], [iot[:, :]])
            S.copy(COORD[:, :, :, 0], jf[:, :].unsqueeze(2).to_broadcast([128, NT, 16]))
            S.copy(COORD[:, :, :, 1], pf[:, 0:1].unsqueeze(2).to_broadcast([128, NT, 16]))
            S.copy(COORD[:, :, :, 2], AFF[:, :, :])
            S.tt(COORD[:, :, :, 3], AFF[:, :, :], COORD[:, :, :, 2], ALU.subtract)
            n = 0
            for e_ in range(16):
                for j in range(NT):
                    oh = OH[n % 3]
                    n += 1
                    S.ts(oh[:, :], iot[:, :], POS[:, j, e_:e_ + 1], ALU.is_equal, s2=SEL[:, j, e_:e_ + 1], op1=ALU.mult)
                    for sc in range(4):
                        S.mm(psI[sc][:, 0:4], oh[:, sc * 128:(sc + 1) * 128], COORD[:, j, e_, :], start=(j == 0), stop=(j == NT - 1))
                for sc in range(4):
                    r = r4[(e_ * 4 + sc) % 2]
                    S.copy(r[:, 0:4], psI[sc][:, 0:4], eng='act')
                    S.stt(r[:, 4:5], r[:, 0:1], 128.0, r[:, 1:2], ALU.mult, ALU.add)
                    S.copy(P.IDX[:, e_, sc:sc + 1], r[:, 4:5])
                    S.tt(P.GW[:, e_, sc:sc + 1], r[:, 2:3], r[:, 3:4], ALU.add)
        if C.debug:
            d_ = C.dbg_out('IDX', [128, 16, 4], I32)
            S.dma(d_[:, :, :], P.IDX[:, :, :])
            d_ = C.dbg_out('GW', [128, 16, 4])
            S.dma(d_[:, :, :], P.GW[:, :, :])


def phase_route(C):
    pass


def phase_experts(C):
    S, D, P, sb, pst, nc = C.S, C.D, C.P, C.sb, C.pst, C.nc
    with S.scope() as ph:
        stg = [sb(f"stg{i}", [128, 8, 512], F32, ph) for i in range(4)]
        wb = [sb(f"wb{i}", [128, 8, 512], BF16, ph) for i in range(8)]
        xs = sb("xs", [128, 4, DM], BF16, ph)
        xsT = sb("xsT", [128, 8, 512], BF16, ph)
        hidT = sb("hidT", [128, 16, 512], BF16, ph)
        sg = [sb(f"sg{i}", [128, 512], F32, ph) for i in range(2)]
        ysb = [sb(f"ysb{i}", [128, DM], F32, ph) for i in range(4)]
        psT = pst("psTe", [128, 1024], BF16, ph)
        psG = [pst(f"psGe{i}", [128, 512], F32, ph) for i in range(2)]
        psU = pst("psUe", [128, 512], F32, ph)
        psY = [pst(f"psYe{i}", [128, 512], F32, ph) for i in range(4)]
        pieces = []
        for e_ in range(16):
            for fb in range(4):
                for nm in ('w_gate', 'w_up'):
                    pieces.append(D[nm][e_ * 1024:(e_ + 1) * 1024, fb * 512:(fb + 1) * 512].rearrange("(k p) c -> p k c", p=128))
            for dh in range(2):
                for fh in range(2):
                    r0 = e_ * 2048 + fh * 1024
                    pieces.append(D['w_down'][r0:r0 + 1024, dh * 512:(dh + 1) * 512].rearrange("(k p) c -> p k c", p=128))
        issued = [0]
        cast_eng = ['act', 'dve']
        LOOK = 5

        def issue_upto(n):
            while issued[0] < min(n, len(pieces)):
                i = issued[0]
                s_ = stg[i % 4]
                S.dma(s_[:, :, :], pieces[i])
                S.copy(wb[i % 8][:, :, :], s_[:, :, :], eng=cast_eng[i % 2])
                issued[0] += 1
        pc = [0]

        def next_piece():
            i = pc[0]
            issue_upto(i + 1 + LOOK)
            pc[0] += 1
            return wb[i % 8]
        ng = 0
        for e_ in range(16):
            for st in range(4):
                idx_ap = P.IDX[:, e_, st:st + 1]
                S.op('pool', lambda e, st=st, idx_ap=idx_ap: e.indirect_dma_start(
                    out=xs[:, st, :], out_offset=None, in_=D['HFd'][:, :],
                    in_offset=bass.IndirectOffsetOnAxis(ap=idx_ap, axis=0)),
                    [D['HFd'][:, :], idx_ap], [xs[:, st, :]], dma=True)
            for st in range(4):
                for k in range(8):
                    S.transpose(psT[:, k * 128:(k + 1) * 128], xs[:, st, k * 128:(k + 1) * 128], P.ident_bf[:, :])
                S.copy(xsT[:, :, st * 128:(st + 1) * 128], psT[:, :].rearrange("p (k t) -> p k t", k=8), eng=('act' if st % 2 else 'dve'))
            for fb in range(4):
                wg = next_piece()
                wu = next_piece()
                for fc in range(4):
                    pg = psG[ng % 2]
                    sgt = sg[ng % 2]
                    ng += 1
                    for k in range(8):
                        S.mm(pg[:, :], wg[:, k, fc * 128:(fc + 1) * 128], xsT[:, k, :], start=(k == 0), stop=(k == 7))
                    for k in range(8):
                        S.mm(psU[:, :], wu[:, k, fc * 128:(fc + 1) * 128], xsT[:, k, :], start=(k == 0), stop=(k == 7))
                    S.act(sgt[:, :], pg[:, :], AF.Silu)
                    S.tt(hidT[:, fb * 4 + fc, :], sgt[:, :], psU[:, :], ALU.mult)
            for dh in range(2):
                for fh in range(2):
                    wd = next_piece()
                    for st in range(4):
                        for f8 in range(8):
                            S.mm(psY[st][:, :], hidT[:, fh * 8 + f8, st * 128:(st + 1) * 128], wd[:, f8, :],
                                 start=(fh == 0 and f8 == 0), stop=(fh == 1 and f8 == 7))
                for st in range(4):
                    S.stt(ysb[st][:, dh * 512:(dh + 1) * 512], psY[st][:, :], P.GW[:, e_, st:st + 1],
                          P.gt2rep[:, dh * 512:(dh + 1) * 512], ALU.mult, ALU.mult)
            for st in range(4):
                idx_ap = P.IDX[:, e_, st:st + 1]
                S.op('pool', lambda e, st=st, idx_ap=idx_ap: e.indirect_dma_start(
                    out=D['acc'][:, :], out_offset=bass.IndirectOffsetOnAxis(ap=idx_ap, axis=0),
                    in_=ysb[st][:, :], in_offset=None, compute_op=ALU.add),
                    [ysb[st][:, :], idx_ap, D['acc'][:, :]], [D['acc'][:, :]], dma=True)


def phase_final(C):
    S, D, P, sb, pst = C.S, C.D, C.P, C.sb, C.pst
    with S.scope() as ph:
        gfin = sb("gfin", [128, DM], F32, ph)
        xb = [sb(f"fxb{i}", [128, DM], F32, ph) for i in range(2)]
        ob = [sb(f"fob{i}", [128, DM], F32, ph) for i in range(2)]
        junk = sb("fjunk", [128, DM], BF16, ph)
        sm = sb("fsm", [128, NT, 2], F32, ph)
        S.dma(gfin[:, :], D['gfin_row'][0:1, :].partition_broadcast(128))
        for j in range(NT):
            a = j % 2
            S.dma(xb[a][:, :], D['acc'][j * 128:(j + 1) * 128, :])
            ss = sm[:, j, 0:1]
            rs = sm[:, j, 1:2]
            S.act(junk[:, :], xb[a][:, :], AF.Square, accum_out=ss)
            S.act(rs, ss, AF.Ln, scale=1.0 / DM, bias=EPS)
            S.act(rs, rs, AF.Exp, scale=-0.5)
            S.stt(ob[a][:, :], xb[a][:, :], rs, gfin[:, :], ALU.mult, ALU.mult)
            S.dma(D['out'][j * 128:(j + 1) * 128, :], ob[a][:, :])


_PROG = {}


def kernel(**inputs):
    if 'nc' not in _PROG:
        _PROG['nc'] = build()[0]
    nc = _PROG['nc']
    B = inputs['x'].shape[0]
    in_maps = [layout_inputs(inputs, b) for b in range(B)]
    res = run_bass_kernel_spmd(nc, in_maps, core_ids=list(range(B)))
    out = np.stack([np.asarray(r["out"], dtype=np.float32) for r in res.results], axis=0)
    return out
```

```python
import math
import numpy as np
import ml_dtypes
import concourse.bass as bass
import concourse.mybir as mybir
from concourse.bass_utils import run_bass_kernel_spmd
from contextlib import ExitStack

F32 = mybir.dt.float32
BF16 = mybir.dt.bfloat16
I32 = mybir.dt.int32
AF = mybir.ActivationFunctionType
ALU = mybir.AluOpType
AX = mybir.AxisListType

SEQ = 4096
DM = 1024
NT = 32
EPS = 1e-6
EMIT_UNTIL = [None]
COMPUTE = ('pe', 'act', 'dve', 'pool')
SELF_SYNC = {'act': True, 'dve': True, 'pool': True, 'pe': False}
NDMA_SEMS = 6


def ap_box(ap):
    t = ap.tensor
    name = t.name
    dims = list(ap.ap)
    off = int(ap.offset)
    sp = str(ap.space() if callable(ap.space) else ap.space)
    is_dram = 'DRAM' in sp.upper() or 'HBM' in sp.upper() or type(t).__name__.startswith('DRAM') or type(t).__name__.startswith('Dram')
    if is_dram:
        lo = off
        hi = off
        for (st, cnt) in dims:
            st = int(st); cnt = int(cnt)
            if st >= 0:
                hi += st * (cnt - 1)
            else:
                lo += st * (cnt - 1)
        return (name, 0, 1, lo, hi + 1)
    if 'PSUM' in sp.upper() or type(t).__name__.startswith('PSum'):
        return (name, 0, 128, 0, 1 << 30)
    p0 = int(ap.start_partition())
    pc = int(dims[0][1])
    lo = off
    hi = off
    for (st, cnt) in dims[1:]:
        st = int(st); cnt = int(cnt)
        if st >= 0:
            hi += st * (cnt - 1)
        else:
            lo += st * (cnt - 1)
    return (name, p0, p0 + pc, lo, hi + 1)


class Op:
    __slots__ = ('stream', 'fn', 'deps', 'is_dma', 'signal', 'semval', 'dma_slot', 'idx', 'extra_waits')


class Sched:
    def __init__(self, nc, es):
        self.nc = nc
        self.es = es
        self.ops = []
        self.track = {}
        self.eng = {'pe': nc.tensor, 'act': nc.scalar, 'dve': nc.vector, 'pool': nc.gpsimd, 'sp': nc.sync}
        self.sem = {s: es.enter_context(nc.semaphore('sem_' + s)) for s in COMPUTE}
        self.dma_sems = {}
        for s in ('sp', 'pool', 'act'):
            self.dma_sems[s] = [es.enter_context(nc.semaphore(f'dsem_{s}{i}')) for i in range(NDMA_SEMS)]
        self.dma_count = {'sp': 0, 'pool': 0, 'act': 0}
        self.last_dma_ops = {'sp': [], 'pool': [], 'act': []}

    def _deps(self, boxes_r, boxes_w, idx, stream, is_dma):
        deps = set()
        for (boxes, is_w) in ((boxes_r, False), (boxes_w, True)):
            for b in boxes:
                lst = self.track.setdefault(b[0], [])
                keep = []
                for ent in lst:
                    eb, eidx, ew = ent
                    ov = not (eb[2] <= b[1] or b[2] <= eb[1] or eb[4] <= b[3] or b[4] <= eb[3])
                    if ov and (is_w or ew):
                        deps.add(eidx)
                    covered = (b[1] <= eb[1] and eb[2] <= b[2] and b[3] <= eb[3] and eb[4] <= b[4])
                    if is_w and covered:
                        continue
                    if (not is_w) and (not ew) and covered and (not is_dma):
                        eo = self.ops[eidx]
                        if eo.stream == stream and not eo.is_dma:
                            continue
                    keep.append(ent)
                keep.append([b, idx, is_w])
                self.track[b[0]] = keep
        deps.discard(idx)
        return deps

    def op(self, stream, fn, reads=(), writes=(), dma=False):
        o = Op()
        o.idx = len(self.ops)
        o.stream = stream
        o.fn = fn
        o.is_dma = dma
        o.signal = False
        o.semval = None
        o.dma_slot = None
        o.extra_waits = []
        br = [ap_box(a) for a in reads if a is not None and not isinstance(a, (int, float))]
        bw = [ap_box(a) for a in writes]
        self.ops.append(o)
        o.deps = self._deps(br, bw, o.idx, stream, dma)
        return o

    def emit(self):
        ops = self.ops
        for o in ops:
            for d in o.deps:
                po = ops[d]
                if po.is_dma:
                    continue
                if po.stream != o.stream or o.is_dma or SELF_SYNC.get(po.stream, True):
                    po.signal = True
        cnt = {s: 0 for s in COMPUTE}
        dcnt = {'sp': 0, 'pool': 0, 'act': 0}
        for o in ops:
            if o.is_dma:
                d = dcnt[o.stream]
                o.dma_slot = (d % NDMA_SEMS, 16 * (d // NDMA_SEMS + 1))
                dcnt[o.stream] = d + 1
            elif o.signal:
                cnt[o.stream] += 1
                o.semval = cnt[o.stream]
        waited = {s: {} for s in self.eng}
        nw = 0
        for o in ops:
            e = self.eng[o.stream]
            w = waited[o.stream]
            toks = []
            if o.is_dma:
                j, v = o.dma_slot
                if v > 16:
                    toks.append((('d', o.stream, j), self.dma_sems[o.stream][j], v - 16))
            for d in o.deps:
                po = ops[d]
                if po.is_dma:
                    j, v = po.dma_slot
                    toks.append((('d', po.stream, j), self.dma_sems[po.stream][j], v))
                else:
                    if po.stream == o.stream and not o.is_dma and not SELF_SYNC.get(po.stream, True):
                        continue
                    toks.append((('c', po.stream), self.sem[po.stream], po.semval))
            best = {}
            for key, sem, v in toks:
                if v is None:
                    raise RuntimeError('dep on non-signaling op')
                if w.get(key, 0) >= v:
                    continue
                if key not in best or best[key][1] < v:
                    best[key] = (sem, v)
            for key, (sem, v) in best.items():
                e.wait_ge(sem, v)
                w[key] = v
                nw += 1
            ins = o.fn(e)
            if ins is None:
                continue
            if o.is_dma:
                j, v = o.dma_slot
                ins.then_inc(self.dma_sems[o.stream][j], 16)
            elif o.signal:
                ins.then_inc(self.sem[o.stream], 1)
        self.n_waits = nw
        return cnt, dcnt

    def dma(self, out, in_, q='sp', **kw):
        return self.op(q, lambda e: e.dma_start(out=out, in_=in_, **kw), [in_], [out], dma=True)

    def mm(self, out, lhsT, rhs, start=True, stop=True, **kw):
        return self.op('pe', lambda e: e.matmul(out, lhsT, rhs, start=start, stop=stop, **kw),
                       [lhsT, rhs] + ([] if start else [out]), [out])

    def transpose(self, out, in_, ident):
        return self.op('pe', lambda e: e.transpose(out, in_, ident), [in_, ident], [out])

    def act(self, out, in_, func, scale=1.0, bias=0.0, accum_out=None, eng='act'):
        rd = [in_]
        if not isinstance(scale, (int, float)):
            rd.append(scale)
            if func == AF.Copy:
                func = AF.Identity
        if not isinstance(bias, (int, float)):
            rd.append(bias)
            if func == AF.Copy:
                func = AF.Identity
        wr = [out] + ([accum_out] if accum_out is not None else [])
        kw = {}
        if accum_out is not None:
            kw['accum_out'] = accum_out
        return self.op('act', lambda e: e.activation(out=out, in_=in_, func=func, scale=scale, bias=bias, **kw), rd, wr)

    def tt(self, out, in0, in1, op, eng='dve'):
        return self.op(eng, lambda e: e.tensor_tensor(out=out, in0=in0, in1=in1, op=op), [in0, in1], [out])

    def ts(self, out, in0, s1, op0, s2=None, op1=None, eng='dve', accum_out=None):
        rd = [in0]
        if not isinstance(s1, (int, float)):
            rd.append(s1)
        if s2 is not None and not isinstance(s2, (int, float)):
            rd.append(s2)
        kw = {}
        if op1 is not None:
            kw['op1'] = op1
        if accum_out is not None:
            kw['accum_out'] = accum_out
        wr = [out] + ([accum_out] if accum_out is not None else [])
        return self.op(eng, lambda e: e.tensor_scalar(out=out, in0=in0, scalar1=s1, scalar2=s2, op0=op0, **kw), rd, wr)

    def stt(self, out, in0, scalar, in1, op0, op1, eng='dve'):
        rd = [in0, in1]
        if not isinstance(scalar, (int, float)):
            rd.append(scalar)
        return self.op(eng, lambda e: e.scalar_tensor_tensor(out=out, in0=in0, scalar=scalar, in1=in1, op0=op0, op1=op1), rd, [out])

    def copy(self, out, in_, eng='dve'):
        if eng == 'act':
            return self.act(out, in_, AF.Copy)
        return self.op(eng, lambda e: e.tensor_copy(out=out, in_=in_), [in_], [out])

    def memset(self, out, val, eng='dve'):
        return self.op(eng, lambda e: e.memset(out, val), [], [out])

    def reduce(self, out, in_, op, axis=None, eng='dve'):
        axis = axis or AX.X
        return self.op(eng, lambda e: e.tensor_reduce(out=out, in_=in_, op=op, axis=axis), [in_], [out])

    def recip(self, out, in_, eng='dve'):
        return self.op(eng, lambda e: e.reciprocal(out=out, in_=in_), [in_], [out])

    def barrier(self):
        alld = set()
        for lst in self.track.values():
            for ent in lst:
                alld.add(ent[1])
        self.track = {}
        for s_ in ('pe', 'act', 'dve', 'pool', 'sp'):
            o = self.op(s_, lambda e: None, [], [])
            o.deps = set(alld)

    def scope(self):
        return _Scope(self)

    def finish(self, out_aps):
        boxes = [ap_box(a) for a in out_aps]
        o = self.op('sp', lambda e: None, list(out_aps), [])
        return o


class _Scope:
    def __init__(self, S):
        self.S = S
        self.st = ExitStack()

    def __enter__(self):
        self.st.__enter__()
        return self.st

    def __exit__(self, *a):
        self.S.barrier()
        return self.st.__exit__(*a)

_CONSTS = {}


def _bf(a):
    return np.ascontiguousarray(a.astype(np.float32)).astype(ml_dtypes.bfloat16)


def host_consts():
    if _CONSTS:
        return _CONSTS
    c = {}
    c['ident_bf'] = _bf(np.eye(128))
    c['ident_f'] = np.eye(128, dtype=np.float32)
    s = np.arange(128)[:, None]
    t = np.arange(128)[None, :]
    c['triU'] = (s <= t).astype(np.float32)
    c['triL'] = (s >= t).astype(np.float32)
    c['striU'] = (s < t).astype(np.float32)
    N = 8192
    n1 = np.arange(64); k1 = np.arange(64); n2 = np.arange(128); k2 = np.arange(128)
    ang = 2 * np.pi * np.outer(n1, k1) / 64
    c['F1'] = _bf(np.concatenate([np.cos(ang), -np.sin(ang)], 1))
    th = 2 * np.pi * ((n2[:, None, None] * (k1[None, :, None] + 64 * k2[None, None, :])) % N) / N
    c['GrT'] = _bf(np.cos(th).reshape(128, 64 * 128))
    c['GiT'] = _bf((-np.sin(th)).reshape(128, 64 * 128))
    c['GiNT'] = _bf((np.sin(th)).reshape(128, 64 * 128))
    ph = 2 * np.pi * np.outer(k2, n2) / 128
    Rr = np.cos(ph); Ri = np.sin(ph)
    c['Rc1'] = _bf(np.concatenate([Rr, Ri], 1))
    c['Rc2'] = _bf(np.concatenate([-Ri, Rr], 1))
    n1h = np.arange(32)
    thL = 2 * np.pi * (k1[:, None, None] * n1h[None, None, :] / 64 + k1[:, None, None] * n2[None, :, None] / N)
    c['LrT'] = _bf((np.cos(thL) / N).reshape(64, 128 * 32))
    c['LiNT'] = _bf((-np.sin(thL) / N).reshape(64, 128 * 32))
    L = SEQ
    f32 = np.float32
    tt = np.linspace(0.0, 1.0, L, dtype=f32)[:, None]
    w = (f32(2.0 * math.pi) * np.arange(L, dtype=f32)[:, None] / f32(L)).astype(f32)
    bands = np.linspace(1e-4, 15, 16, dtype=f32)[None, :]
    z = np.concatenate([tt, np.cos(bands * w), -np.sin(bands * w)], axis=-1).astype(f32)
    zr = np.concatenate([z[0:1], z[:0:-1]], 0)
    c['zT'] = np.ascontiguousarray(z.T)
    c['zrT'] = np.ascontiguousarray(zr.T)
    trow = tt[:, 0]
    trr = np.concatenate([trow[0:1], trow[:0:-1]])
    c['t_row'] = np.ascontiguousarray(trow[None, :]).astype(f32)
    c['tr_row'] = np.ascontiguousarray(trr[None, :]).astype(f32)
    _CONSTS.update(c)
    return _CONSTS


def colmaj(v, nk):
    return np.ascontiguousarray(np.asarray(v, dtype=np.float32).reshape(nk, 128).T)


def layout_inputs(inp, b):
    m = {}
    f = lambda a: np.ascontiguousarray(np.asarray(a, dtype=np.float32))
    m['x'] = f(inp['x'][b])
    m['ccol'] = colmaj(inp['c'][b], 8)
    m['w_ada'] = f(inp['w_ada'][0])
    m['b_ada'] = f(inp['b_ada'][0][None, :])
    m['gmix_col'] = colmaj(inp['g_mix'][0], 8)
    m['w_in'] = f(inp['w_in'][0])
    bin_ = np.asarray(inp['b_in'][0], dtype=np.float32)
    m['bin_row'] = f(bin_[None, 1024:2064])
    m['bqk_col'] = colmaj(bin_[0:1024], 8)
    m['bhy_col'] = colmaj(bin_[2064:3600], 12)
    cw = np.asarray(inp['conv_qk_w'][0], dtype=np.float32)
    m['cqk_w'] = np.ascontiguousarray(cw.reshape(3, 8, 128).transpose(2, 1, 0))
    m['cqk_b'] = colmaj(inp['conv_qk_b'][0], 8)
    cw = np.asarray(inp['conv_hy_w'][0], dtype=np.float32)
    m['chy_w'] = np.ascontiguousarray(cw.reshape(3, 12, 128).transpose(2, 1, 0))
    m['chy_b'] = colmaj(inp['conv_hy_b'][0], 12)
    m['mnorm_g'] = f(inp['mlstm_norm_g'][0][None, :])
    m['hy_w1'] = f(inp['hy_w1'][0])
    m['hy_b1c'] = f(inp['hy_b1'][0][:, None])
    m['hy_w2'] = f(inp['hy_w2'][0])
    m['hy_b2c'] = f(inp['hy_b2'][0][:, None])
    m['hy_w3'] = f(inp['hy_w3'][0])
    m['hy_frc'] = f(inp['hy_freq'][0][:, None])
    m['hy_del_col'] = colmaj(inp['hy_deltas'][0], 16)
    m['hy_bias_col'] = colmaj(np.asarray(inp['hy_bias'][0]).reshape(-1), 8)
    m['hnorm_col'] = colmaj(inp['hyena_norm_g'][0], 4)
    m['w_out'] = f(inp['w_out'][0])
    m['gffn_row'] = f(inp['g_ffn'][0][None, :])
    m['w_router'] = f(inp['w_router'][0])
    m['w_gate'] = f(inp['w_gate'][0]).reshape(16 * 1024, 2048)
    m['w_up'] = f(inp['w_up'][0]).reshape(16 * 1024, 2048)
    m['w_down'] = f(inp['w_down'][0]).reshape(16 * 2048, 1024)
    m['gfin_row'] = f(np.asarray(inp['g_final'])[None, :])
    m.update(host_consts())
    return m

INPUT_SPECS = [
    ('x', [SEQ, DM], F32), ('ccol', [128, 8], F32), ('w_ada', [DM, 6144], F32), ('b_ada', [1, 6144], F32),
    ('gmix_col', [128, 8], F32), ('w_in', [DM, 3600], F32), ('bin_row', [1, 1040], F32),
    ('bqk_col', [128, 8], F32), ('bhy_col', [128, 12], F32), ('cqk_w', [128, 8, 3], F32), ('cqk_b', [128, 8], F32),
    ('chy_w', [128, 12, 3], F32), ('chy_b', [128, 12], F32), ('mnorm_g', [1, 512], F32),
    ('hy_w1', [33, 64], F32), ('hy_b1c', [64, 1], F32), ('hy_w2', [64, 64], F32), ('hy_b2c', [64, 1], F32),
    ('hy_w3', [64, 2048], F32), ('hy_frc', [64, 1], F32), ('hy_del_col', [128, 16], F32),
    ('hy_bias_col', [128, 8], F32), ('hnorm_col', [128, 4], F32), ('w_out', [DM, DM], F32),
    ('gffn_row', [1, DM], F32), ('w_router', [DM, 16], F32), ('w_gate', [16 * 1024, 2048], F32),
    ('w_up', [16 * 1024, 2048], F32), ('w_down', [16 * 2048, 1024], F32), ('gfin_row', [1, DM], F32),
    ('ident_bf', [128, 128], BF16), ('ident_f', [128, 128], F32), ('triU', [128, 128], F32),
    ('triL', [128, 128], F32), ('striU', [128, 128], F32), ('F1', [64, 128], BF16),
    ('GrT', [128, 8192], BF16), ('GiT', [128, 8192], BF16), ('GiNT', [128, 8192], BF16),
    ('Rc1', [128, 256], BF16), ('Rc2', [128, 256], BF16), ('LrT', [64, 4096], BF16), ('LiNT', [64, 4096], BF16),
    ('zT', [33, SEQ], F32), ('zrT', [33, SEQ], F32), ('t_row', [1, SEQ], F32), ('tr_row', [1, SEQ], F32),
]


class Ctx:
    pass


def build(stop_after=None, debug=False):
    nc = bass.Bass("TRN2", target_bir_lowering=False)
    es = ExitStack()
    S = Sched(nc, es)
    C = Ctx()
    C.nc, C.S, C.es = nc, S, es
    C.debug = debug
    D = {}
    for name, shape, dt in INPUT_SPECS:
        D[name] = nc.dram_tensor(name, shape, dt, kind="ExternalInput").ap()
    D['out'] = nc.dram_tensor("out", [SEQ, DM], F32, kind="ExternalOutput").ap()

    def scratch(name, shape, dt):
        D[name] = nc.dram_tensor(name, shape, dt, kind="Internal").ap()
    scratch('QTd', [512, SEQ], BF16)
    scratch('KTd', [512, SEQ], BF16)
    scratch('Vd', [SEQ, 512], BF16)
    scratch('Od', [SEQ, 512], BF16)
    scratch('X1d', [512, SEQ], F32)
    scratch('X2d', [512, SEQ], F32)
    scratch('Zd', [512, SEQ], BF16)
    scratch('Z1d', [512, SEQ], BF16)
    scratch('Kd0', [512, 2 * SEQ], BF16)
    scratch('Kd1', [512, 2 * SEQ], BF16)
    scratch('Ycv', [512, SEQ], BF16)
    scratch('Yt', [DM, SEQ], BF16)
    scratch('HFd', [SEQ, DM], BF16)
    scratch('acc', [SEQ, DM], F32)
    C.D = D
    C.dbg = {}

    def dbg_out(name, shape, dt=F32):
        t = nc.dram_tensor("dbg_" + name, shape, dt, kind="ExternalOutput").ap()
        C.dbg[name] = t
        return t
    C.dbg_out = dbg_out

    cnt = [0]

    def sb(name, shape, dt, st=None):
        cnt[0] += 1
        return (st or es).enter_context(nc.sbuf_tensor(f"s{cnt[0]}_{name}", shape, dt))

    def pst(name, shape, dt, st=None):
        cnt[0] += 1
        return (st or es).enter_context(nc.psum_tensor(f"p{cnt[0]}_{name}", shape, dt))
    C.sb, C.pst = sb, pst

    P = Ctx()
    C.P = P
    P.ident_bf = sb("ident_bf", [128, 128], BF16)
    P.ident_f = sb("ident_f", [128, 128], F32)
    P.ones_f = sb("ones_f", [128, 128], F32)
    P.modcol = sb("modcol", [128, 48], F32)
    P.A1col = sb("A1col", [128, 8], F32)
    P.gt1rep = sb("gt1rep", [128, DM], F32)
    P.gt2rep = sb("gt2rep", [128, DM], F32)
    P.A2rep = sb("A2rep", [128, DM], F32)
    P.B2rep = sb("B2rep", [128, DM], F32)
    S.dma(P.ident_bf[:, :], D['ident_bf'][:, :])
    S.dma(P.ident_f[:, :], D['ident_f'][:, :])
    S.memset(P.ones_f[:, :], 1.0)

    phases = [phase_mod, phase_norm_proj, phase_mlstm, phase_hyena, phase_outproj, phase_route, phase_experts, phase_final]
    for ph in phases:
        ph(C)
        if stop_after == ph.__name__:
            break
    outs = [D['out'][:, :]] + [t for t in C.dbg.values()]
    S.finish(outs)
    S.emit()
    return nc, C


def phase_mod(C):
    S, D, P, sb, pst = C.S, C.D, C.P, C.sb, C.pst
    with S.scope() as ph:
        wada = [sb(f"wada{i}", [128, 8, 512], F32, ph) for i in range(2)]
        modrow = sb("modrow", [1, 6144], F32, ph)
        badar = sb("badar", [1, 6144], F32, ph)
        ccol = sb("ccol_sb", [128, 8], F32, ph)
        gmixc = sb("gmixc", [128, 8], F32, ph)
        gffn_rep = sb("gffn_rep", [128, DM], F32, ph)
        sc2rep = sb("sc2rep", [128, DM], F32, ph)
        ps = pst("ps_mod", [128, 512], F32, ph)
        psc = pst("ps_modc", [128, 512], F32, ph)
        psr = [pst(f"ps_modr{i}", [128, 512], F32, ph) for i in range(2)]
        S.dma(badar[:, :], D['b_ada'][:, :])
        S.dma(ccol[:, :], D['ccol'][:, :])
        S.dma(gmixc[:, :], D['gmix_col'][:, :])
        S.dma(gffn_rep[:, :], D['gffn_row'][0:1, :].partition_broadcast(128))
        wsrc = D['w_ada'].rearrange("(k p) c -> p k c", p=128)
        for blk in range(12):
            buf = wada[blk % 2]
            S.dma(buf[:, :, :], wsrc[:, :, blk * 512:(blk + 1) * 512])
            for k in range(8):
                S.mm(ps[0:1, :], ccol[:, k:k + 1], buf[:, k, :], start=(k == 0), stop=(k == 7))
            S.tt(modrow[0:1, blk * 512:(blk + 1) * 512], ps[0:1, :], badar[0:1, blk * 512:(blk + 1) * 512], ALU.add)
        for oc in range(48):
            S.mm(psc[:, oc:oc + 1], modrow[0:1, oc * 128:(oc + 1) * 128], P.ones_f[0:1, 0:1])
        S.copy(P.modcol[:, :], psc[:, 0:48])
        S.stt(P.A1col[:, :], P.modcol[:, 8:16], 1.0, gmixc[:, :], ALU.add, ALU.mult)
        n = 0
        for (dst, j) in ((P.gt1rep, 2), (P.gt2rep, 5), (sc2rep, 4), (P.B2rep, 3)):
            for h in range(2):
                pr = psr[n % 2]
                n += 1
                S.mm(pr[:, :], P.ones_f[0:1, 0:128], modrow[0:1, j * 1024 + h * 512: j * 1024 + (h + 1) * 512])
                S.copy(dst[:, h * 512:(h + 1) * 512], pr[:, :], eng=('act' if n % 2 else 'dve'))
        S.stt(P.A2rep[:, :], sc2rep[:, :], 1.0, gffn_rep[:, :], ALU.add, ALU.mult)
        if C.debug:
            d = C.dbg_out('modcol', [128, 48])
            S.dma(d[:, :], P.modcol[:, :])
            d = C.dbg_out('gt1rep', [128, DM])
            S.dma(d[:, :], P.gt1rep[:, :])


def phase_norm_proj(C):
    S, D, P, sb, pst = C.S, C.D, C.P, C.sb, C.pst
    P.Gt = sb("Gt", [128, NT, 16], F32)
    with S.scope() as ph:
        hT = sb("hT", [128, 8, SEQ], BF16, ph)
        with S.scope() as p1:
            xb = [sb(f"xb{i}", [128, DM], F32, p1) for i in range(2)]
            xn = [sb(f"xn{i}", [128, DM], BF16, p1) for i in range(2)]
            junk = sb("junk", [128, DM], BF16, p1)
            ss = sb("ss", [128, NT], F32, p1)
            rs = sb("rs", [128, NT], F32, p1)
            psT = [pst(f"psT{i}", [128, DM], BF16, p1) for i in range(2)]
            for j in range(NT):
                xt = xb[j % 2]
                S.dma(xt[:, :], D['x'][j * 128:(j + 1) * 128, :])
                S.act(junk[:, :], xt[:, :], AF.Square, accum_out=ss[:, j:j + 1])
                S.act(rs[:, j:j + 1], ss[:, j:j + 1], AF.Ln, scale=1.0 / DM, bias=EPS)
                S.act(rs[:, j:j + 1], rs[:, j:j + 1], AF.Exp, scale=-0.5)
                S.act(xn[j % 2][:, :], xt[:, :], AF.Copy, scale=rs[:, j:j + 1])
                pt = psT[j % 2]
                for k in range(8):
                    S.transpose(pt[:, k * 128:(k + 1) * 128], xn[j % 2][:, k * 128:(k + 1) * 128], P.ident_bf[:, :])
                for k in range(8):
                    S.act(hT[:, k, j * 128:(j + 1) * 128], pt[:, k * 128:(k + 1) * 128], AF.Identity,
                          scale=P.A1col[:, k:k + 1], bias=P.modcol[:, k:k + 1])
        if C.debug:
            d = C.dbg_out('hT', [128, 8, SEQ], BF16)
            for k in range(8):
                S.dma(d[:, k, :], hT[:, k, :])
        with S.scope() as p2:
            wvo = sb("wvo", [128, 8, 1040], BF16, p2)
            brow = sb("brow", [128, 1040], F32, p2)
            vt = [sb(f"vt{i}", [128, 512], BF16, p2) for i in range(2)]
            ot = [sb(f"ot{i}", [128, 512], BF16, p2) for i in range(2)]
            otf = [sb(f"otf{i}", [128, 512], F32, p2) for i in range(2)]
            psV = [pst(f"psV{i}", [128, 512], F32, p2) for i in range(2)]
            psO = [pst(f"psO{i}", [128, 512], F32, p2) for i in range(2)]
            psG = [pst(f"psG{i}", [128, 512], F32, p2) for i in range(2)]
            wsrc = D['w_in'].rearrange("(k p) c -> p k c", p=128)
            S.dma(wvo[:, :, 0:512], wsrc[:, :, 1024:1536], q='pool')
            S.dma(wvo[:, :, 512:1040], wsrc[:, :, 1536:2064], q='pool')
            S.dma(brow[:, :], D['bin_row'][0:1, :].partition_broadcast(128))
            for j in range(NT):
                a = j % 2
                for (pp, c0, c1) in ((psV[a], 0, 512), (psO[a], 512, 1024), (psG[a], 1024, 1040)):
                    for k in range(8):
                        S.mm(pp[:, 0:c1 - c0], hT[:, k, j * 128:(j + 1) * 128], wvo[:, k, c0:c1], start=(k == 0), stop=(k == 7))
                S.tt(vt[a][:, :], psV[a][:, :], brow[:, 0:512], ALU.add)
                S.dma(D['Vd'][j * 128:(j + 1) * 128, :], vt[a][:, :])
                S.tt(otf[a][:, :], psO[a][:, :], brow[:, 512:1024], ALU.add)
                S.act(ot[a][:, :], otf[a][:, :], AF.Sigmoid)
                S.dma(D['Od'][j * 128:(j + 1) * 128, :], ot[a][:, :])
                S.tt(P.Gt[:, j, :], psG[a][:, 0:16], brow[:, 1024:1040], ALU.add)
        if C.debug:
            d = C.dbg_out('Gt', [128, NT, 16])
            S.dma(d[:, :, :], P.Gt[:, :, :])
        with S.scope() as p3:
            wch = [sb(f"wch{i}", [128, 8, 128], BF16, p3) for i in range(3)]
            pre = [sb(f"pre{i}", [128, SEQ + 2], F32, p3) for i in range(2)]
            t0s = [sb(f"cv_t0{i}", [128, 2048], F32, p3) for i in range(2)]
            t2s = [sb(f"cv_t2{i}", [128, 2048], F32, p3) for i in range(2)]
            t3 = [[sb(f"cv_t3{q}{i}", [128, 2048], F32, p3) for i in range(2)] for q in range(2)]
            ob = [[sb(f"cv_ob{q}{i}", [128, 2048], BF16, p3) for i in range(2)] for q in range(2)]
            pending = [None]
            bqk = sb("bqk", [128, 8], F32, p3)
            bhy = sb("bhy", [128, 12], F32, p3)
            cqw = sb("cqw", [128, 8, 3], F32, p3)
            cqb = sb("cqb", [128, 8], F32, p3)
            chw = sb("chw", [128, 12, 3], F32, p3)
            chb = sb("chb", [128, 12], F32, p3)
            psF = [pst(f"psF{i}", [128, 512], F32, p3) for i in range(8)]
            for (tile_, nm) in ((bqk, 'bqk_col'), (bhy, 'bhy_col'), (cqb, 'cqk_b'), (chb, 'chy_b')):
                S.dma(tile_[:, :], D[nm][:, :])
            S.dma(cqw[:, :, :], D['cqk_w'][:, :, :])
            S.dma(chw[:, :, :], D['chy_w'][:, :, :])
            for i in range(2):
                S.memset(pre[i][:, 0:1], 0.0)
                S.memset(pre[i][:, SEQ + 1:SEQ + 2], 0.0)
            wsrc = D['w_in'].rearrange("(k p) c -> p k c", p=128)
            chunks = []
            for cc in range(8):
                chunks.append(('qk', cc, cc * 128))
            for i in range(12):
                chunks.append(('hy', i, 2064 + i * 128))
            nps = 0
            for m_ in range(2):
                S.dma(wch[m_ % 3][:, :, :], wsrc[:, :, chunks[m_][2]:chunks[m_][2] + 128], q='pool')
            for n, (kind, ci, col0) in enumerate(chunks):
                wc = wch[n % 3]
                pr = pre[n % 2]
                if n + 2 < len(chunks):
                    S.dma(wch[(n + 2) % 3][:, :, :], wsrc[:, :, chunks[n + 2][2]:chunks[n + 2][2] + 128], q='pool')
                bcol = bqk[:, ci:ci + 1] if kind == 'qk' else bhy[:, ci:ci + 1]
                cw = cqw if kind == 'qk' else chw
                cb = cqb if kind == 'qk' else chb
                for tb in range(8):
                    pp = psF[nps % 8]
                    nps += 1
                    for k in range(8):
                        S.mm(pp[:, :], wc[:, k, :], hT[:, k, tb * 512:(tb + 1) * 512], start=(k == 0), stop=(k == 7))
                    if tb % 2 == 0:
                        S.act(pr[:, 1 + tb * 512:1 + (tb + 1) * 512], pp[:, :], AF.Identity, bias=bcol)
                    else:
                        S.ts(pr[:, 1 + tb * 512:1 + (tb + 1) * 512], pp[:, :], bcol, ALU.add)
                par = n % 2
                for hh in range(2):
                    o0 = hh * 2048
                    S.act(t0s[hh][:, :], pr[:, o0:o0 + 2048], AF.Identity, scale=cw[:, ci, 0:1], bias=cb[:, ci:ci + 1])
                    S.act(t2s[hh][:, :], pr[:, o0 + 2:o0 + 2050], AF.Identity, scale=cw[:, ci, 2:3])
                for hh in range(2):
                    o0 = hh * 2048
                    S.stt(t0s[hh][:, :], pr[:, o0 + 1:o0 + 2049], cw[:, ci, 1:2], t0s[hh][:, :], ALU.mult, ALU.add)
                for hh in range(2):
                    if kind == 'hy' and ci >= 8:
                        S.tt(ob[par][hh][:, :], t0s[hh][:, :], t2s[hh][:, :], ALU.add, eng='pool')
                    else:
                        S.tt(t3[par][hh][:, :], t0s[hh][:, :], t2s[hh][:, :], ALU.add, eng='pool')
                if pending[0] is not None:
                    pending[0]()

                def fin(kind=kind, ci=ci, par=par):
                    for hh in range(2):
                        o0 = hh * 2048
                        if kind == 'qk':
                            S.act(ob[par][hh][:, :], t3[par][hh][:, :], AF.Silu)
                            dst = D['QTd'] if ci < 4 else D['KTd']
                            S.dma(dst[(ci % 4) * 128:(ci % 4 + 1) * 128, o0:o0 + 2048], ob[par][hh][:, :])
                        elif ci < 8:
                            dst = D['X1d'] if ci < 4 else D['X2d']
                            S.dma(dst[(ci % 4) * 128:(ci % 4 + 1) * 128, o0:o0 + 2048], t3[par][hh][:, :])
                        else:
                            S.dma(D['Zd'][(ci - 8) * 128:(ci - 7) * 128, o0:o0 + 2048], ob[par][hh][:, :])
                pending[0] = fin
            pending[0]()
    if C.debug:
        for nm, shp, dt in (('QTd', [512, SEQ], BF16), ('KTd', [512, SEQ], BF16), ('Vd', [SEQ, 512], BF16),
                            ('Od', [SEQ, 512], BF16), ('X1d', [512, SEQ], F32), ('Zd', [512, SEQ], BF16)):
            d = C.dbg_out(nm, shp, dt)
            with S.scope() as pd:
                if shp[0] == 512:
                    tmp = sb("dbgtmp_" + nm, [128, 4, SEQ], dt, pd)
                    S.dma(tmp[:, :, :], D[nm].rearrange("(a p) n -> p a n", p=128))
                    S.dma(d.rearrange("(a p) n -> p a n", p=128), tmp[:, :, :])
                else:
                    tmp = sb("dbgtmp_" + nm, [128, NT, 512], dt, pd)
                    S.dma(tmp[:, :, :], D[nm].rearrange("(a p) n -> p a n", p=128))
                    S.dma(d.rearrange("(a p) n -> p a n", p=128), tmp[:, :, :])


def phase_mlstm(C):
    S, D, P, sb, pst = C.S, C.D, C.P, C.sb, C.pst
    Gt = P.Gt
    with S.scope() as ph:
        triU = sb("triU", [128, 128], F32, ph)
        triL = sb("triL", [128, 128], F32, ph)
        LF = sb("LF", [128, 2, NT, 4], F32, ph)
        II = sb("II", [128, 2, NT, 4], F32, ph)
        Bc = sb("Bc", [128, 2, NT, 4], F32, ph)
        Wp = sb("Wp", [128, 2, NT, 4], F32, ph)
        ENB = sb("ENB", [128, 2, NT, 4], F32, ph)
        EG = sb("EG", [128, 2, NT, 4], F32, ph)
        zcol = sb("zcol", [128, 1], F32, ph)
        S.dma(triU[:, :], D['triU'][:, :])
        S.dma(triL[:, :], D['triL'][:, :])
        S.memset(zcol[:, :], 0.0)
        with S.scope() as pp:
            psB = pst("psB", [128, 512], F32, pp)
            psGs = pst("psGs", [128, 512], F32, pp)
            for d in range(2):
                S.act(LF[:, d, :, :], Gt[:, :, d * 8 + 4:d * 8 + 8], AF.Exp, scale=-1.0)
                S.copy(II[:, d, :, :], Gt[:, :, d * 8:d * 8 + 4])
            fl = lambda t: t[:, :, :, :].rearrange("p d c e -> p (d c e)")
            S.act(fl(LF), fl(LF), AF.Ln, bias=1.0)
            S.ts(fl(LF), fl(LF), -1.0, ALU.mult)
            S.mm(psB[:, 0:128], triU[:, :], fl(LF)[:, 0:128])
            S.mm(psB[:, 128:256], triL[:, :], fl(LF)[:, 128:256])
            S.mm(psGs[:, 0:256], P.ones_f[:, :], fl(LF))
            S.copy(fl(Bc), psB[:, 0:256])
            S.act(fl(EG), psGs[:, 0:256], AF.Exp)
            S.tt(fl(Wp), fl(II), fl(Bc), ALU.subtract)
            S.act(fl(Wp), fl(Wp), AF.Exp, bias=math.log(128.0 ** -0.5))
            S.act(fl(ENB), fl(Bc), AF.Exp, scale=-1.0)
        if C.debug:
            for nm, t in (('Bc', Bc), ('Wp', Wp), ('EG', EG)):
                d_ = C.dbg_out(nm, [128, 2, NT, 4])
                S.dma(d_[:, :, :, :], t[:, :, :, :])
        for hd in range(4):
            with S.scope() as hs:
                qT = sb("qT", [128, SEQ], BF16, hs)
                kT = sb("kT", [128, SEQ], BF16, hs)
                vaug = sb("vaug", [128, NT, 129], BF16, hs)
                osg = sb("osg", [128, NT, 128], BF16, hs)
                ktm = sb("ktm", [128, NT, 128], BF16, hs)
                Hs = sb("Hs", [128, NT, 128], F32, hs)
                sq = sb("sq", [128, NT, 128], F32, hs)
                gO = sb("gO", [128, NT, 128], BF16, hs)
                ym = sb("ym", [128, NT, 128], BF16, hs)
                ymT = sb("ymT", [128, SEQ], BF16, hs)
                mng = sb("mng", [128, 128], F32, hs)
                STw = [sb(f"STw{i}", [128, 128], BF16, hs) for i in range(4)]
                vw = [sb(f"vw{i}", [128, 129], BF16, hs) for i in range(4)]
                Tst = sb("Tst", [128, 129], F32, hs)
                Tst2 = sb("Tst2", [128, 129], F32, hs)
                Cbfd = [[sb(f"Cbf{d_}{i}", [128, 129], BF16, hs) for i in range(2)] for d_ in range(2)]
                sm = [sb(f"sm{i}", [128, 4], F32, hs) for i in range(2)]
                ssq = sb("ssq", [128, NT], F32, hs)
                pS = [pst(f"pS{i}", [128, 512], F32, hs) for i in range(2)]
                pO = [pst(f"pO{i}", [128, 512], F32, hs) for i in range(2)]
                pC = [pst(f"pC{i}", [128, 512], F32, hs) for i in range(2)]
                pK = pst("pK", [128, 1024], BF16, hs)
                S.dma(qT[:, :], D['QTd'][hd * 128:(hd + 1) * 128, :])
                S.dma(kT[:, :], D['KTd'][hd * 128:(hd + 1) * 128, :])
                S.dma(vaug[:, :, 0:128], D['Vd'][:, hd * 128:(hd + 1) * 128].rearrange("(c p) d -> p c d", p=128))
                S.memset(vaug[:, :, 128:129], 1.0)
                S.dma(osg[:, :, :], D['Od'][:, hd * 128:(hd + 1) * 128].rearrange("(c p) d -> p c d", p=128))
                S.dma(mng[:, :], D['mnorm_g'][0:1, hd * 128:(hd + 1) * 128].partition_broadcast(128))
                for c0 in range(0, NT, 8):
                    for c in range(c0, c0 + 8):
                        S.transpose(pK[:, (c - c0) * 128:(c - c0 + 1) * 128], kT[:, c * 128:(c + 1) * 128], P.ident_bf[:, :])
                    S.copy(ktm[:, c0:c0 + 8, :].rearrange("p c d -> p (c d)"), pK[:, :], eng=('act' if (c0 // 8) % 2 else 'dve'))
                n = 0
                Tsts = [Tst, Tst2]
                prevc = [None, None]
                visited = set()
                for d in range(2):
                    S.memset(Tsts[d][:, :], 0.0)
                    S.memset(Cbfd[d][0][:, :], 0.0)
                nd = [0, 0]
                steps = []
                for i in range(NT):
                    for d in range(2):
                        steps.append((d, i if d == 0 else NT - 1 - i))

                def phase1(n):
                    d, c = steps[n]
                    cs = slice(c * 128, (c + 1) * 128)
                    wcol = Wp[:, d, c, hd:hd + 1]
                    mask = triU if d == 0 else triL
                    S.mm(pS[n % 2][:, 0:128], kT[:, cs], qT[:, cs])
                    S.stt(STw[n % 4][:, :], pS[n % 2][:, 0:128], wcol, mask[:, :], ALU.mult, ALU.mult)
                    S.act(vw[n % 4][:, :], vaug[:, c, :], AF.Copy, scale=wcol)

                def phase2(n):
                    d, c = steps[n]
                    a = n % 2
                    b_ = nd[d] % 2
                    nd[d] += 1
                    cs = slice(c * 128, (c + 1) * 128)
                    S.mm(pO[a][:, 0:129], STw[n % 4][:, :], vaug[:, c, :], start=True, stop=False)
                    S.mm(pO[a][:, 0:129], qT[:, cs], Cbfd[d][b_][:, :], start=False, stop=True)
                    S.mm(pC[a][:, 0:129], ktm[:, c, :], vw[n % 4][:, :])
                    enb = ENB[:, d, c, hd:hd + 1]
                    s_ = sm[a]
                    S.ts(s_[:, 0:1], pO[a][:, 128:129], enb, ALU.max)
                    S.stt(s_[:, 2:3], pO[a][:, 128:129], -1.0, s_[:, 0:1], ALU.mult, ALU.max)
                    S.recip(s_[:, 3:4], s_[:, 2:3])
                    if c not in visited:
                        visited.add(c)
                        S.act(Hs[:, c, :], pO[a][:, 0:128], AF.Copy, scale=s_[:, 3:4])
                    else:
                        S.stt(Hs[:, c, :], pO[a][:, 0:128], s_[:, 3:4], Hs[:, c, :], ALU.mult, ALU.add)
                    egp = zcol[:, 0:1] if prevc[d] is None else EG[:, d, prevc[d], hd:hd + 1]
                    S.stt(Tsts[d][:, :], Tsts[d][:, :], egp, pC[a][:, 0:129], ALU.mult, ALU.add)
                    S.act(Cbfd[d][(b_ + 1) % 2][:, :], Tsts[d][:, :], AF.Copy, scale=EG[:, d, c, hd:hd + 1])
                    prevc[d] = c
                phase1(0)
                for n_ in range(len(steps)):
                    if n_ + 1 < len(steps):
                        phase1(n_ + 1)
                    phase2(n_)
                S.act(sq[:, :, :], Hs[:, :, :], AF.Square)
                S.reduce(ssq[:, :], sq[:, :, :], ALU.add)
                S.ts(ssq[:, :], ssq[:, :], 1.0 / 128.0, ALU.mult, s2=EPS, op1=ALU.add)
                S.act(ssq[:, :], ssq[:, :], AF.Sqrt)
                S.recip(ssq[:, :], ssq[:, :])
                S.tt(gO[:, :, :], osg[:, :, :], mng[:, :].unsqueeze(1).to_broadcast([128, NT, 128]), ALU.mult)
                for c in range(NT):
                    S.stt(ym[:, c, :], Hs[:, c, :], ssq[:, c:c + 1], gO[:, c, :], ALU.mult, ALU.mult)
                for c0 in range(0, NT, 8):
                    for c in range(c0, c0 + 8):
                        S.transpose(pK[:, (c - c0) * 128:(c - c0 + 1) * 128], ym[:, c, :], P.ident_bf[:, :])
                    S.copy(ymT[:, c0 * 128:(c0 + 8) * 128], pK[:, :], eng=('act' if (c0 // 8) % 2 else 'dve'))
                S.dma(D['Yt'][hd * 128:(hd + 1) * 128, :], ymT[:, :])
    if C.debug:
        d_ = C.dbg_out('Yt_m', [512, SEQ], BF16)
        with S.scope() as pd:
            tmp = sb("dbgtmp_ytm", [128, 4, SEQ], BF16, pd)
            S.dma(tmp[:, :, :], D['Yt'][0:512, :].rearrange("(a p) n -> p a n", p=128))
            S.dma(d_.rearrange("(a p) n -> p a n", p=128), tmp[:, :, :])


def phase_hyena(C):
    S, D, P, sb, pst = C.S, C.D, C.P, C.sb, C.pst
    PI = math.pi
    with S.scope() as pa:
        hid2T = sb("hid2T", [64, SEQ], F32, pa)
        hid2rT = sb("hid2rT", [64, SEQ], F32, pa)
        frc = sb("frc", [64, 1], F32, pa)
        frb1 = sb("frb1", [64, 1], F32, pa)
        frb2 = sb("frb2", [64, 1], F32, pa)
        with S.scope() as p1:
            zT = sb("zT", [33, SEQ], F32, p1)
            zrT = sb("zrT", [33, SEQ], F32, p1)
            hid1 = sb("hid1", [64, SEQ], F32, p1)
            w1 = sb("hw1", [33, 64], F32, p1)
            w2 = sb("hw2", [64, 64], F32, p1)
            b1c = sb("b1c", [64, 1], F32, p1)
            b2c = sb("b2c", [64, 1], F32, p1)
            arg = [sb(f"harg{i}", [64, 512], F32, p1) for i in range(2)]
            m1 = [sb(f"hm1{i}", [64, 512], F32, p1) for i in range(2)]
            m2 = [sb(f"hm2{i}", [64, 512], F32, p1) for i in range(2)]
            psM = [pst(f"psM{i}", [128, 512], F32, p1) for i in range(2)]
            S.dma(zT[:, :], D['zT'][:, :])
            S.dma(zrT[:, :], D['zrT'][:, :])
            S.dma(w1[:, :], D['hy_w1'][:, :])
            S.dma(w2[:, :], D['hy_w2'][:, :])
            S.dma(b1c[:, :], D['hy_b1c'][:, :])
            S.dma(b2c[:, :], D['hy_b2c'][:, :])
            S.dma(frc[:, :], D['hy_frc'][:, :])
            S.tt(frb1[:, :], frc[:, :], b1c[:, :], ALU.mult)
            S.tt(frb2[:, :], frc[:, :], b2c[:, :], ALU.mult)
            n = 0

            def sin_layer(ps, frb, dst):
                nonlocal n
                a = n % 2
                n += 1
                S.ts(arg[a][:, :], ps, frc[:, 0:1], ALU.mult, s2=frb[:, 0:1], op1=ALU.add)
                S.ts(m1[a][:, :], arg[a][:, :], PI, ALU.is_gt, s2=-2.0 * PI, op1=ALU.mult)
                S.ts(m2[a][:, :], arg[a][:, :], -PI, ALU.is_lt, s2=2.0 * PI, op1=ALU.mult)
                S.tt(arg[a][:, :], arg[a][:, :], m1[a][:, :], ALU.add)
                S.tt(arg[a][:, :], arg[a][:, :], m2[a][:, :], ALU.add)
                S.act(dst, arg[a][:, :], AF.Sin)
            for (zs, hdst) in ((zT, hid2T), (zrT, hid2rT)):
                for blk in range(8):
                    ps = psM[blk % 2]
                    S.mm(ps[0:64, :], w1[:, :], zs[:, blk * 512:(blk + 1) * 512])
                    sin_layer(ps[0:64, :], frb1, hid1[:, blk * 512:(blk + 1) * 512])
                for blk in range(8):
                    ps = psM[blk % 2]
                    S.mm(ps[0:64, :], w2[:, :], hid1[:, blk * 512:(blk + 1) * 512])
                    sin_layer(ps[0:64, :], frb2, hdst[:, blk * 512:(blk + 1) * 512])
        with S.scope() as p2:
            w3 = sb("hw3", [64, 2048], F32, p2)
            trow = sb("trow", [128, SEQ], F32, p2)
            trrow = sb("trrow", [128, SEQ], F32, p2)
            ndel = sb("ndel", [128, 16], F32, p2)
            hbias = sb("hbias", [128, 8], F32, p2)
            kT = [sb(f"kTb{i}", [128, 2 * SEQ], BF16, p2) for i in range(2)]
            win = [sb(f"win{i}", [128, 512], F32, p2) for i in range(2)]
            k0t = sb("k0t", [128, 4], F32, p2)
            psK_ = [pst(f"psKf{i}", [128, 512], F32, p2) for i in range(3)]
            ps0 = pst("psK0", [128, 512], F32, p2)
            S.dma(w3[:, :], D['hy_w3'][:, :])
            S.dma(trow[:, :], D['t_row'][0:1, :].partition_broadcast(128))
            S.dma(trrow[:, :], D['tr_row'][0:1, :].partition_broadcast(128))
            S.dma(ndel[:, :], D['hy_del_col'][:, :])
            S.dma(hbias[:, :], D['hy_bias_col'][:, :])
            S.stt(ndel[:, :], ndel[:, :], -1.0, ndel[:, :], ALU.mult, ALU.max)
            S.ts(ndel[:, :], ndel[:, :], -1.0, ALU.mult)
            nb = 0
            nk = 0
            for o in range(2):
                for g in range(4):
                    kt = kT[nk % 2]
                    nk += 1
                    for d in range(2):
                        hs = hid2T if d == 0 else hid2rT
                        tr = trow if d == 0 else trrow
                        c0 = (o * 2 + d) * 512 + g * 128
                        di = o * 8 + d * 4 + g
                        for blk in range(8):
                            ps = psK_[nb % 3]
                            wn = win[nb % 2]
                            nb += 1
                            S.mm(ps[:, :], w3[:, c0:c0 + 128], hs[:, blk * 512:(blk + 1) * 512])
                            S.act(wn[:, :], tr[:, blk * 512:(blk + 1) * 512], AF.Exp, scale=ndel[:, di:di + 1])
                            S.stt(kt[:, d * SEQ + blk * 512:d * SEQ + (blk + 1) * 512], wn[:, :], 0.05, ps[:, :], ALU.add, ALU.mult)
                    S.memset(kt[:, SEQ:SEQ + 1], 0.0)
                    cf = (o * 2 + 0) * 512 + g * 128
                    cb = (o * 2 + 1) * 512 + g * 128
                    S.mm(ps0[:, 0:1], w3[:, cf:cf + 128], hid2T[:, 0:1])
                    S.mm(ps0[:, 1:2], w3[:, cb:cb + 128], hid2T[:, 0:1])
                    S.copy(k0t[:, 0:2], ps0[:, 0:2])
                    S.tt(k0t[:, 2:3], k0t[:, 0:1], k0t[:, 1:2], ALU.add)
                    S.ts(k0t[:, 3:4], k0t[:, 2:3], 1.05, ALU.mult)
                    S.tt(kt[:, 0:1], k0t[:, 3:4], hbias[:, o * 4 + g:o * 4 + g + 1], ALU.add)
                    dst = D['Kd0'] if o == 0 else D['Kd1']
                    S.dma(dst[g * 128:(g + 1) * 128, :], kt[:, :])
    if C.debug:
        d_ = C.dbg_out('Kd0', [512, 2 * SEQ], BF16)
        with S.scope() as pd:
            tmp = sb("dbgtmp_kd", [128, 4, 2 * SEQ], BF16, pd)
            S.dma(tmp[:, :, :], D['Kd0'].rearrange("(a p) n -> p a n", p=128))
            S.dma(d_.rearrange("(a p) n -> p a n", p=128), tmp[:, :, :])
    with S.scope() as pb:
        T = Ctx()
        T.F1 = sb("F1", [64, 128], BF16, pb)
        T.GrT = sb("GrT", [128, 64, 128], BF16, pb)
        T.GiT = sb("GiT", [128, 64, 128], BF16, pb)
        T.Rc1 = sb("Rc1", [128, 256], BF16, pb)
        T.Rc2 = sb("Rc2", [128, 256], BF16, pb)
        T.LrT = sb("LrT", [64, 128, 32], BF16, pb)
        T.LiNT = sb("LiNT", [64, 128, 32], BF16, pb)
        hnorm = sb("hnorm", [128, 4], F32, pb)
        S.dma(T.F1[:, :], D['F1'][:, :])
        for nm in ('GrT', 'GiT'):
            S.dma(getattr(T, nm)[:, :, :], D[nm].rearrange("p (k m) -> p k m", k=64))
        S.dma(T.Rc1[:, :], D['Rc1'][:, :])
        S.dma(T.Rc2[:, :], D['Rc2'][:, :])
        S.dma(T.LrT[:, :, :], D['LrT'].rearrange("p (n m) -> p n m", n=128))
        S.dma(T.LiNT[:, :, :], D['LiNT'].rearrange("p (n m) -> p n m", n=128))
        S.dma(hnorm[:, :], D['hnorm_col'][:, :])
        for o in range(2):
            zsrc = D['Zd'] if o == 0 else D['Z1d']
            kd = D['Kd0'] if o == 0 else D['Kd1']
            with S.scope() as pw:
                W = Ctx()
                W.X = sb("fX", [64, 64, 128], BF16, pw)
                W.AT = sb("fAT", [128, 64, 2, 64], BF16, pw)
                W.ATn = sb("fATn", [128, 64, 2, 64], BF16, pw)
                W.Kf = sb("fKf", [128, 64, 2, 64], BF16, pw)
                W.V = sb("fV", [128, 2, 64, 64], BF16, pw)
                W.Wt = sb("fWt", [64, 64, 2, 128], BF16, pw)
                W.Ysb = sb("fY", [32, 64, 128], BF16, pw)
                W.tm = [[sb(f"ftm{i}{j}", [128, 4, 64], F32, pw) for j in range(4)] for i in range(2)]
                W.ring = [pst(f"psR{i}", [128, 512], F32, pw) for i in range(8)]
                W.nr = 0
                W.ne = 0
                W.pref = False
                for b8 in range(8):
                    ch0 = b8 * 64
                    fft_batch(C, T, W, zsrc[ch0:ch0 + 64, :], kd[ch0:ch0 + 64, :], D['Ycv'][ch0:ch0 + 64, :],
                              kd_next=(kd[ch0 + 64:ch0 + 128, :] if b8 < 7 else None))
            with S.scope() as pg:
                ysb = [sb(f"gy{i}", [128, SEQ], BF16, pg) for i in range(2)]
                xsb = [sb(f"gx{i}", [128, SEQ], F32, pg) for i in range(2)]
                zo = [sb(f"gz{i}", [128, SEQ], BF16, pg) for i in range(2)]
                xsrc = D['X1d'] if o == 0 else D['X2d']
                if o == 1:
                    z2 = sb("gz2", [128, SEQ], F32, pg)
                    sq = [sb(f"gsq{i}", [128, 512], F32, pg) for i in range(2)]
                    rr = [sb(f"grr{i}", [128, 512], F32, pg) for i in range(2)]
                    psN = [pst(f"psN{i}", [128, 512], F32, pg) for i in range(2)]
                for g in range(4):
                    a = g % 2
                    S.dma(ysb[a][:, :], D['Ycv'][g * 128:(g + 1) * 128, :])
                    S.dma(xsb[a][:, :], xsrc[g * 128:(g + 1) * 128, :])
                    if o == 0:
                        S.tt(zo[a][:, :], xsb[a][:, :], ysb[a][:, :], ALU.mult)
                        S.dma(D['Z1d'][g * 128:(g + 1) * 128, :], zo[a][:, :], q='pool')
                    else:
                        S.tt(z2[:, :], xsb[a][:, :], ysb[a][:, :], ALU.mult)
                        for blk in range(8):
                            bs = slice(blk * 512, (blk + 1) * 512)
                            q_ = blk % 2
                            S.tt(sq[q_][:, :], z2[:, bs], z2[:, bs], ALU.mult)
                            S.mm(psN[q_][:, :], P.ones_f[:, :], sq[q_][:, :])
                            S.act(rr[q_][:, :], psN[q_][:, :], AF.Ln, scale=1.0 / 128.0, bias=EPS)
                            S.act(rr[q_][:, :], rr[q_][:, :], AF.Exp, scale=-0.5)
                            S.stt(zo[a][:, bs], z2[:, bs], hnorm[:, g:g + 1], rr[q_][:, :], ALU.mult, ALU.mult)
                        S.dma(D['Yt'][512 + g * 128:512 + (g + 1) * 128, :], zo[a][:, :], q='pool')
    if C.debug:
        d_ = C.dbg_out('Yt_h', [512, SEQ], BF16)
        with S.scope() as pd:
            tmp = sb("dbgtmp_yth", [128, 4, SEQ], BF16, pd)
            S.dma(tmp[:, :, :], D['Yt'][512:1024, :].rearrange("(a p) n -> p a n", p=128))
            S.dma(d_.rearrange("(a p) n -> p a n", p=128), tmp[:, :, :])


def fft_batch(C, T, W, zsrc, kdsrc, ydst, kd_next=None):
    S = C.S

    def bank():
        W.nr += 1
        return W.ring[W.nr % 8]

    def ev(dst, src):
        W.ne += 1
        S.copy(dst, src, eng=('act' if W.ne % 2 else 'dve'))

    def forward_d(kdim):
        for cq in range(16):
            pA = bank()
            for i in range(4):
                ch = cq * 4 + i
                S.mm(pA[:, i * 128:(i + 1) * 128], W.X[0:kdim, ch, :], T.F1[0:kdim, :])
            ev(W.AT[:, cq * 4:(cq + 1) * 4, :, :].rearrange("p c r k -> p (c r k)"), pA[:, :])
            src = pA[:, :].rearrange("p (c r k) -> p c r k", c=4, r=2)
            S.act(W.ATn[:, cq * 4:(cq + 1) * 4, 0, :], src[:, :, 1, :], AF.Copy, scale=-1.0)
            S.copy(W.ATn[:, cq * 4:(cq + 1) * 4, 1, :], src[:, :, 0, :])

    def forward_s(mode):
        for kb in range(16):
            pU = bank()
            for i in range(4):
                k1 = kb * 4 + i
                o_ = pU[:, i * 128:(i + 1) * 128]
                S.mm(o_, T.GrT[:, k1, :], W.AT[:, :, :, k1].rearrange("p c r -> p r c"), start=True, stop=False)
                S.mm(o_, T.GiT[:, k1, :], W.ATn[:, :, :, k1].rearrange("p c r -> p r c"), start=False, stop=True)
            if mode == 'kernel':
                ev(W.Kf[:, kb * 4:(kb + 1) * 4, :, :].rearrange("p k r c -> p (k r c)"), pU[:, :])
            else:
                pv = pU[:, :].rearrange("p (k r c) -> p k r c", k=4, r=2)
                Ur = pv[:, :, 0, :]
                Ui = pv[:, :, 1, :]
                Kr = W.Kf[:, kb * 4:(kb + 1) * 4, 0, :]
                Ki = W.Kf[:, kb * 4:(kb + 1) * 4, 1, :]
                t = W.tm[kb % 2]
                S.tt(t[0][:, :, :], Ur, Kr, ALU.mult)
                S.tt(t[1][:, :, :], Ui, Ki, ALU.mult)
                S.tt(t[2][:, :, :], Ur, Ki, ALU.mult)
                S.tt(t[3][:, :, :], Ui, Kr, ALU.mult)
                S.tt(W.V[:, 0, kb * 4:(kb + 1) * 4, :], t[0][:, :, :], t[1][:, :, :], ALU.subtract, eng='pool')
                S.tt(W.V[:, 1, kb * 4:(kb + 1) * 4, :], t[2][:, :, :], t[3][:, :, :], ALU.add, eng='pool')

    if not W.pref:
        S.dma(W.X[:, :, :], kdsrc.rearrange("c (a n) -> a c n", n=128))
    forward_d(64)
    forward_s('kernel')
    S.dma(W.X[0:32, :, :], zsrc.rearrange("c (a n) -> a c n", n=128))
    forward_d(32)
    W.pref = False
    if kd_next is not None:
        S.dma(W.X[:, :, :], kd_next.rearrange("c (a n) -> a c n", n=128))
        W.pref = True
    forward_s('data')
    for cp in range(32):
        pW = bank()
        for i in range(2):
            ch = cp * 2 + i
            o_ = pW[0:64, i * 256:(i + 1) * 256]
            S.mm(o_, W.V[:, 0, :, ch], T.Rc1[:, :], start=True, stop=False)
            S.mm(o_, W.V[:, 1, :, ch], T.Rc2[:, :], start=False, stop=True)
        ev(W.Wt[:, cp * 2:(cp + 1) * 2, :, :].rearrange("p c r n -> p (c r n)"), pW[0:64, :])
    for nb in range(16):
        pY = bank()
        for i in range(8):
            n2 = nb * 8 + i
            o_ = pY[0:32, i * 64:(i + 1) * 64]
            S.mm(o_, T.LrT[:, n2, :], W.Wt[:, :, 0, n2], start=True, stop=False)
            S.mm(o_, T.LiNT[:, n2, :], W.Wt[:, :, 1, n2], start=False, stop=True)
        ev(W.Ysb[:, :, nb * 8:(nb + 1) * 8], pY[0:32, :].rearrange("p (n c) -> p c n", n=8))
    S.dma(ydst.rearrange("c (a n) -> a c n", n=128), W.Ysb[:, :, :])


def phase_outproj(C):
    S, D, P, sb, pst, nc = C.S, C.D, C.P, C.sb, C.pst, C.nc
    P.IDX = sb("IDX", [128, 16, 4], I32)
    P.GW = sb("GW", [128, 16, 4], F32)
    with S.scope() as ph:
        AFF = sb("AFF", [128, NT, 16], F32, ph)
        AFFT = sb("AFFT", [16, SEQ], F32, ph)
        with S.scope() as p1:
            ytT = sb("ytT", [128, 8, SEQ], BF16, p1)
            wout = sb("wout", [128, 8, DM], BF16, p1)
            wr = sb("wr", [128, 8, 16], F32, p1)
            xb = [sb(f"oxb{i}", [128, DM], F32, p1) for i in range(2)]
            x1 = [sb(f"ox1{i}", [128, DM], F32, p1) for i in range(3)]
            tmp = [sb(f"otmp{i}", [128, DM], F32, p1) for i in range(2)]
            hf = [sb(f"ohf{i}", [128, DM], F32, p1) for i in range(3)]
            hfb = [sb(f"ohfb{i}", [128, DM], BF16, p1) for i in range(3)]
            hfT = [sb(f"ohfT{i}", [128, 8, 128], F32, p1) for i in range(2)]
            junk = sb("ojunk", [128, DM], BF16, p1)
            sm = sb("osm", [128, NT, 8], F32, p1)
            ex = [sb(f"oex{i}", [128, 16], F32, p1) for i in range(2)]
            psM = [[pst(f"psMo{i}{h}", [128, 512], F32, p1) for h in range(2)] for i in range(2)]
            psT = [pst(f"psTo{i}", [128, 512], F32, p1) for i in range(2)]
            psR = pst("psR", [128, 512], F32, p1)
            psAT = pst("psAT", [128, 512], F32, p1)
            psAT2 = psT[1]
            for k in range(8):
                S.dma(ytT[:, k, :], D['Yt'][k * 128:(k + 1) * 128, :])
            wsrc = D['w_out'].rearrange("(k p) c -> p k c", p=128)
            S.dma(wout[:, :, 0:512], wsrc[:, :, 0:512], q='pool')
            S.dma(wout[:, :, 512:1024], wsrc[:, :, 512:1024], q='pool')
            S.dma(wr[:, :, :], D['w_router'].rearrange("(k p) e -> p k e", p=128))
            def stage_a1(j):
                a = j % 2
                b = j % 3
                xt = xb[a]
                S.dma(xt[:, :], D['x'][j * 128:(j + 1) * 128, :])
                for h in range(2):
                    for k in range(8):
                        S.mm(psM[a][h][:, :], ytT[:, k, j * 128:(j + 1) * 128], wout[:, k, h * 512:(h + 1) * 512],
                             start=(k == 0), stop=(k == 7))
                for h in range(2):
                    S.tt(tmp[a][:, h * 512:(h + 1) * 512], psM[a][h][:, :], P.gt1rep[:, h * 512:(h + 1) * 512], ALU.mult)
                S.tt(x1[b][:, :], tmp[a][:, :], xt[:, :], ALU.add)
                S.dma(D['acc'][j * 128:(j + 1) * 128, :], x1[b][:, :], q='pool')

            def stage_a2(j):
                b = j % 3
                ss = sm[:, j, 0:1]
                rs = sm[:, j, 1:2]
                S.act(junk[:, :], x1[b][:, :], AF.Square, accum_out=ss)
                S.act(rs, ss, AF.Ln, scale=1.0 / DM, bias=EPS)
                S.act(rs, rs, AF.Exp, scale=-0.5)
                S.stt(hf[b][:, :], x1[b][:, :], rs, P.A2rep[:, :], ALU.mult, ALU.mult)
                S.tt(hf[b][:, :], hf[b][:, :], P.B2rep[:, :], ALU.add)
                S.act(hfb[b][:, :], hf[b][:, :], AF.Copy)
                S.dma(D['HFd'][j * 128:(j + 1) * 128, :], hfb[b][:, :], q='pool')

            def stage_b1(j):
                a = j % 2
                b = j % 3
                for k in range(8):
                    S.transpose(psT[k // 4][:, (k % 4) * 128:(k % 4 + 1) * 128], hf[b][:, k * 128:(k + 1) * 128], P.ident_f[:, :])
                S.copy(hfT[a][:, 0:4, :].rearrange("p k t -> p (k t)"), psT[0][:, :], eng='act')
                S.copy(hfT[a][:, 4:8, :].rearrange("p k t -> p (k t)"), psT[1][:, :], eng='dve')

            def stage_b2(j):
                a = j % 2
                for k in range(8):
                    S.mm(psR[:, 0:16], hfT[a][:, k, :], wr[:, k, :], start=(k == 0), stop=(k == 7))
                mx = sm[:, j, 2:3]
                se = sm[:, j, 3:4]
                S.reduce(mx, psR[:, 0:16], ALU.max)
                S.ts(mx, mx, -1.0, ALU.mult)
                S.act(ex[a][:, :], psR[:, 0:16], AF.Exp, bias=mx, accum_out=se)
                S.recip(se, se)
                S.ts(AFF[:, j, :], ex[a][:, :], se, ALU.mult)
            for step in range(NT + 2):
                if 0 <= step - 2 < NT:
                    stage_b1(step - 2)
                if step < NT:
                    stage_a1(step)
                if 0 <= step - 1 < NT:
                    stage_a2(step - 1)
                if 0 <= step - 2 < NT:
                    stage_b2(step - 2)
            for j in range(NT):
                pat = psAT if j % 2 == 0 else psAT2
                S.transpose(pat[0:16, 0:128], AFF[:, j, :], P.ident_f[:, :])
                S.copy(AFFT[:, j * 128:(j + 1) * 128], pat[0:16, 0:128], eng=('act' if j % 2 else 'dve'))
            if C.debug:
                pass
        if C.debug:
            d_ = C.dbg_out('AFF', [128, NT, 16])
            S.dma(d_[:, :, :], AFF[:, :, :])
        with S.scope() as p2:
            junkA = sb("junkA", [16, SEQ], F32, p2)
            bs = sb("bis", [16, 8], F32, p2)
            THR = sb("THR", [128, 16], F32, p2)
            throw = sb("throw", [1, 16], F32, p2)
            SEL = sb("SEL", [128, NT, 16], F32, p2)
            POS = sb("POS", [128, NT, 16], F32, p2)
            selcum = sb("selcum", [128, 16], F32, p2)
            striU = sb("striU", [128, 128], F32, p2)
            COORD = sb("COORD", [128, NT, 16, 4], BF16, p2)
            jf = sb("jf", [128, NT], F32, p2)
            pf = sb("pf", [128, 1], F32, p2)
            iot = sb("iot", [128, 512], mybir.dt.float16, p2)
            OH = [sb(f"OH{i}", [128, 512], BF16, p2) for i in range(3)]
            r4 = [sb(f"r4{i}", [128, 8], F32, p2) for i in range(2)]
            psI = [pst(f"psI{i}", [128, 512], F32, p2) for i in range(4)]
            psP = [pst(f"psP{i}", [128, 512], F32, p2) for i in range(2)]
            psX = pst("psX", [128, 512], F32, p2)
            psB2 = pst("psB2", [128, 512], F32, p2)
            lo, hi, mid, cnt, ge, d1, d2 = [bs[:, i:i + 1] for i in range(7)]
            S.dma(striU[:, :], D['striU'][:, :])
            S.memset(lo, 0.0)
            S.memset(hi, 1.0)
            for it in range(30):
                S.tt(mid, lo, hi, ALU.add)
                S.ts(mid, mid, 0.5, ALU.mult)
                S.ts(junkA[:, :], AFFT[:, :], mid, ALU.is_ge, s2=0.0, op1=ALU.add, accum_out=cnt)
                S.ts(ge, cnt, 511.5, ALU.is_gt)
                S.tt(d1, mid, lo, ALU.subtract)
                S.tt(d2, hi, mid, ALU.subtract)
                S.stt(lo, d1, ge, lo, ALU.mult, ALU.add)
                S.stt(hi, d2, ge, mid, ALU.mult, ALU.add)
            S.transpose(psX[0:1, 0:16], lo, P.ident_f[0:16, 0:16])
            S.copy(throw[:, :], psX[0:1, 0:16])
            S.mm(psB2[:, 0:16], P.ones_f[0:1, 0:128], throw[0:1, :])
            S.copy(THR[:, :], psB2[:, 0:16])
            S.tt(SEL[:, :, :], AFF[:, :, :], THR[:, :].unsqueeze(1).to_broadcast([128, NT, 16]), ALU.is_ge)
            S.memset(selcum[:, :], 0.0)
            for j in range(NT):
                pp = psP[j % 2]
                S.mm(pp[:, 0:16], striU[:, :], SEL[:, j, :], start=True, stop=False)
                S.mm(pp[:, 0:16], P.ones_f[:, :], selcum[:, :], start=False, stop=True)
                S.copy(POS[:, j, :], pp[:, 0:16], eng='act')
                S.tt(selcum[:, :], selcum[:, :], SEL[:, j, :], ALU.add)
            S.op('pool', lambda e: e.iota(jf[:, :], [[1, NT]], base=0, channel_multiplier=0, allow_small_or_imprecise_dtypes=True), [], [jf[:, :]])
            S.op('pool', lambda e: e.iota(pf[:, :], [[1, 1]], base=0, channel_multiplier=1, allow_small_or_imprecise_dtypes=True), [], [pf[:, :]])
            S.op('pool', lambda e: e.iota(iot[:, :], [[1, 512]], base=0, channel_multiplier=0, allow_small_or_imprecise_dtypes=True), [], [iot[:, :]])
            S.copy(COORD[:, :, :, 0], jf[:, :].unsqueeze(2).to_broadcast([128, NT, 16]))
            S.copy(COORD[:, :, :, 1], pf[:, 0:1].unsqueeze(2).to_broadcast([128, NT, 16]))
            S.copy(COORD[:, :, :, 2], AFF[:, :, :])
            S.tt(COORD[:, :, :, 3], AFF[:, :, :], COORD[:, :, :, 2], ALU.subtract)
            n = 0
            for e_ in range(16):
                for j in range(NT):
                    oh = OH[n % 3]
                    n += 1
                    S.ts(oh[:, :], iot[:, :], POS[:, j, e_:e_ + 1], ALU.is_equal, s2=SEL[:, j, e_:e_ + 1], op1=ALU.mult)
                    for sc in range(4):
                        S.mm(psI[sc][:, 0:4], oh[:, sc * 128:(sc + 1) * 128], COORD[:, j, e_, :], start=(j == 0), stop=(j == NT - 1))
                for sc in range(4):
                    r = r4[(e_ * 4 + sc) % 2]
                    S.copy(r[:, 0:4], psI[sc][:, 0:4], eng='act')
                    S.stt(r[:, 4:5], r[:, 0:1], 128.0, r[:, 1:2], ALU.mult, ALU.add)
                    S.copy(P.IDX[:, e_, sc:sc + 1], r[:, 4:5])
                    S.tt(P.GW[:, e_, sc:sc + 1], r[:, 2:3], r[:, 3:4], ALU.add)
        if C.debug:
            d_ = C.dbg_out('IDX', [128, 16, 4], I32)
            S.dma(d_[:, :, :], P.IDX[:, :, :])
            d_ = C.dbg_out('GW', [128, 16, 4])
            S.dma(d_[:, :, :], P.GW[:, :, :])


def phase_route(C):
    pass


def phase_experts(C):
    S, D, P, sb, pst, nc = C.S, C.D, C.P, C.sb, C.pst, C.nc
    with S.scope() as ph:
        stg = [sb(f"stg{i}", [128, 8, 512], F32, ph) for i in range(4)]
        wb = [sb(f"wb{i}", [128, 8, 512], BF16, ph) for i in range(8)]
        xs = sb("xs", [128, 4, DM], BF16, ph)
        xsT = sb("xsT", [128, 8, 512], BF16, ph)
        hidT = sb("hidT", [128, 16, 512], BF16, ph)
        sg = [sb(f"sg{i}", [128, 512], F32, ph) for i in range(2)]
        ysb = [sb(f"ysb{i}", [128, DM], F32, ph) for i in range(4)]
        psT = pst("psTe", [128, 1024], BF16, ph)
        psG = [pst(f"psGe{i}", [128, 512], F32, ph) for i in range(2)]
        psU = pst("psUe", [128, 512], F32, ph)
        psY = [pst(f"psYe{i}", [128, 512], F32, ph) for i in range(4)]
        pieces = []
        for e_ in range(16):
            for fb in range(4):
                for nm in ('w_gate', 'w_up'):
                    pieces.append(D[nm][e_ * 1024:(e_ + 1) * 1024, fb * 512:(fb + 1) * 512].rearrange("(k p) c -> p k c", p=128))
            for dh in range(2):
                for fh in range(2):
                    r0 = e_ * 2048 + fh * 1024
                    pieces.append(D['w_down'][r0:r0 + 1024, dh * 512:(dh + 1) * 512].rearrange("(k p) c -> p k c", p=128))
        issued = [0]
        cast_eng = ['act', 'dve']
        LOOK = 5

        def issue_upto(n):
            while issued[0] < min(n, len(pieces)):
                i = issued[0]
                s_ = stg[i % 4]
                S.dma(s_[:, :, :], pieces[i])
                S.copy(wb[i % 8][:, :, :], s_[:, :, :], eng=cast_eng[i % 2])
                issued[0] += 1
        pc = [0]

        def next_piece():
            i = pc[0]
            issue_upto(i + 1 + LOOK)
            pc[0] += 1
            return wb[i % 8]
        ng = 0
        for e_ in range(16):
            for st in range(4):
                idx_ap = P.IDX[:, e_, st:st + 1]
                S.op('pool', lambda e, st=st, idx_ap=idx_ap: e.indirect_dma_start(
                    out=xs[:, st, :], out_offset=None, in_=D['HFd'][:, :],
                    in_offset=bass.IndirectOffsetOnAxis(ap=idx_ap, axis=0)),
                    [D['HFd'][:, :], idx_ap], [xs[:, st, :]], dma=True)
            for st in range(4):
                for k in range(8):
                    S.transpose(psT[:, k * 128:(k + 1) * 128], xs[:, st, k * 128:(k + 1) * 128], P.ident_bf[:, :])
                S.copy(xsT[:, :, st * 128:(st + 1) * 128], psT[:, :].rearrange("p (k t) -> p k t", k=8), eng=('act' if st % 2 else 'dve'))
            for fb in range(4):
                wg = next_piece()
                wu = next_piece()
                for fc in range(4):
                    pg = psG[ng % 2]
                    sgt = sg[ng % 2]
                    ng += 1
                    for k in range(8):
                        S.mm(pg[:, :], wg[:, k, fc * 128:(fc + 1) * 128], xsT[:, k, :], start=(k == 0), stop=(k == 7))
                    for k in range(8):
                        S.mm(psU[:, :], wu[:, k, fc * 128:(fc + 1) * 128], xsT[:, k, :], start=(k == 0), stop=(k == 7))
                    S.act(sgt[:, :], pg[:, :], AF.Silu)
                    S.tt(hidT[:, fb * 4 + fc, :], sgt[:, :], psU[:, :], ALU.mult)
            for dh in range(2):
                for fh in range(2):
                    wd = next_piece()
                    for st in range(4):
                        for f8 in range(8):
                            S.mm(psY[st][:, :], hidT[:, fh * 8 + f8, st * 128:(st + 1) * 128], wd[:, f8, :],
                                 start=(fh == 0 and f8 == 0), stop=(fh == 1 and f8 == 7))
                for st in range(4):
                    S.stt(ysb[st][:, dh * 512:(dh + 1) * 512], psY[st][:, :], P.GW[:, e_, st:st + 1],
                          P.gt2rep[:, dh * 512:(dh + 1) * 512], ALU.mult, ALU.mult)
            for st in range(4):
                idx_ap = P.IDX[:, e_, st:st + 1]
                S.op('pool', lambda e, st=st, idx_ap=idx_ap: e.indirect_dma_start(
                    out=D['acc'][:, :], out_offset=bass.IndirectOffsetOnAxis(ap=idx_ap, axis=0),
                    in_=ysb[st][:, :], in_offset=None, compute_op=ALU.add),
                    [ysb[st][:, :], idx_ap, D['acc'][:, :]], [D['acc'][:, :]], dma=True)


def phase_final(C):
    S, D, P, sb, pst = C.S, C.D, C.P, C.sb, C.pst
    with S.scope() as ph:
        gfin = sb("gfin", [128, DM], F32, ph)
        xb = [sb(f"fxb{i}", [128, DM], F32, ph) for i in range(4)]
        ob = [sb(f"fob{i}", [128, DM], F32, ph) for i in range(4)]
        junk = sb("fjunk", [128, DM], BF16, ph)
        sm = sb("fsm", [128, NT, 2], F32, ph)
        S.dma(gfin[:, :], D['gfin_row'][0:1, :].partition_broadcast(128))
        for j in range(NT):
            a = j % 4
            S.dma(xb[a][:, :], D['acc'][j * 128:(j + 1) * 128, :])
            ss = sm[:, j, 0:1]
            rs = sm[:, j, 1:2]
            S.act(junk[:, :], xb[a][:, :], AF.Square, accum_out=ss)
            S.act(rs, ss, AF.Ln, scale=1.0 / DM, bias=EPS)
            S.act(rs, rs, AF.Exp, scale=-0.5)
            S.stt(ob[a][:, :], xb[a][:, :], rs, gfin[:, :], ALU.mult, ALU.mult)
            S.dma(D['out'][j * 128:(j + 1) * 128, :], ob[a][:, :], q='pool')


_PROG = {}


def kernel(**inputs):
    if 'nc' not in _PROG:
        _PROG['nc'] = build()[0]
    nc = _PROG['nc']
    B = inputs['x'].shape[0]
    in_maps = [layout_inputs(inputs, b) for b in range(B)]
    res = run_bass_kernel_spmd(nc, in_maps, core_ids=list(range(B)))
    out = np.stack([np.asarray(r["out"], dtype=np.float32) for r in res.results], axis=0)
    return out
```

```python
import math
import numpy as np
import ml_dtypes
import concourse.bass as bass
import concourse.mybir as mybir
from concourse.bass_utils import run_bass_kernel_spmd
from contextlib import ExitStack

F32 = mybir.dt.float32
BF16 = mybir.dt.bfloat16
I32 = mybir.dt.int32
AF = mybir.ActivationFunctionType
ALU = mybir.AluOpType
AX = mybir.AxisListType

SEQ = 4096
DM = 1024
NT = 32
EPS = 1e-6
EMIT_UNTIL = [None]
COMPUTE = ('pe', 'act', 'dve', 'pool')
SELF_SYNC = {'act': True, 'dve': True, 'pool': True, 'pe': False}
NDMA_SEMS = 6


def ap_box(ap):
    t = ap.tensor
    name = t.name
    dims = list(ap.ap)
    off = int(ap.offset)
    sp = str(ap.space() if callable(ap.space) else ap.space)
    is_dram = 'DRAM' in sp.upper() or 'HBM' in sp.upper() or type(t).__name__.startswith('DRAM') or type(t).__name__.startswith('Dram')
    if is_dram:
        lo = off
        hi = off
        for (st, cnt) in dims:
            st = int(st); cnt = int(cnt)
            if st >= 0:
                hi += st * (cnt - 1)
            else:
                lo += st * (cnt - 1)
        return (name, 0, 1, lo, hi + 1)
    if 'PSUM' in sp.upper() or type(t).__name__.startswith('PSum'):
        return (name, 0, 128, 0, 1 << 30)
    p0 = int(ap.start_partition())
    pc = int(dims[0][1])
    lo = off
    hi = off
    for (st, cnt) in dims[1:]:
        st = int(st); cnt = int(cnt)
        if st >= 0:
            hi += st * (cnt - 1)
        else:
            lo += st * (cnt - 1)
    return (name, p0, p0 + pc, lo, hi + 1)


class Op:
    __slots__ = ('stream', 'fn', 'deps', 'is_dma', 'signal', 'semval', 'dma_slot', 'idx', 'extra_waits')


class Sched:
    def __init__(self, nc, es):
        self.nc = nc
        self.es = es
        self.ops = []
        self.track = {}
        self.eng = {'pe': nc.tensor, 'act': nc.scalar, 'dve': nc.vector, 'pool': nc.gpsimd, 'sp': nc.sync}
        self.sem = {s: es.enter_context(nc.semaphore('sem_' + s)) for s in COMPUTE}
        self.dma_sems = {}
        for s in ('sp', 'pool', 'act'):
            self.dma_sems[s] = [es.enter_context(nc.semaphore(f'dsem_{s}{i}')) for i in range(NDMA_SEMS)]
        self.dma_count = {'sp': 0, 'pool': 0, 'act': 0}
        self.last_dma_ops = {'sp': [], 'pool': [], 'act': []}

    def _deps(self, boxes_r, boxes_w, idx, stream, is_dma):
        deps = set()
        for (boxes, is_w) in ((boxes_r, False), (boxes_w, True)):
            for b in boxes:
                lst = self.track.setdefault(b[0], [])
                keep = []
                for ent in lst:
                    eb, eidx, ew = ent
                    ov = not (eb[2] <= b[1] or b[2] <= eb[1] or eb[4] <= b[3] or b[4] <= eb[3])
                    if ov and (is_w or ew):
                        deps.add(eidx)
                    covered = (b[1] <= eb[1] and eb[2] <= b[2] and b[3] <= eb[3] and eb[4] <= b[4])
                    if is_w and covered:
                        continue
                    if (not is_w) and (not ew) and covered and (not is_dma):
                        eo = self.ops[eidx]
                        if eo.stream == stream and not eo.is_dma:
                            continue
                    keep.append(ent)
                keep.append([b, idx, is_w])
                self.track[b[0]] = keep
        deps.discard(idx)
        return deps

    def op(self, stream, fn, reads=(), writes=(), dma=False):
        o = Op()
        o.idx = len(self.ops)
        o.stream = stream
        o.fn = fn
        o.is_dma = dma
        o.signal = False
        o.semval = None
        o.dma_slot = None
        o.extra_waits = []
        br = [ap_box(a) for a in reads if a is not None and not isinstance(a, (int, float))]
        bw = [ap_box(a) for a in writes]
        self.ops.append(o)
        o.deps = self._deps(br, bw, o.idx, stream, dma)
        return o

    def emit(self):
        ops = self.ops
        for o in ops:
            for d in o.deps:
                po = ops[d]
                if po.is_dma:
                    continue
                if po.stream != o.stream or o.is_dma or SELF_SYNC.get(po.stream, True):
                    po.signal = True
        cnt = {s: 0 for s in COMPUTE}
        dcnt = {'sp': 0, 'pool': 0, 'act': 0}
        for o in ops:
            if o.is_dma:
                d = dcnt[o.stream]
                o.dma_slot = (d % NDMA_SEMS, 16 * (d // NDMA_SEMS + 1))
                dcnt[o.stream] = d + 1
            elif o.signal:
                cnt[o.stream] += 1
                o.semval = cnt[o.stream]
        waited = {s: {} for s in self.eng}
        nw = 0
        for o in ops:
            e = self.eng[o.stream]
            w = waited[o.stream]
            toks = []
            if o.is_dma:
                j, v = o.dma_slot
                if v > 16:
                    toks.append((('d', o.stream, j), self.dma_sems[o.stream][j], v - 16))
            for d in o.deps:
                po = ops[d]
                if po.is_dma:
                    j, v = po.dma_slot
                    toks.append((('d', po.stream, j), self.dma_sems[po.stream][j], v))
                else:
                    if po.stream == o.stream and not o.is_dma and not SELF_SYNC.get(po.stream, True):
                        continue
                    toks.append((('c', po.stream), self.sem[po.stream], po.semval))
            best = {}
            for key, sem, v in toks:
                if v is None:
                    raise RuntimeError('dep on non-signaling op')
                if w.get(key, 0) >= v:
                    continue
                if key not in best or best[key][1] < v:
                    best[key] = (sem, v)
            for key, (sem, v) in best.items():
                e.wait_ge(sem, v)
                w[key] = v
                nw += 1
            ins = o.fn(e)
            if ins is None:
                continue
            if o.is_dma:
                j, v = o.dma_slot
                ins.then_inc(self.dma_sems[o.stream][j], 16)
            elif o.signal:
                ins.then_inc(self.sem[o.stream], 1)
        self.n_waits = nw
        return cnt, dcnt

    def dma(self, out, in_, q='sp', **kw):
        return self.op(q, lambda e: e.dma_start(out=out, in_=in_, **kw), [in_], [out], dma=True)

    def mm(self, out, lhsT, rhs, start=True, stop=True, **kw):
        return self.op('pe', lambda e: e.matmul(out, lhsT, rhs, start=start, stop=stop, **kw),
                       [lhsT, rhs] + ([] if start else [out]), [out])

    def transpose(self, out, in_, ident):
        return self.op('pe', lambda e: e.transpose(out, in_, ident), [in_, ident], [out])

    def act(self, out, in_, func, scale=1.0, bias=0.0, accum_out=None, eng='act'):
        rd = [in_]
        if not isinstance(scale, (int, float)):
            rd.append(scale)
            if func == AF.Copy:
                func = AF.Identity
        if not isinstance(bias, (int, float)):
            rd.append(bias)
            if func == AF.Copy:
                func = AF.Identity
        wr = [out] + ([accum_out] if accum_out is not None else [])
        kw = {}
        if accum_out is not None:
            kw['accum_out'] = accum_out
        return self.op('act', lambda e: e.activation(out=out, in_=in_, func=func, scale=scale, bias=bias, **kw), rd, wr)

    def tt(self, out, in0, in1, op, eng='dve'):
        return self.op(eng, lambda e: e.tensor_tensor(out=out, in0=in0, in1=in1, op=op), [in0, in1], [out])

    def ts(self, out, in0, s1, op0, s2=None, op1=None, eng='dve', accum_out=None):
        rd = [in0]
        if not isinstance(s1, (int, float)):
            rd.append(s1)
        if s2 is not None and not isinstance(s2, (int, float)):
            rd.append(s2)
        kw = {}
        if op1 is not None:
            kw['op1'] = op1
        if accum_out is not None:
            kw['accum_out'] = accum_out
        wr = [out] + ([accum_out] if accum_out is not None else [])
        return self.op(eng, lambda e: e.tensor_scalar(out=out, in0=in0, scalar1=s1, scalar2=s2, op0=op0, **kw), rd, wr)

    def stt(self, out, in0, scalar, in1, op0, op1, eng='dve'):
        rd = [in0, in1]
        if not isinstance(scalar, (int, float)):
            rd.append(scalar)
        return self.op(eng, lambda e: e.scalar_tensor_tensor(out=out, in0=in0, scalar=scalar, in1=in1, op0=op0, op1=op1), rd, [out])

    def copy(self, out, in_, eng='dve'):
        if eng == 'act':
            return self.act(out, in_, AF.Copy)
        return self.op(eng, lambda e: e.tensor_copy(out=out, in_=in_), [in_], [out])

    def memset(self, out, val, eng='dve'):
        return self.op(eng, lambda e: e.memset(out, val), [], [out])

    def reduce(self, out, in_, op, axis=None, eng='dve'):
        axis = axis or AX.X
        return self.op(eng, lambda e: e.tensor_reduce(out=out, in_=in_, op=op, axis=axis), [in_], [out])

    def recip(self, out, in_, eng='dve'):
        return self.op(eng, lambda e: e.reciprocal(out=out, in_=in_), [in_], [out])

    def barrier(self):
        alld = set()
        for lst in self.track.values():
            for ent in lst:
                alld.add(ent[1])
        self.track = {}
        for s_ in ('pe', 'act', 'dve', 'pool', 'sp'):
            o = self.op(s_, lambda e: None, [], [])
            o.deps = set(alld)

    def scope(self):
        return _Scope(self)

    def finish(self, out_aps):
        boxes = [ap_box(a) for a in out_aps]
        o = self.op('sp', lambda e: None, list(out_aps), [])
        return o


class _Scope:
    def __init__(self, S):
        self.S = S
        self.st = ExitStack()

    def __enter__(self):
        self.st.__enter__()
        return self.st

    def __exit__(self, *a):
        self.S.barrier()
        return self.st.__exit__(*a)

_CONSTS = {}


def _bf(a):
    return np.ascontiguousarray(a.astype(np.float32)).astype(ml_dtypes.bfloat16)


def host_consts():
    if _CONSTS:
        return _CONSTS
    c = {}
    c['ident_bf'] = _bf(np.eye(128))
    c['ident_f'] = np.eye(128, dtype=np.float32)
    s = np.arange(128)[:, None]
    t = np.arange(128)[None, :]
    c['triU'] = (s <= t).astype(np.float32)
    c['triL'] = (s >= t).astype(np.float32)
    c['striU'] = (s < t).astype(np.float32)
    N = 8192
    n1 = np.arange(64); k1 = np.arange(64); n2 = np.arange(128); k2 = np.arange(128)
    ang = 2 * np.pi * np.outer(n1, k1) / 64
    c['F1'] = _bf(np.concatenate([np.cos(ang), -np.sin(ang)], 1))
    th = 2 * np.pi * ((n2[:, None, None] * (k1[None, :, None] + 64 * k2[None, None, :])) % N) / N
    c['GrT'] = _bf(np.cos(th).reshape(128, 64 * 128))
    c['GiT'] = _bf((-np.sin(th)).reshape(128, 64 * 128))
    c['GiNT'] = _bf((np.sin(th)).reshape(128, 64 * 128))
    ph = 2 * np.pi * np.outer(k2, n2) / 128
    Rr = np.cos(ph); Ri = np.sin(ph)
    c['Rc1'] = _bf(np.concatenate([Rr, Ri], 1))
    c['Rc2'] = _bf(np.concatenate([-Ri, Rr], 1))
    n1h = np.arange(32)
    thL = 2 * np.pi * (k1[:, None, None] * n1h[None, None, :] / 64 + k1[:, None, None] * n2[None, :, None] / N)
    c['LrT'] = _bf((np.cos(thL) / N).reshape(64, 128 * 32))
    c['LiNT'] = _bf((-np.sin(thL) / N).reshape(64, 128 * 32))
    L = SEQ
    f32 = np.float32
    tt = np.linspace(0.0, 1.0, L, dtype=f32)[:, None]
    w = (f32(2.0 * math.pi) * np.arange(L, dtype=f32)[:, None] / f32(L)).astype(f32)
    bands = np.linspace(1e-4, 15, 16, dtype=f32)[None, :]
    z = np.concatenate([tt, np.cos(bands * w), -np.sin(bands * w)], axis=-1).astype(f32)
    zr = np.concatenate([z[0:1], z[:0:-1]], 0)
    c['zT'] = np.ascontiguousarray(z.T)
    c['zrT'] = np.ascontiguousarray(zr.T)
    trow = tt[:, 0]
    trr = np.concatenate([trow[0:1], trow[:0:-1]])
    c['t_row'] = np.ascontiguousarray(trow[None, :]).astype(f32)
    c['tr_row'] = np.ascontiguousarray(trr[None, :]).astype(f32)
    _CONSTS.update(c)
    return _CONSTS


def colmaj(v, nk):
    return np.ascontiguousarray(np.asarray(v, dtype=np.float32).reshape(nk, 128).T)


def layout_inputs(inp, b):
    m = {}
    f = lambda a: np.ascontiguousarray(np.asarray(a, dtype=np.float32))
    m['x'] = f(inp['x'][b])
    m['ccol'] = colmaj(inp['c'][b], 8)
    m['w_ada'] = f(inp['w_ada'][0])
    m['b_ada'] = f(inp['b_ada'][0][None, :])
    m['gmix_col'] = colmaj(inp['g_mix'][0], 8)
    m['w_in'] = f(inp['w_in'][0])
    bin_ = np.asarray(inp['b_in'][0], dtype=np.float32)
    m['bin_row'] = f(bin_[None, 1024:2064])
    m['bqk_col'] = colmaj(bin_[0:1024], 8)
    m['bhy_col'] = colmaj(bin_[2064:3600], 12)
    cw = np.asarray(inp['conv_qk_w'][0], dtype=np.float32)
    m['cqk_w'] = np.ascontiguousarray(cw.reshape(3, 8, 128).transpose(2, 1, 0))
    m['cqk_b'] = colmaj(inp['conv_qk_b'][0], 8)
    cw = np.asarray(inp['conv_hy_w'][0], dtype=np.float32)
    m['chy_w'] = np.ascontiguousarray(cw.reshape(3, 12, 128).transpose(2, 1, 0))
    m['chy_b'] = colmaj(inp['conv_hy_b'][0], 12)
    m['mnorm_g'] = f(inp['mlstm_norm_g'][0][None, :])
    m['hy_w1'] = f(inp['hy_w1'][0])
    m['hy_b1c'] = f(inp['hy_b1'][0][:, None])
    m['hy_w2'] = f(inp['hy_w2'][0])
    m['hy_b2c'] = f(inp['hy_b2'][0][:, None])
    m['hy_w3'] = f(inp['hy_w3'][0])
    m['hy_frc'] = f(inp['hy_freq'][0][:, None])
    m['hy_del_col'] = colmaj(inp['hy_deltas'][0], 16)
    m['hy_bias_col'] = colmaj(np.asarray(inp['hy_bias'][0]).reshape(-1), 8)
    m['hnorm_col'] = colmaj(inp['hyena_norm_g'][0], 4)
    m['w_out'] = f(inp['w_out'][0])
    m['gffn_row'] = f(inp['g_ffn'][0][None, :])
    m['w_router'] = f(inp['w_router'][0])
    m['w_gate'] = f(inp['w_gate'][0]).reshape(16 * 1024, 2048)
    m['w_up'] = f(inp['w_up'][0]).reshape(16 * 1024, 2048)
    m['w_down'] = f(inp['w_down'][0]).reshape(16 * 2048, 1024)
    m['gfin_row'] = f(np.asarray(inp['g_final'])[None, :])
    m.update(host_consts())
    return m

INPUT_SPECS = [
    ('x', [SEQ, DM], F32), ('ccol', [128, 8], F32), ('w_ada', [DM, 6144], F32), ('b_ada', [1, 6144], F32),
    ('gmix_col', [128, 8], F32), ('w_in', [DM, 3600], F32), ('bin_row', [1, 1040], F32),
    ('bqk_col', [128, 8], F32), ('bhy_col', [128, 12], F32), ('cqk_w', [128, 8, 3], F32), ('cqk_b', [128, 8], F32),
    ('chy_w', [128, 12, 3], F32), ('chy_b', [128, 12], F32), ('mnorm_g', [1, 512], F32),
    ('hy_w1', [33, 64], F32), ('hy_b1c', [64, 1], F32), ('hy_w2', [64, 64], F32), ('hy_b2c', [64, 1], F32),
    ('hy_w3', [64, 2048], F32), ('hy_frc', [64, 1], F32), ('hy_del_col', [128, 16], F32),
    ('hy_bias_col', [128, 8], F32), ('hnorm_col', [128, 4], F32), ('w_out', [DM, DM], F32),
    ('gffn_row', [1, DM], F32), ('w_router', [DM, 16], F32), ('w_gate', [16 * 1024, 2048], F32),
    ('w_up', [16 * 1024, 2048], F32), ('w_down', [16 * 2048, 1024], F32), ('gfin_row', [1, DM], F32),
    ('ident_bf', [128, 128], BF16), ('ident_f', [128, 128], F32), ('triU', [128, 128], F32),
    ('triL', [128, 128], F32), ('striU', [128, 128], F32), ('F1', [64, 128], BF16),
    ('GrT', [128, 8192], BF16), ('GiT', [128, 8192], BF16), ('GiNT', [128, 8192], BF16),
    ('Rc1', [128, 256], BF16), ('Rc2', [128, 256], BF16), ('LrT', [64, 4096], BF16), ('LiNT', [64, 4096], BF16),
    ('zT', [33, SEQ], F32), ('zrT', [33, SEQ], F32), ('t_row', [1, SEQ], F32), ('tr_row', [1, SEQ], F32),
]


class Ctx:
    pass


def build(stop_after=None, debug=False):
    nc = bass.Bass("TRN2", target_bir_lowering=False)
    es = ExitStack()
    S = Sched(nc, es)
    C = Ctx()
    C.nc, C.S, C.es = nc, S, es
    C.debug = debug
    D = {}
    for name, shape, dt in INPUT_SPECS:
        D[name] = nc.dram_tensor(name, shape, dt, kind="ExternalInput").ap()
    D['out'] = nc.dram_tensor("out", [SEQ, DM], F32, kind="ExternalOutput").ap()

    def scratch(name, shape, dt):
        D[name] = nc.dram_tensor(name, shape, dt, kind="Internal").ap()
    scratch('QTd', [512, SEQ], BF16)
    scratch('KTd', [512, SEQ], BF16)
    scratch('Vd', [SEQ, 512], BF16)
    scratch('Od', [SEQ, 512], BF16)
    scratch('X1d', [512, SEQ], F32)
    scratch('X2d', [512, SEQ], F32)
    scratch('Zd', [512, SEQ], BF16)
    scratch('Z1d', [512, SEQ], BF16)
    scratch('Kd0', [512, 2 * SEQ], BF16)
    scratch('Kd1', [512, 2 * SEQ], BF16)
    scratch('Ycv', [512, SEQ], BF16)
    scratch('Yt', [DM, SEQ], BF16)
    scratch('HFd', [SEQ, DM], BF16)
    scratch('acc', [SEQ, DM], F32)
    C.D = D
    C.dbg = {}

    def dbg_out(name, shape, dt=F32):
        t = nc.dram_tensor("dbg_" + name, shape, dt, kind="ExternalOutput").ap()
        C.dbg[name] = t
        return t
    C.dbg_out = dbg_out

    cnt = [0]

    def sb(name, shape, dt, st=None):
        cnt[0] += 1
        return (st or es).enter_context(nc.sbuf_tensor(f"s{cnt[0]}_{name}", shape, dt))

    def pst(name, shape, dt, st=None):
        cnt[0] += 1
        return (st or es).enter_context(nc.psum_tensor(f"p{cnt[0]}_{name}", shape, dt))
    C.sb, C.pst = sb, pst

    P = Ctx()
    C.P = P
    P.ident_bf = sb("ident_bf", [128, 128], BF16)
    P.ident_f = sb("ident_f", [128, 128], F32)
    P.ones_f = sb("ones_f", [128, 128], F32)
    P.modcol = sb("modcol", [128, 48], F32)
    P.A1col = sb("A1col", [128, 8], F32)
    P.gt1rep = sb("gt1rep", [128, DM], F32)
    P.gt2rep = sb("gt2rep", [128, DM], F32)
    P.A2rep = sb("A2rep", [128, DM], F32)
    P.B2rep = sb("B2rep", [128, DM], F32)
    S.dma(P.ident_bf[:, :], D['ident_bf'][:, :])
    S.dma(P.ident_f[:, :], D['ident_f'][:, :])
    S.memset(P.ones_f[:, :], 1.0)

    phases = [phase_mod, phase_norm_proj, phase_mlstm, phase_hyena, phase_outproj, phase_route, phase_experts, phase_final]
    for ph in phases:
        ph(C)
        if stop_after == ph.__name__:
            break
    outs = [D['out'][:, :]] + [t for t in C.dbg.values()]
    S.finish(outs)
    S.emit()
    return nc, C


def phase_mod(C):
    S, D, P, sb, pst = C.S, C.D, C.P, C.sb, C.pst
    with S.scope() as ph:
        wada = [sb(f"wada{i}", [128, 8, 512], F32, ph) for i in range(2)]
        modrow = sb("modrow", [1, 6144], F32, ph)
        badar = sb("badar", [1, 6144], F32, ph)
        ccol = sb("ccol_sb", [128, 8], F32, ph)
        gmixc = sb("gmixc", [128, 8], F32, ph)
        gffn_rep = sb("gffn_rep", [128, DM], F32, ph)
        sc2rep = sb("sc2rep", [128, DM], F32, ph)
        ps = pst("ps_mod", [128, 512], F32, ph)
        psc = pst("ps_modc", [128, 512], F32, ph)
        psr = [pst(f"ps_modr{i}", [128, 512], F32, ph) for i in range(2)]
        S.dma(badar[:, :], D['b_ada'][:, :])
        S.dma(ccol[:, :], D['ccol'][:, :])
        S.dma(gmixc[:, :], D['gmix_col'][:, :])
        S.dma(gffn_rep[:, :], D['gffn_row'][0:1, :].partition_broadcast(128))
        wsrc = D['w_ada'].rearrange("(k p) c -> p k c", p=128)
        for blk in range(12):
            buf = wada[blk % 2]
            S.dma(buf[:, :, :], wsrc[:, :, blk * 512:(blk + 1) * 512])
            for k in range(8):
                S.mm(ps[0:1, :], ccol[:, k:k + 1], buf[:, k, :], start=(k == 0), stop=(k == 7))
            S.tt(modrow[0:1, blk * 512:(blk + 1) * 512], ps[0:1, :], badar[0:1, blk * 512:(blk + 1) * 512], ALU.add)
        for oc in range(48):
            S.mm(psc[:, oc:oc + 1], modrow[0:1, oc * 128:(oc + 1) * 128], P.ones_f[0:1, 0:1])
        S.copy(P.modcol[:, :], psc[:, 0:48])
        S.stt(P.A1col[:, :], P.modcol[:, 8:16], 1.0, gmixc[:, :], ALU.add, ALU.mult)
        n = 0
        for (dst, j) in ((P.gt1rep, 2), (P.gt2rep, 5), (sc2rep, 4), (P.B2rep, 3)):
            for h in range(2):
                pr = psr[n % 2]
                n += 1
                S.mm(pr[:, :], P.ones_f[0:1, 0:128], modrow[0:1, j * 1024 + h * 512: j * 1024 + (h + 1) * 512])
                S.copy(dst[:, h * 512:(h + 1) * 512], pr[:, :], eng=('act' if n % 2 else 'dve'))
        S.stt(P.A2rep[:, :], sc2rep[:, :], 1.0, gffn_rep[:, :], ALU.add, ALU.mult)
        if C.debug:
            d = C.dbg_out('modcol', [128, 48])
            S.dma(d[:, :], P.modcol[:, :])
            d = C.dbg_out('gt1rep', [128, DM])
            S.dma(d[:, :], P.gt1rep[:, :])


def phase_norm_proj(C):
    S, D, P, sb, pst = C.S, C.D, C.P, C.sb, C.pst
    P.Gt = sb("Gt", [128, NT, 16], F32)
    with S.scope() as ph:
        hT = sb("hT", [128, 8, SEQ], BF16, ph)
        with S.scope() as p1:
            xb = [sb(f"xb{i}", [128, DM], F32, p1) for i in range(2)]
            xn = [sb(f"xn{i}", [128, DM], BF16, p1) for i in range(2)]
            junk = sb("junk", [128, DM], BF16, p1)
            ss = sb("ss", [128, NT], F32, p1)
            rs = sb("rs", [128, NT], F32, p1)
            psT = [pst(f"psT{i}", [128, DM], BF16, p1) for i in range(2)]
            for j in range(NT):
                xt = xb[j % 2]
                S.dma(xt[:, :], D['x'][j * 128:(j + 1) * 128, :])
                S.act(junk[:, :], xt[:, :], AF.Square, accum_out=ss[:, j:j + 1])
                S.act(rs[:, j:j + 1], ss[:, j:j + 1], AF.Ln, scale=1.0 / DM, bias=EPS)
                S.act(rs[:, j:j + 1], rs[:, j:j + 1], AF.Exp, scale=-0.5)
                S.act(xn[j % 2][:, :], xt[:, :], AF.Copy, scale=rs[:, j:j + 1])
                pt = psT[j % 2]
                for k in range(8):
                    S.transpose(pt[:, k * 128:(k + 1) * 128], xn[j % 2][:, k * 128:(k + 1) * 128], P.ident_bf[:, :])
                for k in range(8):
                    S.act(hT[:, k, j * 128:(j + 1) * 128], pt[:, k * 128:(k + 1) * 128], AF.Identity,
                          scale=P.A1col[:, k:k + 1], bias=P.modcol[:, k:k + 1])
        if C.debug:
            d = C.dbg_out('hT', [128, 8, SEQ], BF16)
            for k in range(8):
                S.dma(d[:, k, :], hT[:, k, :])
        with S.scope() as p2:
            wvo = sb("wvo", [128, 8, 1040], BF16, p2)
            brow = sb("brow", [128, 1040], F32, p2)
            vt = [sb(f"vt{i}", [128, 512], BF16, p2) for i in range(2)]
            ot = [sb(f"ot{i}", [128, 512], BF16, p2) for i in range(2)]
            otf = [sb(f"otf{i}", [128, 512], F32, p2) for i in range(2)]
            psV = [pst(f"psV{i}", [128, 512], F32, p2) for i in range(2)]
            psO = [pst(f"psO{i}", [128, 512], F32, p2) for i in range(2)]
            psG = [pst(f"psG{i}", [128, 512], F32, p2) for i in range(2)]
            wsrc = D['w_in'].rearrange("(k p) c -> p k c", p=128)
            S.dma(wvo[:, :, 0:512], wsrc[:, :, 1024:1536], q='pool')
            S.dma(wvo[:, :, 512:1040], wsrc[:, :, 1536:2064], q='pool')
            S.dma(brow[:, :], D['bin_row'][0:1, :].partition_broadcast(128))
            for j in range(NT):
                a = j % 2
                for (pp, c0, c1) in ((psV[a], 0, 512), (psO[a], 512, 1024), (psG[a], 1024, 1040)):
                    for k in range(8):
                        S.mm(pp[:, 0:c1 - c0], hT[:, k, j * 128:(j + 1) * 128], wvo[:, k, c0:c1], start=(k == 0), stop=(k == 7))
                S.tt(vt[a][:, :], psV[a][:, :], brow[:, 0:512], ALU.add)
                S.dma(D['Vd'][j * 128:(j + 1) * 128, :], vt[a][:, :])
                S.tt(otf[a][:, :], psO[a][:, :], brow[:, 512:1024], ALU.add)
                S.act(ot[a][:, :], otf[a][:, :], AF.Sigmoid)
                S.dma(D['Od'][j * 128:(j + 1) * 128, :], ot[a][:, :])
                S.tt(P.Gt[:, j, :], psG[a][:, 0:16], brow[:, 1024:1040], ALU.add)
        if C.debug:
            d = C.dbg_out('Gt', [128, NT, 16])
            S.dma(d[:, :, :], P.Gt[:, :, :])
        with S.scope() as p3:
            wch = [sb(f"wch{i}", [128, 8, 128], BF16, p3) for i in range(3)]
            pre = [sb(f"pre{i}", [128, SEQ + 2], F32, p3) for i in range(2)]
            t0s = [sb(f"cv_t0{i}", [128, 2048], F32, p3) for i in range(2)]
            t2s = [sb(f"cv_t2{i}", [128, 2048], F32, p3) for i in range(2)]
            t3 = [[sb(f"cv_t3{q}{i}", [128, 2048], F32, p3) for i in range(2)] for q in range(2)]
            ob = [[sb(f"cv_ob{q}{i}", [128, 2048], BF16, p3) for i in range(2)] for q in range(2)]
            pending = [None]
            bqk = sb("bqk", [128, 8], F32, p3)
            bhy = sb("bhy", [128, 12], F32, p3)
            cqw = sb("cqw", [128, 8, 3], F32, p3)
            cqb = sb("cqb", [128, 8], F32, p3)
            chw = sb("chw", [128, 12, 3], F32, p3)
            chb = sb("chb", [128, 12], F32, p3)
            psF = [pst(f"psF{i}", [128, 512], F32, p3) for i in range(8)]
            for (tile_, nm) in ((bqk, 'bqk_col'), (bhy, 'bhy_col'), (cqb, 'cqk_b'), (chb, 'chy_b')):
                S.dma(tile_[:, :], D[nm][:, :])
            S.dma(cqw[:, :, :], D['cqk_w'][:, :, :])
            S.dma(chw[:, :, :], D['chy_w'][:, :, :])
            for i in range(2):
                S.memset(pre[i][:, 0:1], 0.0)
                S.memset(pre[i][:, SEQ + 1:SEQ + 2], 0.0)
            wsrc = D['w_in'].rearrange("(k p) c -> p k c", p=128)
            chunks = []
            for cc in range(8):
                chunks.append(('qk', cc, cc * 128))
            for i in range(12):
                chunks.append(('hy', i, 2064 + i * 128))
            nps = 0
            for m_ in range(2):
                S.dma(wch[m_ % 3][:, :, :], wsrc[:, :, chunks[m_][2]:chunks[m_][2] + 128], q='pool')
            for n, (kind, ci, col0) in enumerate(chunks):
                wc = wch[n % 3]
                pr = pre[n % 2]
                if n + 2 < len(chunks):
                    S.dma(wch[(n + 2) % 3][:, :, :], wsrc[:, :, chunks[n + 2][2]:chunks[n + 2][2] + 128], q='pool')
                bcol = bqk[:, ci:ci + 1] if kind == 'qk' else bhy[:, ci:ci + 1]
                cw = cqw if kind == 'qk' else chw
                cb = cqb if kind == 'qk' else chb
                for tb in range(8):
                    pp = psF[nps % 8]
                    nps += 1
                    for k in range(8):
                        S.mm(pp[:, :], wc[:, k, :], hT[:, k, tb * 512:(tb + 1) * 512], start=(k == 0), stop=(k == 7))
                    if tb % 2 == 0:
                        S.act(pr[:, 1 + tb * 512:1 + (tb + 1) * 512], pp[:, :], AF.Identity, bias=bcol)
                    else:
                        S.ts(pr[:, 1 + tb * 512:1 + (tb + 1) * 512], pp[:, :], bcol, ALU.add)
                par = n % 2
                for hh in range(2):
                    o0 = hh * 2048
                    S.act(t0s[hh][:, :], pr[:, o0:o0 + 2048], AF.Identity, scale=cw[:, ci, 0:1], bias=cb[:, ci:ci + 1])
                    S.act(t2s[hh][:, :], pr[:, o0 + 2:o0 + 2050], AF.Identity, scale=cw[:, ci, 2:3])
                for hh in range(2):
                    o0 = hh * 2048
                    S.stt(t0s[hh][:, :], pr[:, o0 + 1:o0 + 2049], cw[:, ci, 1:2], t0s[hh][:, :], ALU.mult, ALU.add)
                for hh in range(2):
                    if kind == 'hy' and ci >= 8:
                        S.tt(ob[par][hh][:, :], t0s[hh][:, :], t2s[hh][:, :], ALU.add, eng='pool')
                    else:
                        S.tt(t3[par][hh][:, :], t0s[hh][:, :], t2s[hh][:, :], ALU.add, eng='pool')
                if pending[0] is not None:
                    pending[0]()

                def fin(kind=kind, ci=ci, par=par):
                    for hh in range(2):
                        o0 = hh * 2048
                        if kind == 'qk':
                            S.act(ob[par][hh][:, :], t3[par][hh][:, :], AF.Silu)
                            dst = D['QTd'] if ci < 4 else D['KTd']
                            S.dma(dst[(ci % 4) * 128:(ci % 4 + 1) * 128, o0:o0 + 2048], ob[par][hh][:, :])
                        elif ci < 8:
                            dst = D['X1d'] if ci < 4 else D['X2d']
                            S.dma(dst[(ci % 4) * 128:(ci % 4 + 1) * 128, o0:o0 + 2048], t3[par][hh][:, :])
                        else:
                            S.dma(D['Zd'][(ci - 8) * 128:(ci - 7) * 128, o0:o0 + 2048], ob[par][hh][:, :])
                pending[0] = fin
            pending[0]()
    if C.debug:
        for nm, shp, dt in (('QTd', [512, SEQ], BF16), ('KTd', [512, SEQ], BF16), ('Vd', [SEQ, 512], BF16),
                            ('Od', [SEQ, 512], BF16), ('X1d', [512, SEQ], F32), ('Zd', [512, SEQ], BF16)):
            d = C.dbg_out(nm, shp, dt)
            with S.scope() as pd:
                if shp[0] == 512:
                    tmp = sb("dbgtmp_" + nm, [128, 4, SEQ], dt, pd)
                    S.dma(tmp[:, :, :], D[nm].rearrange("(a p) n -> p a n", p=128))
                    S.dma(d.rearrange("(a p) n -> p a n", p=128), tmp[:, :, :])
                else:
                    tmp = sb("dbgtmp_" + nm, [128, NT, 512], dt, pd)
                    S.dma(tmp[:, :, :], D[nm].rearrange("(a p) n -> p a n", p=128))
                    S.dma(d.rearrange("(a p) n -> p a n", p=128), tmp[:, :, :])


def phase_mlstm(C):
    S, D, P, sb, pst = C.S, C.D, C.P, C.sb, C.pst
    Gt = P.Gt
    with S.scope() as ph:
        triU = sb("triU", [128, 128], F32, ph)
        triL = sb("triL", [128, 128], F32, ph)
        LF = sb("LF", [128, 2, NT, 4], F32, ph)
        II = sb("II", [128, 2, NT, 4], F32, ph)
        Bc = sb("Bc", [128, 2, NT, 4], F32, ph)
        Wp = sb("Wp", [128, 2, NT, 4], F32, ph)
        ENB = sb("ENB", [128, 2, NT, 4], F32, ph)
        EG = sb("EG", [128, 2, NT, 4], F32, ph)
        zcol = sb("zcol", [128, 1], F32, ph)
        S.dma(triU[:, :], D['triU'][:, :])
        S.dma(triL[:, :], D['triL'][:, :])
        S.memset(zcol[:, :], 0.0)
        with S.scope() as pp:
            psB = pst("psB", [128, 512], F32, pp)
            psGs = pst("psGs", [128, 512], F32, pp)
            for d in range(2):
                S.act(LF[:, d, :, :], Gt[:, :, d * 8 + 4:d * 8 + 8], AF.Exp, scale=-1.0)
                S.copy(II[:, d, :, :], Gt[:, :, d * 8:d * 8 + 4])
            fl = lambda t: t[:, :, :, :].rearrange("p d c e -> p (d c e)")
            S.act(fl(LF), fl(LF), AF.Ln, bias=1.0)
            S.ts(fl(LF), fl(LF), -1.0, ALU.mult)
            S.mm(psB[:, 0:128], triU[:, :], fl(LF)[:, 0:128])
            S.mm(psB[:, 128:256], triL[:, :], fl(LF)[:, 128:256])
            S.mm(psGs[:, 0:256], P.ones_f[:, :], fl(LF))
            S.copy(fl(Bc), psB[:, 0:256])
            S.act(fl(EG), psGs[:, 0:256], AF.Exp)
            S.tt(fl(Wp), fl(II), fl(Bc), ALU.subtract)
            S.act(fl(Wp), fl(Wp), AF.Exp, bias=math.log(128.0 ** -0.5))
            S.act(fl(ENB), fl(Bc), AF.Exp, scale=-1.0)
        if C.debug:
            for nm, t in (('Bc', Bc), ('Wp', Wp), ('EG', EG)):
                d_ = C.dbg_out(nm, [128, 2, NT, 4])
                S.dma(d_[:, :, :, :], t[:, :, :, :])
        for hd in range(4):
            with S.scope() as hs:
                qT = sb("qT", [128, SEQ], BF16, hs)
                kT = sb("kT", [128, SEQ], BF16, hs)
                vaug = sb("vaug", [128, NT, 129], BF16, hs)
                osg = sb("osg", [128, NT, 128], BF16, hs)
                ktm = sb("ktm", [128, NT, 128], BF16, hs)
                Hs = sb("Hs", [128, NT, 128], F32, hs)
                sq = sb("sq", [128, NT, 128], F32, hs)
                gO = sb("gO", [128, NT, 128], BF16, hs)
                ym = sb("ym", [128, NT, 128], BF16, hs)
                ymT = sb("ymT", [128, SEQ], BF16, hs)
                mng = sb("mng", [128, 128], F32, hs)
                STw = [sb(f"STw{i}", [128, 128], BF16, hs) for i in range(4)]
                vw = [sb(f"vw{i}", [128, 129], BF16, hs) for i in range(4)]
                Tst = sb("Tst", [128, 129], F32, hs)
                Tst2 = sb("Tst2", [128, 129], F32, hs)
                Cbfd = [[sb(f"Cbf{d_}{i}", [128, 129], BF16, hs) for i in range(2)] for d_ in range(2)]
                sm = [sb(f"sm{i}", [128, 4], F32, hs) for i in range(2)]
                ssq = sb("ssq", [128, NT], F32, hs)
                pS = [pst(f"pS{i}", [128, 512], F32, hs) for i in range(2)]
                pO = [pst(f"pO{i}", [128, 512], F32, hs) for i in range(2)]
                pC = [pst(f"pC{i}", [128, 512], F32, hs) for i in range(2)]
                pK = pst("pK", [128, 1024], BF16, hs)
                S.dma(qT[:, :], D['QTd'][hd * 128:(hd + 1) * 128, :])
                S.dma(kT[:, :], D['KTd'][hd * 128:(hd + 1) * 128, :])
                S.dma(vaug[:, :, 0:128], D['Vd'][:, hd * 128:(hd + 1) * 128].rearrange("(c p) d -> p c d", p=128))
                S.memset(vaug[:, :, 128:129], 1.0)
                S.dma(osg[:, :, :], D['Od'][:, hd * 128:(hd + 1) * 128].rearrange("(c p) d -> p c d", p=128))
                S.dma(mng[:, :], D['mnorm_g'][0:1, hd * 128:(hd + 1) * 128].partition_broadcast(128))
                for c0 in range(0, NT, 8):
                    for c in range(c0, c0 + 8):
                        S.transpose(pK[:, (c - c0) * 128:(c - c0 + 1) * 128], kT[:, c * 128:(c + 1) * 128], P.ident_bf[:, :])
                    S.copy(ktm[:, c0:c0 + 8, :].rearrange("p c d -> p (c d)"), pK[:, :], eng=('act' if (c0 // 8) % 2 else 'dve'))
                n = 0
                Tsts = [Tst, Tst2]
                prevc = [None, None]
                visited = set()
                for d in range(2):
                    S.memset(Tsts[d][:, :], 0.0)
                    S.memset(Cbfd[d][0][:, :], 0.0)
                nd = [0, 0]
                steps = []
                for i in range(NT):
                    for d in range(2):
                        steps.append((d, i if d == 0 else NT - 1 - i))

                def phase1(n):
                    d, c = steps[n]
                    cs = slice(c * 128, (c + 1) * 128)
                    wcol = Wp[:, d, c, hd:hd + 1]
                    mask = triU if d == 0 else triL
                    S.mm(pS[n % 2][:, 0:128], kT[:, cs], qT[:, cs])
                    S.stt(STw[n % 4][:, :], pS[n % 2][:, 0:128], wcol, mask[:, :], ALU.mult, ALU.mult)
                    S.act(vw[n % 4][:, :], vaug[:, c, :], AF.Copy, scale=wcol)

                def phase2(n):
                    d, c = steps[n]
                    a = n % 2
                    b_ = nd[d] % 2
                    nd[d] += 1
                    cs = slice(c * 128, (c + 1) * 128)
                    S.mm(pO[a][:, 0:129], STw[n % 4][:, :], vaug[:, c, :], start=True, stop=False)
                    S.mm(pO[a][:, 0:129], qT[:, cs], Cbfd[d][b_][:, :], start=False, stop=True)
                    S.mm(pC[a][:, 0:129], ktm[:, c, :], vw[n % 4][:, :])
                    enb = ENB[:, d, c, hd:hd + 1]
                    s_ = sm[a]
                    S.ts(s_[:, 0:1], pO[a][:, 128:129], enb, ALU.max)
                    S.stt(s_[:, 2:3], pO[a][:, 128:129], -1.0, s_[:, 0:1], ALU.mult, ALU.max)
                    S.recip(s_[:, 3:4], s_[:, 2:3])
                    if c not in visited:
                        visited.add(c)
                        S.act(Hs[:, c, :], pO[a][:, 0:128], AF.Copy, scale=s_[:, 3:4])
                    else:
                        S.stt(Hs[:, c, :], pO[a][:, 0:128], s_[:, 3:4], Hs[:, c, :], ALU.mult, ALU.add)
                    egp = zcol[:, 0:1] if prevc[d] is None else EG[:, d, prevc[d], hd:hd + 1]
                    S.stt(Tsts[d][:, :], Tsts[d][:, :], egp, pC[a][:, 0:129], ALU.mult, ALU.add)
                    S.act(Cbfd[d][(b_ + 1) % 2][:, :], Tsts[d][:, :], AF.Copy, scale=EG[:, d, c, hd:hd + 1])
                    prevc[d] = c
                phase1(0)
                for n_ in range(len(steps)):
                    if n_ + 1 < len(steps):
                        phase1(n_ + 1)
                    phase2(n_)
                S.act(sq[:, :, :], Hs[:, :, :], AF.Square)
                S.reduce(ssq[:, :], sq[:, :, :], ALU.add)
                S.ts(ssq[:, :], ssq[:, :], 1.0 / 128.0, ALU.mult, s2=EPS, op1=ALU.add)
                S.act(ssq[:, :], ssq[:, :], AF.Sqrt)
                S.recip(ssq[:, :], ssq[:, :])
                S.tt(gO[:, :, :], osg[:, :, :], mng[:, :].unsqueeze(1).to_broadcast([128, NT, 128]), ALU.mult)
                for c in range(NT):
                    S.stt(ym[:, c, :], Hs[:, c, :], ssq[:, c:c + 1], gO[:, c, :], ALU.mult, ALU.mult)
                for c0 in range(0, NT, 8):
                    for c in range(c0, c0 + 8):
                        S.transpose(pK[:, (c - c0) * 128:(c - c0 + 1) * 128], ym[:, c, :], P.ident_bf[:, :])
                    S.copy(ymT[:, c0 * 128:(c0 + 8) * 128], pK[:, :], eng=('act' if (c0 // 8) % 2 else 'dve'))
                S.dma(D['Yt'][hd * 128:(hd + 1) * 128, :], ymT[:, :])
    if C.debug:
        d_ = C.dbg_out('Yt_m', [512, SEQ], BF16)
        with S.scope() as pd:
            tmp = sb("dbgtmp_ytm", [128, 4, SEQ], BF16, pd)
            S.dma(tmp[:, :, :], D['Yt'][0:512, :].rearrange("(a p) n -> p a n", p=128))
            S.dma(d_.rearrange("(a p) n -> p a n", p=128), tmp[:, :, :])


def phase_hyena(C):
    S, D, P, sb, pst = C.S, C.D, C.P, C.sb, C.pst
    PI = math.pi
    with S.scope() as pa:
        hid2T = sb("hid2T", [64, SEQ], F32, pa)
        hid2rT = sb("hid2rT", [64, SEQ], F32, pa)
        frc = sb("frc", [64, 1], F32, pa)
        frb1 = sb("frb1", [64, 1], F32, pa)
        frb2 = sb("frb2", [64, 1], F32, pa)
        with S.scope() as p1:
            zT = sb("zT", [33, SEQ], F32, p1)
            zrT = sb("zrT", [33, SEQ], F32, p1)
            hid1 = sb("hid1", [64, SEQ], F32, p1)
            w1 = sb("hw1", [33, 64], F32, p1)
            w2 = sb("hw2", [64, 64], F32, p1)
            b1c = sb("b1c", [64, 1], F32, p1)
            b2c = sb("b2c", [64, 1], F32, p1)
            arg = [sb(f"harg{i}", [64, 512], F32, p1) for i in range(2)]
            m1 = [sb(f"hm1{i}", [64, 512], F32, p1) for i in range(2)]
            m2 = [sb(f"hm2{i}", [64, 512], F32, p1) for i in range(2)]
            psM = [pst(f"psM{i}", [128, 512], F32, p1) for i in range(2)]
            S.dma(zT[:, :], D['zT'][:, :])
            S.dma(zrT[:, :], D['zrT'][:, :])
            S.dma(w1[:, :], D['hy_w1'][:, :])
            S.dma(w2[:, :], D['hy_w2'][:, :])
            S.dma(b1c[:, :], D['hy_b1c'][:, :])
            S.dma(b2c[:, :], D['hy_b2c'][:, :])
            S.dma(frc[:, :], D['hy_frc'][:, :])
            S.tt(frb1[:, :], frc[:, :], b1c[:, :], ALU.mult)
            S.tt(frb2[:, :], frc[:, :], b2c[:, :], ALU.mult)
            n = 0

            def sin_layer(ps, frb, dst):
                nonlocal n
                a = n % 2
                n += 1
                S.ts(arg[a][:, :], ps, frc[:, 0:1], ALU.mult, s2=frb[:, 0:1], op1=ALU.add)
                S.ts(m1[a][:, :], arg[a][:, :], PI, ALU.is_gt, s2=-2.0 * PI, op1=ALU.mult)
                S.ts(m2[a][:, :], arg[a][:, :], -PI, ALU.is_lt, s2=2.0 * PI, op1=ALU.mult)
                S.tt(arg[a][:, :], arg[a][:, :], m1[a][:, :], ALU.add)
                S.tt(arg[a][:, :], arg[a][:, :], m2[a][:, :], ALU.add)
                S.act(dst, arg[a][:, :], AF.Sin)
            for (zs, hdst) in ((zT, hid2T), (zrT, hid2rT)):
                for blk in range(8):
                    ps = psM[blk % 2]
                    S.mm(ps[0:64, :], w1[:, :], zs[:, blk * 512:(blk + 1) * 512])
                    sin_layer(ps[0:64, :], frb1, hid1[:, blk * 512:(blk + 1) * 512])
                for blk in range(8):
                    ps = psM[blk % 2]
                    S.mm(ps[0:64, :], w2[:, :], hid1[:, blk * 512:(blk + 1) * 512])
                    sin_layer(ps[0:64, :], frb2, hdst[:, blk * 512:(blk + 1) * 512])
        with S.scope() as p2:
            w3 = sb("hw3", [64, 2048], F32, p2)
            trow = sb("trow", [128, SEQ], F32, p2)
            trrow = sb("trrow", [128, SEQ], F32, p2)
            ndel = sb("ndel", [128, 16], F32, p2)
            hbias = sb("hbias", [128, 8], F32, p2)
            kT = [sb(f"kTb{i}", [128, 2 * SEQ], BF16, p2) for i in range(2)]
            win = [sb(f"win{i}", [128, 512], F32, p2) for i in range(2)]
            k0t = sb("k0t", [128, 4], F32, p2)
            psK_ = [pst(f"psKf{i}", [128, 512], F32, p2) for i in range(3)]
            ps0 = pst("psK0", [128, 512], F32, p2)
            S.dma(w3[:, :], D['hy_w3'][:, :])
            S.dma(trow[:, :], D['t_row'][0:1, :].partition_broadcast(128))
            S.dma(trrow[:, :], D['tr_row'][0:1, :].partition_broadcast(128))
            S.dma(ndel[:, :], D['hy_del_col'][:, :])
            S.dma(hbias[:, :], D['hy_bias_col'][:, :])
            S.stt(ndel[:, :], ndel[:, :], -1.0, ndel[:, :], ALU.mult, ALU.max)
            S.ts(ndel[:, :], ndel[:, :], -1.0, ALU.mult)
            nb = 0
            nk = 0
            for o in range(2):
                for g in range(4):
                    kt = kT[nk % 2]
                    nk += 1
                    for d in range(2):
                        hs = hid2T if d == 0 else hid2rT
                        tr = trow if d == 0 else trrow
                        c0 = (o * 2 + d) * 512 + g * 128
                        di = o * 8 + d * 4 + g
                        for blk in range(8):
                            ps = psK_[nb % 3]
                            wn = win[nb % 2]
                            nb += 1
                            S.mm(ps[:, :], w3[:, c0:c0 + 128], hs[:, blk * 512:(blk + 1) * 512])
                            S.act(wn[:, :], tr[:, blk * 512:(blk + 1) * 512], AF.Exp, scale=ndel[:, di:di + 1])
                            S.stt(kt[:, d * SEQ + blk * 512:d * SEQ + (blk + 1) * 512], wn[:, :], 0.05, ps[:, :], ALU.add, ALU.mult)
                    S.memset(kt[:, SEQ:SEQ + 1], 0.0)
                    cf = (o * 2 + 0) * 512 + g * 128
                    cb = (o * 2 + 1) * 512 + g * 128
                    S.mm(ps0[:, 0:1], w3[:, cf:cf + 128], hid2T[:, 0:1])
                    S.mm(ps0[:, 1:2], w3[:, cb:cb + 128], hid2T[:, 0:1])
                    S.copy(k0t[:, 0:2], ps0[:, 0:2])
                    S.tt(k0t[:, 2:3], k0t[:, 0:1], k0t[:, 1:2], ALU.add)
                    S.ts(k0t[:, 3:4], k0t[:, 2:3], 1.05, ALU.mult)
                    S.tt(kt[:, 0:1], k0t[:, 3:4], hbias[:, o * 4 + g:o * 4 + g + 1], ALU.add)
                    dst = D['Kd0'] if o == 0 else D['Kd1']
                    S.dma(dst[g * 128:(g + 1) * 128, :], kt[:, :], q='pool')
    if C.debug:
        d_ = C.dbg_out('Kd0', [512, 2 * SEQ], BF16)
        with S.scope() as pd:
            tmp = sb("dbgtmp_kd", [128, 4, 2 * SEQ], BF16, pd)
            S.dma(tmp[:, :, :], D['Kd0'].rearrange("(a p) n -> p a n", p=128))
            S.dma(d_.rearrange("(a p) n -> p a n", p=128), tmp[:, :, :])
    with S.scope() as pb:
        T = Ctx()
        T.F1 = sb("F1", [64, 128], BF16, pb)
        T.GrT = sb("GrT", [128, 64, 128], BF16, pb)
        T.GiT = sb("GiT", [128, 64, 128], BF16, pb)
        T.Rc1 = sb("Rc1", [128, 256], BF16, pb)
        T.Rc2 = sb("Rc2", [128, 256], BF16, pb)
        T.LrT = sb("LrT", [64, 128, 32], BF16, pb)
        T.LiNT = sb("LiNT", [64, 128, 32], BF16, pb)
        hnorm = sb("hnorm", [128, 4], F32, pb)
        S.dma(T.F1[:, :], D['F1'][:, :])
        for nm in ('GrT', 'GiT'):
            S.dma(getattr(T, nm)[:, :, :], D[nm].rearrange("p (k m) -> p k m", k=64))
        S.dma(T.Rc1[:, :], D['Rc1'][:, :])
        S.dma(T.Rc2[:, :], D['Rc2'][:, :])
        S.dma(T.LrT[:, :, :], D['LrT'].rearrange("p (n m) -> p n m", n=128))
        S.dma(T.LiNT[:, :, :], D['LiNT'].rearrange("p (n m) -> p n m", n=128))
        S.dma(hnorm[:, :], D['hnorm_col'][:, :])
        for o in range(2):
            zsrc = D['Zd'] if o == 0 else D['Z1d']
            kd = D['Kd0'] if o == 0 else D['Kd1']
            with S.scope() as pw:
                W = Ctx()
                W.X = sb("fX", [64, 64, 128], BF16, pw)
                W.AT = sb("fAT", [128, 64, 2, 64], BF16, pw)
                W.ATn = sb("fATn", [128, 64, 2, 64], BF16, pw)
                W.Kf = sb("fKf", [128, 64, 2, 64], BF16, pw)
                W.V = sb("fV", [128, 2, 64, 64], BF16, pw)
                W.Wt = sb("fWt", [64, 64, 2, 128], BF16, pw)
                W.Ysb = sb("fY", [32, 64, 128], BF16, pw)
                W.tm = [[sb(f"ftm{i}{j}", [128, 4, 64], F32, pw) for j in range(4)] for i in range(2)]
                W.ring = [pst(f"psR{i}", [128, 512], F32, pw) for i in range(8)]
                W.nr = 0
                W.ne = 0
                W.pref = False
                for b8 in range(8):
                    ch0 = b8 * 64
                    fft_batch(C, T, W, zsrc[ch0:ch0 + 64, :], kd[ch0:ch0 + 64, :], D['Ycv'][ch0:ch0 + 64, :],
                              kd_next=(kd[ch0 + 64:ch0 + 128, :] if b8 < 7 else None))
            with S.scope() as pg:
                ysb = [sb(f"gy{i}", [128, SEQ], BF16, pg) for i in range(2)]
                xsb = [sb(f"gx{i}", [128, SEQ], F32, pg) for i in range(2)]
                zo = [sb(f"gz{i}", [128, SEQ], BF16, pg) for i in range(2)]
                xsrc = D['X1d'] if o == 0 else D['X2d']
                if o == 1:
                    z2 = sb("gz2", [128, SEQ], F32, pg)
                    sq = [sb(f"gsq{i}", [128, 512], F32, pg) for i in range(2)]
                    rr = [sb(f"grr{i}", [128, 512], F32, pg) for i in range(2)]
                    psN = [pst(f"psN{i}", [128, 512], F32, pg) for i in range(2)]
                for g in range(4):
                    a = g % 2
                    S.dma(ysb[a][:, :], D['Ycv'][g * 128:(g + 1) * 128, :])
                    S.dma(xsb[a][:, :], xsrc[g * 128:(g + 1) * 128, :])
                    if o == 0:
                        S.tt(zo[a][:, :], xsb[a][:, :], ysb[a][:, :], ALU.mult)
                        S.dma(D['Z1d'][g * 128:(g + 1) * 128, :], zo[a][:, :], q='pool')
                    else:
                        S.tt(z2[:, :], xsb[a][:, :], ysb[a][:, :], ALU.mult)
                        for blk in range(8):
                            bs = slice(blk * 512, (blk + 1) * 512)
                            q_ = blk % 2
                            S.tt(sq[q_][:, :], z2[:, bs], z2[:, bs], ALU.mult)
                            S.mm(psN[q_][:, :], P.ones_f[:, :], sq[q_][:, :])
                            S.act(rr[q_][:, :], psN[q_][:, :], AF.Ln, scale=1.0 / 128.0, bias=EPS)
                            S.act(rr[q_][:, :], rr[q_][:, :], AF.Exp, scale=-0.5)
                            S.stt(zo[a][:, bs], z2[:, bs], hnorm[:, g:g + 1], rr[q_][:, :], ALU.mult, ALU.mult)
                        S.dma(D['Yt'][512 + g * 128:512 + (g + 1) * 128, :], zo[a][:, :], q='pool')
    if C.debug:
        d_ = C.dbg_out('Yt_h', [512, SEQ], BF16)
        with S.scope() as pd:
            tmp = sb("dbgtmp_yth", [128, 4, SEQ], BF16, pd)
            S.dma(tmp[:, :, :], D['Yt'][512:1024, :].rearrange("(a p) n -> p a n", p=128))
            S.dma(d_.rearrange("(a p) n -> p a n", p=128), tmp[:, :, :])


def fft_batch(C, T, W, zsrc, kdsrc, ydst, kd_next=None):
    S = C.S

    def bank():
        W.nr += 1
        return W.ring[W.nr % 8]

    def ev(dst, src):
        W.ne += 1
        S.copy(dst, src, eng=('act' if W.ne % 2 else 'dve'))

    def forward_d(kdim):
        for cq in range(16):
            pA = bank()
            for i in range(4):
                ch = cq * 4 + i
                S.mm(pA[:, i * 128:(i + 1) * 128], W.X[0:kdim, ch, :], T.F1[0:kdim, :])
            ev(W.AT[:, cq * 4:(cq + 1) * 4, :, :].rearrange("p c r k -> p (c r k)"), pA[:, :])
            src = pA[:, :].rearrange("p (c r k) -> p c r k", c=4, r=2)
            S.act(W.ATn[:, cq * 4:(cq + 1) * 4, 0, :], src[:, :, 1, :], AF.Copy, scale=-1.0)
            S.copy(W.ATn[:, cq * 4:(cq + 1) * 4, 1, :], src[:, :, 0, :])

    def forward_s(mode):
        for kb in range(16):
            pU = bank()
            for i in range(4):
                k1 = kb * 4 + i
                o_ = pU[:, i * 128:(i + 1) * 128]
                S.mm(o_, T.GrT[:, k1, :], W.AT[:, :, :, k1].rearrange("p c r -> p r c"), start=True, stop=False)
                S.mm(o_, T.GiT[:, k1, :], W.ATn[:, :, :, k1].rearrange("p c r -> p r c"), start=False, stop=True)
            if mode == 'kernel':
                ev(W.Kf[:, kb * 4:(kb + 1) * 4, :, :].rearrange("p k r c -> p (k r c)"), pU[:, :])
            else:
                pv = pU[:, :].rearrange("p (k r c) -> p k r c", k=4, r=2)
                Ur = pv[:, :, 0, :]
                Ui = pv[:, :, 1, :]
                Kr = W.Kf[:, kb * 4:(kb + 1) * 4, 0, :]
                Ki = W.Kf[:, kb * 4:(kb + 1) * 4, 1, :]
                t = W.tm[kb % 2]
                S.tt(t[0][:, :, :], Ur, Kr, ALU.mult)
                S.tt(t[1][:, :, :], Ui, Ki, ALU.mult)
                S.tt(t[2][:, :, :], Ur, Ki, ALU.mult)
                S.tt(t[3][:, :, :], Ui, Kr, ALU.mult)
                S.tt(W.V[:, 0, kb * 4:(kb + 1) * 4, :], t[0][:, :, :], t[1][:, :, :], ALU.subtract, eng='pool')
                S.tt(W.V[:, 1, kb * 4:(kb + 1) * 4, :], t[2][:, :, :], t[3][:, :, :], ALU.add, eng='pool')

    if not W.pref:
        S.dma(W.X[:, :, :], kdsrc.rearrange("c (a n) -> a c n", n=128))
    forward_d(64)
    forward_s('kernel')
    S.dma(W.X[0:32, :, :], zsrc.rearrange("c (a n) -> a c n", n=128))
    forward_d(32)
    W.pref = False
    if kd_next is not None:
        S.dma(W.X[:, :, :], kd_next.rearrange("c (a n) -> a c n", n=128))
        W.pref = True
    forward_s('data')
    for cp in range(32):
        pW = bank()
        for i in range(2):
            ch = cp * 2 + i
            o_ = pW[0:64, i * 256:(i + 1) * 256]
            S.mm(o_, W.V[:, 0, :, ch], T.Rc1[:, :], start=True, stop=False)
            S.mm(o_, W.V[:, 1, :, ch], T.Rc2[:, :], start=False, stop=True)
        ev(W.Wt[:, cp * 2:(cp + 1) * 2, :, :].rearrange("p c r n -> p (c r n)"), pW[0:64, :])
    for nb in range(16):
        pY = bank()
        for i in range(8):
            n2 = nb * 8 + i
            o_ = pY[0:32, i * 64:(i + 1) * 64]
            S.mm(o_, T.LrT[:, n2, :], W.Wt[:, :, 0, n2], start=True, stop=False)
            S.mm(o_, T.LiNT[:, n2, :], W.Wt[:, :, 1, n2], start=False, stop=True)
        ev(W.Ysb[:, :, nb * 8:(nb + 1) * 8], pY[0:32, :].rearrange("p (n c) -> p c n", n=8))
    S.dma(ydst.rearrange("c (a n) -> a c n", n=128), W.Ysb[:, :, :], q='pool')


def phase_outproj(C):
    S, D, P, sb, pst, nc = C.S, C.D, C.P, C.sb, C.pst, C.nc
    P.IDX = sb("IDX", [128, 16, 4], I32)
    P.GW = sb("GW", [128, 16, 4], F32)
    with S.scope() as ph:
        AFF = sb("AFF", [128, NT, 16], F32, ph)
        AFFT = sb("AFFT", [16, SEQ], F32, ph)
        with S.scope() as p1:
            ytT = sb("ytT", [128, 8, SEQ], BF16, p1)
            wout = sb("wout", [128, 8, DM], BF16, p1)
            wr = sb("wr", [128, 8, 16], F32, p1)
            xb = [sb(f"oxb{i}", [128, DM], F32, p1) for i in range(2)]
            x1 = [sb(f"ox1{i}", [128, DM], F32, p1) for i in range(3)]
            tmp = [sb(f"otmp{i}", [128, DM], F32, p1) for i in range(2)]
            hf = [sb(f"ohf{i}", [128, DM], F32, p1) for i in range(3)]
            hfb = [sb(f"ohfb{i}", [128, DM], BF16, p1) for i in range(3)]
            hfT = [sb(f"ohfT{i}", [128, 8, 128], F32, p1) for i in range(2)]
            junk = sb("ojunk", [128, DM], BF16, p1)
            sm = sb("osm", [128, NT, 8], F32, p1)
            ex = [sb(f"oex{i}", [128, 16], F32, p1) for i in range(2)]
            psM = [[pst(f"psMo{i}{h}", [128, 512], F32, p1) for h in range(2)] for i in range(2)]
            psT = [pst(f"psTo{i}", [128, 512], F32, p1) for i in range(2)]
            psR = pst("psR", [128, 512], F32, p1)
            psAT = pst("psAT", [128, 512], F32, p1)
            psAT2 = psT[1]
            for k in range(8):
                S.dma(ytT[:, k, :], D['Yt'][k * 128:(k + 1) * 128, :])
            wsrc = D['w_out'].rearrange("(k p) c -> p k c", p=128)
            S.dma(wout[:, :, 0:512], wsrc[:, :, 0:512], q='pool')
            S.dma(wout[:, :, 512:1024], wsrc[:, :, 512:1024], q='pool')
            S.dma(wr[:, :, :], D['w_router'].rearrange("(k p) e -> p k e", p=128))
            def stage_a1(j):
                a = j % 2
                b = j % 3
                xt = xb[a]
                S.dma(xt[:, :], D['x'][j * 128:(j + 1) * 128, :])
                for h in range(2):
                    for k in range(8):
                        S.mm(psM[a][h][:, :], ytT[:, k, j * 128:(j + 1) * 128], wout[:, k, h * 512:(h + 1) * 512],
                             start=(k == 0), stop=(k == 7))
                for h in range(2):
                    S.tt(tmp[a][:, h * 512:(h + 1) * 512], psM[a][h][:, :], P.gt1rep[:, h * 512:(h + 1) * 512], ALU.mult)
                S.tt(x1[b][:, :], tmp[a][:, :], xt[:, :], ALU.add)
                S.dma(D['acc'][j * 128:(j + 1) * 128, :], x1[b][:, :], q='pool')

            def stage_a2(j):
                b = j % 3
                ss = sm[:, j, 0:1]
                rs = sm[:, j, 1:2]
                S.act(junk[:, :], x1[b][:, :], AF.Square, accum_out=ss)
                S.act(rs, ss, AF.Ln, scale=1.0 / DM, bias=EPS)
                S.act(rs, rs, AF.Exp, scale=-0.5)
                S.stt(hf[b][:, :], x1[b][:, :], rs, P.A2rep[:, :], ALU.mult, ALU.mult)
                S.tt(hf[b][:, :], hf[b][:, :], P.B2rep[:, :], ALU.add)
                S.act(hfb[b][:, :], hf[b][:, :], AF.Copy)
                S.dma(D['HFd'][j * 128:(j + 1) * 128, :], hfb[b][:, :], q='pool')

            def stage_b1(j):
                a = j % 2
                b = j % 3
                for k in range(8):
                    S.transpose(psT[k // 4][:, (k % 4) * 128:(k % 4 + 1) * 128], hf[b][:, k * 128:(k + 1) * 128], P.ident_f[:, :])
                S.copy(hfT[a][:, 0:4, :].rearrange("p k t -> p (k t)"), psT[0][:, :], eng='act')
                S.copy(hfT[a][:, 4:8, :].rearrange("p k t -> p (k t)"), psT[1][:, :], eng='dve')

            def stage_b2(j):
                a = j % 2
                for k in range(8):
                    S.mm(psR[:, 0:16], hfT[a][:, k, :], wr[:, k, :], start=(k == 0), stop=(k == 7))
                mx = sm[:, j, 2:3]
                se = sm[:, j, 3:4]
                S.reduce(mx, psR[:, 0:16], ALU.max)
                S.ts(mx, mx, -1.0, ALU.mult)
                S.act(ex[a][:, :], psR[:, 0:16], AF.Exp, bias=mx, accum_out=se)
                S.recip(se, se)
                S.ts(AFF[:, j, :], ex[a][:, :], se, ALU.mult)
            for step in range(NT + 2):
                if 0 <= step - 2 < NT:
                    stage_b1(step - 2)
                if step < NT:
                    stage_a1(step)
                if 0 <= step - 1 < NT:
                    stage_a2(step - 1)
                if 0 <= step - 2 < NT:
                    stage_b2(step - 2)
            for j in range(NT):
                pat = psAT if j % 2 == 0 else psAT2
                S.transpose(pat[0:16, 0:128], AFF[:, j, :], P.ident_f[:, :])
                S.copy(AFFT[:, j * 128:(j + 1) * 128], pat[0:16, 0:128], eng=('act' if j % 2 else 'dve'))
            if C.debug:
                pass
        if C.debug:
            d_ = C.dbg_out('AFF', [128, NT, 16])
            S.dma(d_[:, :, :], AFF[:, :, :])
        with S.scope() as p2:
            junkA = sb("junkA", [16, SEQ], F32, p2)
            bs = sb("bis", [16, 8], F32, p2)
            THR = sb("THR", [128, 16], F32, p2)
            throw = sb("throw", [1, 16], F32, p2)
            SEL = sb("SEL", [128, NT, 16], F32, p2)
            POS = sb("POS", [128, NT, 16], F32, p2)
            selcum = sb("selcum", [128, 16], F32, p2)
            striU = sb("striU", [128, 128], F32, p2)
            COORD = sb("COORD", [128, NT, 16, 4], BF16, p2)
            jf = sb("jf", [128, NT], F32, p2)
            pf = sb("pf", [128, 1], F32, p2)
            iot = sb("iot", [128, 512], mybir.dt.float16, p2)
            OH = [sb(f"OH{i}", [128, 512], BF16, p2) for i in range(3)]
            r4 = [sb(f"r4{i}", [128, 8], F32, p2) for i in range(2)]
            psI = [pst(f"psI{i}", [128, 512], F32, p2) for i in range(4)]
            psP = [pst(f"psP{i}", [128, 512], F32, p2) for i in range(2)]
            psX = pst("psX", [128, 512], F32, p2)
            psB2 = pst("psB2", [128, 512], F32, p2)
            lo, hi, mid, cnt, ge, d1, d2 = [bs[:, i:i + 1] for i in range(7)]
            S.dma(striU[:, :], D['striU'][:, :])
            S.memset(lo, 0.0)
            S.memset(hi, 1.0)
            for it in range(30):
                S.tt(mid, lo, hi, ALU.add)
                S.ts(mid, mid, 0.5, ALU.mult)
                S.ts(junkA[:, :], AFFT[:, :], mid, ALU.is_ge, s2=0.0, op1=ALU.add, accum_out=cnt)
                S.ts(ge, cnt, 511.5, ALU.is_gt)
                S.tt(d1, mid, lo, ALU.subtract)
                S.tt(d2, hi, mid, ALU.subtract)
                S.stt(lo, d1, ge, lo, ALU.mult, ALU.add)
                S.stt(hi, d2, ge, mid, ALU.mult, ALU.add)
            S.transpose(psX[0:1, 0:16], lo, P.ident_f[0:16, 0:16])
            S.copy(throw[:, :], psX[0:1, 0:16])
            S.mm(psB2[:, 0:16], P.ones_f[0:1, 0:128], throw[0:1, :])
            S.copy(THR[:, :], psB2[:, 0:16])
            S.tt(SEL[:, :, :], AFF[:, :, :], THR[:, :].unsqueeze(1).to_broadcast([128, NT, 16]), ALU.is_ge)
            S.memset(selcum[:, :], 0.0)
            for j in range(NT):
                pp = psP[j % 2]
                S.mm(pp[:, 0:16], striU[:, :], SEL[:, j, :], start=True, stop=False)
                S.mm(pp[:, 0:16], P.ones_f[:, :], selcum[:, :], start=False, stop=True)
                S.copy(POS[:, j, :], pp[:, 0:16], eng='act')
                S.tt(selcum[:, :], selcum[:, :], SEL[:, j, :], ALU.add)
            S.op('pool', lambda e: e.iota(jf[:, :], [[1, NT]], base=0, channel_multiplier=0, allow_small_or_imprecise_dtypes=True), [], [jf[:, :]])
            S.op('pool', lambda e: e.iota(pf[:, :], [[1, 1]], base=0, channel_multiplier=1, allow_small_or_imprecise_dtypes=True), [], [pf[:, :]])
            S.op('pool', lambda e: e.iota(iot[:, :], [[1, 512]], base=0, channel_multiplier=0, allow_small_or_imprecise_dtypes=True), [], [iot[:, :]])
            S.copy(COORD[:, :, :, 0], jf[:, :].unsqueeze(2).to_broadcast([128, NT, 16]))
            S.copy(COORD[:, :, :, 1], pf[:, 0:1].unsqueeze(2).to_broadcast([128, NT, 16]))
            S.copy(COORD[:, :, :, 2], AFF[:, :, :])
            S.tt(COORD[:, :, :, 3], AFF[:, :, :], COORD[:, :, :, 2], ALU.subtract)
            n = 0
            for e_ in range(16):
                for j in range(NT):
                    oh = OH[n % 3]
                    n += 1
                    S.ts(oh[:, :], iot[:, :], POS[:, j, e_:e_ + 1], ALU.is_equal, s2=SEL[:, j, e_:e_ + 1], op1=ALU.mult)
                    for sc in range(4):
                        S.mm(psI[sc][:, 0:4], oh[:, sc * 128:(sc + 1) * 128], COORD[:, j, e_, :], start=(j == 0), stop=(j == NT - 1))
                for sc in range(4):
                    r = r4[(e_ * 4 + sc) % 2]
                    S.copy(r[:, 0:4], psI[sc][:, 0:4], eng='act')
                    S.stt(r[:, 4:5], r[:, 0:1], 128.0, r[:, 1:2], ALU.mult, ALU.add)
                    S.copy(P.IDX[:, e_, sc:sc + 1], r[:, 4:5])
                    S.tt(P.GW[:, e_, sc:sc + 1], r[:, 2:3], r[:, 3:4], ALU.add)
        if C.debug:
            d_ = C.dbg_out('IDX', [128, 16, 4], I32)
            S.dma(d_[:, :, :], P.IDX[:, :, :])
            d_ = C.dbg_out('GW', [128, 16, 4])
            S.dma(d_[:, :, :], P.GW[:, :, :])


def phase_route(C):
    pass


def phase_experts(C):
    S, D, P, sb, pst, nc = C.S, C.D, C.P, C.sb, C.pst, C.nc
    with S.scope() as ph:
        stg = [sb(f"stg{i}", [128, 8, 512], F32, ph) for i in range(4)]
        wb = [sb(f"wb{i}", [128, 8, 512], BF16, ph) for i in range(8)]
        xs = sb("xs", [128, 4, DM], BF16, ph)
        xsT = sb("xsT", [128, 8, 512], BF16, ph)
        hidT = sb("hidT", [128, 16, 512], BF16, ph)
        sg = [sb(f"sg{i}", [128, 512], F32, ph) for i in range(2)]
        ysb = [sb(f"ysb{i}", [128, DM], F32, ph) for i in range(4)]
        psT = pst("psTe", [128, 1024], BF16, ph)
        psG = [pst(f"psGe{i}", [128, 512], F32, ph) for i in range(2)]
        psU = pst("psUe", [128, 512], F32, ph)
        psY = [pst(f"psYe{i}", [128, 512], F32, ph) for i in range(4)]
        pieces = []
        for e_ in range(16):
            for fb in range(4):
                for nm in ('w_gate', 'w_up'):
                    pieces.append(D[nm][e_ * 1024:(e_ + 1) * 1024, fb * 512:(fb + 1) * 512].rearrange("(k p) c -> p k c", p=128))
            for dh in range(2):
                for fh in range(2):
                    r0 = e_ * 2048 + fh * 1024
                    pieces.append(D['w_down'][r0:r0 + 1024, dh * 512:(dh + 1) * 512].rearrange("(k p) c -> p k c", p=128))
        issued = [0]
        cast_eng = ['act', 'dve']
        LOOK = 6

        def issue_upto(n):
            while issued[0] < min(n, len(pieces)):
                i = issued[0]
                s_ = stg[i % 4]
                S.dma(s_[:, :, :], pieces[i])
                S.copy(wb[i % 8][:, :, :], s_[:, :, :], eng=cast_eng[i % 2])
                issued[0] += 1
        pc = [0]

        def next_piece():
            i = pc[0]
            issue_upto(i + 1 + LOOK)
            pc[0] += 1
            return wb[i % 8]
        ng = 0
        for e_ in range(16):
            for st in range(4):
                idx_ap = P.IDX[:, e_, st:st + 1]
                S.op('pool', lambda e, st=st, idx_ap=idx_ap: e.indirect_dma_start(
                    out=xs[:, st, :], out_offset=None, in_=D['HFd'][:, :],
                    in_offset=bass.IndirectOffsetOnAxis(ap=idx_ap, axis=0)),
                    [D['HFd'][:, :], idx_ap], [xs[:, st, :]], dma=True)
            for st in range(4):
                for k in range(8):
                    S.transpose(psT[:, k * 128:(k + 1) * 128], xs[:, st, k * 128:(k + 1) * 128], P.ident_bf[:, :])
                S.copy(xsT[:, :, st * 128:(st + 1) * 128], psT[:, :].rearrange("p (k t) -> p k t", k=8), eng=('act' if st % 2 else 'dve'))
            for fb in range(4):
                wg = next_piece()
                wu = next_piece()
                for fc in range(4):
                    pg = psG[ng % 2]
                    sgt = sg[ng % 2]
                    ng += 1
                    for k in range(8):
                        S.mm(pg[:, :], wg[:, k, fc * 128:(fc + 1) * 128], xsT[:, k, :], start=(k == 0), stop=(k == 7))
                    for k in range(8):
                        S.mm(psU[:, :], wu[:, k, fc * 128:(fc + 1) * 128], xsT[:, k, :], start=(k == 0), stop=(k == 7))
                    S.act(sgt[:, :], pg[:, :], AF.Silu)
                    S.tt(hidT[:, fb * 4 + fc, :], sgt[:, :], psU[:, :], ALU.mult)
            for dh in range(2):
                for fh in range(2):
                    wd = next_piece()
                    for st in range(4):
                        for f8 in range(8):
                            S.mm(psY[st][:, :], hidT[:, fh * 8 + f8, st * 128:(st + 1) * 128], wd[:, f8, :],
                                 start=(fh == 0 and f8 == 0), stop=(fh == 1 and f8 == 7))
                for st in range(4):
                    S.stt(ysb[st][:, dh * 512:(dh + 1) * 512], psY[st][:, :], P.GW[:, e_, st:st + 1],
                          P.gt2rep[:, dh * 512:(dh + 1) * 512], ALU.mult, ALU.mult)
            for st in range(4):
                idx_ap = P.IDX[:, e_, st:st + 1]
                S.op('pool', lambda e, st=st, idx_ap=idx_ap: e.indirect_dma_start(
                    out=D['acc'][:, :], out_offset=bass.IndirectOffsetOnAxis(ap=idx_ap, axis=0),
                    in_=ysb[st][:, :], in_offset=None, compute_op=ALU.add),
                    [ysb[st][:, :], idx_ap, D['acc'][:, :]], [D['acc'][:, :]], dma=True)


def phase_final(C):
    S, D, P, sb, pst = C.S, C.D, C.P, C.sb, C.pst
    with S.scope() as ph:
        gfin = sb("gfin", [128, DM], F32, ph)
        xb = [sb(f"fxb{i}", [128, DM], F32, ph) for i in range(4)]
        ob = [sb(f"fob{i}", [128, DM], F32, ph) for i in range(4)]
        junk = sb("fjunk", [128, DM], BF16, ph)
        sm = sb("fsm", [128, NT, 2], F32, ph)
        S.dma(gfin[:, :], D['gfin_row'][0:1, :].partition_broadcast(128))
        for j in range(NT):
            a = j % 4
            S.dma(xb[a][:, :], D['acc'][j * 128:(j + 1) * 128, :])
            ss = sm[:, j, 0:1]
            rs = sm[:, j, 1:2]
            S.act(junk[:, :], xb[a][:, :], AF.Square, accum_out=ss)
            S.act(rs, ss, AF.Ln, scale=1.0 / DM, bias=EPS)
            S.act(rs, rs, AF.Exp, scale=-0.5)
            S.stt(ob[a][:, :], xb[a][:, :], rs, gfin[:, :], ALU.mult, ALU.mult)
            S.dma(D['out'][j * 128:(j + 1) * 128, :], ob[a][:, :], q='pool')


_PROG = {}


def kernel(**inputs):
    if 'nc' not in _PROG:
        _PROG['nc'] = build()[0]
    nc = _PROG['nc']
    B = inputs['x'].shape[0]
    in_maps = [layout_inputs(inputs, b) for b in range(B)]
    res = run_bass_kernel_spmd(nc, in_maps, core_ids=list(range(B)))
    out = np.stack([np.asarray(r["out"], dtype=np.float32) for r in res.results], axis=0)
    return out
```

```python
import math
import numpy as np
import ml_dtypes
import concourse.bass as bass
import concourse.mybir as mybir
from concourse.bass_utils import run_bass_kernel_spmd
from contextlib import ExitStack

F32 = mybir.dt.float32
BF16 = mybir.dt.bfloat16
I32 = mybir.dt.int32
AF = mybir.ActivationFunctionType
ALU = mybir.AluOpType
AX = mybir.AxisListType

SEQ = 4096
DM = 1024
NT = 32
EPS = 1e-6
EMIT_UNTIL = [None]
COMPUTE = ('pe', 'act', 'dve', 'pool')
SELF_SYNC = {'act': True, 'dve': True, 'pool': True, 'pe': False}
NDMA_SEMS = 6


def ap_box(ap):
    t = ap.tensor
    name = t.name
    dims = list(ap.ap)
    off = int(ap.offset)
    sp = str(ap.space() if callable(ap.space) else ap.space)
    is_dram = 'DRAM' in sp.upper() or 'HBM' in sp.upper() or type(t).__name__.startswith('DRAM') or type(t).__name__.startswith('Dram')
    if is_dram:
        lo = off
        hi = off
        for (st, cnt) in dims:
            st = int(st); cnt = int(cnt)
            if st >= 0:
                hi += st * (cnt - 1)
            else:
                lo += st * (cnt - 1)
        return (name, 0, 1, lo, hi + 1)
    if 'PSUM' in sp.upper() or type(t).__name__.startswith('PSum'):
        return (name, 0, 128, 0, 1 << 30)
    p0 = int(ap.start_partition())
    pc = int(dims[0][1])
    lo = off
    hi = off
    for (st, cnt) in dims[1:]:
        st = int(st); cnt = int(cnt)
        if st >= 0:
            hi += st * (cnt - 1)
        else:
            lo += st * (cnt - 1)
    return (name, p0, p0 + pc, lo, hi + 1)


class Op:
    __slots__ = ('stream', 'fn', 'deps', 'is_dma', 'signal', 'semval', 'dma_slot', 'idx', 'extra_waits')


class Sched:
    def __init__(self, nc, es):
        self.nc = nc
        self.es = es
        self.ops = []
        self.track = {}
        self.eng = {'pe': nc.tensor, 'act': nc.scalar, 'dve': nc.vector, 'pool': nc.gpsimd, 'sp': nc.sync}
        self.sem = {s: es.enter_context(nc.semaphore('sem_' + s)) for s in COMPUTE}
        self.dma_sems = {}
        for s in ('sp', 'pool', 'act'):
            self.dma_sems[s] = [es.enter_context(nc.semaphore(f'dsem_{s}{i}')) for i in range(NDMA_SEMS)]
        self.dma_count = {'sp': 0, 'pool': 0, 'act': 0}
        self.last_dma_ops = {'sp': [], 'pool': [], 'act': []}

    def _deps(self, boxes_r, boxes_w, idx, stream, is_dma):
        deps = set()
        for (boxes, is_w) in ((boxes_r, False), (boxes_w, True)):
            for b in boxes:
                lst = self.track.setdefault(b[0], [])
                keep = []
                for ent in lst:
                    eb, eidx, ew = ent
                    ov = not (eb[2] <= b[1] or b[2] <= eb[1] or eb[4] <= b[3] or b[4] <= eb[3])
                    if ov and (is_w or ew):
                        deps.add(eidx)
                    covered = (b[1] <= eb[1] and eb[2] <= b[2] and b[3] <= eb[3] and eb[4] <= b[4])
                    if is_w and covered:
                        continue
                    if (not is_w) and (not ew) and covered and (not is_dma):
                        eo = self.ops[eidx]
                        if eo.stream == stream and not eo.is_dma:
                            continue
                    keep.append(ent)
                keep.append([b, idx, is_w])
                self.track[b[0]] = keep
        deps.discard(idx)
        return deps

    def op(self, stream, fn, reads=(), writes=(), dma=False):
        o = Op()
        o.idx = len(self.ops)
        o.stream = stream
        o.fn = fn
        o.is_dma = dma
        o.signal = False
        o.semval = None
        o.dma_slot = None
        o.extra_waits = []
        br = [ap_box(a) for a in reads if a is not None and not isinstance(a, (int, float))]
        bw = [ap_box(a) for a in writes]
        self.ops.append(o)
        o.deps = self._deps(br, bw, o.idx, stream, dma)
        return o

    def emit(self):
        ops = self.ops
        for o in ops:
            for d in o.deps:
                po = ops[d]
                if po.is_dma:
                    continue
                if po.stream != o.stream or o.is_dma or SELF_SYNC.get(po.stream, True):
                    po.signal = True
        cnt = {s: 0 for s in COMPUTE}
        dcnt = {'sp': 0, 'pool': 0, 'act': 0}
        for o in ops:
            if o.is_dma:
                d = dcnt[o.stream]
                o.dma_slot = (d % NDMA_SEMS, 16 * (d // NDMA_SEMS + 1))
                dcnt[o.stream] = d + 1
            elif o.signal:
                cnt[o.stream] += 1
                o.semval = cnt[o.stream]
        waited = {s: {} for s in self.eng}
        nw = 0
        for o in ops:
            e = self.eng[o.stream]
            w = waited[o.stream]
            toks = []
            if o.is_dma:
                j, v = o.dma_slot
                if v > 16:
                    toks.append((('d', o.stream, j), self.dma_sems[o.stream][j], v - 16))
            for d in o.deps:
                po = ops[d]
                if po.is_dma:
                    j, v = po.dma_slot
                    toks.append((('d', po.stream, j), self.dma_sems[po.stream][j], v))
                else:
                    if po.stream == o.stream and not o.is_dma and not SELF_SYNC.get(po.stream, True):
                        continue
                    toks.append((('c', po.stream), self.sem[po.stream], po.semval))
            best = {}
            for key, sem, v in toks:
                if v is None:
                    raise RuntimeError('dep on non-signaling op')
                if w.get(key, 0) >= v:
                    continue
                if key not in best or best[key][1] < v:
                    best[key] = (sem, v)
            for key, (sem, v) in best.items():
                e.wait_ge(sem, v)
                w[key] = v
                nw += 1
            ins = o.fn(e)
            if ins is None:
                continue
            if o.is_dma:
                j, v = o.dma_slot
                ins.then_inc(self.dma_sems[o.stream][j], 16)
            elif o.signal:
                ins.then_inc(self.sem[o.stream], 1)
        self.n_waits = nw
        return cnt, dcnt

    def dma(self, out, in_, q='sp', **kw):
        return self.op(q, lambda e: e.dma_start(out=out, in_=in_, **kw), [in_], [out], dma=True)

    def mm(self, out, lhsT, rhs, start=True, stop=True, **kw):
        return self.op('pe', lambda e: e.matmul(out, lhsT, rhs, start=start, stop=stop, **kw),
                       [lhsT, rhs] + ([] if start else [out]), [out])

    def transpose(self, out, in_, ident):
        return self.op('pe', lambda e: e.transpose(out, in_, ident), [in_, ident], [out])

    def act(self, out, in_, func, scale=1.0, bias=0.0, accum_out=None, eng='act'):
        rd = [in_]
        if not isinstance(scale, (int, float)):
            rd.append(scale)
            if func == AF.Copy:
                func = AF.Identity
        if not isinstance(bias, (int, float)):
            rd.append(bias)
            if func == AF.Copy:
                func = AF.Identity
        wr = [out] + ([accum_out] if accum_out is not None else [])
        kw = {}
        if accum_out is not None:
            kw['accum_out'] = accum_out
        return self.op('act', lambda e: e.activation(out=out, in_=in_, func=func, scale=scale, bias=bias, **kw), rd, wr)

    def tt(self, out, in0, in1, op, eng='dve'):
        return self.op(eng, lambda e: e.tensor_tensor(out=out, in0=in0, in1=in1, op=op), [in0, in1], [out])

    def ts(self, out, in0, s1, op0, s2=None, op1=None, eng='dve', accum_out=None):
        rd = [in0]
        if not isinstance(s1, (int, float)):
            rd.append(s1)
        if s2 is not None and not isinstance(s2, (int, float)):
            rd.append(s2)
        kw = {}
        if op1 is not None:
            kw['op1'] = op1
        if accum_out is not None:
            kw['accum_out'] = accum_out
        wr = [out] + ([accum_out] if accum_out is not None else [])
        return self.op(eng, lambda e: e.tensor_scalar(out=out, in0=in0, scalar1=s1, scalar2=s2, op0=op0, **kw), rd, wr)

    def stt(self, out, in0, scalar, in1, op0, op1, eng='dve'):
        rd = [in0, in1]
        if not isinstance(scalar, (int, float)):
            rd.append(scalar)
        return self.op(eng, lambda e: e.scalar_tensor_tensor(out=out, in0=in0, scalar=scalar, in1=in1, op0=op0, op1=op1), rd, [out])

    def copy(self, out, in_, eng='dve'):
        if eng == 'act':
            return self.act(out, in_, AF.Copy)
        return self.op(eng, lambda e: e.tensor_copy(out=out, in_=in_), [in_], [out])

    def memset(self, out, val, eng='dve'):
        return self.op(eng, lambda e: e.memset(out, val), [], [out])

    def reduce(self, out, in_, op, axis=None, eng='dve'):
        axis = axis or AX.X
        return self.op(eng, lambda e: e.tensor_reduce(out=out, in_=in_, op=op, axis=axis), [in_], [out])

    def recip(self, out, in_, eng='dve'):
        return self.op(eng, lambda e: e.reciprocal(out=out, in_=in_), [in_], [out])

    def barrier(self):
        alld = set()
        for lst in self.track.values():
            for ent in lst:
                alld.add(ent[1])
        self.track = {}
        for s_ in ('pe', 'act', 'dve', 'pool', 'sp'):
            o = self.op(s_, lambda e: None, [], [])
            o.deps = set(alld)

    def scope(self):
        return _Scope(self)

    def finish(self, out_aps):
        boxes = [ap_box(a) for a in out_aps]
        o = self.op('sp', lambda e: None, list(out_aps), [])
        return o


class _Scope:
    def __init__(self, S):
        self.S = S
        self.st = ExitStack()

    def __enter__(self):
        self.st.__enter__()
        return self.st

    def __exit__(self, *a):
        self.S.barrier()
        return self.st.__exit__(*a)

_CONSTS = {}


def _bf(a):
    return np.ascontiguousarray(a.astype(np.float32)).astype(ml_dtypes.bfloat16)


def host_consts():
    if _CONSTS:
        return _CONSTS
    c = {}
    c['ident_bf'] = _bf(np.eye(128))
    c['ident_f'] = np.eye(128, dtype=np.float32)
    s = np.arange(128)[:, None]
    t = np.arange(128)[None, :]
    c['triU'] = (s <= t).astype(np.float32)
    c['triL'] = (s >= t).astype(np.float32)
    c['striU'] = (s < t).astype(np.float32)
    N = 8192
    n1 = np.arange(64); k1 = np.arange(64); n2 = np.arange(128); k2 = np.arange(128)
    ang = 2 * np.pi * np.outer(n1, k1) / 64
    c['F1'] = _bf(np.concatenate([np.cos(ang), -np.sin(ang)], 1))
    th = 2 * np.pi * ((n2[:, None, None] * (k1[None, :, None] + 64 * k2[None, None, :])) % N) / N
    c['GrT'] = _bf(np.cos(th).reshape(128, 64 * 128))
    c['GiT'] = _bf((-np.sin(th)).reshape(128, 64 * 128))
    c['GiNT'] = _bf((np.sin(th)).reshape(128, 64 * 128))
    ph = 2 * np.pi * np.outer(k2, n2) / 128
    Rr = np.cos(ph); Ri = np.sin(ph)
    c['Rc1'] = _bf(np.concatenate([Rr, Ri], 1))
    c['Rc2'] = _bf(np.concatenate([-Ri, Rr], 1))
    n1h = np.arange(32)
    thL = 2 * np.pi * (k1[:, None, None] * n1h[None, None, :] / 64 + k1[:, None, None] * n2[None, :, None] / N)
    c['LrT'] = _bf((np.cos(thL) / N).reshape(64, 128 * 32))
    c['LiNT'] = _bf((-np.sin(thL) / N).reshape(64, 128 * 32))
    L = SEQ
    f32 = np.float32
    tt = np.linspace(0.0, 1.0, L, dtype=f32)[:, None]
    w = (f32(2.0 * math.pi) * np.arange(L, dtype=f32)[:, None] / f32(L)).astype(f32)
    bands = np.linspace(1e-4, 15, 16, dtype=f32)[None, :]
    z = np.concatenate([tt, np.cos(bands * w), -np.sin(bands * w)], axis=-1).astype(f32)
    zr = np.concatenate([z[0:1], z[:0:-1]], 0)
    c['zT'] = np.ascontiguousarray(z.T)
    c['zrT'] = np.ascontiguousarray(zr.T)
    trow = tt[:, 0]
    trr = np.concatenate([trow[0:1], trow[:0:-1]])
    c['t_row'] = np.ascontiguousarray(trow[None, :]).astype(f32)
    c['tr_row'] = np.ascontiguousarray(trr[None, :]).astype(f32)
    _CONSTS.update(c)
    return _CONSTS


def colmaj(v, nk):
    return np.ascontiguousarray(np.asarray(v, dtype=np.float32).reshape(nk, 128).T)


def layout_inputs(inp, b):
    m = {}
    f = lambda a: np.ascontiguousarray(np.asarray(a, dtype=np.float32))
    m['x'] = f(inp['x'][b])
    m['ccol'] = colmaj(inp['c'][b], 8)
    m['w_ada'] = f(inp['w_ada'][0])
    m['b_ada'] = f(inp['b_ada'][0][None, :])
    m['gmix_col'] = colmaj(inp['g_mix'][0], 8)
    m['w_in'] = f(inp['w_in'][0])
    bin_ = np.asarray(inp['b_in'][0], dtype=np.float32)
    m['bin_row'] = f(bin_[None, 1024:2064])
    m['bqk_col'] = colmaj(bin_[0:1024], 8)
    m['bhy_col'] = colmaj(bin_[2064:3600], 12)
    cw = np.asarray(inp['conv_qk_w'][0], dtype=np.float32)
    m['cqk_w'] = np.ascontiguousarray(cw.reshape(3, 8, 128).transpose(2, 1, 0))
    m['cqk_b'] = colmaj(inp['conv_qk_b'][0], 8)
    cw = np.asarray(inp['conv_hy_w'][0], dtype=np.float32)
    m['chy_w'] = np.ascontiguousarray(cw.reshape(3, 12, 128).transpose(2, 1, 0))
    m['chy_b'] = colmaj(inp['conv_hy_b'][0], 12)
    m['mnorm_g'] = f(inp['mlstm_norm_g'][0][None, :])
    m['hy_w1'] = f(inp['hy_w1'][0])
    m['hy_b1c'] = f(inp['hy_b1'][0][:, None])
    m['hy_w2'] = f(inp['hy_w2'][0])
    m['hy_b2c'] = f(inp['hy_b2'][0][:, None])
    m['hy_w3'] = f(inp['hy_w3'][0])
    m['hy_frc'] = f(inp['hy_freq'][0][:, None])
    m['hy_del_col'] = colmaj(inp['hy_deltas'][0], 16)
    m['hy_bias_col'] = colmaj(np.asarray(inp['hy_bias'][0]).reshape(-1), 8)
    m['hnorm_col'] = colmaj(inp['hyena_norm_g'][0], 4)
    m['w_out'] = f(inp['w_out'][0])
    m['gffn_row'] = f(inp['g_ffn'][0][None, :])
    m['w_router'] = f(inp['w_router'][0])
    m['w_gate'] = f(inp['w_gate'][0]).reshape(16 * 1024, 2048)
    m['w_up'] = f(inp['w_up'][0]).reshape(16 * 1024, 2048)
    m['w_down'] = f(inp['w_down'][0]).reshape(16 * 2048, 1024)
    m['gfin_row'] = f(np.asarray(inp['g_final'])[None, :])
    m.update(host_consts())
    return m

INPUT_SPECS = [
    ('x', [SEQ, DM], F32), ('ccol', [128, 8], F32), ('w_ada', [DM, 6144], F32), ('b_ada', [1, 6144], F32),
    ('gmix_col', [128, 8], F32), ('w_in', [DM, 3600], F32), ('bin_row', [1, 1040], F32),
    ('bqk_col', [128, 8], F32), ('bhy_col', [128, 12], F32), ('cqk_w', [128, 8, 3], F32), ('cqk_b', [128, 8], F32),
    ('chy_w', [128, 12, 3], F32), ('chy_b', [128, 12], F32), ('mnorm_g', [1, 512], F32),
    ('hy_w1', [33, 64], F32), ('hy_b1c', [64, 1], F32), ('hy_w2', [64, 64], F32), ('hy_b2c', [64, 1], F32),
    ('hy_w3', [64, 2048], F32), ('hy_frc', [64, 1], F32), ('hy_del_col', [128, 16], F32),
    ('hy_bias_col', [128, 8], F32), ('hnorm_col', [128, 4], F32), ('w_out', [DM, DM], F32),
    ('gffn_row', [1, DM], F32), ('w_router', [DM, 16], F32), ('w_gate', [16 * 1024, 2048], F32),
    ('w_up', [16 * 1024, 2048], F32), ('w_down', [16 * 2048, 1024], F32), ('gfin_row', [1, DM], F32),
    ('ident_bf', [128, 128], BF16), ('ident_f', [128, 128], F32), ('triU', [128, 128], F32),
    ('triL', [128, 128], F32), ('striU', [128, 128], F32), ('F1', [64, 128], BF16),
    ('GrT', [128, 8192], BF16), ('GiT', [128, 8192], BF16), ('GiNT', [128, 8192], BF16),
    ('Rc1', [128, 256], BF16), ('Rc2', [128, 256], BF16), ('LrT', [64, 4096], BF16), ('LiNT', [64, 4096], BF16),
    ('zT', [33, SEQ], F32), ('zrT', [33, SEQ], F32), ('t_row', [1, SEQ], F32), ('tr_row', [1, SEQ], F32),
]


class Ctx:
    pass


def build(stop_after=None, debug=False):
    nc = bass.Bass("TRN2", target_bir_lowering=False)
    es = ExitStack()
    S = Sched(nc, es)
    C = Ctx()
    C.nc, C.S, C.es = nc, S, es
    C.debug = debug
    D = {}
    for name, shape, dt in INPUT_SPECS:
        D[name] = nc.dram_tensor(name, shape, dt, kind="ExternalInput").ap()
    D['out'] = nc.dram_tensor("out", [SEQ, DM], F32, kind="ExternalOutput").ap()

    def scratch(name, shape, dt):
        D[name] = nc.dram_tensor(name, shape, dt, kind="Internal").ap()
    scratch('QTd', [512, SEQ], BF16)
    scratch('KTd', [512, SEQ], BF16)
    scratch('Vd', [SEQ, 512], BF16)
    scratch('Od', [SEQ, 512], BF16)
    scratch('X1d', [512, SEQ], F32)
    scratch('X2d', [512, SEQ], F32)
    scratch('Zd', [512, SEQ], BF16)
    scratch('Z1d', [512, SEQ], BF16)
    scratch('Kd0', [512, 2 * SEQ], BF16)
    scratch('Kd1', [512, 2 * SEQ], BF16)
    scratch('Ycv', [512, SEQ], BF16)
    scratch('Yt', [DM, SEQ], BF16)
    scratch('HFd', [SEQ, DM], BF16)
    scratch('acc', [SEQ, DM], F32)
    C.D = D
    C.dbg = {}

    def dbg_out(name, shape, dt=F32):
        t = nc.dram_tensor("dbg_" + name, shape, dt, kind="ExternalOutput").ap()
        C.dbg[name] = t
        return t
    C.dbg_out = dbg_out

    cnt = [0]

    def sb(name, shape, dt, st=None):
        cnt[0] += 1
        return (st or es).enter_context(nc.sbuf_tensor(f"s{cnt[0]}_{name}", shape, dt))

    def pst(name, shape, dt, st=None):
        cnt[0] += 1
        return (st or es).enter_context(nc.psum_tensor(f"p{cnt[0]}_{name}", shape, dt))
    C.sb, C.pst = sb, pst

    P = Ctx()
    C.P = P
    P.ident_bf = sb("ident_bf", [128, 128], BF16)
    P.ident_f = sb("ident_f", [128, 128], F32)
    P.ones_f = sb("ones_f", [128, 128], F32)
    P.modcol = sb("modcol", [128, 48], F32)
    P.A1col = sb("A1col", [128, 8], F32)
    P.gt1rep = sb("gt1rep", [128, DM], F32)
    P.gt2rep = sb("gt2rep", [128, DM], F32)
    P.A2rep = sb("A2rep", [128, DM], F32)
    P.B2rep = sb("B2rep", [128, DM], F32)
    S.dma(P.ident_bf[:, :], D['ident_bf'][:, :])
    S.dma(P.ident_f[:, :], D['ident_f'][:, :])
    S.memset(P.ones_f[:, :], 1.0)

    phases = [phase_mod, phase_norm_proj, phase_mlstm, phase_hyena, phase_outproj, phase_route, phase_experts, phase_final]
    for ph in phases:
        ph(C)
        if stop_after == ph.__name__:
            break
    outs = [D['out'][:, :]] + [t for t in C.dbg.values()]
    S.finish(outs)
    S.emit()
    return nc, C


def phase_mod(C):
    S, D, P, sb, pst = C.S, C.D, C.P, C.sb, C.pst
    with S.scope() as ph:
        wada = [sb(f"wada{i}", [128, 8, 512], F32, ph) for i in range(2)]
        modrow = sb("modrow", [1, 6144], F32, ph)
        badar = sb("badar", [1, 6144], F32, ph)
        ccol = sb("ccol_sb", [128, 8], F32, ph)
        gmixc = sb("gmixc", [128, 8], F32, ph)
        gffn_rep = sb("gffn_rep", [128, DM], F32, ph)
        sc2rep = sb("sc2rep", [128, DM], F32, ph)
        ps = pst("ps_mod", [128, 512], F32, ph)
        psc = pst("ps_modc", [128, 512], F32, ph)
        psr = [pst(f"ps_modr{i}", [128, 512], F32, ph) for i in range(2)]
        S.dma(badar[:, :], D['b_ada'][:, :])
        S.dma(ccol[:, :], D['ccol'][:, :])
        S.dma(gmixc[:, :], D['gmix_col'][:, :])
        S.dma(gffn_rep[:, :], D['gffn_row'][0:1, :].partition_broadcast(128))
        wsrc = D['w_ada'].rearrange("(k p) c -> p k c", p=128)
        for blk in range(12):
            buf = wada[blk % 2]
            S.dma(buf[:, :, :], wsrc[:, :, blk * 512:(blk + 1) * 512])
            for k in range(8):
                S.mm(ps[0:1, :], ccol[:, k:k + 1], buf[:, k, :], start=(k == 0), stop=(k == 7))
            S.tt(modrow[0:1, blk * 512:(blk + 1) * 512], ps[0:1, :], badar[0:1, blk * 512:(blk + 1) * 512], ALU.add)
        for oc in range(48):
            S.mm(psc[:, oc:oc + 1], modrow[0:1, oc * 128:(oc + 1) * 128], P.ones_f[0:1, 0:1])
        S.copy(P.modcol[:, :], psc[:, 0:48])
        S.stt(P.A1col[:, :], P.modcol[:, 8:16], 1.0, gmixc[:, :], ALU.add, ALU.mult)
        n = 0
        for (dst, j) in ((P.gt1rep, 2), (P.gt2rep, 5), (sc2rep, 4), (P.B2rep, 3)):
            for h in range(2):
                pr = psr[n % 2]
                n += 1
                S.mm(pr[:, :], P.ones_f[0:1, 0:128], modrow[0:1, j * 1024 + h * 512: j * 1024 + (h + 1) * 512])
                S.copy(dst[:, h * 512:(h + 1) * 512], pr[:, :], eng=('act' if n % 2 else 'dve'))
        S.stt(P.A2rep[:, :], sc2rep[:, :], 1.0, gffn_rep[:, :], ALU.add, ALU.mult)
        if C.debug:
            d = C.dbg_out('modcol', [128, 48])
            S.dma(d[:, :], P.modcol[:, :])
            d = C.dbg_out('gt1rep', [128, DM])
            S.dma(d[:, :], P.gt1rep[:, :])


def phase_norm_proj(C):
    S, D, P, sb, pst = C.S, C.D, C.P, C.sb, C.pst
    P.Gt = sb("Gt", [128, NT, 16], F32)
    with S.scope() as ph:
        hT = sb("hT", [128, 8, SEQ], BF16, ph)
        with S.scope() as p1:
            xb = [sb(f"xb{i}", [128, DM], F32, p1) for i in range(2)]
            xn = [sb(f"xn{i}", [128, DM], BF16, p1) for i in range(2)]
            junk = sb("junk", [128, DM], BF16, p1)
            ss = sb("ss", [128, NT], F32, p1)
            rs = sb("rs", [128, NT], F32, p1)
            psT = [pst(f"psT{i}", [128, DM], BF16, p1) for i in range(2)]
            for j in range(NT):
                xt = xb[j % 2]
                S.dma(xt[:, :], D['x'][j * 128:(j + 1) * 128, :])
                S.act(junk[:, :], xt[:, :], AF.Square, accum_out=ss[:, j:j + 1])
                S.act(rs[:, j:j + 1], ss[:, j:j + 1], AF.Ln, scale=1.0 / DM, bias=EPS)
                S.act(rs[:, j:j + 1], rs[:, j:j + 1], AF.Exp, scale=-0.5)
                S.act(xn[j % 2][:, :], xt[:, :], AF.Copy, scale=rs[:, j:j + 1])
                pt = psT[j % 2]
                for k in range(8):
                    S.transpose(pt[:, k * 128:(k + 1) * 128], xn[j % 2][:, k * 128:(k + 1) * 128], P.ident_bf[:, :])
                for k in range(8):
                    S.act(hT[:, k, j * 128:(j + 1) * 128], pt[:, k * 128:(k + 1) * 128], AF.Identity,
                          scale=P.A1col[:, k:k + 1], bias=P.modcol[:, k:k + 1])
        if C.debug:
            d = C.dbg_out('hT', [128, 8, SEQ], BF16)
            for k in range(8):
                S.dma(d[:, k, :], hT[:, k, :])
        with S.scope() as p2:
            wvo = sb("wvo", [128, 8, 1040], BF16, p2)
            brow = sb("brow", [128, 1040], F32, p2)
            vt = [sb(f"vt{i}", [128, 512], BF16, p2) for i in range(2)]
            ot = [sb(f"ot{i}", [128, 512], BF16, p2) for i in range(2)]
            otf = [sb(f"otf{i}", [128, 512], F32, p2) for i in range(2)]
            psV = [pst(f"psV{i}", [128, 512], F32, p2) for i in range(2)]
            psO = [pst(f"psO{i}", [128, 512], F32, p2) for i in range(2)]
            psG = [pst(f"psG{i}", [128, 512], F32, p2) for i in range(2)]
            wsrc = D['w_in'].rearrange("(k p) c -> p k c", p=128)
            S.dma(wvo[:, :, 0:512], wsrc[:, :, 1024:1536], q='pool')
            S.dma(wvo[:, :, 512:1040], wsrc[:, :, 1536:2064], q='pool')
            S.dma(brow[:, :], D['bin_row'][0:1, :].partition_broadcast(128))
            for j in range(NT):
                a = j % 2
                for (pp, c0, c1) in ((psV[a], 0, 512), (psO[a], 512, 1024), (psG[a], 1024, 1040)):
                    for k in range(8):
                        S.mm(pp[:, 0:c1 - c0], hT[:, k, j * 128:(j + 1) * 128], wvo[:, k, c0:c1], start=(k == 0), stop=(k == 7))
                S.tt(vt[a][:, :], psV[a][:, :], brow[:, 0:512], ALU.add)
                S.dma(D['Vd'][j * 128:(j + 1) * 128, :], vt[a][:, :])
                S.tt(otf[a][:, :], psO[a][:, :], brow[:, 512:1024], ALU.add)
                S.act(ot[a][:, :], otf[a][:, :], AF.Sigmoid)
                S.dma(D['Od'][j * 128:(j + 1) * 128, :], ot[a][:, :])
                S.tt(P.Gt[:, j, :], psG[a][:, 0:16], brow[:, 1024:1040], ALU.add)
        if C.debug:
            d = C.dbg_out('Gt', [128, NT, 16])
            S.dma(d[:, :, :], P.Gt[:, :, :])
        with S.scope() as p3:
            wch = [sb(f"wch{i}", [128, 8, 128], BF16, p3) for i in range(3)]
            pre = [sb(f"pre{i}", [128, SEQ + 2], F32, p3) for i in range(2)]
            t0s = [sb(f"cv_t0{i}", [128, 2048], F32, p3) for i in range(2)]
            t2s = [sb(f"cv_t2{i}", [128, 2048], F32, p3) for i in range(2)]
            t3 = [[sb(f"cv_t3{q}{i}", [128, 2048], F32, p3) for i in range(2)] for q in range(2)]
            ob = [[sb(f"cv_ob{q}{i}", [128, 2048], BF16, p3) for i in range(2)] for q in range(2)]
            pending = [None]
            bqk = sb("bqk", [128, 8], F32, p3)
            bhy = sb("bhy", [128, 12], F32, p3)
            cqw = sb("cqw", [128, 8, 3], F32, p3)
            cqb = sb("cqb", [128, 8], F32, p3)
            chw = sb("chw", [128, 12, 3], F32, p3)
            chb = sb("chb", [128, 12], F32, p3)
            psF = [pst(f"psF{i}", [128, 512], F32, p3) for i in range(8)]
            for (tile_, nm) in ((bqk, 'bqk_col'), (bhy, 'bhy_col'), (cqb, 'cqk_b'), (chb, 'chy_b')):
                S.dma(tile_[:, :], D[nm][:, :])
            S.dma(cqw[:, :, :], D['cqk_w'][:, :, :])
            S.dma(chw[:, :, :], D['chy_w'][:, :, :])
            for i in range(2):
                S.memset(pre[i][:, 0:1], 0.0)
                S.memset(pre[i][:, SEQ + 1:SEQ + 2], 0.0)
            wsrc = D['w_in'].rearrange("(k p) c -> p k c", p=128)
            chunks = []
            for cc in range(8):
                chunks.append(('qk', cc, cc * 128))
            for i in range(12):
                chunks.append(('hy', i, 2064 + i * 128))
            nps = 0
            for m_ in range(2):
                S.dma(wch[m_ % 3][:, :, :], wsrc[:, :, chunks[m_][2]:chunks[m_][2] + 128], q='pool')
            for n, (kind, ci, col0) in enumerate(chunks):
                wc = wch[n % 3]
                pr = pre[n % 2]
                if n + 2 < len(chunks):
                    S.dma(wch[(n + 2) % 3][:, :, :], wsrc[:, :, chunks[n + 2][2]:chunks[n + 2][2] + 128], q='pool')
                bcol = bqk[:, ci:ci + 1] if kind == 'qk' else bhy[:, ci:ci + 1]
                cw = cqw if kind == 'qk' else chw
                cb = cqb if kind == 'qk' else chb
                for tb in range(8):
                    pp = psF[nps % 8]
                    nps += 1
                    for k in range(8):
                        S.mm(pp[:, :], wc[:, k, :], hT[:, k, tb * 512:(tb + 1) * 512], start=(k == 0), stop=(k == 7))
                    if tb % 2 == 0:
                        S.act(pr[:, 1 + tb * 512:1 + (tb + 1) * 512], pp[:, :], AF.Identity, bias=bcol)
                    else:
                        S.ts(pr[:, 1 + tb * 512:1 + (tb + 1) * 512], pp[:, :], bcol, ALU.add)
                par = n % 2
                for hh in range(2):
                    o0 = hh * 2048
                    S.act(t0s[hh][:, :], pr[:, o0:o0 + 2048], AF.Identity, scale=cw[:, ci, 0:1], bias=cb[:, ci:ci + 1])
                    S.act(t2s[hh][:, :], pr[:, o0 + 2:o0 + 2050], AF.Identity, scale=cw[:, ci, 2:3])
                for hh in range(2):
                    o0 = hh * 2048
                    S.stt(t0s[hh][:, :], pr[:, o0 + 1:o0 + 2049], cw[:, ci, 1:2], t0s[hh][:, :], ALU.mult, ALU.add)
                for hh in range(2):
                    if kind == 'hy' and ci >= 8:
                        S.tt(ob[par][hh][:, :], t0s[hh][:, :], t2s[hh][:, :], ALU.add, eng='pool')
                    else:
                        S.tt(t3[par][hh][:, :], t0s[hh][:, :], t2s[hh][:, :], ALU.add, eng='pool')
                if pending[0] is not None:
                    pending[0]()

                def fin(kind=kind, ci=ci, par=par):
                    for hh in range(2):
                        o0 = hh * 2048
                        if kind == 'qk':
                            S.act(ob[par][hh][:, :], t3[par][hh][:, :], AF.Silu)
                            dst = D['QTd'] if ci < 4 else D['KTd']
                            S.dma(dst[(ci % 4) * 128:(ci % 4 + 1) * 128, o0:o0 + 2048], ob[par][hh][:, :])
                        elif ci < 8:
                            dst = D['X1d'] if ci < 4 else D['X2d']
                            S.dma(dst[(ci % 4) * 128:(ci % 4 + 1) * 128, o0:o0 + 2048], t3[par][hh][:, :])
                        else:
                            S.dma(D['Zd'][(ci - 8) * 128:(ci - 7) * 128, o0:o0 + 2048], ob[par][hh][:, :])
                pending[0] = fin
            pending[0]()
    if C.debug:
        for nm, shp, dt in (('QTd', [512, SEQ], BF16), ('KTd', [512, SEQ], BF16), ('Vd', [SEQ, 512], BF16),
                            ('Od', [SEQ, 512], BF16), ('X1d', [512, SEQ], F32), ('Zd', [512, SEQ], BF16)):
            d = C.dbg_out(nm, shp, dt)
            with S.scope() as pd:
                if shp[0] == 512:
                    tmp = sb("dbgtmp_" + nm, [128, 4, SEQ], dt, pd)
                    S.dma(tmp[:, :, :], D[nm].rearrange("(a p) n -> p a n", p=128))
                    S.dma(d.rearrange("(a p) n -> p a n", p=128), tmp[:, :, :])
                else:
                    tmp = sb("dbgtmp_" + nm, [128, NT, 512], dt, pd)
                    S.dma(tmp[:, :, :], D[nm].rearrange("(a p) n -> p a n", p=128))
                    S.dma(d.rearrange("(a p) n -> p a n", p=128), tmp[:, :, :])


def phase_mlstm(C):
    S, D, P, sb, pst = C.S, C.D, C.P, C.sb, C.pst
    Gt = P.Gt
    with S.scope() as ph:
        triU = sb("triU", [128, 128], F32, ph)
        triL = sb("triL", [128, 128], F32, ph)
        LF = sb("LF", [128, 2, NT, 4], F32, ph)
        II = sb("II", [128, 2, NT, 4], F32, ph)
        Bc = sb("Bc", [128, 2, NT, 4], F32, ph)
        Wp = sb("Wp", [128, 2, NT, 4], F32, ph)
        ENB = sb("ENB", [128, 2, NT, 4], F32, ph)
        EG = sb("EG", [128, 2, NT, 4], F32, ph)
        zcol = sb("zcol", [128, 1], F32, ph)
        S.dma(triU[:, :], D['triU'][:, :])
        S.dma(triL[:, :], D['triL'][:, :])
        S.memset(zcol[:, :], 0.0)
        with S.scope() as pp:
            psB = pst("psB", [128, 512], F32, pp)
            psGs = pst("psGs", [128, 512], F32, pp)
            for d in range(2):
                S.act(LF[:, d, :, :], Gt[:, :, d * 8 + 4:d * 8 + 8], AF.Exp, scale=-1.0)
                S.copy(II[:, d, :, :], Gt[:, :, d * 8:d * 8 + 4])
            fl = lambda t: t[:, :, :, :].rearrange("p d c e -> p (d c e)")
            S.act(fl(LF), fl(LF), AF.Ln, bias=1.0)
            S.ts(fl(LF), fl(LF), -1.0, ALU.mult)
            S.mm(psB[:, 0:128], triU[:, :], fl(LF)[:, 0:128])
            S.mm(psB[:, 128:256], triL[:, :], fl(LF)[:, 128:256])
            S.mm(psGs[:, 0:256], P.ones_f[:, :], fl(LF))
            S.copy(fl(Bc), psB[:, 0:256])
            S.act(fl(EG), psGs[:, 0:256], AF.Exp)
            S.tt(fl(Wp), fl(II), fl(Bc), ALU.subtract)
            S.act(fl(Wp), fl(Wp), AF.Exp, bias=math.log(128.0 ** -0.5))
            S.act(fl(ENB), fl(Bc), AF.Exp, scale=-1.0)
        if C.debug:
            for nm, t in (('Bc', Bc), ('Wp', Wp), ('EG', EG)):
                d_ = C.dbg_out(nm, [128, 2, NT, 4])
                S.dma(d_[:, :, :, :], t[:, :, :, :])
        for hd in range(4):
            with S.scope() as hs:
                qT = sb("qT", [128, SEQ], BF16, hs)
                kT = sb("kT", [128, SEQ], BF16, hs)
                vaug = sb("vaug", [128, NT, 129], BF16, hs)
                osg = sb("osg", [128, NT, 128], BF16, hs)
                ktm = sb("ktm", [128, NT, 128], BF16, hs)
                Hs = sb("Hs", [128, NT, 128], F32, hs)
                sq = sb("sq", [128, NT, 128], F32, hs)
                gO = sb("gO", [128, NT, 128], BF16, hs)
                ym = sb("ym", [128, NT, 128], BF16, hs)
                ymT = sb("ymT", [128, SEQ], BF16, hs)
                mng = sb("mng", [128, 128], F32, hs)
                STw = [sb(f"STw{i}", [128, 128], BF16, hs) for i in range(4)]
                vw = [sb(f"vw{i}", [128, 129], BF16, hs) for i in range(4)]
                Tst = sb("Tst", [128, 129], F32, hs)
                Tst2 = sb("Tst2", [128, 129], F32, hs)
                Cbfd = [[sb(f"Cbf{d_}{i}", [128, 129], BF16, hs) for i in range(2)] for d_ in range(2)]
                sm = [sb(f"sm{i}", [128, 4], F32, hs) for i in range(2)]
                ssq = sb("ssq", [128, NT], F32, hs)
                pS = [pst(f"pS{i}", [128, 512], F32, hs) for i in range(2)]
                pO = [pst(f"pO{i}", [128, 512], F32, hs) for i in range(2)]
                pC = [pst(f"pC{i}", [128, 512], F32, hs) for i in range(2)]
                pK = pst("pK", [128, 1024], BF16, hs)
                S.dma(qT[:, :], D['QTd'][hd * 128:(hd + 1) * 128, :])
                S.dma(kT[:, :], D['KTd'][hd * 128:(hd + 1) * 128, :])
                S.dma(vaug[:, :, 0:128], D['Vd'][:, hd * 128:(hd + 1) * 128].rearrange("(c p) d -> p c d", p=128))
                S.memset(vaug[:, :, 128:129], 1.0)
                S.dma(osg[:, :, :], D['Od'][:, hd * 128:(hd + 1) * 128].rearrange("(c p) d -> p c d", p=128))
                S.dma(mng[:, :], D['mnorm_g'][0:1, hd * 128:(hd + 1) * 128].partition_broadcast(128))
                for c0 in range(0, NT, 8):
                    for c in range(c0, c0 + 8):
                        S.transpose(pK[:, (c - c0) * 128:(c - c0 + 1) * 128], kT[:, c * 128:(c + 1) * 128], P.ident_bf[:, :])
                    S.copy(ktm[:, c0:c0 + 8, :].rearrange("p c d -> p (c d)"), pK[:, :], eng=('act' if (c0 // 8) % 2 else 'dve'))
                n = 0
                Tsts = [Tst, Tst2]
                prevc = [None, None]
                visited = set()
                for d in range(2):
                    S.memset(Tsts[d][:, :], 0.0)
                    S.memset(Cbfd[d][0][:, :], 0.0)
                nd = [0, 0]
                steps = []
                for i in range(NT):
                    for d in range(2):
                        steps.append((d, i if d == 0 else NT - 1 - i))

                def phase1(n):
                    d, c = steps[n]
                    cs = slice(c * 128, (c + 1) * 128)
                    wcol = Wp[:, d, c, hd:hd + 1]
                    mask = triU if d == 0 else triL
                    S.mm(pS[n % 2][:, 0:128], kT[:, cs], qT[:, cs])
                    S.stt(STw[n % 4][:, :], pS[n % 2][:, 0:128], wcol, mask[:, :], ALU.mult, ALU.mult)
                    S.act(vw[n % 4][:, :], vaug[:, c, :], AF.Copy, scale=wcol)

                def phase2(n):
                    d, c = steps[n]
                    a = n % 2
                    b_ = nd[d] % 2
                    nd[d] += 1
                    cs = slice(c * 128, (c + 1) * 128)
                    S.mm(pO[a][:, 0:129], STw[n % 4][:, :], vaug[:, c, :], start=True, stop=False)
                    S.mm(pO[a][:, 0:129], qT[:, cs], Cbfd[d][b_][:, :], start=False, stop=True)
                    S.mm(pC[a][:, 0:129], ktm[:, c, :], vw[n % 4][:, :])
                    enb = ENB[:, d, c, hd:hd + 1]
                    s_ = sm[a]
                    S.ts(s_[:, 0:1], pO[a][:, 128:129], enb, ALU.max)
                    S.stt(s_[:, 2:3], pO[a][:, 128:129], -1.0, s_[:, 0:1], ALU.mult, ALU.max)
                    S.recip(s_[:, 3:4], s_[:, 2:3])
                    if c not in visited:
                        visited.add(c)
                        S.act(Hs[:, c, :], pO[a][:, 0:128], AF.Copy, scale=s_[:, 3:4])
                    else:
                        S.stt(Hs[:, c, :], pO[a][:, 0:128], s_[:, 3:4], Hs[:, c, :], ALU.mult, ALU.add)
                    egp = zcol[:, 0:1] if prevc[d] is None else EG[:, d, prevc[d], hd:hd + 1]
                    S.stt(Tsts[d][:, :], Tsts[d][:, :], egp, pC[a][:, 0:129], ALU.mult, ALU.add)
                    S.act(Cbfd[d][(b_ + 1) % 2][:, :], Tsts[d][:, :], AF.Copy, scale=EG[:, d, c, hd:hd + 1])
                    prevc[d] = c
                phase1(0)
                for n_ in range(len(steps)):
                    if n_ + 1 < len(steps):
                        phase1(n_ + 1)
                    phase2(n_)
                S.act(sq[:, :, :], Hs[:, :, :], AF.Square)
                S.reduce(ssq[:, :], sq[:, :, :], ALU.add)
                S.ts(ssq[:, :], ssq[:, :], 1.0 / 128.0, ALU.mult, s2=EPS, op1=ALU.add)
                S.act(ssq[:, :], ssq[:, :], AF.Sqrt)
                S.recip(ssq[:, :], ssq[:, :])
                S.tt(gO[:, :, :], osg[:, :, :], mng[:, :].unsqueeze(1).to_broadcast([128, NT, 128]), ALU.mult)
                for c in range(NT):
                    S.stt(ym[:, c, :], Hs[:, c, :], ssq[:, c:c + 1], gO[:, c, :], ALU.mult, ALU.mult)
                for c0 in range(0, NT, 8):
                    for c in range(c0, c0 + 8):
                        S.transpose(pK[:, (c - c0) * 128:(c - c0 + 1) * 128], ym[:, c, :], P.ident_bf[:, :])
                    S.copy(ymT[:, c0 * 128:(c0 + 8) * 128], pK[:, :], eng=('act' if (c0 // 8) % 2 else 'dve'))
                S.dma(D['Yt'][hd * 128:(hd + 1) * 128, :], ymT[:, :])
    if C.debug:
        d_ = C.dbg_out('Yt_m', [512, SEQ], BF16)
        with S.scope() as pd:
            tmp = sb("dbgtmp_ytm", [128, 4, SEQ], BF16, pd)
            S.dma(tmp[:, :, :], D['Yt'][0:512, :].rearrange("(a p) n -> p a n", p=128))
            S.dma(d_.rearrange("(a p) n -> p a n", p=128), tmp[:, :, :])


def phase_hyena(C):
    S, D, P, sb, pst = C.S, C.D, C.P, C.sb, C.pst
    PI = math.pi
    with S.scope() as pa:
        hid2T = sb("hid2T", [64, SEQ], F32, pa)
        hid2rT = sb("hid2rT", [64, SEQ], F32, pa)
        frc = sb("frc", [64, 1], F32, pa)
        frb1 = sb("frb1", [64, 1], F32, pa)
        frb2 = sb("frb2", [64, 1], F32, pa)
        with S.scope() as p1:
            zT = sb("zT", [33, SEQ], F32, p1)
            zrT = sb("zrT", [33, SEQ], F32, p1)
            hid1 = sb("hid1", [64, SEQ], F32, p1)
            w1 = sb("hw1", [33, 64], F32, p1)
            w2 = sb("hw2", [64, 64], F32, p1)
            b1c = sb("b1c", [64, 1], F32, p1)
            b2c = sb("b2c", [64, 1], F32, p1)
            arg = [sb(f"harg{i}", [64, 512], F32, p1) for i in range(2)]
            m1 = [sb(f"hm1{i}", [64, 512], F32, p1) for i in range(2)]
            m2 = [sb(f"hm2{i}", [64, 512], F32, p1) for i in range(2)]
            psM = [pst(f"psM{i}", [128, 512], F32, p1) for i in range(2)]
            S.dma(zT[:, :], D['zT'][:, :])
            S.dma(zrT[:, :], D['zrT'][:, :])
            S.dma(w1[:, :], D['hy_w1'][:, :])
            S.dma(w2[:, :], D['hy_w2'][:, :])
            S.dma(b1c[:, :], D['hy_b1c'][:, :])
            S.dma(b2c[:, :], D['hy_b2c'][:, :])
            S.dma(frc[:, :], D['hy_frc'][:, :])
            S.tt(frb1[:, :], frc[:, :], b1c[:, :], ALU.mult)
            S.tt(frb2[:, :], frc[:, :], b2c[:, :], ALU.mult)
            n = 0

            def sin_layer(ps, frb, dst):
                nonlocal n
                a = n % 2
                n += 1
                S.ts(arg[a][:, :], ps, frc[:, 0:1], ALU.mult, s2=frb[:, 0:1], op1=ALU.add)
                S.ts(m1[a][:, :], arg[a][:, :], PI, ALU.is_gt, s2=-2.0 * PI, op1=ALU.mult)
                S.ts(m2[a][:, :], arg[a][:, :], -PI, ALU.is_lt, s2=2.0 * PI, op1=ALU.mult)
                S.tt(arg[a][:, :], arg[a][:, :], m1[a][:, :], ALU.add)
                S.tt(arg[a][:, :], arg[a][:, :], m2[a][:, :], ALU.add)
                S.act(dst, arg[a][:, :], AF.Sin)
            for (zs, hdst) in ((zT, hid2T), (zrT, hid2rT)):
                for blk in range(8):
                    ps = psM[blk % 2]
                    S.mm(ps[0:64, :], w1[:, :], zs[:, blk * 512:(blk + 1) * 512])
                    sin_layer(ps[0:64, :], frb1, hid1[:, blk * 512:(blk + 1) * 512])
                for blk in range(8):
                    ps = psM[blk % 2]
                    S.mm(ps[0:64, :], w2[:, :], hid1[:, blk * 512:(blk + 1) * 512])
                    sin_layer(ps[0:64, :], frb2, hdst[:, blk * 512:(blk + 1) * 512])
        with S.scope() as p2:
            w3 = sb("hw3", [64, 2048], F32, p2)
            trow = sb("trow", [128, SEQ], F32, p2)
            trrow = sb("trrow", [128, SEQ], F32, p2)
            ndel = sb("ndel", [128, 16], F32, p2)
            hbias = sb("hbias", [128, 8], F32, p2)
            kT = [sb(f"kTb{i}", [128, 2 * SEQ], BF16, p2) for i in range(2)]
            win = [sb(f"win{i}", [128, 512], F32, p2) for i in range(2)]
            k0t = sb("k0t", [128, 4], F32, p2)
            psK_ = [pst(f"psKf{i}", [128, 512], F32, p2) for i in range(3)]
            ps0 = pst("psK0", [128, 512], F32, p2)
            S.dma(w3[:, :], D['hy_w3'][:, :])
            S.dma(trow[:, :], D['t_row'][0:1, :].partition_broadcast(128))
            S.dma(trrow[:, :], D['tr_row'][0:1, :].partition_broadcast(128))
            S.dma(ndel[:, :], D['hy_del_col'][:, :])
            S.dma(hbias[:, :], D['hy_bias_col'][:, :])
            S.stt(ndel[:, :], ndel[:, :], -1.0, ndel[:, :], ALU.mult, ALU.max)
            S.ts(ndel[:, :], ndel[:, :], -1.0, ALU.mult)
            nb = 0
            nk = 0
            for o in range(2):
                for g in range(4):
                    kt = kT[nk % 2]
                    nk += 1
                    for d in range(2):
                        hs = hid2T if d == 0 else hid2rT
                        tr = trow if d == 0 else trrow
                        c0 = (o * 2 + d) * 512 + g * 128
                        di = o * 8 + d * 4 + g
                        for blk in range(8):
                            ps = psK_[nb % 3]
                            wn = win[nb % 2]
                            nb += 1
                            S.mm(ps[:, :], w3[:, c0:c0 + 128], hs[:, blk * 512:(blk + 1) * 512])
                            S.act(wn[:, :], tr[:, blk * 512:(blk + 1) * 512], AF.Exp, scale=ndel[:, di:di + 1])
                            S.stt(kt[:, d * SEQ + blk * 512:d * SEQ + (blk + 1) * 512], wn[:, :], 0.05, ps[:, :], ALU.add, ALU.mult)
                    S.memset(kt[:, SEQ:SEQ + 1], 0.0)
                    cf = (o * 2 + 0) * 512 + g * 128
                    cb = (o * 2 + 1) * 512 + g * 128
                    S.mm(ps0[:, 0:1], w3[:, cf:cf + 128], hid2T[:, 0:1])
                    S.mm(ps0[:, 1:2], w3[:, cb:cb + 128], hid2T[:, 0:1])
                    S.copy(k0t[:, 0:2], ps0[:, 0:2])
                    S.tt(k0t[:, 2:3], k0t[:, 0:1], k0t[:, 1:2], ALU.add)
                    S.ts(k0t[:, 3:4], k0t[:, 2:3], 1.05, ALU.mult)
                    S.tt(kt[:, 0:1], k0t[:, 3:4], hbias[:, o * 4 + g:o * 4 + g + 1], ALU.add)
                    dst = D['Kd0'] if o == 0 else D['Kd1']
                    S.dma(dst[g * 128:(g + 1) * 128, :], kt[:, :], q='pool')
    if C.debug:
        d_ = C.dbg_out('Kd0', [512, 2 * SEQ], BF16)
        with S.scope() as pd:
            tmp = sb("dbgtmp_kd", [128, 4, 2 * SEQ], BF16, pd)
            S.dma(tmp[:, :, :], D['Kd0'].rearrange("(a p) n -> p a n", p=128))
            S.dma(d_.rearrange("(a p) n -> p a n", p=128), tmp[:, :, :])
    with S.scope() as pb:
        T = Ctx()
        T.F1 = sb("F1", [64, 128], BF16, pb)
        T.GrT = sb("GrT", [128, 64, 128], BF16, pb)
        T.GiT = sb("GiT", [128, 64, 128], BF16, pb)
        T.Rc1 = sb("Rc1", [128, 256], BF16, pb)
        T.Rc2 = sb("Rc2", [128, 256], BF16, pb)
        T.LrT = sb("LrT", [64, 128, 32], BF16, pb)
        T.LiNT = sb("LiNT", [64, 128, 32], BF16, pb)
        hnorm = sb("hnorm", [128, 4], F32, pb)
        S.dma(T.F1[:, :], D['F1'][:, :])
        for nm in ('GrT', 'GiT'):
            S.dma(getattr(T, nm)[:, :, :], D[nm].rearrange("p (k m) -> p k m", k=64))
        S.dma(T.Rc1[:, :], D['Rc1'][:, :])
        S.dma(T.Rc2[:, :], D['Rc2'][:, :])
        S.dma(T.LrT[:, :, :], D['LrT'].rearrange("p (n m) -> p n m", n=128))
        S.dma(T.LiNT[:, :, :], D['LiNT'].rearrange("p (n m) -> p n m", n=128))
        S.dma(hnorm[:, :], D['hnorm_col'][:, :])
        for o in range(2):
            zsrc = D['Zd'] if o == 0 else D['Z1d']
            kd = D['Kd0'] if o == 0 else D['Kd1']
            with S.scope() as pw:
                W = Ctx()
                W.X = sb("fX", [64, 64, 128], BF16, pw)
                W.AT = sb("fAT", [128, 64, 2, 64], BF16, pw)
                W.ATn = sb("fATn", [128, 64, 2, 64], BF16, pw)
                W.Kf = sb("fKf", [128, 64, 2, 64], BF16, pw)
                W.V = sb("fV", [128, 2, 64, 64], BF16, pw)
                W.Wt = sb("fWt", [64, 64, 2, 128], BF16, pw)
                W.Ysb = sb("fY", [32, 64, 128], BF16, pw)
                W.tm = [[sb(f"ftm{i}{j}", [128, 4, 64], F32, pw) for j in range(4)] for i in range(2)]
                W.ring = [pst(f"psR{i}", [128, 512], F32, pw) for i in range(8)]
                W.nr = 0
                W.ne = 0
                W.pref = False
                for b8 in range(8):
                    ch0 = b8 * 64
                    fft_batch(C, T, W, zsrc[ch0:ch0 + 64, :], kd[ch0:ch0 + 64, :], D['Ycv'][ch0:ch0 + 64, :],
                              kd_next=(kd[ch0 + 64:ch0 + 128, :] if b8 < 7 else None))
            with S.scope() as pg:
                ysb = [sb(f"gy{i}", [128, SEQ], BF16, pg) for i in range(2)]
                xsb = [sb(f"gx{i}", [128, SEQ], F32, pg) for i in range(2)]
                zo = [sb(f"gz{i}", [128, SEQ], BF16, pg) for i in range(2)]
                xsrc = D['X1d'] if o == 0 else D['X2d']
                if o == 1:
                    z2 = sb("gz2", [128, SEQ], F32, pg)
                    sq = [sb(f"gsq{i}", [128, 512], F32, pg) for i in range(2)]
                    rr = [sb(f"grr{i}", [128, 512], F32, pg) for i in range(2)]
                    psN = [pst(f"psN{i}", [128, 512], F32, pg) for i in range(2)]
                for g in range(4):
                    a = g % 2
                    S.dma(ysb[a][:, :], D['Ycv'][g * 128:(g + 1) * 128, :])
                    S.dma(xsb[a][:, :], xsrc[g * 128:(g + 1) * 128, :])
                    if o == 0:
                        S.tt(zo[a][:, :], xsb[a][:, :], ysb[a][:, :], ALU.mult)
                        S.dma(D['Z1d'][g * 128:(g + 1) * 128, :], zo[a][:, :], q='pool')
                    else:
                        S.tt(z2[:, :], xsb[a][:, :], ysb[a][:, :], ALU.mult)
                        for blk in range(9):
                            if blk < 8:
                                bs = slice(blk * 512, (blk + 1) * 512)
                                q_ = blk % 2
                                S.tt(sq[q_][:, :], z2[:, bs], z2[:, bs], ALU.mult)
                                S.mm(psN[q_][:, :], P.ones_f[:, :], sq[q_][:, :])
                                S.act(rr[q_][:, :], psN[q_][:, :], AF.Ln, scale=1.0 / 128.0, bias=EPS)
                                S.act(rr[q_][:, :], rr[q_][:, :], AF.Exp, scale=-0.5)
                            if blk >= 1:
                                pb_ = slice((blk - 1) * 512, blk * 512)
                                S.stt(zo[a][:, pb_], z2[:, pb_], hnorm[:, g:g + 1], rr[(blk - 1) % 2][:, :], ALU.mult, ALU.mult)
                        S.dma(D['Yt'][512 + g * 128:512 + (g + 1) * 128, :], zo[a][:, :], q='pool')
    if C.debug:
        d_ = C.dbg_out('Yt_h', [512, SEQ], BF16)
        with S.scope() as pd:
            tmp = sb("dbgtmp_yth", [128, 4, SEQ], BF16, pd)
            S.dma(tmp[:, :, :], D['Yt'][512:1024, :].rearrange("(a p) n -> p a n", p=128))
            S.dma(d_.rearrange("(a p) n -> p a n", p=128), tmp[:, :, :])


def fft_batch(C, T, W, zsrc, kdsrc, ydst, kd_next=None):
    S = C.S

    def bank():
        W.nr += 1
        return W.ring[W.nr % 8]

    def ev(dst, src):
        W.ne += 1
        S.copy(dst, src, eng=('act' if W.ne % 2 else 'dve'))

    def forward_d(kdim):
        for cq in range(16):
            pA = bank()
            for i in range(4):
                ch = cq * 4 + i
                S.mm(pA[:, i * 128:(i + 1) * 128], W.X[0:kdim, ch, :], T.F1[0:kdim, :])
            ev(W.AT[:, cq * 4:(cq + 1) * 4, :, :].rearrange("p c r k -> p (c r k)"), pA[:, :])
            src = pA[:, :].rearrange("p (c r k) -> p c r k", c=4, r=2)
            S.act(W.ATn[:, cq * 4:(cq + 1) * 4, 0, :], src[:, :, 1, :], AF.Copy, scale=-1.0)
            S.copy(W.ATn[:, cq * 4:(cq + 1) * 4, 1, :], src[:, :, 0, :])

    def forward_s(mode):
        for kb in range(16):
            pU = bank()
            for i in range(4):
                k1 = kb * 4 + i
                o_ = pU[:, i * 128:(i + 1) * 128]
                S.mm(o_, T.GrT[:, k1, :], W.AT[:, :, :, k1].rearrange("p c r -> p r c"), start=True, stop=False)
                S.mm(o_, T.GiT[:, k1, :], W.ATn[:, :, :, k1].rearrange("p c r -> p r c"), start=False, stop=True)
            if mode == 'kernel':
                ev(W.Kf[:, kb * 4:(kb + 1) * 4, :, :].rearrange("p k r c -> p (k r c)"), pU[:, :])
            else:
                pv = pU[:, :].rearrange("p (k r c) -> p k r c", k=4, r=2)
                Ur = pv[:, :, 0, :]
                Ui = pv[:, :, 1, :]
                Kr = W.Kf[:, kb * 4:(kb + 1) * 4, 0, :]
                Ki = W.Kf[:, kb * 4:(kb + 1) * 4, 1, :]
                t = W.tm[kb % 2]
                S.tt(t[0][:, :, :], Ur, Kr, ALU.mult)
                S.tt(t[1][:, :, :], Ui, Ki, ALU.mult)
                S.tt(t[2][:, :, :], Ur, Ki, ALU.mult)
                S.tt(t[3][:, :, :], Ui, Kr, ALU.mult)
                S.tt(W.V[:, 0, kb * 4:(kb + 1) * 4, :], t[0][:, :, :], t[1][:, :, :], ALU.subtract, eng='pool')
                S.tt(W.V[:, 1, kb * 4:(kb + 1) * 4, :], t[2][:, :, :], t[3][:, :, :], ALU.add, eng='pool')

    if not W.pref:
        S.dma(W.X[:, :, :], kdsrc.rearrange("c (a n) -> a c n", n=128))
    forward_d(64)
    forward_s('kernel')
    S.dma(W.X[0:32, :, :], zsrc.rearrange("c (a n) -> a c n", n=128))
    forward_d(32)
    W.pref = False
    if kd_next is not None:
        S.dma(W.X[:, :, :], kd_next.rearrange("c (a n) -> a c n", n=128))
        W.pref = True
    forward_s('data')
    for cp in range(32):
        pW = bank()
        for i in range(2):
            ch = cp * 2 + i
            o_ = pW[0:64, i * 256:(i + 1) * 256]
            S.mm(o_, W.V[:, 0, :, ch], T.Rc1[:, :], start=True, stop=False)
            S.mm(o_, W.V[:, 1, :, ch], T.Rc2[:, :], start=False, stop=True)
        ev(W.Wt[:, cp * 2:(cp + 1) * 2, :, :].rearrange("p c r n -> p (c r n)"), pW[0:64, :])
    for nb in range(16):
        pY = bank()
        for i in range(8):
            n2 = nb * 8 + i
            o_ = pY[0:32, i * 64:(i + 1) * 64]
            S.mm(o_, T.LrT[:, n2, :], W.Wt[:, :, 0, n2], start=True, stop=False)
            S.mm(o_, T.LiNT[:, n2, :], W.Wt[:, :, 1, n2], start=False, stop=True)
        ev(W.Ysb[:, :, nb * 8:(nb + 1) * 8], pY[0:32, :].rearrange("p (n c) -> p c n", n=8))
    S.dma(ydst.rearrange("c (a n) -> a c n", n=128), W.Ysb[:, :, :], q='pool')


def phase_outproj(C):
    S, D, P, sb, pst, nc = C.S, C.D, C.P, C.sb, C.pst, C.nc
    P.IDX = sb("IDX", [128, 16, 4], I32)
    P.GW = sb("GW", [128, 16, 4], F32)
    with S.scope() as ph:
        AFF = sb("AFF", [128, NT, 16], F32, ph)
        AFFT = sb("AFFT", [16, SEQ], F32, ph)
        with S.scope() as p1:
            ytT = sb("ytT", [128, 8, SEQ], BF16, p1)
            wout = sb("wout", [128, 8, DM], BF16, p1)
            wr = sb("wr", [128, 8, 16], F32, p1)
            xb = [sb(f"oxb{i}", [128, DM], F32, p1) for i in range(2)]
            x1 = [sb(f"ox1{i}", [128, DM], F32, p1) for i in range(3)]
            tmp = [sb(f"otmp{i}", [128, DM], F32, p1) for i in range(2)]
            hf = [sb(f"ohf{i}", [128, DM], F32, p1) for i in range(3)]
            hfb = [sb(f"ohfb{i}", [128, DM], BF16, p1) for i in range(3)]
            hfT = [sb(f"ohfT{i}", [128, 8, 128], F32, p1) for i in range(2)]
            junk = sb("ojunk", [128, DM], BF16, p1)
            sm = sb("osm", [128, NT, 8], F32, p1)
            ex = [sb(f"oex{i}", [128, 16], F32, p1) for i in range(2)]
            psM = [[pst(f"psMo{i}{h}", [128, 512], F32, p1) for h in range(2)] for i in range(2)]
            psT = [pst(f"psTo{i}", [128, 512], F32, p1) for i in range(2)]
            psR = pst("psR", [128, 512], F32, p1)
            psAT = pst("psAT", [128, 512], F32, p1)
            psAT2 = psT[1]
            for k in range(8):
                S.dma(ytT[:, k, :], D['Yt'][k * 128:(k + 1) * 128, :])
            wsrc = D['w_out'].rearrange("(k p) c -> p k c", p=128)
            S.dma(wout[:, :, 0:512], wsrc[:, :, 0:512], q='pool')
            S.dma(wout[:, :, 512:1024], wsrc[:, :, 512:1024], q='pool')
            S.dma(wr[:, :, :], D['w_router'].rearrange("(k p) e -> p k e", p=128))
            def stage_a1(j):
                a = j % 2
                b = j % 3
                xt = xb[a]
                S.dma(xt[:, :], D['x'][j * 128:(j + 1) * 128, :])
                for h in range(2):
                    for k in range(8):
                        S.mm(psM[a][h][:, :], ytT[:, k, j * 128:(j + 1) * 128], wout[:, k, h * 512:(h + 1) * 512],
                             start=(k == 0), stop=(k == 7))
                for h in range(2):
                    S.tt(tmp[a][:, h * 512:(h + 1) * 512], psM[a][h][:, :], P.gt1rep[:, h * 512:(h + 1) * 512], ALU.mult)
                S.tt(x1[b][:, :], tmp[a][:, :], xt[:, :], ALU.add)
                S.dma(D['acc'][j * 128:(j + 1) * 128, :], x1[b][:, :], q='pool')

            def stage_a2(j):
                b = j % 3
                ss = sm[:, j, 0:1]
                rs = sm[:, j, 1:2]
                S.act(junk[:, :], x1[b][:, :], AF.Square, accum_out=ss)
                S.act(rs, ss, AF.Ln, scale=1.0 / DM, bias=EPS)
                S.act(rs, rs, AF.Exp, scale=-0.5)
                S.stt(hf[b][:, :], x1[b][:, :], rs, P.A2rep[:, :], ALU.mult, ALU.mult)
                S.tt(hf[b][:, :], hf[b][:, :], P.B2rep[:, :], ALU.add)
                S.act(hfb[b][:, :], hf[b][:, :], AF.Copy)
                S.dma(D['HFd'][j * 128:(j + 1) * 128, :], hfb[b][:, :], q='pool')

            def stage_b1(j):
                a = j % 2
                b = j % 3
                for k in range(8):
                    S.transpose(psT[k // 4][:, (k % 4) * 128:(k % 4 + 1) * 128], hf[b][:, k * 128:(k + 1) * 128], P.ident_f[:, :])
                S.copy(hfT[a][:, 0:4, :].rearrange("p k t -> p (k t)"), psT[0][:, :], eng='act')
                S.copy(hfT[a][:, 4:8, :].rearrange("p k t -> p (k t)"), psT[1][:, :], eng='dve')

            def stage_b2(j):
                a = j % 2
                for k in range(8):
                    S.mm(psR[:, 0:16], hfT[a][:, k, :], wr[:, k, :], start=(k == 0), stop=(k == 7))
                mx = sm[:, j, 2:3]
                se = sm[:, j, 3:4]
                S.reduce(mx, psR[:, 0:16], ALU.max)
                S.ts(mx, mx, -1.0, ALU.mult)
                S.act(ex[a][:, :], psR[:, 0:16], AF.Exp, bias=mx, accum_out=se)
                S.recip(se, se)
                S.ts(AFF[:, j, :], ex[a][:, :], se, ALU.mult)
            for step in range(NT + 2):
                if 0 <= step - 2 < NT:
                    stage_b1(step - 2)
                if step < NT:
                    stage_a1(step)
                if 0 <= step - 1 < NT:
                    stage_a2(step - 1)
                if 0 <= step - 2 < NT:
                    stage_b2(step - 2)
            for j in range(NT):
                pat = psAT if j % 2 == 0 else psAT2
                S.transpose(pat[0:16, 0:128], AFF[:, j, :], P.ident_f[:, :])
                S.copy(AFFT[:, j * 128:(j + 1) * 128], pat[0:16, 0:128], eng=('act' if j % 2 else 'dve'))
            if C.debug:
                pass
        if C.debug:
            d_ = C.dbg_out('AFF', [128, NT, 16])
            S.dma(d_[:, :, :], AFF[:, :, :])
        with S.scope() as p2:
            junkA = sb("junkA", [16, SEQ], F32, p2)
            bs = sb("bis", [16, 8], F32, p2)
            THR = sb("THR", [128, 16], F32, p2)
            throw = sb("throw", [1, 16], F32, p2)
            SEL = sb("SEL", [128, NT, 16], F32, p2)
            POS = sb("POS", [128, NT, 16], F32, p2)
            selcum = sb("selcum", [128, 16], F32, p2)
            striU = sb("striU", [128, 128], F32, p2)
            COORD = sb("COORD", [128, NT, 16, 4], BF16, p2)
            jf = sb("jf", [128, NT], F32, p2)
            pf = sb("pf", [128, 1], F32, p2)
            iot = sb("iot", [128, 512], mybir.dt.float16, p2)
            OH = [sb(f"OH{i}", [128, 512], BF16, p2) for i in range(3)]
            r4 = [sb(f"r4{i}", [128, 8], F32, p2) for i in range(2)]
            psI = [pst(f"psI{i}", [128, 512], F32, p2) for i in range(4)]
            psP = [pst(f"psP{i}", [128, 512], F32, p2) for i in range(2)]
            psX = pst("psX", [128, 512], F32, p2)
            psB2 = pst("psB2", [128, 512], F32, p2)
            lo, hi, mid, cnt, ge, d1, d2 = [bs[:, i:i + 1] for i in range(7)]
            S.dma(striU[:, :], D['striU'][:, :])
            S.memset(lo, 0.0)
            S.memset(hi, 1.0)
            for it in range(30):
                S.tt(mid, lo, hi, ALU.add)
                S.ts(mid, mid, 0.5, ALU.mult)
                S.ts(junkA[:, :], AFFT[:, :], mid, ALU.is_ge, s2=0.0, op1=ALU.add, accum_out=cnt)
                S.ts(ge, cnt, 511.5, ALU.is_gt)
                S.tt(d1, mid, lo, ALU.subtract)
                S.tt(d2, hi, mid, ALU.subtract)
                S.stt(lo, d1, ge, lo, ALU.mult, ALU.add)
                S.stt(hi, d2, ge, mid, ALU.mult, ALU.add)
            S.transpose(psX[0:1, 0:16], lo, P.ident_f[0:16, 0:16])
            S.copy(throw[:, :], psX[0:1, 0:16])
            S.mm(psB2[:, 0:16], P.ones_f[0:1, 0:128], throw[0:1, :])
            S.copy(THR[:, :], psB2[:, 0:16])
            S.tt(SEL[:, :, :], AFF[:, :, :], THR[:, :].unsqueeze(1).to_broadcast([128, NT, 16]), ALU.is_ge)
            S.memset(selcum[:, :], 0.0)
            for j in range(NT):
                pp = psP[j % 2]
                S.mm(pp[:, 0:16], striU[:, :], SEL[:, j, :], start=True, stop=False)
                S.mm(pp[:, 0:16], P.ones_f[:, :], selcum[:, :], start=False, stop=True)
                S.copy(POS[:, j, :], pp[:, 0:16], eng='act')
                S.tt(selcum[:, :], selcum[:, :], SEL[:, j, :], ALU.add)
            S.op('pool', lambda e: e.iota(jf[:, :], [[1, NT]], base=0, channel_multiplier=0, allow_small_or_imprecise_dtypes=True), [], [jf[:, :]])
            S.op('pool', lambda e: e.iota(pf[:, :], [[1, 1]], base=0, channel_multiplier=1, allow_small_or_imprecise_dtypes=True), [], [pf[:, :]])
            S.op('pool', lambda e: e.iota(iot[:, :], [[1, 512]], base=0, channel_multiplier=0, allow_small_or_imprecise_dtypes=True), [], [iot[:, :]])
            S.copy(COORD[:, :, :, 0], jf[:, :].unsqueeze(2).to_broadcast([128, NT, 16]))
            S.copy(COORD[:, :, :, 1], pf[:, 0:1].unsqueeze(2).to_broadcast([128, NT, 16]))
            S.copy(COORD[:, :, :, 2], AFF[:, :, :])
            S.tt(COORD[:, :, :, 3], AFF[:, :, :], COORD[:, :, :, 2], ALU.subtract)
            n = 0
            for e_ in range(16):
                for j in range(NT):
                    oh = OH[n % 3]
                    n += 1
                    S.ts(oh[:, :], iot[:, :], POS[:, j, e_:e_ + 1], ALU.is_equal, s2=SEL[:, j, e_:e_ + 1], op1=ALU.mult)
                    for sc in range(4):
                        S.mm(psI[sc][:, 0:4], oh[:, sc * 128:(sc + 1) * 128], COORD[:, j, e_, :], start=(j == 0), stop=(j == NT - 1))
                for sc in range(4):
                    r = r4[(e_ * 4 + sc) % 2]
                    S.copy(r[:, 0:4], psI[sc][:, 0:4], eng='act')
                    S.stt(r[:, 4:5], r[:, 0:1], 128.0, r[:, 1:2], ALU.mult, ALU.add)
                    S.copy(P.IDX[:, e_, sc:sc + 1], r[:, 4:5])
                    S.tt(P.GW[:, e_, sc:sc + 1], r[:, 2:3], r[:, 3:4], ALU.add)
        if C.debug:
            d_ = C.dbg_out('IDX', [128, 16, 4], I32)
            S.dma(d_[:, :, :], P.IDX[:, :, :])
            d_ = C.dbg_out('GW', [128, 16, 4])
            S.dma(d_[:, :, :], P.GW[:, :, :])


def phase_route(C):
    pass


def phase_experts(C):
    S, D, P, sb, pst, nc = C.S, C.D, C.P, C.sb, C.pst, C.nc
    with S.scope() as ph:
        stg = [sb(f"stg{i}", [128, 8, 512], F32, ph) for i in range(4)]
        wb = [sb(f"wb{i}", [128, 8, 512], BF16, ph) for i in range(8)]
        xs = sb("xs", [128, 4, DM], BF16, ph)
        xsT = sb("xsT", [128, 8, 512], BF16, ph)
        hidT = sb("hidT", [128, 16, 512], BF16, ph)
        sg = [sb(f"sg{i}", [128, 512], F32, ph) for i in range(2)]
        ysb = [sb(f"ysb{i}", [128, DM], F32, ph) for i in range(4)]
        psT = pst("psTe", [128, 1024], BF16, ph)
        psG = [pst(f"psGe{i}", [128, 512], F32, ph) for i in range(2)]
        psU = pst("psUe", [128, 512], F32, ph)
        psY = [pst(f"psYe{i}", [128, 512], F32, ph) for i in range(4)]
        pieces = []
        for e_ in range(16):
            for fb in range(4):
                for nm in ('w_gate', 'w_up'):
                    pieces.append(D[nm][e_ * 1024:(e_ + 1) * 1024, fb * 512:(fb + 1) * 512].rearrange("(k p) c -> p k c", p=128))
            for dh in range(2):
                for fh in range(2):
                    r0 = e_ * 2048 + fh * 1024
                    pieces.append(D['w_down'][r0:r0 + 1024, dh * 512:(dh + 1) * 512].rearrange("(k p) c -> p k c", p=128))
        issued = [0]
        cast_eng = ['act', 'dve']
        LOOK = 6

        def issue_upto(n):
            while issued[0] < min(n, len(pieces)):
                i = issued[0]
                s_ = stg[i % 4]
                S.dma(s_[:, :, :], pieces[i])
                S.copy(wb[i % 8][:, :, :], s_[:, :, :], eng=cast_eng[i % 2])
                issued[0] += 1
        pc = [0]

        def next_piece():
            i = pc[0]
            issue_upto(i + 1 + LOOK)
            pc[0] += 1
            return wb[i % 8]
        ng = 0
        for e_ in range(16):
            for st in range(4):
                idx_ap = P.IDX[:, e_, st:st + 1]
                S.op('pool', lambda e, st=st, idx_ap=idx_ap: e.indirect_dma_start(
                    out=xs[:, st, :], out_offset=None, in_=D['HFd'][:, :],
                    in_offset=bass.IndirectOffsetOnAxis(ap=idx_ap, axis=0)),
                    [D['HFd'][:, :], idx_ap], [xs[:, st, :]], dma=True)
            for st in range(4):
                for k in range(8):
                    S.transpose(psT[:, k * 128:(k + 1) * 128], xs[:, st, k * 128:(k + 1) * 128], P.ident_bf[:, :])
                S.copy(xsT[:, :, st * 128:(st + 1) * 128], psT[:, :].rearrange("p (k t) -> p k t", k=8), eng=('act' if st % 2 else 'dve'))
            for fb in range(4):
                wg = next_piece()
                wu = next_piece()
                for fc in range(4):
                    pg = psG[ng % 2]
                    sgt = sg[ng % 2]
                    ng += 1
                    for k in range(8):
                        S.mm(pg[:, :], wg[:, k, fc * 128:(fc + 1) * 128], xsT[:, k, :], start=(k == 0), stop=(k == 7))
                    for k in range(8):
                        S.mm(psU[:, :], wu[:, k, fc * 128:(fc + 1) * 128], xsT[:, k, :], start=(k == 0), stop=(k == 7))
                    S.act(sgt[:, :], pg[:, :], AF.Silu)
                    S.tt(hidT[:, fb * 4 + fc, :], sgt[:, :], psU[:, :], ALU.mult)
            for dh in range(2):
                for fh in range(2):
                    wd = next_piece()
                    for st in range(4):
                        for f8 in range(8):
                            S.mm(psY[st][:, :], hidT[:, fh * 8 + f8, st * 128:(st + 1) * 128], wd[:, f8, :],
                                 start=(fh == 0 and f8 == 0), stop=(fh == 1 and f8 == 7))
                for st in range(4):
                    S.stt(ysb[st][:, dh * 512:(dh + 1) * 512], psY[st][:, :], P.GW[:, e_, st:st + 1],
                          P.gt2rep[:, dh * 512:(dh + 1) * 512], ALU.mult, ALU.mult)
            for st in range(4):
                idx_ap = P.IDX[:, e_, st:st + 1]
                S.op('pool', lambda e, st=st, idx_ap=idx_ap: e.indirect_dma_start(
                    out=D['acc'][:, :], out_offset=bass.IndirectOffsetOnAxis(ap=idx_ap, axis=0),
                    in_=ysb[st][:, :], in_offset=None, compute_op=ALU.add),
                    [ysb[st][:, :], idx_ap, D['acc'][:, :]], [D['acc'][:, :]], dma=True)


def phase_final(C):
    S, D, P, sb, pst = C.S, C.D, C.P, C.sb, C.pst
    with S.scope() as ph:
        gfin = sb("gfin", [128, DM], F32, ph)
        xb = [sb(f"fxb{i}", [128, DM], F32, ph) for i in range(4)]
        ob = [sb(f"fob{i}", [128, DM], F32, ph) for i in range(4)]
        junk = sb("fjunk", [128, DM], BF16, ph)
        sm = sb("fsm", [128, NT, 2], F32, ph)
        S.dma(gfin[:, :], D['gfin_row'][0:1, :].partition_broadcast(128))
        for j in range(NT):
            a = j % 4
            S.dma(xb[a][:, :], D['acc'][j * 128:(j + 1) * 128, :])
            ss = sm[:, j, 0:1]
            rs = sm[:, j, 1:2]
            S.act(junk[:, :], xb[a][:, :], AF.Square, accum_out=ss)
            S.act(rs, ss, AF.Ln, scale=1.0 / DM, bias=EPS)
            S.act(rs, rs, AF.Exp, scale=-0.5)
            S.stt(ob[a][:, :], xb[a][:, :], rs, gfin[:, :], ALU.mult, ALU.mult)
            S.dma(D['out'][j * 128:(j + 1) * 128, :], ob[a][:, :], q='pool')


_PROG = {}


def kernel(**inputs):
    if 'nc' not in _PROG:
        _PROG['nc'] = build()[0]
    nc = _PROG['nc']
    B = inputs['x'].shape[0]
    in_maps = [layout_inputs(inputs, b) for b in range(B)]
    res = run_bass_kernel_spmd(nc, in_maps, core_ids=list(range(B)))
    out = np.stack([np.asarray(r["out"], dtype=np.float32) for r in res.results], axis=0)
    return out
```
